# Optimizing a Trainium2 kernel written in Bass

```python
import math
import jax, jax.numpy as jnp
from jax import lax
import numpy as np

D_MODEL = 1024
BATCH = 4
SEQ = 8192
DEPTH = 4

GRID_W = 64
CTX_LEN = 256
HEAD_DIM = 64
NA_HEADS = 6
NA_W = NA_HEADS * HEAD_DIM
NA_ROWS = 8
NA_COLS = 16
NA_QBLK = 16
NA_KBLK = 32
S5_GROUP_CH = 16
S5_CH = 256
S5_GROUPS = S5_CH // S5_GROUP_CH
S5_STATE = 64
S5_DT_MIN = 1e-3
S5_DT_MAX = 1e-1
S5_EIG_MAX = -1e-4
SW_HEADS = 6
SW_KV_HEADS = 2
SW_W = SW_HEADS * HEAD_DIM
SW_KV_W = SW_KV_HEADS * HEAD_DIM
SW_WINDOW = 128
SW_BLK = 128
ROPE_BASE = 10000.0
MIX_W = NA_W + S5_CH + SW_W
IN_COLS = 3 * NA_W + S5_CH + SW_W + 2 * SW_KV_W
PROJ_SPLITS = (NA_W, 2 * NA_W, 3 * NA_W, 3 * NA_W + S5_CH, 3 * NA_W + S5_CH + SW_W, 3 * NA_W + S5_CH + SW_W + SW_KV_W)
N_EXPERTS = 32
TOP_K = 4
EXPERT_FF = D_MODEL
MOE_BLK = 256
SWIGLU_LIMIT = 7.0
SWIGLU_ALPHA = 1.702
RMS_EPS = 1e-6
NEG_INF = -1e30

kernel_name = 'hybrid_na_s5_swa_moe_dit'


def _rmsnorm(x, g):
    x32 = x.astype(jnp.float32)
    y = x32 * lax.rsqrt(jnp.mean(x32 * x32, axis=-1, keepdims=True) + RMS_EPS)
    return (y * g.astype(jnp.float32)).astype(x.dtype)


def _modulate(h, shift, scale):
    return h * (1.0 + scale) + shift


def _heads(t):
    return t.reshape(t.shape[:-1] + (t.shape[-1] // HEAD_DIM, HEAD_DIM))


def _axial_rope(length, dim):
    t = jnp.arange(length)
    row = (t // GRID_W).astype(jnp.float32)
    col = (t % GRID_W).astype(jnp.float32)
    nf = dim // 4
    inv = ROPE_BASE ** (-jnp.arange(nf, dtype=jnp.float32) / nf)
    ar = row[:, None] * inv
    ac = col[:, None] * inv
    ang = jnp.concatenate([ar, ar, ac, ac], axis=-1)[:, None, :]
    return jnp.cos(ang), jnp.sin(ang)


def _apply_rope(x, cos, sin):
    xs = x.reshape(x.shape[:-1] + (2, 2, x.shape[-1] // 4))
    rot = jnp.stack([-xs[..., 1, :], xs[..., 0, :]], axis=-2).reshape(x.shape)
    return (x * cos + rot * sin).astype(x.dtype)


def _na_col_tables():
    n_qb = GRID_W // NA_QBLK
    kc0 = np.clip(np.arange(n_qb) * NA_QBLK - NA_COLS // 2, 0, GRID_W - NA_KBLK)
    qcol = np.arange(n_qb)[:, None] * NA_QBLK + np.arange(NA_QBLK)[None, :]
    kcol = kc0[:, None] + np.arange(NA_KBLK)[None, :]
    ws = np.clip(qcol - NA_COLS // 2, 0, GRID_W - NA_COLS)
    valid = (kcol[:, None, :] >= ws[:, :, None]) & (kcol[:, None, :] < ws[:, :, None] + NA_COLS)
    dc = np.clip(kcol[:, None, :] - qcol[:, :, None] + NA_COLS - 1, 0, 2 * NA_COLS - 2)
    return kc0, valid, dc


def _neighbourhood_attention(q, k, v, qc, kc, vc, rpb, with_ctx):
    bsz, seq, nh, hd = q.shape
    ctx_len = kc.shape[1]
    rows = seq // GRID_W
    kr = min(NA_ROWS, rows)
    scale = hd ** -0.5
    kc0, valid, dc = _na_col_tables()
    n_qb = kc0.shape[0]
    nk = kr * NA_KBLK
    mask = np.broadcast_to(valid[:, :, None, :], (n_qb, NA_QBLK, kr, NA_KBLK)).reshape(n_qb, NA_QBLK, nk)
    rpb_c = rpb[:, :, dc]
    kg = k.reshape(bsz, rows, GRID_W, nh, hd)
    vg = v.reshape(bsz, rows, GRID_W, nh, hd)
    qg = jnp.moveaxis(q.reshape(bsz, rows, GRID_W, nh, hd), 1, 0)

    def row_block(args):
        q_r, r = args
        rs = jnp.clip(r - kr // 2, 0, rows - kr)
        k_win = lax.dynamic_slice_in_dim(kg, rs, kr, axis=1)
        v_win = lax.dynamic_slice_in_dim(vg, rs, kr, axis=1)
        kb = jnp.stack([k_win[:, :, int(s):int(s) + NA_KBLK] for s in kc0], axis=1).reshape(bsz, n_qb, nk, nh, hd)
        vb = jnp.stack([v_win[:, :, int(s):int(s) + NA_KBLK] for s in kc0], axis=1).reshape(bsz, n_qb, nk, nh, hd)
        qb = q_r.reshape(bsz, n_qb, NA_QBLK, nh, hd)
        dr_idx = rs + jnp.arange(kr) - r + NA_ROWS - 1
        bias = jnp.take(rpb_c, dr_idx, axis=1)
        bias = bias.transpose(0, 2, 3, 1, 4).reshape(nh, n_qb, NA_QBLK, nk).astype(jnp.float32)
        s_nb = jnp.einsum('bjqhd,bjkhd->bhjqk', qb, kb).astype(jnp.float32) * scale + bias
        s_nb = jnp.where(mask, s_nb, NEG_INF)
        s_cx = jnp.einsum('bjqhd,bchd->bhjqc', qb, kc).astype(jnp.float32) * scale
        p = jax.nn.softmax(jnp.concatenate([s_nb, s_cx], axis=-1), axis=-1).astype(q.dtype)
        o = (jnp.einsum('bhjqk,bjkhd->bjqhd', p[..., :nk], vb)
             + jnp.einsum('bhjqc,bchd->bjqhd', p[..., nk:], vc))
        return o.reshape(bsz, GRID_W, nh, hd)

    o = lax.map(row_block, (qg, jnp.arange(rows)))
    o = jnp.moveaxis(o, 0, 1).reshape(bsz, seq, nh * hd)
    oc = None
    if with_ctx:
        sc = jnp.einsum('bqhd,bkhd->bhqk', qc, kc).astype(jnp.float32) * scale
        pc = jax.nn.softmax(sc, axis=-1).astype(q.dtype)
        oc = jnp.einsum('bhqk,bkhd->bqhd', pc, vc).reshape(bsz, ctx_len, nh * hd)
    return o, oc


def _window_gqa(q, k, v, qc, kc, vc, sinks, with_ctx):
    bsz, seq, n_q, hd = q.shape
    n_kv = k.shape[2]
    grp = n_q // n_kv
    ctx_len = kc.shape[1]
    nb = seq // SW_BLK
    scale = hd ** -0.5
    sink_logit = sinks.astype(jnp.float32).reshape(n_kv, grp, 1, 1)
    qb = q.reshape(bsz, nb, SW_BLK, n_kv, grp, hd)

    def band(t):
        tp = jnp.pad(t, ((0, 0), (SW_BLK, SW_BLK), (0, 0), (0, 0))).reshape(bsz, nb + 2, SW_BLK, n_kv, hd)
        return jnp.concatenate([tp[:, :-2], tp[:, 1:-1], tp[:, 2:]], axis=2)

    kb, vb = band(k), band(v)
    qpos = np.arange(nb)[:, None] * SW_BLK + np.arange(SW_BLK)[None, :]
    kpos = (np.arange(nb)[:, None] - 1) * SW_BLK + np.arange(3 * SW_BLK)[None, :]
    valid = ((np.abs(qpos[:, :, None] - kpos[:, None, :]) <= SW_WINDOW)
             & (kpos[:, None, :] >= 0) & (kpos[:, None, :] < seq))
    s_loc = jnp.einsum('bnqhgd,bnkhd->bnhgqk', qb, kb).astype(jnp.float32) * scale
    s_loc = jnp.where(valid[:, None, None], s_loc, NEG_INF)
    s_ctx = jnp.einsum('bnqhgd,bchd->bnhgqc', qb, kc).astype(jnp.float32) * scale
    s_sink = jnp.broadcast_to(sink_logit, s_ctx.shape[:-1] + (1,))
    p = jax.nn.softmax(jnp.concatenate([s_loc, s_ctx, s_sink], axis=-1), axis=-1).astype(q.dtype)
    nk = 3 * SW_BLK
    o = (jnp.einsum('bnhgqk,bnkhd->bnqhgd', p[..., :nk], vb)
         + jnp.einsum('bnhgqc,bchd->bnqhgd', p[..., nk:nk + ctx_len], vc))
    o = o.reshape(bsz, seq, n_q * hd)
    oc = None
    if with_ctx:
        qcb = qc.reshape(bsz, ctx_len, n_kv, grp, hd)
        sc = jnp.einsum('bqhgd,bkhd->bhgqk', qcb, kc).astype(jnp.float32) * scale
        sc_sink = jnp.broadcast_to(sink_logit, sc.shape[:-1] + (1,))
        pc = jax.nn.softmax(jnp.concatenate([sc, sc_sink], axis=-1), axis=-1).astype(q.dtype)
        oc = jnp.einsum('bhgqk,bkhd->bqhgd', pc[..., :ctx_len], vc).reshape(bsz, ctx_len, n_q * hd)
    return o, oc


def _ssm_drive(u, b_bar):
    return lax.complex(jnp.einsum('blgh,gph->blgp', u, jnp.real(b_bar)),
                       jnp.einsum('blgh,gph->blgp', u, jnp.imag(b_bar)))


def _ssm_readout(xs, c_re, c_im):
    return (jnp.einsum('blgp,ghp->blgh', jnp.real(xs), c_re.astype(jnp.float32))
            - jnp.einsum('blgp,ghp->blgh', jnp.imag(xs), c_im.astype(jnp.float32)))


def _diag_scan(lam_bar, drive):
    a = jnp.broadcast_to(lam_bar, drive.shape)

    def combine(e1, e2):
        a1, b1 = e1
        a2, b2 = e2
        return a1 * a2, a2 * b1 + b2

    _, xs = lax.associative_scan(combine, (a, drive), axis=1)
    return xs


def _s5_mixer(u, uc, a_re, a_im, log_step, b_re, b_im, c_re, c_im, d_skip, w_glu, b_glu, with_ctx):
    f32 = jnp.float32
    bsz, seq, _ = u.shape
    ctx_len = uc.shape[1]
    u32 = u.astype(f32).reshape(bsz, seq, S5_GROUPS, S5_GROUP_CH)
    uc32 = uc.astype(f32).reshape(bsz, ctx_len, S5_GROUPS, S5_GROUP_CH)
    lam = lax.complex(jnp.minimum(a_re.astype(f32), S5_EIG_MAX), a_im.astype(f32))
    step = jnp.exp(log_step.astype(f32))[..., None]
    lam_bar = jnp.exp(lam * step)
    b_bar = ((lam_bar - 1.0) / lam)[..., None] * lax.complex(b_re.astype(f32), b_im.astype(f32))
    d32 = d_skip.astype(f32).reshape(S5_GROUPS, S5_GROUP_CH)
    y = d32 * u32
    yc = d32 * uc32
    for direction in range(2):
        rev = direction == 1
        seq_u = jnp.flip(u32, axis=1) if rev else u32
        seq_c = jnp.flip(uc32, axis=1) if rev else uc32
        xc = _diag_scan(lam_bar[direction], _ssm_drive(seq_c, b_bar[direction]))
        drive = _ssm_drive(seq_u, b_bar[direction])
        drive = drive.at[:, 0].add(lam_bar[direction] * xc[:, -1])
        xs = _diag_scan(lam_bar[direction], drive)
        out = _ssm_readout(xs, c_re[direction], c_im[direction])
        y = y + (jnp.flip(out, axis=1) if rev else out)
        if with_ctx:
            outc = _ssm_readout(xc, c_re[direction], c_im[direction])
            yc = yc + (jnp.flip(outc, axis=1) if rev else outc)

    def glu(t):
        g = jax.nn.gelu(t.reshape(t.shape[:2] + (S5_CH,)))
        return (g * jax.nn.sigmoid(g @ w_glu.astype(f32) + b_glu.astype(f32))).astype(u.dtype)

    return glu(y), (glu(yc) if with_ctx else None)


def _clamped_swiglu(gu):
    gate, up = jnp.split(gu, 2, axis=-1)
    gate = jnp.minimum(gate, SWIGLU_LIMIT)
    up = jnp.clip(up, -SWIGLU_LIMIT, SWIGLU_LIMIT)
    return gate * jax.nn.sigmoid(SWIGLU_ALPHA * gate) * (up + 1.0)


def _moe_ffn(h, w_router, b_router, w_gate_up, b_gate_up, w_down, b_down):
    f32 = jnp.float32
    n_tok, dim = h.shape
    logits = (h @ w_router + b_router).astype(f32)
    top_val, top_idx = lax.top_k(logits, TOP_K)
    top_w = jax.nn.softmax(top_val, axis=-1)
    n_assign = n_tok * TOP_K
    e_flat = top_idx.reshape(n_assign)
    order = jnp.argsort(e_flat)
    e_sorted = e_flat[order]
    tok_sorted = (order // TOP_K).astype(jnp.int32)
    w_sorted = top_w.reshape(n_assign)[order]
    counts = jnp.bincount(e_flat, length=N_EXPERTS)
    padded = (counts + MOE_BLK - 1) // MOE_BLK * MOE_BLK
    ends = jnp.cumsum(padded)
    pstarts = ends - padded
    starts = jnp.cumsum(counts) - counts
    dest = pstarts[e_sorted] + jnp.arange(n_assign) - starts[e_sorted]
    n_blocks = -(-(n_assign + N_EXPERTS * (MOE_BLK - 1)) // MOE_BLK)
    n_rows = n_blocks * MOE_BLK
    tok_buf = jnp.zeros((n_rows,), jnp.int32).at[dest].set(tok_sorted)
    w_buf = jnp.zeros((n_rows,), f32).at[dest].set(w_sorted)
    blk_exp = jnp.minimum(jnp.searchsorted(ends, jnp.arange(n_blocks) * MOE_BLK, side='right'), N_EXPERTS - 1)

    def expert_block(args):
        tok, e = args
        gu = h[tok] @ w_gate_up[e] + b_gate_up[e]
        return _clamped_swiglu(gu) @ w_down[e] + b_down[e]

    y = lax.map(expert_block, (tok_buf.reshape(n_blocks, MOE_BLK), blk_exp))
    y = y.reshape(n_rows, dim).astype(f32) * w_buf[:, None]
    return jnp.zeros((n_tok, dim), f32).at[tok_buf].add(y).astype(h.dtype)


def setup_inputs(seed: int = 0) -> dict:
    key = jax.random.key(seed)
    ks = iter(jax.random.split(key, 40))
    f32 = jnp.float32

    def nrm(shape, s):
        return jax.random.normal(next(ks), shape, f32) * s

    d = D_MODEL
    x = nrm((BATCH, SEQ, d), 1.0)
    c = nrm((BATCH, d), 1.0)
    ctx = nrm((BATCH, CTX_LEN, d), 1.0)
    c_ctx = nrm((d,), 1.0)
    w_mod = nrm((DEPTH, d, 6 * d), 0.5 * d ** -0.5)
    b_mod = nrm((DEPTH, 6 * d), 0.02)
    g_mix = 1.0 + nrm((DEPTH, d), 0.05)
    w_in = nrm((DEPTH, d, IN_COLS), d ** -0.5)
    w_out = nrm((DEPTH, MIX_W, d), MIX_W ** -0.5)
    na_rpb = nrm((DEPTH, NA_HEADS, 2 * NA_ROWS - 1, 2 * NA_COLS - 1), 0.1)
    s5_a_re = -0.5 + nrm((DEPTH, 2, S5_GROUPS, S5_STATE), 0.01)
    s5_a_im = jnp.pi * jnp.arange(S5_STATE, dtype=f32) + nrm((DEPTH, 2, S5_GROUPS, S5_STATE), 0.01)
    s5_log_step = jax.random.uniform(next(ks), (DEPTH, 2, S5_GROUPS), f32, math.log(S5_DT_MIN), math.log(S5_DT_MAX))
    s5_b_re = nrm((DEPTH, 2, S5_GROUPS, S5_STATE, S5_GROUP_CH), (2 * S5_GROUP_CH) ** -0.5)
    s5_b_im = nrm((DEPTH, 2, S5_GROUPS, S5_STATE, S5_GROUP_CH), (2 * S5_GROUP_CH) ** -0.5)
    s5_c_re = nrm((DEPTH, 2, S5_GROUPS, S5_GROUP_CH, S5_STATE), S5_STATE ** -0.5)
    s5_c_im = nrm((DEPTH, 2, S5_GROUPS, S5_GROUP_CH, S5_STATE), S5_STATE ** -0.5)
    s5_d = nrm((DEPTH, S5_CH), 1.0)
    s5_w_glu = nrm((DEPTH, S5_CH, S5_CH), S5_CH ** -0.5)
    s5_b_glu = nrm((DEPTH, S5_CH), 0.01)
    sw_sinks = nrm((DEPTH, SW_HEADS), 0.5)
    g_ffn = 1.0 + nrm((DEPTH, d), 0.05)
    w_router = nrm((DEPTH, d, N_EXPERTS), d ** -0.5)
    b_router = nrm((DEPTH, N_EXPERTS), 0.01)
    w_gate_up = nrm((DEPTH, N_EXPERTS, d, 2 * EXPERT_FF), d ** -0.5)
    b_gate_up = nrm((DEPTH, N_EXPERTS, 2 * EXPERT_FF), 0.01)
    w_down = nrm((DEPTH, N_EXPERTS, EXPERT_FF, d), EXPERT_FF ** -0.5)
    b_down = nrm((DEPTH, N_EXPERTS, d), 0.01)
    g_final = 1.0 + nrm((d,), 0.05)
    return {'x': x, 'c': c, 'ctx': ctx, 'c_ctx': c_ctx, 'w_mod': w_mod, 'b_mod': b_mod,
            'g_mix': g_mix, 'w_in': w_in, 'w_out': w_out, 'na_rpb': na_rpb,
            's5_a_re': s5_a_re, 's5_a_im': s5_a_im, 's5_log_step': s5_log_step,
            's5_b_re': s5_b_re, 's5_b_im': s5_b_im, 's5_c_re': s5_c_re, 's5_c_im': s5_c_im,
            's5_d': s5_d, 's5_w_glu': s5_w_glu, 's5_b_glu': s5_b_glu, 'sw_sinks': sw_sinks,
            'g_ffn': g_ffn, 'w_router': w_router, 'b_router': b_router,
            'w_gate_up': w_gate_up, 'b_gate_up': b_gate_up, 'w_down': w_down, 'b_down': b_down,
            'g_final': g_final}


def reference(x, c, ctx, c_ctx, w_mod, b_mod, g_mix, w_in, w_out, na_rpb,
              s5_a_re, s5_a_im, s5_log_step, s5_b_re, s5_b_im, s5_c_re, s5_c_im,
              s5_d, s5_w_glu, s5_b_glu, sw_sinks, g_ffn, w_router, b_router,
              w_gate_up, b_gate_up, w_down, b_down, g_final):
    bsz, seq, dim = x.shape
    ctx_len = ctx.shape[1]
    cos, sin = _axial_rope(seq, HEAD_DIM)
    cond = jax.nn.silu(c)
    cond_ctx = jax.nn.silu(c_ctx)
    for l in range(DEPTH):
        with_ctx = l < DEPTH - 1
        mod = (cond @ w_mod[l] + b_mod[l]).reshape(bsz, 6, 1, dim)
        mod_c = (cond_ctx @ w_mod[l] + b_mod[l]).reshape(6, dim)
        h = _modulate(_rmsnorm(x, g_mix[l]), mod[:, 0], mod[:, 1])
        hc = _modulate(_rmsnorm(ctx, g_mix[l]), mod_c[0], mod_c[1])
        aq, ak, av, su, sq, sk, sv = jnp.split(h @ w_in[l], PROJ_SPLITS, axis=-1)
        caq, cak, cav, csu, csq, csk, csv = jnp.split(hc @ w_in[l], PROJ_SPLITS, axis=-1)
        ya, yca = _neighbourhood_attention(_heads(aq), _heads(ak), _heads(av),
                                           _heads(caq), _heads(cak), _heads(cav), na_rpb[l], with_ctx)
        yb, ycb = _s5_mixer(su, csu, s5_a_re[l], s5_a_im[l], s5_log_step[l], s5_b_re[l], s5_b_im[l],
                            s5_c_re[l], s5_c_im[l], s5_d[l], s5_w_glu[l], s5_b_glu[l], with_ctx)
        yc, ycc = _window_gqa(_apply_rope(_heads(sq), cos, sin), _apply_rope(_heads(sk), cos, sin), _heads(sv),
                              _heads(csq), _heads(csk), _heads(csv), sw_sinks[l], with_ctx)
        x = x + mod[:, 2] * (jnp.concatenate([ya, yb, yc], axis=-1) @ w_out[l])
        h = _modulate(_rmsnorm(x, g_ffn[l]), mod[:, 3], mod[:, 4])
        if with_ctx:
            ctx = ctx + mod_c[2] * (jnp.concatenate([yca, ycb, ycc], axis=-1) @ w_out[l])
            hc = _modulate(_rmsnorm(ctx, g_ffn[l]), mod_c[3], mod_c[4])
            f = _moe_ffn(jnp.concatenate([h.reshape(-1, dim), hc.reshape(-1, dim)], axis=0),
                         w_router[l], b_router[l], w_gate_up[l], b_gate_up[l], w_down[l], b_down[l])
            ctx = ctx + mod_c[5] * f[bsz * seq:].reshape(bsz, ctx_len, dim)
            f = f[:bsz * seq]
        else:
            f = _moe_ffn(h.reshape(-1, dim), w_router[l], b_router[l], w_gate_up[l], b_gate_up[l],
                         w_down[l], b_down[l])
        x = x + mod[:, 5] * f.reshape(bsz, seq, dim)
    return _rmsnorm(x, g_final)
```

```python
import numpy as np
import ml_dtypes
from contextlib import ExitStack
import concourse.bass as bass
import concourse.mybir as mybir
from concourse.bass_utils import run_bass_kernel_spmd

F32 = mybir.dt.float32
BF16 = mybir.dt.bfloat16
I32 = mybir.dt.int32
U32 = mybir.dt.uint32
AF = mybir.ActivationFunctionType
ALU = mybir.AluOpType
AX = mybir.AxisListType

D = 1024
SEQ = 8192
CTXL = 256
NTOK = SEQ + CTXL
DEPTH = 4
NE = 32
BLK = 896
NB = -(-(NTOK * 4 + NE * (BLK - 1)) // BLK)
NSLOT = NB * BLK
EPS = 1e-6
NEG = -8.0e30


class Buf:
    __slots__ = ("w", "r")

    def __init__(self):
        self.w = {}
        self.r = {}


class T:
    def __init__(self, t, name):
        self.t = t
        self.name = name
        self.b = Buf()

    def __getitem__(self, idx):
        return self.t[idx]


class FW:
    NDMA = 32

    def __init__(self, nc, es):
        self.nc = nc
        self.es = es
        self.engs = {"pe": nc.tensor, "act": nc.scalar, "dve": nc.vector, "pool": nc.gpsimd, "sp": nc.sync}
        self.sem = {}
        self.cnt = {}
        self.sems = {}
        for k in self.engs:
            s = es.enter_context(nc.semaphore("s_" + k))
            self.sem[k] = s
            self.sems["e_" + k] = s
            self.cnt[k] = 0
        self.dma_sems = []
        for i in range(self.NDMA):
            s = es.enter_context(nc.semaphore("d%d" % i))
            self.sems["d%d" % i] = s
            self.dma_sems.append(["d%d" % i, 0])
        self.dma_rr = 0
        self.waited = {k: {} for k in self.engs}
        self.n_inst = 0
        self.n_wait = 0
        self.uid = 0

    def sb(self, name, shape, dt, es=None):
        self.uid += 1
        t = (es or self.es).enter_context(self.nc.sbuf_tensor("%s_%d" % (name, self.uid), list(shape), dt))
        return T(t, name)

    def ps(self, name, shape, dt, es=None):
        self.uid += 1
        t = (es or self.es).enter_context(self.nc.psum_tensor("%s_%d" % (name, self.uid), list(shape), dt))
        return T(t, name)

    def dram(self, name, shape, dt, kind="Internal"):
        t = self.nc.dram_tensor(name, list(shape), dt, kind=kind)
        return T(t.ap(), name)

    def _deps(self, eng, reads, writes, skip_own=False):
        deps = {}

        def add(ev):
            s, v = ev
            if deps.get(s, 0) < v:
                deps[s] = v
        for b in reads:
            for ev in b.b.w.items():
                add(ev)
        for b in writes:
            for ev in b.b.w.items():
                add(ev)
            for ev in b.b.r.items():
                add(ev)
        own = "e_" + eng
        for s, v in deps.items():
            if skip_own and s == own:
                continue
            if self.waited[eng].get(s, 0) >= v:
                continue
            self.engs[eng].wait_ge(self.sems[s], v)
            self.waited[eng][s] = v
            self.n_wait += 1

    def _commit(self, ev, reads, writes):
        s, v = ev
        for b in reads:
            if b.b.r.get(s, 0) < v:
                b.b.r[s] = v
        for b in writes:
            if b.b.w.get(s, 0) < v:
                b.b.w[s] = v
            b.b.r = {}

    def op(self, eng, fn, reads=(), writes=(), chain=False):
        self._deps(eng, reads, writes, skip_own=chain)
        inst = fn()
        self.cnt[eng] += 1
        inst.then_inc(self.sem[eng], 1)
        self._commit(("e_" + eng, self.cnt[eng]), reads, writes)
        self.n_inst += 1
        return inst

    def dma(self, q, fn, reads=(), writes=()):
        slot = self.dma_sems[self.dma_rr]
        self.dma_rr = (self.dma_rr + 1) % self.NDMA
        sid, used = slot
        if used > 0 and self.waited[q].get(sid, 0) < used * 16:
            self.engs[q].wait_ge(self.sems[sid], used * 16)
            self.waited[q][sid] = used * 16
        self._deps(q, reads, writes)
        inst = fn()
        slot[1] = used + 1
        inst.then_inc(self.sems[sid], 16)
        self._commit((sid, (used + 1) * 16), reads, writes)
        self.n_inst += 1
        return inst

    def barrier(self):
        evs = {}
        for k in self.engs:
            if self.cnt[k] > 0:
                evs["e_" + k] = self.cnt[k]
        for sid, used in self.dma_sems:
            if used > 0:
                evs[sid] = used * 16
        for k in self.engs:
            for s, v in evs.items():
                if s == "e_" + k and k in ("sp",):
                    continue
                if self.waited[k].get(s, 0) >= v:
                    continue
                self.engs[k].wait_ge(self.sems[s], v)
                self.waited[k][s] = v
                self.n_wait += 1

    def finish(self, outs, eng="sp"):
        for b in outs:
            for s, v in b.b.w.items():
                self.engs[eng].wait_ge(self.sems[s], v)


def host_consts():
    c = {}
    c["ident_f"] = np.eye(128, dtype=np.float32)
    c["ident_b"] = np.eye(128).astype(ml_dtypes.bfloat16)
    c["ones_b"] = np.ones((128, 128)).astype(ml_dtypes.bfloat16)
    c["ones_f"] = np.ones((128, 128), dtype=np.float32)
    t = np.arange(SEQ)
    row = (t // 64).astype(np.float32)
    col = (t % 64).astype(np.float32)
    inv = (10000.0 ** (-np.arange(16, dtype=np.float32) / 16)).astype(np.float32)
    ar = row[:, None] * inv
    ac = col[:, None] * inv
    ang = np.concatenate([ar, ar, ac, ac], axis=-1)
    cos = np.cos(ang).astype(np.float32).T
    sin = np.sin(ang).astype(np.float32).T
    sign = np.where((np.arange(64) % 32) < 16, -1.0, 1.0).astype(np.float32)[:, None]
    c["rope_c"] = np.ascontiguousarray(np.concatenate([cos, cos], 0))
    c["rope_s"] = np.ascontiguousarray(np.concatenate([sin * sign, sin * sign], 0))
    k = np.arange(128)[:, None]
    q = np.arange(128)[None, :]
    c["mask_l"] = np.where(k >= q, 0.0, NEG).astype(ml_dtypes.bfloat16)
    c["mask_u"] = np.where(k <= q, 0.0, NEG).astype(ml_dtypes.bfloat16)
    qc = np.arange(64)[None, :]
    kc = np.arange(64)[:, None]
    ws = np.clip(qc - 8, 0, 48)
    valid = (kc >= ws) & (kc < ws + 16)
    v2 = np.concatenate([valid, valid], 0)
    c["na_valid8"] = (v2 * 8.0).astype(np.float32)
    c["na_pen"] = np.where(v2, 0.0, NEG).astype(np.float32)
    c["iota32"] = np.broadcast_to(np.arange(32, dtype=np.float32), (128, 32)).copy()
    c["iota512"] = np.broadcast_to(np.arange(512, dtype=np.float32), (128, 512)).copy()
    c["iotablk"] = np.broadcast_to((np.arange(NB) * BLK).astype(np.float32), (128, NB)).copy()
    c["iota_p"] = np.arange(128, dtype=np.float32).reshape(128, 1)
    c["ustrict"] = (np.arange(128)[:, None] < np.arange(128)[None, :]).astype(np.float32)
    return c


CONST_SPECS = None


def groups():
    g = [(i * 512, 512, False) for i in range(SEQ // 512)]
    g.append((SEQ, CTXL, True))
    return g


def build(nlayers=DEPTH, debug=()):
    nc = bass.Bass("TRN2", target_bir_lowering=False)
    es = ExitStack()
    fw = FW(nc, es)

    def din(name, shape, dt=F32):
        return T(nc.dram_tensor(name, list(shape), dt, kind="ExternalInput").ap(), name)

    def dout(name, shape, dt=F32):
        return T(nc.dram_tensor(name, list(shape), dt, kind="ExternalOutput").ap(), name)

    I = {}
    I["xin"] = din("xin", [NTOK, D])
    I["cvec"] = din("cvec", [2, D])
    specs = {"w_mod": [DEPTH, D, 6 * D], "b_mod": [DEPTH, 6 * D], "g_mix": [DEPTH, D], "w_in": [DEPTH, D, 2048],
             "w_out": [DEPTH, D, D], "na_rpb": [DEPTH, 6, 15, 31], "s5_a_re": [DEPTH, 2, 16, 64], "s5_a_im": [DEPTH, 2, 16, 64],
             "s5_log_step": [DEPTH, 2, 16], "s5_b_re": [DEPTH, 2, 16, 64, 16], "s5_b_im": [DEPTH, 2, 16, 64, 16],
             "s5_c_re": [DEPTH, 2, 16, 16, 64], "s5_c_im": [DEPTH, 2, 16, 16, 64], "s5_d": [DEPTH, 256],
             "s5_w_glu": [DEPTH, 256, 256], "s5_b_glu": [DEPTH, 256], "sw_sinks": [DEPTH, 6], "g_ffn": [DEPTH, D],
             "w_router": [DEPTH, D, NE], "b_router": [DEPTH, NE], "w_gate_up": [DEPTH, NE, D, 2 * D],
             "b_gate_up": [DEPTH, NE, 2 * D], "w_down": [DEPTH, NE, D, D], "b_down": [DEPTH, NE, D], "g_final": [D]}
    for k, s in specs.items():
        I[k] = din(k, s)
    hc = host_consts()
    for k, v in hc.items():
        I[k] = din(k, list(v.shape), BF16 if v.dtype == ml_dtypes.bfloat16 else F32)
    OUT = dout("out", [SEQ, D])
    DBG = {}

    xs = fw.dram("xs", [128, 8, NTOK], F32)
    qa = fw.dram("qa", [3, 128, NTOK], BF16)
    ka = fw.dram("ka", [3, 128, NTOK], BF16)
    va = fw.dram("va", [NTOK, 6 * 65], BF16)
    u5 = fw.dram("u5", [2, 128, NTOK], F32)
    qs = fw.dram("qs", [3, 128, NTOK], BF16)
    ks = fw.dram("ks", [3, 128, NTOK], BF16)
    vs = fw.dram("vs", [NTOK, 2 * 65], BF16)
    yT = fw.dram("yT", [8, 128, NTOK], BF16)

    ident_f = fw.sb("ident_f", [128, 128], F32)
    ident_b = fw.sb("ident_b", [128, 128], BF16)
    ones_b = fw.sb("ones_b", [128, 128], BF16)
    ones_f = fw.sb("ones_f", [128, 128], F32)
    for tl, nm in ((ident_f, "ident_f"), (ident_b, "ident_b"), (ones_b, "ones_b"), (ones_f, "ones_f")):
        fw.dma("sp", lambda tl=tl, nm=nm: nc.sync.dma_start(out=tl[:], in_=I[nm][:]), reads=[I[nm]], writes=[tl])
    modT = fw.sb("modT", [128, DEPTH, 48, 2], F32)
    gmix = fw.sb("gmix", [128, DEPTH, 8], F32)
    gffn = fw.sb("gffn", [128, DEPTH, 8], F32)
    gfin = fw.sb("gfin", [128, 8], F32)
    with nc.allow_non_contiguous_dma(reason="tiny per-feature vectors"):
        fw.dma("sp", lambda: nc.sync.dma_start(out=gmix[:], in_=I["g_mix"][:].rearrange("l (c p) -> p l c", p=128)), reads=[I["g_mix"]], writes=[gmix])
        fw.dma("sp", lambda: nc.sync.dma_start(out=gffn[:], in_=I["g_ffn"][:].rearrange("l (c p) -> p l c", p=128)), reads=[I["g_ffn"]], writes=[gffn])
        fw.dma("sp", lambda: nc.sync.dma_start(out=gfin[:], in_=I["g_final"][:].rearrange("(c p) -> p c", p=128)), reads=[I["g_final"]], writes=[gfin])

    def phase0():
        with ExitStack() as pes:
            condT = fw.sb("condT", [128, 2, 8], F32, pes)
            bmT = fw.sb("bmT", [128, DEPTH, 48], F32, pes)
            with nc.allow_non_contiguous_dma(reason="tiny"):
                for jj in range(2):
                    fw.dma("sp", lambda: nc.sync.dma_start(out=condT[:, jj, :], in_=I["cvec"][jj, :].rearrange("(c p) -> p c", p=128)), reads=[I["cvec"]], writes=[condT])
                fw.dma("sp", lambda: nc.sync.dma_start(out=bmT[:], in_=I["b_mod"][:].rearrange("l (c p) -> p l c", p=128)), reads=[I["b_mod"]], writes=[bmT])
            fw.op("act", lambda: nc.scalar.activation(out=condT[:], in_=condT[:], func=AF.Silu), reads=[condT], writes=[condT])
            wbuf = [fw.sb("wmod%d" % i, [128, 8, 768], F32, pes) for i in range(2)]
            pm = [fw.ps("pmod%d" % i, [128, 6, 2], F32, pes) for i in range(2)]
            it = 0
            for l in range(nlayers):
                for piece in range(8):
                    wb = wbuf[it % 2]
                    pp = pm[it % 2]
                    it += 1
                    q = "sp" if piece % 2 == 0 else "act"
                    eng = nc.sync if piece % 2 == 0 else nc.scalar
                    fw.dma(q, lambda: eng.dma_start(out=wb[:], in_=I["w_mod"][l, :, piece * 768:(piece + 1) * 768].rearrange("(c p) n -> p c n", p=128)),
                           reads=[I["w_mod"]], writes=[wb])
                    for cc in range(6):
                        for kc in range(8):
                            fw.op("pe", lambda: nc.tensor.matmul(pp[:, cc, :], lhsT=wb[:, kc, cc * 128:(cc + 1) * 128], rhs=condT[:, :, kc],
                                                               start=(kc == 0), stop=(kc == 7)),
                                  reads=[wb, condT], writes=[pp], chain=True)
                    for j in range(2):
                        fw.op("dve", lambda: nc.vector.tensor_tensor(out=modT[:, l, piece * 6:(piece + 1) * 6, j], in0=pp[:, :, j],
                                                                   in1=bmT[:, l, piece * 6:(piece + 1) * 6], op=ALU.add),
                              reads=[pp, bmT], writes=[modT])
            for l in range(nlayers):
                for i in (1, 4):
                    fw.op("dve", lambda: nc.vector.tensor_scalar_add(out=modT[:, l, i * 8:(i + 1) * 8, :], in0=modT[:, l, i * 8:(i + 1) * 8, :], scalar1=1.0),
                          reads=[modT], writes=[modT])

    def prephase():
        with ExitStack() as pes:
            xt = [fw.sb("xt%d" % i, [128, D], F32, pes) for i in range(2)]
            xo = [fw.sb("xo%d" % i, [128, 8, 128], F32, pes) for i in range(2)]
            pt = [fw.ps("ptr%d" % i, [128, 8, 128], F32, pes) for i in range(2)]
            for ti in range(NTOK // 128):
                a = xt[ti % 2]; o = xo[ti % 2]; p = pt[ti % 2]
                fw.dma("sp", lambda: nc.sync.dma_start(out=a[:], in_=I["xin"][ti * 128:(ti + 1) * 128, :]), reads=[I["xin"]], writes=[a])
                for c in range(8):
                    fw.op("pe", lambda: nc.tensor.transpose(out=p[:, c, :], in_=a[:, c * 128:(c + 1) * 128], identity=ident_f[:]),
                          reads=[a, ident_f], writes=[p], chain=True)
                if ti % 2 == 0:
                    fw.op("act", lambda: nc.scalar.copy(out=o[:], in_=p[:]), reads=[p], writes=[o])
                else:
                    fw.op("dve", lambda: nc.vector.tensor_copy(out=o[:], in_=p[:]), reads=[p], writes=[o])
                fw.dma("act", lambda: nc.scalar.dma_start(out=xs[:, :, ti * 128:(ti + 1) * 128], in_=o[:]), reads=[o], writes=[xs])

    def norm_mod(pes_bufs, xg, n, gvec, l, ishift, iscale, j, out_bf=None, out_f=None):
        sq, pss, rstd, GS = pes_bufs
        fw.op("act", lambda: nc.scalar.activation(out=sq[:, :, :n], in_=xg[:, :, :n], func=AF.Square), reads=[xg], writes=[sq])
        for c in range(8):
            fw.op("pe", lambda: nc.tensor.matmul(pss[:, :n], lhsT=ones_b[:], rhs=sq[:, c, :n], start=(c == 0), stop=(c == 7)),
                  reads=[sq, ones_b], writes=[pss], chain=True)
        fw.op("act", lambda: nc.scalar.activation(out=rstd[:, :n], in_=pss[:, :n], func=AF.Sqrt, bias=EPS, scale=1.0 / D), reads=[pss], writes=[rstd])
        fw.op("dve", lambda: nc.vector.reciprocal(out=rstd[:, :n], in_=rstd[:, :n]), reads=[rstd], writes=[rstd])
        fw.op("dve", lambda: nc.vector.tensor_tensor(out=GS[:, 0, :], in0=gvec, in1=modT[:, l, iscale * 8:(iscale + 1) * 8, j], op=ALU.mult),
              reads=[modT, gmix, gffn], writes=[GS])
        for c in range(8):
            tgt = out_f if out_f is not None else sq
            if out_f is not None:
                fw.op("dve", lambda: nc.vector.scalar_tensor_tensor(out=out_f[:, c, :n], in0=xg[:, c, :n], scalar=GS[:, 0, c:c + 1], in1=rstd[:, :n],
                                                                  op0=ALU.mult, op1=ALU.mult), reads=[xg, GS, rstd], writes=[out_f])
                fw.op("dve", lambda: nc.vector.tensor_scalar_add(out=out_f[:, c, :n], in0=out_f[:, c, :n], scalar1=modT[:, l, ishift * 8 + c, j:j + 1]),
                      reads=[out_f, modT], writes=[out_f])
                if out_bf is not None:
                    fw.op("act", lambda: nc.scalar.copy(out=out_bf[:, c, :n], in_=out_f[:, c, :n]), reads=[out_f], writes=[out_bf])
            else:
                fw.op("dve", lambda: nc.vector.scalar_tensor_tensor(out=xg[:, c, :n], in0=xg[:, c, :n], scalar=GS[:, 0, c:c + 1], in1=rstd[:, :n],
                                                                  op0=ALU.mult, op1=ALU.mult), reads=[xg, GS, rstd], writes=[xg])
                fw.op("act", lambda: nc.scalar.activation(out=out_bf[:, c, :n], in_=xg[:, c, :n], func=AF.Identity,
                                                        bias=modT[:, l, ishift * 8 + c, j:j + 1], scale=1.0), reads=[xg, modT], writes=[out_bf])

    NCOLF = 20 * 128

    def phaseA(l):
        with ExitStack() as pes:
            w2 = fw.sb("w2", [128, 8, NCOLF], BF16, pes)
            wv = fw.sb("wv", [128, 8, 512], BF16, pes)
            win = I["w_in"]

            def wl(dst, dlo, slo, n, q="pool"):
                fw.dma("pool", lambda: nc.gpsimd.dma_start(out=dst[:, :, dlo:dlo + n], in_=win[l, :, slo:slo + n].rearrange("(c p) n -> p c n", p=128)),
                       reads=[win], writes=[dst])
            wl(w2, 0, 0, 384)
            wl(w2, 384, 384, 384)
            wl(w2, 768, 1152, 256)
            wl(w2, 1024, 1408, 384)
            for ti, (h0, h1) in enumerate(((0, 0), (0, 1), (1, 1))):
                wl(w2, 1792 + ti * 128, 1792 + h0 * 64, 64)
                wl(w2, 1792 + ti * 128 + 64, 1792 + h1 * 64, 64)
            wl(wv, 0, 768, 384)
            wl(wv, 384, 1920, 128)
            for (src, dst, nh) in ((1024, 1408, 6), (1792, 2176, 6)):
                sv = w2[:, :, src:src + nh * 64].rearrange("p c (h a j f) -> p c h a j f", h=nh, a=2, j=2, f=16)
                dv = w2[:, :, dst:dst + nh * 64].rearrange("p c (h a j f) -> p c h a j f", h=nh, a=2, j=2, f=16)
                for c in range(8):
                    for jj in range(2):
                        fw.op("pool", lambda: nc.gpsimd.tensor_copy(out=dv[:, c, :, :, jj, :], in_=sv[:, c, :, :, 1 - jj, :]), reads=[w2], writes=[w2])
            xg = [fw.sb("xg%d" % i, [128, 8, 512], F32, pes) for i in range(2)]
            hT = [fw.sb("hT%d" % i, [128, 8, 512], BF16, pes) for i in range(2)]
            sq = fw.sb("sq", [128, 8, 512], BF16, pes)
            rstd = fw.sb("rstd", [128, 512], F32, pes)
            GS = fw.sb("GS", [128, 1, 8], F32, pes)
            pss = fw.ps("pss", [128, 512], F32, pes)
            pp = [fw.ps("ppA%d" % i, [128, 512], F32, pes) for i in range(4)]
            pv = [fw.ps("ppV%d" % i, [128, 512], F32, pes) for i in range(2)]
            ob = [fw.sb("obA%d" % i, [128, 512], BF16, pes) for i in range(4)]
            of = [fw.sb("ofA%d" % i, [128, 512], F32, pes) for i in range(2)]
            t1 = [fw.sb("t1A%d" % i, [128, 512], F32, pes) for i in range(2)]
            t2 = [fw.sb("t2A%d" % i, [128, 512], F32, pes) for i in range(2)]
            rc = [fw.sb("rc%d" % i, [128, 512], F32, pes) for i in range(2)]
            rs_ = [fw.sb("rs%d" % i, [128, 512], F32, pes) for i in range(2)]
            vst = [fw.sb("vst%d" % i, [128, 8, 65], BF16, pes) for i in range(2)]
            for v in vst:
                fw.op("dve", lambda: nc.vector.memset(v[:], 1.0), writes=[v])
            k = 0
            ko = 0
            for gi, (t0, n, isctx) in enumerate(groups()):
                j = 1 if isctx else 0
                x_ = xg[gi % 2]; h_ = hT[gi % 2]
                fw.dma("sp", lambda: nc.sync.dma_start(out=x_[:, :, :n], in_=xs[:, :, t0:t0 + n]), reads=[xs], writes=[x_])
                if not isctx:
                    r_c = rc[gi % 2]; r_s = rs_[gi % 2]
                    fw.dma("act", lambda: nc.scalar.dma_start(out=r_c[:], in_=I["rope_c"][:, t0:t0 + n]), reads=[I["rope_c"]], writes=[r_c])
                    fw.dma("act", lambda: nc.scalar.dma_start(out=r_s[:], in_=I["rope_s"][:, t0:t0 + n]), reads=[I["rope_s"]], writes=[r_s])
                norm_mod((sq, pss, rstd, GS), x_, n, gmix[:, l, :], l, 0, 1, j, out_bf=h_)

                def proj(colblk):
                    nonlocal k
                    p = pp[k % 4]; k += 1
                    for kc in range(8):
                        fw.op("pe", lambda: nc.tensor.matmul(p[:, :n], lhsT=w2[:, kc, colblk * 128:(colblk + 1) * 128], rhs=h_[:, kc, :n],
                                                           start=(kc == 0), stop=(kc == 7)), reads=[w2, h_], writes=[p], chain=True)
                    return p
                for blk in range(8):
                    p = proj(blk)
                    if blk < 6:
                        o = ob[ko % 4]; ko += 1
                        if blk % 2 == 0:
                            fw.op("act", lambda: nc.scalar.copy(out=o[:, :n], in_=p[:, :n]), reads=[p], writes=[o])
                        else:
                            fw.op("dve", lambda: nc.vector.tensor_copy(out=o[:, :n], in_=p[:, :n]), reads=[p], writes=[o])
                        dst = qa if blk < 3 else ka
                        fw.dma("sp", lambda: nc.sync.dma_start(out=dst[blk % 3, :, t0:t0 + n], in_=o[:, :n]), reads=[o], writes=[dst])
                    else:
                        o = of[blk % 2]
                        fw.op("act", lambda: nc.scalar.copy(out=o[:, :n], in_=p[:, :n]), reads=[p], writes=[o])
                        fw.dma("sp", lambda: nc.sync.dma_start(out=u5[blk - 6, :, t0:t0 + n], in_=o[:, :n]), reads=[o], writes=[u5])
                for which, base, dst in ((0, 8, qs), (1, 14, ks)):
                    for ti in range(3):
                        p = proj(base + ti)
                        o = ob[ko % 4]; ko += 1
                        if isctx:
                            fw.op("act", lambda: nc.scalar.copy(out=o[:, :n], in_=p[:, :n]), reads=[p], writes=[o])
                        else:
                            pr = proj(base + 3 + ti)
                            a = t1[ti % 2]; b = t2[ti % 2]
                            fw.op("dve", lambda: nc.vector.tensor_tensor(out=a[:, :n], in0=p[:, :n], in1=r_c[:, :n], op=ALU.mult), reads=[p, r_c], writes=[a])
                            fw.op("dve", lambda: nc.vector.tensor_tensor(out=b[:, :n], in0=pr[:, :n], in1=r_s[:, :n], op=ALU.mult), reads=[pr, r_s], writes=[b])
                            fw.op("pool", lambda: nc.gpsimd.tensor_tensor(out=o[:, :n], in0=a[:, :n], in1=b[:, :n], op=ALU.add), reads=[a, b], writes=[o])
                        fw.dma("sp", lambda: nc.sync.dma_start(out=dst[ti, :, t0:t0 + n], in_=o[:, :n]), reads=[o], writes=[dst])
                for tt in range(n // 128):
                    p = pv[tt % 2]; v = vst[tt % 2]
                    for kc in range(8):
                        fw.op("pe", lambda: nc.tensor.matmul(p[:, :], lhsT=h_[:, kc, tt * 128:(tt + 1) * 128], rhs=wv[:, kc, :],
                                                           start=(kc == 0), stop=(kc == 7)), reads=[wv, h_], writes=[p], chain=True)
                    fw.op("act", lambda: nc.scalar.copy(out=v[:, :, 0:64], in_=p[:, :].rearrange("p (h d) -> p h d", d=64)), reads=[p], writes=[v])
                    r0 = t0 + tt * 128
                    fw.dma("act", lambda: nc.scalar.dma_start(out=va[r0:r0 + 128, :].rearrange("t (h d) -> t h d", d=65), in_=v[:, 0:6, :]), reads=[v], writes=[va])
                    fw.dma("act", lambda: nc.scalar.dma_start(out=vs[r0:r0 + 128, :].rearrange("t (h d) -> t h d", d=65), in_=v[:, 6:8, :]), reads=[v], writes=[vs])


    def attn_norm(pes_t, po, n, h, t0, chtile, base, sink=None):
        rden, osb, ysb, pbc = pes_t
        if sink is not None:
            fw.op("dve", lambda: nc.vector.tensor_scalar(out=rden[64:65, :n], in0=po[64:65, :n], scalar1=sink[64:65, h:h + 1], scalar2=None, op0=ALU.add),
                  reads=[po, sink], writes=[rden])
            fw.op("dve", lambda: nc.vector.reciprocal(out=rden[64:65, :n], in_=rden[64:65, :n]), reads=[rden], writes=[rden])
        else:
            fw.op("dve", lambda: nc.vector.reciprocal(out=rden[64:65, :n], in_=po[64:65, :n]), reads=[po], writes=[rden])
        fw.op("pe", lambda: nc.tensor.matmul(pbc[0:64, :n], lhsT=ones_f[64:65, 0:64], rhs=rden[64:65, :n], start=True, stop=True),
              reads=[rden, ones_f], writes=[pbc])
        fw.op("act", lambda: nc.scalar.copy(out=osb[0:64, :n], in_=po[0:64, :n]), reads=[po], writes=[osb])
        fw.op("dve", lambda: nc.vector.tensor_tensor(out=ysb[0:64, :n], in0=osb[0:64, :n], in1=pbc[0:64, :n], op=ALU.mult), reads=[osb, pbc], writes=[ysb])
        fw.dma("sp", lambda: nc.sync.dma_start(out=yT[chtile, base:base + 64, t0:t0 + n], in_=ysb[0:64, :n]), reads=[ysb], writes=[yT])

    rp = fw.dram("rp", [1, 3072], F32)

    def phaseB(l, with_ctx):
        with ExitStack() as pes:
            z = fw.sb("zrp", [1, 3072], F32, pes)
            fw.op("dve", lambda: nc.vector.memset(z[:], 0.0), writes=[z])
            fw.dma("sp", lambda: nc.sync.dma_start(out=z[0:1, 64:64 + 2790], in_=I["na_rpb"][l:l + 1].rearrange("o h a b -> o (h a b)")), reads=[I["na_rpb"]], writes=[z])
            fw.dma("sp", lambda: nc.sync.dma_start(out=rp[:], in_=z[:]), reads=[z], writes=[rp])
            BB = fw.sb("BB", [64, 6, 15, 64], F32, pes)
            for h in range(6):
                src = bass.AP(tensor=rp.t.tensor, offset=64 + h * 465 + 15 - 63, ap=[[1, 64], [31, 15], [1, 64]])
                fw.dma("sp", lambda: nc.sync.dma_start(out=BB[:, h, :, :], in_=src), reads=[rp], writes=[BB])
            pen = fw.sb("napen", [128, 64], F32, pes)
            fw.dma("sp", lambda: nc.sync.dma_start(out=pen[:], in_=I["na_pen"][:]), reads=[I["na_pen"]], writes=[pen])
            biasT = fw.sb("biasT", [128, 6, 14, 64], BF16, pes)
            with ExitStack() as pes2:
                pb = [fw.ps("pbias%d" % i, [128, 64], F32, pes2) for i in range(2)]
                it = 0
                for h in range(6):
                    for a in range(14):
                        p = pb[it % 2]; it += 1
                        fw.op("pe", lambda: nc.tensor.transpose(out=p[:, :], in_=BB[:, h, a:a + 2, :].rearrange("p a k -> p (a k)"), identity=ident_f[0:64, 0:64]),
                              reads=[BB, ident_f], writes=[p])
                        fw.op("dve", lambda: nc.vector.scalar_tensor_tensor(out=biasT[:, h, a, :], in0=p[:, ::-1], scalar=8.0, in1=pen[:], op0=ALU.mult, op1=ALU.add),
                              reads=[p, pen], writes=[biasT])
                fw.barrier()
            qT = fw.sb("qT", [128, NTOK], BF16, pes)
            kT = fw.sb("kT", [128, NTOK], BF16, pes)
            v0 = fw.sb("v0", [128, 66, 2, 65], BF16, pes)
            v1 = fw.sb("v1", [128, 63, 2, 65], BF16, pes)
            pc = fw.ps("pc", [128, 2, 512], F32, pes)
            pn = [fw.ps("pn%d" % i, [128, 4, 4, 64], F32, pes) for i in range(2)]
            po = fw.ps("po", [128, 512], F32, pes)
            pbc = fw.ps("pbc", [128, 512], F32, pes)
            PcT = fw.sb("PcT", [128, 2, 512], BF16, pes)
            PnT = [fw.sb("PnT%d" % i, [128, 4, 4, 64], BF16, pes) for i in range(2)]
            rden = fw.sb("rden", [128, 512], F32, pes)
            osb = fw.sb("osb", [128, 512], F32, pes)
            ysb = fw.sb("ysb", [128, 512], BF16, pes)
            nt = (rden, osb, ysb, pbc)
            ih = 0
            for jp in range(3):
                fw.dma("sp", lambda: nc.sync.dma_start(out=qT[:], in_=qa[jp]), reads=[qa], writes=[qT])
                fw.dma("act", lambda: nc.scalar.dma_start(out=kT[:], in_=ka[jp]), reads=[ka], writes=[kT])
                fw.dma("sp", lambda: nc.sync.dma_start(out=v0[:].rearrange("p t h d -> p t (h d)"),
                                                      in_=va[:, jp * 130:(jp + 1) * 130].rearrange("(t p) c -> p t c", p=128)), reads=[va], writes=[v0])
                fw.dma("act", lambda: nc.scalar.dma_start(out=v1[:].rearrange("p t h d -> p t (h d)"),
                                                        in_=va[64:64 + 63 * 128, jp * 130:(jp + 1) * 130].rearrange("(t p) c -> p t c", p=128)), reads=[va], writes=[v1])
                for hh in range(2):
                    h = 2 * jp + hh
                    b0 = 64 * hh
                    for G in range(16):
                        t0 = 512 * G
                        for ct in range(2):
                            fw.op("pe", lambda: nc.tensor.matmul(pc[:, ct, :], lhsT=kT[b0:b0 + 64, SEQ + 128 * ct:SEQ + 128 * ct + 128], rhs=qT[b0:b0 + 64, t0:t0 + 512],
                                                               start=True, stop=True), reads=[kT, qT], writes=[pc])
                        fw.op("act", lambda: nc.scalar.activation(out=PcT[:], in_=pc[:], func=AF.Exp, scale=0.125), reads=[pc], writes=[PcT])
                        for half in range(2):
                            p_ = pn[ih % 2]; P_ = PnT[ih % 2]; ih += 1
                            for rr in range(4):
                                r = 8 * G + 4 * half + rr
                                rs = min(max(r - 4, 0), 120)
                                for j in range(4):
                                    k0 = (rs + 2 * j) * 64
                                    a = rs + 2 * j - r + 7
                                    fw.op("pe", lambda: nc.tensor.matmul(p_[:, rr, j, :], lhsT=kT[b0:b0 + 64, k0:k0 + 128], rhs=qT[b0:b0 + 64, r * 64:r * 64 + 64],
                                                                       start=True, stop=False), reads=[kT, qT], writes=[p_], chain=True)
                                    fw.op("pe", lambda: nc.tensor.matmul(p_[:, rr, j, :], lhsT=ident_b[:, :], rhs=biasT[:, h, a, :],
                                                                       start=False, stop=True), reads=[biasT, ident_b], writes=[p_], chain=True)
                            fw.op("act", lambda: nc.scalar.activation(out=P_[:], in_=p_[:], func=AF.Exp, scale=0.125), reads=[p_], writes=[P_])
                            for rr in range(4):
                                r = 8 * G + 4 * half + rr
                                rs = min(max(r - 4, 0), 120)
                                c0 = (4 * half + rr) * 64
                                for j in range(4):
                                    k0 = (rs + 2 * j) * 64
                                    vt = v0[:, k0 // 128, hh, :] if rs % 2 == 0 else v1[:, (k0 - 64) // 128, hh, :]
                                    fw.op("pe", lambda: nc.tensor.matmul(po[0:65, c0:c0 + 64], lhsT=vt, rhs=P_[:, rr, j, :], start=(j == 0), stop=False),
                                          reads=[v0, v1, P_], writes=[po], chain=True)
                                for ct in range(2):
                                    fw.op("pe", lambda: nc.tensor.matmul(po[0:65, c0:c0 + 64], lhsT=v0[:, 64 + ct, hh, :], rhs=PcT[:, ct, c0:c0 + 64], start=False, stop=(ct == 1)),
                                          reads=[v0, PcT], writes=[po], chain=True)
                        attn_norm(nt, po, 512, h, t0, jp, b0)
                    if with_ctx:
                        for ct in range(2):
                            fw.op("pe", lambda: nc.tensor.matmul(pc[:, ct, :CTXL], lhsT=kT[b0:b0 + 64, SEQ + 128 * ct:SEQ + 128 * ct + 128], rhs=qT[b0:b0 + 64, SEQ:SEQ + CTXL],
                                                               start=True, stop=True), reads=[kT, qT], writes=[pc])
                        fw.op("act", lambda: nc.scalar.activation(out=PcT[:, :, :CTXL], in_=pc[:, :, :CTXL], func=AF.Exp, scale=0.125), reads=[pc], writes=[PcT])
                        for ct in range(2):
                            fw.op("pe", lambda: nc.tensor.matmul(po[0:65, :CTXL], lhsT=v0[:, 64 + ct, hh, :], rhs=PcT[:, ct, :CTXL], start=(ct == 0), stop=(ct == 1)),
                                  reads=[v0, PcT], writes=[po], chain=True)
                        attn_norm(nt, po, CTXL, h, SEQ, jp, b0)

    def phaseD(l, with_ctx):
        with ExitStack() as pes:
            sk = fw.sb("sinks", [128, 6], F32, pes)
            fw.dma("sp", lambda: nc.sync.dma_start(out=sk[64:65, :], in_=I["sw_sinks"][l:l + 1, :]), reads=[I["sw_sinks"]], writes=[sk])
            fw.op("act", lambda: nc.scalar.activation(out=sk[64:65, :], in_=sk[64:65, :], func=AF.Exp), reads=[sk], writes=[sk])
            ml = fw.sb("mask_l", [128, 128], BF16, pes)
            mu = fw.sb("mask_u", [128, 128], BF16, pes)
            fw.dma("sp", lambda: nc.sync.dma_start(out=ml[:], in_=I["mask_l"][:]), reads=[I["mask_l"]], writes=[ml])
            fw.dma("sp", lambda: nc.sync.dma_start(out=mu[:], in_=I["mask_u"][:]), reads=[I["mask_u"]], writes=[mu])
            qT = fw.sb("qTs", [128, NTOK], BF16, pes)
            kT = fw.sb("kTs", [128, NTOK], BF16, pes)
            vS = fw.sb("vS", [128, 66, 2, 65], BF16, pes)
            fw.dma("sp", lambda: nc.sync.dma_start(out=vS[:].rearrange("p t h d -> p t (h d)"), in_=vs[:, :].rearrange("(t p) c -> p t c", p=128)), reads=[vs], writes=[vS])
            pc = fw.ps("pcs", [128, 2, 512], F32, pes)
            pn = [fw.ps("pns%d" % i, [128, 3, 128], F32, pes) for i in range(2)]
            po = fw.ps("pos", [128, 512], F32, pes)
            pbc = fw.ps("pbcs", [128, 512], F32, pes)
            PcT = fw.sb("PcTs", [128, 2, 512], BF16, pes)
            PnT = [fw.sb("PnTs%d" % i, [128, 3, 128], BF16, pes) for i in range(2)]
            rden = fw.sb("rdens", [128, 512], F32, pes)
            osb = fw.sb("osbs", [128, 512], F32, pes)
            ysb = fw.sb("ysbs", [128, 512], BF16, pes)
            nt = (rden, osb, ysb, pbc)
            ih = 0
            NB = SEQ // 128
            for jp in range(3):
                fw.dma("sp", lambda: nc.sync.dma_start(out=qT[:], in_=qs[jp]), reads=[qs], writes=[qT])
                fw.dma("act", lambda: nc.scalar.dma_start(out=kT[:], in_=ks[jp]), reads=[ks], writes=[kT])
                for hh in range(2):
                    h = 2 * jp + hh
                    kvh = h // 3
                    b0 = 64 * hh
                    for G in range(16):
                        t0 = 512 * G
                        for ct in range(2):
                            fw.op("pe", lambda: nc.tensor.matmul(pc[:, ct, :], lhsT=kT[b0:b0 + 64, SEQ + 128 * ct:SEQ + 128 * ct + 128], rhs=qT[b0:b0 + 64, t0:t0 + 512],
                                                               start=True, stop=True), reads=[kT, qT], writes=[pc])
                        fw.op("act", lambda: nc.scalar.activation(out=PcT[:], in_=pc[:], func=AF.Exp, scale=0.125), reads=[pc], writes=[PcT])
                        for nb_ in range(4):
                            n = 4 * G + nb_
                            p_ = pn[ih % 2]; P_ = PnT[ih % 2]; ih += 1
                            kbs = [kb for kb in (n - 1, n, n + 1) if 0 <= kb < NB]
                            lo = kbs[0] - (n - 1)
                            hi = kbs[-1] - (n - 1) + 1
                            for kb in kbs:
                                ki = kb - (n - 1)
                                fw.op("pe", lambda: nc.tensor.matmul(p_[:, ki, :], lhsT=kT[b0:b0 + 64, kb * 128:kb * 128 + 128], rhs=qT[b0:b0 + 64, n * 128:n * 128 + 128],
                                                                   start=True, stop=(kb == n)), reads=[kT, qT], writes=[p_], chain=True)
                                if kb != n:
                                    mk = ml if kb < n else mu
                                    fw.op("pe", lambda: nc.tensor.matmul(p_[:, ki, :], lhsT=ident_b[:, :], rhs=mk[:, :], start=False, stop=True),
                                          reads=[mk, ident_b], writes=[p_], chain=True)
                            fw.op("act", lambda: nc.scalar.activation(out=P_[:, lo:hi, :], in_=p_[:, lo:hi, :], func=AF.Exp, scale=0.125), reads=[p_], writes=[P_])
                            c0 = nb_ * 128
                            for kb in kbs:
                                ki = kb - (n - 1)
                                fw.op("pe", lambda: nc.tensor.matmul(po[0:65, c0:c0 + 128], lhsT=vS[:, kb, kvh, :], rhs=P_[:, ki, :], start=(kb == kbs[0]), stop=False),
                                      reads=[vS, P_], writes=[po], chain=True)
                            for ct in range(2):
                                fw.op("pe", lambda: nc.tensor.matmul(po[0:65, c0:c0 + 128], lhsT=vS[:, 64 + ct, kvh, :], rhs=PcT[:, ct, c0:c0 + 128], start=False, stop=(ct == 1)),
                                      reads=[vS, PcT], writes=[po], chain=True)
                        attn_norm(nt, po, 512, h, t0, 5 + jp, b0, sink=sk)
                    if with_ctx:
                        for ct in range(2):
                            fw.op("pe", lambda: nc.tensor.matmul(pc[:, ct, :CTXL], lhsT=kT[b0:b0 + 64, SEQ + 128 * ct:SEQ + 128 * ct + 128], rhs=qT[b0:b0 + 64, SEQ:SEQ + CTXL],
                                                               start=True, stop=True), reads=[kT, qT], writes=[pc])
                        fw.op("act", lambda: nc.scalar.activation(out=PcT[:, :, :CTXL], in_=pc[:, :, :CTXL], func=AF.Exp, scale=0.125), reads=[pc], writes=[PcT])
                        for ct in range(2):
                            fw.op("pe", lambda: nc.tensor.matmul(po[0:65, :CTXL], lhsT=vS[:, 64 + ct, kvh, :], rhs=PcT[:, ct, :CTXL], start=(ct == 0), stop=(ct == 1)),
                                  reads=[vS, PcT], writes=[po], chain=True)
                        attn_norm(nt, po, CTXL, h, SEQ, 5 + jp, b0, sink=sk)


    yf = fw.dram("yf", [2, 128, NTOK], F32)
    TWO_PI = 6.283185307179586
    C1 = 6.28125
    C2 = TWO_PI - 6.28125
    PI = 3.141592653589793

    def phaseC(l, with_ctx):
        with ExitStack() as pes:
            def tl(name, shape, dt=F32):
                return fw.sb(name, shape, dt, pes)
            scr_i = tl("scr_i", [128, 512], I32)
            scr_k = tl("scr_k", [128, 512])
            scr_t = tl("scr_t", [128, 512])
            scr_a = tl("scr_a", [128, 512])
            scr_b = tl("scr_b", [128, 512])

            def dve(fn, reads, writes):
                fw.op("dve", fn, reads=reads, writes=writes)

            def reduce_angle(ang, n, srcs):
                dve(lambda: nc.vector.tensor_scalar(out=scr_k[:, :n], in0=ang, scalar1=1.0 / TWO_PI, scalar2=None, op0=ALU.mult), srcs, [scr_k])
                dve(lambda: nc.vector.tensor_copy(out=scr_i[:, :n], in_=scr_k[:, :n]), [scr_k], [scr_i])
                dve(lambda: nc.vector.tensor_copy(out=scr_k[:, :n], in_=scr_i[:, :n]), [scr_i], [scr_k])
                dve(lambda: nc.vector.scalar_tensor_tensor(out=scr_a[:, :n], in0=scr_k[:, :n], scalar=-C1, in1=ang, op0=ALU.mult, op1=ALU.add), [scr_k] + srcs, [scr_a])
                dve(lambda: nc.vector.scalar_tensor_tensor(out=scr_a[:, :n], in0=scr_k[:, :n], scalar=-C2, in1=scr_a[:, :n], op0=ALU.mult, op1=ALU.add), [scr_k, scr_a], [scr_a])
                wrap(scr_a, n)

            def wrap(t, n):
                dve(lambda: nc.vector.tensor_scalar(out=scr_t[:, :n], in0=t[:, :n], scalar1=PI, scalar2=None, op0=ALU.is_gt), [t], [scr_t])
                dve(lambda: nc.vector.scalar_tensor_tensor(out=t[:, :n], in0=scr_t[:, :n], scalar=-TWO_PI, in1=t[:, :n], op0=ALU.mult, op1=ALU.add), [scr_t, t], [t])
                dve(lambda: nc.vector.tensor_scalar(out=scr_t[:, :n], in0=t[:, :n], scalar1=-PI, scalar2=None, op0=ALU.is_lt), [t], [scr_t])
                dve(lambda: nc.vector.scalar_tensor_tensor(out=t[:, :n], in0=scr_t[:, :n], scalar=TWO_PI, in1=t[:, :n], op0=ALU.mult, op1=ALU.add), [scr_t, t], [t])

            def sincos(ang, n, srcs, out_s, out_c, outs):
                reduce_angle(ang, n, srcs)
                fw.op("act", lambda: nc.scalar.activation(out=out_s, in_=scr_a[:, :n], func=AF.Sin), reads=[scr_a], writes=outs)
                dve(lambda: nc.vector.tensor_scalar(out=scr_b[:, :n], in0=scr_a[:, :n], scalar1=PI / 2, scalar2=None, op0=ALU.add), [scr_a], [scr_b])
                wrap(scr_b, n)
                fw.op("act", lambda: nc.scalar.activation(out=out_c, in_=scr_b[:, :n], func=AF.Sin), reads=[scr_b], writes=outs)

            are = tl("are", [128, 16]); aim = tl("aim", [128, 16]); stp = tl("stp", [128, 16])
            with nc.allow_non_contiguous_dma(reason="tiny s5 params"):
                for nm, dst in (("s5_a_re", are), ("s5_a_im", aim)):
                    for d_ in range(2):
                        fw.dma("sp", lambda: nc.sync.dma_start(out=dst[:, d_ * 8:(d_ + 1) * 8], in_=I[nm][l, d_].rearrange("g p -> (g p)").rearrange("(i q) -> q i", q=128)),
                               reads=[I[nm]], writes=[dst])
                for d_ in range(2):
                    for g2 in range(2):
                        src = bass.AP(tensor=I["s5_log_step"].t.tensor, offset=l * 32 + d_ * 16 + g2, ap=[[0, 64], [2, 8]])
                        fw.dma("sp", lambda: nc.sync.dma_start(out=stp[64 * g2:64 * g2 + 64, d_ * 8:(d_ + 1) * 8], in_=src), reads=[I["s5_log_step"]], writes=[stp])
            fw.op("act", lambda: nc.scalar.activation(out=stp[:], in_=stp[:], func=AF.Exp), reads=[stp], writes=[stp])
            dve(lambda: nc.vector.tensor_scalar(out=are[:], in0=are[:], scalar1=-1e-4, scalar2=None, op0=ALU.min), [are], [are])
            lr = tl("lr", [128, 16]); li = tl("li", [128, 16]); rr = tl("rr", [128, 16])
            dve(lambda: nc.vector.tensor_tensor(out=lr[:], in0=are[:], in1=stp[:], op=ALU.mult), [are, stp], [lr])
            dve(lambda: nc.vector.tensor_tensor(out=li[:], in0=aim[:], in1=stp[:], op=ALU.mult), [aim, stp], [li])
            fw.op("act", lambda: nc.scalar.activation(out=rr[:], in_=lr[:], func=AF.Exp), reads=[lr], writes=[rr])
            s1 = tl("s1", [128, 16]); c1 = tl("c1", [128, 16])
            sincos(li[:, :], 16, [li], s1[:, :], c1[:, :], [s1, c1])
            sL = {}; cL = {}
            angL = tl("angL", [128, 16])
            for L in (512, 256):
                sL[L] = tl("sL%d" % L, [128, 16]); cL[L] = tl("cL%d" % L, [128, 16])
                dve(lambda: nc.vector.tensor_scalar(out=angL[:], in0=li[:], scalar1=float(L), scalar2=None, op0=ALU.mult), [li], [angL])
                sincos(angL[:, :], 16, [angL], sL[L][:, :], cL[L][:, :], [sL[L], cL[L]])
            lbr = tl("lbr", [128, 16]); lbi = tl("lbi", [128, 16]); den = tl("den", [128, 16]); tq = tl("tq", [128, 16])
            cr = tl("cr", [128, 16]); ci = tl("ci", [128, 16]); nci = tl("nci", [128, 16])
            dve(lambda: nc.vector.tensor_tensor(out=lbr[:], in0=rr[:], in1=c1[:], op=ALU.mult), [rr, c1], [lbr])
            dve(lambda: nc.vector.tensor_scalar(out=lbr[:], in0=lbr[:], scalar1=-1.0, scalar2=None, op0=ALU.add), [lbr], [lbr])
            dve(lambda: nc.vector.tensor_tensor(out=lbi[:], in0=rr[:], in1=s1[:], op=ALU.mult), [rr, s1], [lbi])
            dve(lambda: nc.vector.tensor_tensor(out=den[:], in0=are[:], in1=are[:], op=ALU.mult), [are], [den])
            dve(lambda: nc.vector.tensor_tensor(out=tq[:], in0=aim[:], in1=aim[:], op=ALU.mult), [aim], [tq])
            dve(lambda: nc.vector.tensor_tensor(out=den[:], in0=den[:], in1=tq[:], op=ALU.add), [den, tq], [den])
            dve(lambda: nc.vector.reciprocal(out=den[:], in_=den[:]), [den], [den])
            dve(lambda: nc.vector.tensor_tensor(out=cr[:], in0=lbr[:], in1=are[:], op=ALU.mult), [lbr, are], [cr])
            dve(lambda: nc.vector.tensor_tensor(out=tq[:], in0=lbi[:], in1=aim[:], op=ALU.mult), [lbi, aim], [tq])
            dve(lambda: nc.vector.tensor_tensor(out=cr[:], in0=cr[:], in1=tq[:], op=ALU.add), [cr, tq], [cr])
            dve(lambda: nc.vector.tensor_tensor(out=cr[:], in0=cr[:], in1=den[:], op=ALU.mult), [cr, den], [cr])
            dve(lambda: nc.vector.tensor_tensor(out=ci[:], in0=lbi[:], in1=are[:], op=ALU.mult), [lbi, are], [ci])
            dve(lambda: nc.vector.tensor_tensor(out=tq[:], in0=lbr[:], in1=aim[:], op=ALU.mult), [lbr, aim], [tq])
            dve(lambda: nc.vector.tensor_tensor(out=ci[:], in0=ci[:], in1=tq[:], op=ALU.subtract), [ci, tq], [ci])
            dve(lambda: nc.vector.tensor_tensor(out=ci[:], in0=ci[:], in1=den[:], op=ALU.mult), [ci, den], [ci])
            dve(lambda: nc.vector.tensor_scalar(out=nci[:], in0=ci[:], scalar1=-1.0, scalar2=None, op0=ALU.mult), [ci], [nci])
            bsr = tl("bsr", [128, 2, 2, 128]); bsi = tl("bsi", [128, 2, 2, 128])
            bbr = tl("bbr", [128, 2, 2, 128]); bbi = tl("bbi", [128, 2, 2, 128])
            csr = tl("csr", [128, 2, 2, 128]); csi = tl("csi", [128, 2, 2, 128])
            for t_ in (bsr, bsi, csr, csi):
                dve(lambda: nc.vector.memset(t_[:], 0.0), [], [t_])
            qn = 0
            for d_ in range(2):
                for g in range(16):
                    i = g // 2; g2 = g % 2; i4 = i // 4; im = i % 4
                    for nm, dst in (("s5_b_re", bsr), ("s5_b_im", bsi)):
                        q = ("sp", nc.sync) if qn % 2 == 0 else ("act", nc.scalar); qn += 1
                        fw.dma(q[0], lambda: q[1].dma_start(out=dst[64 * g2:64 * g2 + 64, d_, i4, 32 * im + 16 * g2:32 * im + 16 * g2 + 16], in_=I[nm][l, d_, g]),
                               reads=[I[nm]], writes=[dst])
                    for nm, dst in (("s5_c_re", csr), ("s5_c_im", csi)):
                        q = ("sp", nc.sync) if qn % 2 == 0 else ("act", nc.scalar); qn += 1
                        fw.dma(q[0], lambda: q[1].dma_start(out=dst[32 * im + 16 * g2:32 * im + 16 * g2 + 16, d_, i4, 64 * g2:64 * g2 + 64], in_=I[nm][l, d_, g]),
                               reads=[I[nm]], writes=[dst])
            for d_ in range(2):
                for i in range(8):
                    col = d_ * 8 + i; i4 = i // 4; im = i % 4
                    sl = slice(32 * im, 32 * im + 32)
                    dve(lambda: nc.vector.tensor_scalar(out=bbr[:, d_, i4, sl], in0=bsr[:, d_, i4, sl], scalar1=cr[:, col:col + 1], scalar2=None, op0=ALU.mult), [bsr, cr], [bbr])
                    dve(lambda: nc.vector.scalar_tensor_tensor(out=bbr[:, d_, i4, sl], in0=bsi[:, d_, i4, sl], scalar=nci[:, col:col + 1], in1=bbr[:, d_, i4, sl],
                                                               op0=ALU.mult, op1=ALU.add), [bsi, nci, bbr], [bbr])
                    dve(lambda: nc.vector.tensor_scalar(out=bbi[:, d_, i4, sl], in0=bsi[:, d_, i4, sl], scalar1=cr[:, col:col + 1], scalar2=None, op0=ALU.mult), [bsi, cr], [bbi])
                    dve(lambda: nc.vector.scalar_tensor_tensor(out=bbi[:, d_, i4, sl], in0=bsr[:, d_, i4, sl], scalar=ci[:, col:col + 1], in1=bbi[:, d_, i4, sl],
                                                               op0=ALU.mult, op1=ALU.add), [bsr, ci, bbi], [bbi])
            BbT = tl("BbT", [128, 2, 2, 2, 128], BF16)
            CT = tl("CT", [128, 2, 2, 2, 128], BF16)
            with ExitStack() as pes2:
                ptp = [fw.ps("ptp%d" % k_, [128, 128], F32, pes2) for k_ in range(2)]
                it = 0
                for d_ in range(2):
                    for i4 in range(2):
                        for ri, (bsrc, csrc) in enumerate(((bbr, csr), (bbi, csi))):
                            p = ptp[it % 2]; it += 1
                            fw.op("pe", lambda: nc.tensor.transpose(out=p[:, :], in_=bsrc[:, d_, i4, :], identity=ident_f[:]), reads=[bsrc, ident_f], writes=[p])
                            fw.op("act", lambda: nc.scalar.copy(out=BbT[:, d_, i4, ri, :], in_=p[:, :]), reads=[p], writes=[BbT])
                            p = ptp[it % 2]; it += 1
                            fw.op("pe", lambda: nc.tensor.transpose(out=p[:, :], in_=csrc[:, d_, i4, :], identity=ident_f[:]), reads=[csrc, ident_f], writes=[p])
                            fw.op("act", lambda: nc.scalar.mul(out=CT[:, d_, i4, ri, :], in_=p[:, :], mul=(1.0 if ri == 0 else -1.0)), reads=[p], writes=[CT])
                fw.barrier()
            tcos = tl("tcos", [128, 16, 512]); tsin = tl("tsin", [128, 16, 512])
            rfl = [tl("rfl%d" % k_, [128, 512]) for k_ in range(2)]
            io = tl("io512", [128, 512])
            fw.dma("sp", lambda: nc.sync.dma_start(out=io[:], in_=I["iota512"][:]), reads=[I["iota512"]], writes=[io])
            angt = tl("angt", [128, 512])
            for col in range(16):
                dve(lambda: nc.vector.tensor_scalar(out=angt[:], in0=io[:], scalar1=li[:, col:col + 1], scalar2=None, op0=ALU.mult), [io, li], [angt])
                sincos(angt[:, :], 512, [angt], tsin[:, col, :], tcos[:, col, :], [tsin, tcos])
            dsk = tl("dsk", [128, 2]); bgl = tl("bgl", [128, 2])
            wgl = tl("wgl", [128, 2, 256], BF16)
            with nc.allow_non_contiguous_dma(reason="tiny"):
                fw.dma("sp", lambda: nc.sync.dma_start(out=dsk[:], in_=I["s5_d"][l].rearrange("(c p) -> p c", p=128)), reads=[I["s5_d"]], writes=[dsk])
                fw.dma("sp", lambda: nc.sync.dma_start(out=bgl[:], in_=I["s5_b_glu"][l].rearrange("(c p) -> p c", p=128)), reads=[I["s5_b_glu"]], writes=[bgl])
            fw.dma("pool", lambda: nc.gpsimd.dma_start(out=wgl[:], in_=I["s5_w_glu"][l].rearrange("(c p) n -> p c n", p=128)), reads=[I["s5_w_glu"]], writes=[wgl])
            zst = tl("zst", [128, 16, 2])
            zin = tl("zin", [128, 16, 2])
            dve(lambda: nc.vector.memset(zin[:], 0.0), [], [zin])
            uf = [tl("uf%d" % k_, [128, 2, 512]) for k_ in range(2)]
            ub = [tl("ub%d" % k_, [128, 2, 512], BF16) for k_ in range(2)]
            pA = [fw.ps("pA%d" % k_, [128, 512], F32, pes) for k_ in range(2)]
            pB = [fw.ps("pB%d" % k_, [128, 512], F32, pes) for k_ in range(2)]
            py = [fw.ps("py%d" % k_, [128, 512], F32, pes) for k_ in range(2)]
            pg = [fw.ps("pg%d" % k_, [128, 512], F32, pes) for k_ in range(2)]
            W = {}
            for nm in ("t1", "t2", "t3", "t4", "dr", "di", "zr", "zi"):
                W[nm] = [tl(nm + "_%d" % k_, [128, 512]) for k_ in range(2)]
            xr = [tl("xr%d" % k_, [128, 512], BF16) for k_ in range(2)]
            xi = [tl("xi%d" % k_, [128, 512], BF16) for k_ in range(2)]
            ysb = [tl("ysbC%d" % k_, [128, 2, 512]) for k_ in range(2)]
            yfl = tl("yfl", [128, 2, 512])
            gq = tl("gq", [128, 2, 512]); gp = tl("gp", [128, 2, 512]); gg = tl("gg", [128, 2, 512])
            gb = tl("gb", [128, 2, 512], BF16); sg = tl("sg", [128, 2, 512]); yo = tl("yo", [128, 2, 512], BF16)
            lat = [(g_ * 512, 512) for g_ in range(16)]
            order = {0: [(SEQ, CTXL)] + lat, 1: [(SEQ, CTXL)] + lat[::-1]}
            it = 0
            ci_ = 0
            for d_ in range(2):
                prevL = None
                for (t0, L) in order[d_]:
                    isctx = t0 >= SEQ
                    u_ = uf[ci_ % 2]; ub_ = ub[ci_ % 2]; ys_ = ysb[ci_ % 2]; ci_ += 1
                    fw.dma("sp", lambda: nc.sync.dma_start(out=u_[:, :, :L], in_=u5[:, :, t0:t0 + L].rearrange("c p t -> p c t")), reads=[u5], writes=[u_])
                    if d_ == 0:
                        fw.op("act", lambda: nc.scalar.copy(out=ub_[:, :, :L], in_=u_[:, :, :L]), reads=[u_], writes=[ub_])
                    else:
                        fw.op("act", lambda: nc.scalar.copy(out=ub_[:, :, :L], in_=u_[:, :, L - 1::-1] if False else u_[:, :, :L][:, :, ::-1]), reads=[u_], writes=[ub_])
                        fw.dma("act", lambda: nc.scalar.dma_start(out=yfl[:, :, :L], in_=yf[:, :, t0:t0 + L].rearrange("c p t -> p c t")), reads=[yf], writes=[yfl])
                    for i in range(8):
                        col = d_ * 8 + i; i4 = i // 4; im = i % 4
                        k_ = it % 2; it += 1
                        A = pA[k_]; B = pB[k_]
                        t1, t2, t3, t4 = W["t1"][k_], W["t2"][k_], W["t3"][k_], W["t4"][k_]
                        dr, di, zr, zi = W["dr"][k_], W["di"][k_], W["zr"][k_], W["zi"][k_]
                        u1, u2, u3, u4 = t1, t2, t3, t4
                        rf = rfl[k_]
                        fw.op("pool", lambda: nc.gpsimd.tensor_scalar(out=rf[:, :L], in0=io[:, :L], scalar1=0.0, scalar2=rr[:, col:col + 1], op0=ALU.mult, op1=ALU.add), reads=[io, rr], writes=[rf])
                        xr_, xi_ = xr[k_], xi[k_]
                        ps_ = slice(32 * im, 32 * im + 32)
                        fw.op("pe", lambda: nc.tensor.matmul(A[:, :L], lhsT=BbT[ps_, d_, i4, 0, :], rhs=ub_[ps_, i4, :L], start=True, stop=True, tile_position=(32 * im, 0)),
                              reads=[BbT, ub_], writes=[A])
                        fw.op("pe", lambda: nc.tensor.matmul(B[:, :L], lhsT=BbT[ps_, d_, i4, 1, :], rhs=ub_[ps_, i4, :L], start=True, stop=True, tile_position=(32 * im, 0)),
                              reads=[BbT, ub_], writes=[B])
                        if prevL is not None:
                            dve(lambda: nc.vector.tensor_scalar(out=zin[:, col, 0:1], in0=zst[:, col, 1:2], scalar1=sL[prevL][:, col:col + 1], scalar2=None, op0=ALU.mult), [zst, sL[prevL]], [zin])
                            dve(lambda: nc.vector.scalar_tensor_tensor(out=zin[:, col, 0:1], in0=zst[:, col, 0:1], scalar=cL[prevL][:, col:col + 1], in1=zin[:, col, 0:1],
                                                                       op0=ALU.mult, op1=ALU.subtract), [zst, cL[prevL], zin], [zin])
                            dve(lambda: nc.vector.tensor_scalar(out=zin[:, col, 1:2], in0=zst[:, col, 1:2], scalar1=cL[prevL][:, col:col + 1], scalar2=None, op0=ALU.mult), [zst, cL[prevL]], [zin])
                            dve(lambda: nc.vector.scalar_tensor_tensor(out=zin[:, col, 1:2], in0=zst[:, col, 0:1], scalar=sL[prevL][:, col:col + 1], in1=zin[:, col, 1:2],
                                                                       op0=ALU.mult, op1=ALU.add), [zst, sL[prevL], zin], [zin])
                        cs = tcos[:, col, :L]; sn = tsin[:, col, :L]
                        dve(lambda: nc.vector.tensor_tensor(out=t1[:, :L], in0=A[:, :L], in1=cs, op=ALU.mult), [A, tcos], [t1])
                        dve(lambda: nc.vector.tensor_tensor(out=t2[:, :L], in0=B[:, :L], in1=sn, op=ALU.mult), [B, tsin], [t2])
                        fw.op("pool", lambda: nc.gpsimd.tensor_tensor(out=dr[:, :L], in0=t1[:, :L], in1=t2[:, :L], op=ALU.add), reads=[t1, t2], writes=[dr])
                        dve(lambda: nc.vector.tensor_tensor(out=t3[:, :L], in0=B[:, :L], in1=cs, op=ALU.mult), [B, tcos], [t3])
                        dve(lambda: nc.vector.tensor_tensor(out=t4[:, :L], in0=A[:, :L], in1=sn, op=ALU.mult), [A, tsin], [t4])
                        fw.op("pool", lambda: nc.gpsimd.tensor_tensor(out=di[:, :L], in0=t3[:, :L], in1=t4[:, :L], op=ALU.subtract), reads=[t3, t4], writes=[di])
                        dve(lambda: nc.vector.tensor_tensor_scan(out=zr[:, :L], data0=rf[:, :L], data1=dr[:, :L], initial=zin[:, col, 0:1], op0=ALU.mult, op1=ALU.add),
                            [rf, dr, zin], [zr])
                        dve(lambda: nc.vector.tensor_tensor_scan(out=zi[:, :L], data0=rf[:, :L], data1=di[:, :L], initial=zin[:, col, 1:2], op0=ALU.mult, op1=ALU.add),
                            [rf, di, zin], [zi])
                        dve(lambda: nc.vector.tensor_copy(out=zst[:, col, 0:1], in_=zr[:, L - 1:L]), [zr], [zst])
                        dve(lambda: nc.vector.tensor_copy(out=zst[:, col, 1:2], in_=zi[:, L - 1:L]), [zi], [zst])
                        fw.op("pool", lambda: nc.gpsimd.tensor_tensor(out=u1[:, :L], in0=zr[:, :L], in1=cs, op=ALU.mult), reads=[zr, tcos], writes=[u1])
                        fw.op("pool", lambda: nc.gpsimd.tensor_tensor(out=u2[:, :L], in0=zi[:, :L], in1=sn, op=ALU.mult), reads=[zi, tsin], writes=[u2])
                        fw.op("pool", lambda: nc.gpsimd.tensor_tensor(out=xr_[:, :L], in0=u1[:, :L], in1=u2[:, :L], op=ALU.subtract), reads=[u1, u2], writes=[xr_])
                        dve(lambda: nc.vector.tensor_tensor(out=u3[:, :L], in0=zr[:, :L], in1=sn, op=ALU.mult), [zr, tsin], [u3])
                        fw.op("pool", lambda: nc.gpsimd.tensor_tensor(out=u4[:, :L], in0=zi[:, :L], in1=cs, op=ALU.mult), reads=[zi, tcos], writes=[u4])
                        fw.op("pool", lambda: nc.gpsimd.tensor_tensor(out=xi_[:, :L], in0=u3[:, :L], in1=u4[:, :L], op=ALU.add), reads=[u3, u4], writes=[xi_])
                        if isctx and not with_ctx:
                            continue
                        yq = py[i4]
                        fw.op("pe", lambda: nc.tensor.matmul(yq[ps_, :L], lhsT=CT[:, d_, i4, 0, ps_], rhs=xr_[:, :L], start=True, stop=False, tile_position=(0, 32 * im)),
                              reads=[CT, xr_], writes=[yq])
                        fw.op("pe", lambda: nc.tensor.matmul(yq[ps_, :L], lhsT=CT[:, d_, i4, 1, ps_], rhs=xi_[:, :L], start=False, stop=True, tile_position=(0, 32 * im)),
                              reads=[CT, xi_], writes=[yq])
                    prevL = L
                    if isctx and not with_ctx:
                        continue
                    if d_ == 0:
                        for ct in range(2):
                            dve(lambda: nc.vector.scalar_tensor_tensor(out=ys_[:, ct, :L], in0=u_[:, ct, :L], scalar=dsk[:, ct:ct + 1], in1=py[ct][:, :L], op0=ALU.mult, op1=ALU.add),
                                [u_, dsk, py[ct]], [ys_])
                        fw.dma("sp", lambda: nc.sync.dma_start(out=yf[:, :, t0:t0 + L].rearrange("c p t -> p c t"), in_=ys_[:, :, :L]), reads=[ys_], writes=[yf])
                    else:
                        for ct in range(2):
                            dve(lambda: nc.vector.tensor_tensor(out=ys_[:, ct, :L], in0=py[ct][:, :L][:, ::-1], in1=yfl[:, ct, :L], op=ALU.add), [py[ct], yfl], [ys_])
                        fw.op("act", lambda: nc.scalar.activation(out=gq[:, :, :L], in_=ys_[:, :, :L], func=AF.Square), reads=[ys_], writes=[gq])
                        dve(lambda: nc.vector.tensor_scalar(out=gq[:, :, :L], in0=gq[:, :, :L], scalar1=0.044715, scalar2=1.0, op0=ALU.mult, op1=ALU.add), [gq], [gq])
                        fw.op("pool", lambda: nc.gpsimd.tensor_tensor(out=gp[:, :, :L], in0=gq[:, :, :L], in1=ys_[:, :, :L], op=ALU.mult), reads=[gq, ys_], writes=[gp])
                        fw.op("act", lambda: nc.scalar.activation(out=gp[:, :, :L], in_=gp[:, :, :L], func=AF.Sigmoid, scale=1.5957691216057308), reads=[gp], writes=[gp])
                        fw.op("pool", lambda: nc.gpsimd.tensor_tensor(out=gg[:, :, :L], in0=gp[:, :, :L], in1=ys_[:, :, :L], op=ALU.mult), reads=[gp, ys_], writes=[gg])
                        fw.op("act", lambda: nc.scalar.copy(out=gb[:, :, :L], in_=gg[:, :, :L]), reads=[gg], writes=[gb])
                        for co in range(2):
                            for cin in range(2):
                                fw.op("pe", lambda: nc.tensor.matmul(pg[co][:, :L], lhsT=wgl[:, cin, co * 128:(co + 1) * 128], rhs=gb[:, cin, :L], start=(cin == 0), stop=(cin == 1)),
                                      reads=[wgl, gb], writes=[pg[co]], chain=True)
                            fw.op("act", lambda: nc.scalar.activation(out=sg[:, co, :L], in_=pg[co][:, :L], func=AF.Sigmoid, bias=bgl[:, co:co + 1], scale=1.0), reads=[pg[co], bgl], writes=[sg])
                        dve(lambda: nc.vector.tensor_tensor(out=yo[:, :, :L], in0=gg[:, :, :L], in1=sg[:, :, :L], op=ALU.mult), [gg, sg], [yo])
                        fw.dma("sp", lambda: nc.sync.dma_start(out=yT[3:5, :, t0:t0 + L].rearrange("c p t -> p c t"), in_=yo[:, :, :L]), reads=[yo], writes=[yT])


    Xs = fw.dram("Xs", [NSLOT + 128, D], BF16)
    Ys = fw.dram("Ys", [NSLOT, D], F32)
    h2tok = fw.dram("h2tok", [NTOK, D], BF16)
    NTILE = NTOK // 128
    dest_i = fw.sb("dest_i", [128, NTILE, 4], U32)
    wk = fw.sb("wk", [128, NTILE, 4], F32)
    idxw = fw.sb("idxw", [128, NB, 8], U32)
    idxbg = fw.sb("idxbg", [128, NB], U32)
    idxbd = fw.sb("idxbd", [128, NB], U32)
    iop = fw.sb("iop", [128, 1], F32)
    fw.dma("sp", lambda: nc.sync.dma_start(out=iop[:], in_=I["iota_p"][:]), reads=[I["iota_p"]], writes=[iop])

    def phaseE(l, with_ctx):
        with ExitStack() as pes:
            def tl(name, shape, dt=F32):
                return fw.sb(name, shape, dt, pes)
            wout = tl("wout", [128, 8, D], BF16)
            fw.dma("pool", lambda: nc.gpsimd.dma_start(out=wout[:], in_=I["w_out"][l].rearrange("(c p) n -> p c n", p=128)), reads=[I["w_out"]], writes=[wout])
            wr = tl("wr", [128, 8, NE])
            fw.dma("sp", lambda: nc.sync.dma_start(out=wr[:], in_=I["w_router"][l].rearrange("(c p) e -> p c e", p=128)), reads=[I["w_router"]], writes=[wr])
            brow = tl("brow", [128, NE])
            fw.dma("sp", lambda: nc.sync.dma_start(out=brow[:], in_=I["b_router"][l].partition_broadcast(128)), reads=[I["b_router"]], writes=[brow])
            io32 = tl("io32", [128, NE]); ust = tl("ust", [128, 128]); iob = tl("iob", [128, NB])
            fw.dma("sp", lambda: nc.sync.dma_start(out=io32[:], in_=I["iota32"][:]), reads=[I["iota32"]], writes=[io32])
            fw.dma("sp", lambda: nc.sync.dma_start(out=ust[:], in_=I["ustrict"][:]), reads=[I["ustrict"]], writes=[ust])
            fw.dma("sp", lambda: nc.sync.dma_start(out=iob[:], in_=I["iotablk"][:]), reads=[I["iotablk"]], writes=[iob])
            runm = tl("runm", [128, NE])
            fw.op("dve", lambda: nc.vector.memset(runm[:], 0.0), writes=[runm])
            posall = tl("posall", [128, NTILE, NE]); idxall = tl("idxall", [128, NTILE, 4])
            xg = [tl("xgE%d" % i, [128, 8, 512]) for i in range(2)]
            yg = [tl("ygE%d" % i, [128, 8, 512], BF16) for i in range(2)]
            h2f = tl("h2f", [128, 8, 512]); h2b = tl("h2b", [128, 8, 512], BF16)
            sq = tl("sqE", [128, 8, 512], BF16); rstd = tl("rstdE", [128, 512]); GS = tl("GSE", [128, 1, 8])
            pss = fw.ps("pssE", [128, 512], F32, pes)
            pp = [fw.ps("ppE%d" % i, [128, 512], F32, pes) for i in range(2)]
            plg = fw.ps("plg", [128, NE], F32, pes)
            ppos = fw.ps("ppos", [128, NE], F32, pes)
            ptr = [fw.ps("ptrE%d" % i, [128, 8, 128], BF16, pes) for i in range(2)]
            htok = [tl("htok%d" % i, [128, D], BF16) for i in range(2)]
            lg = tl("lg", [128, NE]); m8 = tl("m8", [128, 8]); idx8 = tl("idx8", [128, 8], U32); mask = tl("mask", [128, NE])
            negmx = tl("negmx", [128, 1]); e4 = tl("e4", [128, 4]); ssum = tl("ssum", [128, 1])
            oh = tl("oh", [128, NE]); junk = tl("junk", [128, NE]); posk = tl("posk", [128, 4]); posf = tl("posf", [128, NE])
            gl = groups() if with_ctx else groups()[:-1]
            tiles_done = []
            for gi, (t0, n, isctx) in enumerate(gl):
                j = 1 if isctx else 0
                x_ = xg[gi % 2]; y_ = yg[gi % 2]
                fw.dma("sp", lambda: nc.sync.dma_start(out=x_[:, :, :n], in_=xs[:, :, t0:t0 + n]), reads=[xs], writes=[x_])
                fw.dma("act", lambda: nc.scalar.dma_start(out=y_[:, :, :n], in_=yT[:, :, t0:t0 + n].rearrange("c p t -> p c t")), reads=[yT], writes=[y_])
                for dc in range(8):
                    p = pp[dc % 2]
                    for ct in range(8):
                        fw.op("pe", lambda: nc.tensor.matmul(p[:, :n], lhsT=wout[:, ct, dc * 128:(dc + 1) * 128], rhs=y_[:, ct, :n], start=(ct == 0), stop=(ct == 7)),
                              reads=[wout, y_], writes=[p], chain=True)
                    fw.op("dve", lambda: nc.vector.scalar_tensor_tensor(out=x_[:, dc, :n], in0=p[:, :n], scalar=modT[:, l, 16 + dc, j:j + 1], in1=x_[:, dc, :n],
                                                                      op0=ALU.mult, op1=ALU.add), reads=[p, modT, x_], writes=[x_])
                fw.dma("sp", lambda: nc.sync.dma_start(out=xs[:, :, t0:t0 + n], in_=x_[:, :, :n]), reads=[x_], writes=[xs])
                norm_mod((sq, pss, rstd, GS), x_, n, gffn[:, l, :], l, 3, 4, j, out_bf=h2b, out_f=h2f)
                for tt in range(n // 128):
                    ti = (t0 // 128) + tt
                    tiles_done.append(ti)
                    tsl = slice(tt * 128, (tt + 1) * 128)
                    for kc in range(8):
                        fw.op("pe", lambda: nc.tensor.matmul(plg[:, :], lhsT=h2f[:, kc, tsl], rhs=wr[:, kc, :], start=(kc == 0), stop=(kc == 7)),
                              reads=[h2f, wr], writes=[plg], chain=True)
                    fw.op("dve", lambda: nc.vector.tensor_tensor(out=lg[:], in0=plg[:], in1=brow[:], op=ALU.add), reads=[plg, brow], writes=[lg])
                    fw.op("dve", lambda: nc.vector.max(out=m8[:], in_=lg[:]), reads=[lg], writes=[m8])
                    fw.op("dve", lambda: nc.vector.max_index(out=idx8[:], in_max=m8[:], in_values=lg[:]), reads=[lg, m8], writes=[idx8])
                    fw.op("dve", lambda: nc.vector.tensor_scalar(out=mask[:], in0=lg[:], scalar1=m8[:, 3:4], scalar2=None, op0=ALU.is_ge), reads=[lg, m8], writes=[mask])
                    fw.op("dve", lambda: nc.vector.tensor_scalar(out=negmx[:], in0=m8[:, 0:1], scalar1=-1.0, scalar2=None, op0=ALU.mult), reads=[m8], writes=[negmx])
                    fw.op("act", lambda: nc.scalar.activation(out=e4[:], in_=m8[:, 0:4], func=AF.Exp, bias=negmx[:, 0:1], scale=1.0, accum_out=ssum[:, 0:1]),
                          reads=[m8, negmx], writes=[e4, ssum])
                    fw.op("dve", lambda: nc.vector.reciprocal(out=ssum[:], in_=ssum[:]), reads=[ssum], writes=[ssum])
                    fw.op("dve", lambda: nc.vector.tensor_scalar(out=wk[:, ti, :], in0=e4[:], scalar1=ssum[:, 0:1], scalar2=None, op0=ALU.mult), reads=[e4, ssum], writes=[wk])
                    fw.op("pe", lambda: nc.tensor.matmul(ppos[:, :], lhsT=ust[:, :], rhs=mask[:, :], start=True, stop=False), reads=[ust, mask], writes=[ppos])
                    fw.op("pe", lambda: nc.tensor.matmul(ppos[:, :], lhsT=ones_f[:, :], rhs=runm[:, :], start=False, stop=True), reads=[ones_f, runm], writes=[ppos], chain=True)
                    fw.op("act", lambda: nc.scalar.copy(out=posall[:, ti, :], in_=ppos[:]), reads=[ppos], writes=[posall])
                    fw.op("pool", lambda: nc.gpsimd.tensor_tensor(out=runm[:], in0=runm[:], in1=mask[:], op=ALU.add), reads=[runm, mask], writes=[runm])
                    fw.op("dve", lambda: nc.vector.tensor_copy(out=idxall[:, ti, :], in_=idx8[:, 0:4]), reads=[idx8], writes=[idxall])
                    pt_ = ptr[ti % 2]; ht = htok[ti % 2]
                    for kc in range(8):
                        fw.op("pe", lambda: nc.tensor.transpose(out=pt_[:, kc, :], in_=h2b[:, kc, tsl], identity=ident_b[:]), reads=[h2b, ident_b], writes=[pt_], chain=True)
                    fw.op("act", lambda: nc.scalar.copy(out=ht[:], in_=pt_[:].rearrange("p c d -> p (c d)")), reads=[pt_], writes=[ht])
                    fw.dma("act", lambda: nc.scalar.dma_start(out=h2tok[ti * 128:(ti + 1) * 128, :], in_=ht[:]), reads=[ht], writes=[h2tok])
            cnt = tl("cnt", [128, NE]); pad = tl("pad", [128, NE]); ends = tl("ends", [128, NE]); pst = tl("pst", [128, NE])
            qi = tl("qi", [128, NE], I32); qf = tl("qf", [128, NE]); gt = tl("gt", [128, NE]); onesr = tl("onesr", [128, NE])
            acc = tl("accb", [128, NB])
            fw.op("pe", lambda: nc.tensor.matmul(ppos[:, :], lhsT=ones_f[:, :], rhs=runm[:, :], start=True, stop=True), reads=[ones_f, runm], writes=[ppos])
            fw.op("dve", lambda: nc.vector.tensor_scalar(out=cnt[:], in0=ppos[:], scalar1=float(BLK - 1), scalar2=1.0 / BLK, op0=ALU.add, op1=ALU.mult), reads=[ppos], writes=[cnt])
            fw.op("dve", lambda: nc.vector.tensor_copy(out=qi[:], in_=cnt[:]), reads=[cnt], writes=[qi])
            fw.op("dve", lambda: nc.vector.tensor_copy(out=qf[:], in_=qi[:]), reads=[qi], writes=[qf])
            fw.op("dve", lambda: nc.vector.tensor_tensor(out=gt[:], in0=qf[:], in1=cnt[:], op=ALU.is_gt), reads=[qf, cnt], writes=[gt])
            fw.op("dve", lambda: nc.vector.tensor_tensor(out=qf[:], in0=qf[:], in1=gt[:], op=ALU.subtract), reads=[qf, gt], writes=[qf])
            fw.op("dve", lambda: nc.vector.tensor_scalar(out=pad[:], in0=qf[:], scalar1=float(BLK), scalar2=None, op0=ALU.mult), reads=[qf], writes=[pad])
            fw.op("dve", lambda: nc.vector.memset(onesr[:], 1.0), writes=[onesr])
            fw.op("dve", lambda: nc.vector.tensor_tensor_scan(out=ends[:], data0=onesr[:], data1=pad[:], initial=0.0, op0=ALU.mult, op1=ALU.add), reads=[onesr, pad], writes=[ends])
            fw.op("dve", lambda: nc.vector.tensor_tensor(out=pst[:], in0=ends[:], in1=pad[:], op=ALU.subtract), reads=[ends, pad], writes=[pst])
            fw.op("dve", lambda: nc.vector.memset(acc[:], 0.0), writes=[acc])
            for e in range(NE):
                fw.op("dve", lambda: nc.vector.scalar_tensor_tensor(out=acc[:], in0=iob[:], scalar=ends[:, e:e + 1], in1=acc[:], op0=ALU.is_ge, op1=ALU.add),
                      reads=[iob, ends, acc], writes=[acc])
            fw.op("dve", lambda: nc.vector.tensor_scalar(out=acc[:], in0=acc[:], scalar1=float(NE - 1), scalar2=None, op0=ALU.min), reads=[acc], writes=[acc])
            tix = tl("tix", [128, NB])
            for kc in range(8):
                fw.op("dve", lambda: nc.vector.tensor_scalar(out=tix[:], in0=acc[:], scalar1=1024.0, scalar2=float(l * NE * 1024 + kc * 128), op0=ALU.mult, op1=ALU.add), reads=[acc], writes=[tix])
                fw.op("dve", lambda: nc.vector.tensor_scalar(out=tix[:], in0=tix[:], scalar1=iop[:, 0:1], scalar2=None, op0=ALU.add), reads=[tix, iop], writes=[tix])
                fw.op("dve", lambda: nc.vector.tensor_copy(out=idxw[:, :, kc], in_=tix[:]), reads=[tix], writes=[idxw])
            fw.op("dve", lambda: nc.vector.tensor_scalar(out=tix[:], in0=acc[:], scalar1=16.0, scalar2=float(l * NE * 16), op0=ALU.mult, op1=ALU.add), reads=[acc], writes=[tix])
            fw.op("dve", lambda: nc.vector.tensor_scalar(out=tix[:], in0=tix[:], scalar1=iop[:, 0:1], scalar2=None, op0=ALU.add), reads=[tix, iop], writes=[tix])
            fw.op("dve", lambda: nc.vector.tensor_copy(out=idxbg[:], in_=tix[:]), reads=[tix], writes=[idxbg])
            fw.op("dve", lambda: nc.vector.tensor_scalar(out=tix[:], in0=acc[:], scalar1=float(l * NE), scalar2=None, op0=ALU.add), reads=[acc], writes=[tix])
            fw.op("dve", lambda: nc.vector.tensor_copy(out=idxbd[:], in_=tix[:]), reads=[tix], writes=[idxbd])
            for ti in tiles_done:
                ht = htok[ti % 2]
                fw.dma("sp", lambda: nc.sync.dma_start(out=ht[:], in_=h2tok[ti * 128:(ti + 1) * 128, :]), reads=[h2tok], writes=[ht])
                fw.op("dve", lambda: nc.vector.tensor_tensor(out=posf[:], in0=posall[:, ti, :], in1=pst[:], op=ALU.add), reads=[posall, pst], writes=[posf])
                for k_ in range(4):
                    fw.op("dve", lambda: nc.vector.tensor_scalar(out=oh[:], in0=io32[:], scalar1=idxall[:, ti, k_:k_ + 1], scalar2=None, op0=ALU.is_equal), reads=[io32, idxall], writes=[oh])
                    fw.op("dve", lambda: nc.vector.scalar_tensor_tensor(out=junk[:], in0=oh[:], scalar=1.0, in1=posf[:], op0=ALU.mult, op1=ALU.mult,
                                                                      accum_out=posk[:, k_:k_ + 1]), reads=[oh, posf], writes=[junk, posk])
                fw.op("dve", lambda: nc.vector.tensor_copy(out=dest_i[:, ti, :], in_=posk[:]), reads=[posk], writes=[dest_i])
                for k_ in range(4):
                    fw.dma("pool", lambda: nc.gpsimd.indirect_dma_start(out=Xs[:, :], out_offset=bass.IndirectOffsetOnAxis(ap=dest_i[:, ti, k_:k_ + 1], axis=0),
                                                                      in_=ht[:, :], in_offset=None), reads=[ht, dest_i], writes=[Xs])

    def phaseF(l):
        with ExitStack() as pes:
            def tl(name, shape, dt=F32):
                return fw.sb(name, shape, dt, pes)
            NST = BLK // 128
            chunks = [(c0, min(512, BLK - c0)) for c0 in range(0, BLK, 512)]
            wgu = [tl("wgu%d" % i, [128, 8, 2 * D], BF16) for i in range(2)]
            wdn = [tl("wdn%d" % i, [128, 8, D], BF16) for i in range(2)]
            bdr = [tl("bdr%d" % i, [128, D]) for i in range(2)]
            bgc = [tl("bgc%d" % i, [128, 16]) for i in range(2)]
            xtok = tl("xtok", [128, NST, D], BF16)
            XT = tl("XT", [128, 8, BLK], BF16)
            actT = tl("actT", [128, 8, BLK], BF16)
            gs = [tl("gs%d" % i, [128, 512]) for i in range(2)]
            sgm = [tl("sgm%d" % i, [128, 512]) for i in range(2)]
            uu = [tl("uu%d" % i, [128, 512]) for i in range(2)]
            aa = [tl("aa%d" % i, [128, 512]) for i in range(2)]
            yo = [tl("yoF%d" % i, [128, D]) for i in range(2)]
            ptr = fw.ps("ptrF", [128, 8, 128], BF16, pes)
            pg = [fw.ps("pgF%d" % i, [128, 512], F32, pes) for i in range(2)]
            pu = [fw.ps("puF%d" % i, [128, 512], F32, pes) for i in range(2)]
            pd = [fw.ps("pdF%d" % i, [128, 512], F32, pes) for i in range(2)]

            wgu_flat = I["w_gate_up"][:].rearrange("l e k n -> (l e k) n")
            wdn_flat = I["w_down"][:].rearrange("l e k n -> (l e k) n")
            bgu_flat = I["b_gate_up"][:].rearrange("l e (c p) -> (l e c) p", p=128)
            bdn_flat = I["b_down"][:].rearrange("l e d -> (l e) d")
            bgrow = [tl("bgrow%d" % i, [16, 128]) for i in range(2)]
            pbg = fw.ps("pbgF", [128, 16], F32, pes)

            def load_w(b):
                wg_ = wgu[b % 2]; wd_ = wdn[b % 2]; bd_ = bdr[b % 2]; bg_ = bgc[b % 2]; br_ = bgrow[b % 2]
                for kc in range(8):
                    fw.dma("pool", lambda: nc.gpsimd.indirect_dma_start(out=wg_[:, kc, :], out_offset=None, in_=wgu_flat,
                                                                      in_offset=bass.IndirectOffsetOnAxis(ap=idxw[:, b, kc:kc + 1], axis=0)),
                           reads=[I["w_gate_up"], idxw], writes=[wg_])
                for fc in range(8):
                    fw.dma("pool", lambda: nc.gpsimd.indirect_dma_start(out=wd_[:, fc, :], out_offset=None, in_=wdn_flat,
                                                                      in_offset=bass.IndirectOffsetOnAxis(ap=idxw[:, b, fc:fc + 1], axis=0)),
                           reads=[I["w_down"], idxw], writes=[wd_])
                fw.dma("pool", lambda: nc.gpsimd.indirect_dma_start(out=br_[:, :], out_offset=None, in_=bgu_flat,
                                                                  in_offset=bass.IndirectOffsetOnAxis(ap=idxbg[0:16, b:b + 1], axis=0)),
                       reads=[I["b_gate_up"], idxbg], writes=[br_])
                fw.dma("pool", lambda: nc.gpsimd.indirect_dma_start(out=bd_[:, :], out_offset=None, in_=bdn_flat,
                                                                  in_offset=bass.IndirectOffsetOnAxis(ap=idxbd[:, b:b + 1], axis=0)),
                       reads=[I["b_down"], idxbd], writes=[bd_])
                fw.op("pe", lambda: nc.tensor.transpose(out=pbg[:, :], in_=br_[:, :], identity=ident_f[0:16, 0:16]), reads=[br_, ident_f], writes=[pbg])
                fw.op("act", lambda: nc.scalar.copy(out=bg_[:], in_=pbg[:]), reads=[pbg], writes=[bg_])
            load_w(0)
            kk = 0
            for b in range(NB):
                if b + 1 < NB:
                    load_w(b + 1)
                wg_ = wgu[b % 2]; wd_ = wdn[b % 2]; bd_ = bdr[b % 2]; bg_ = bgc[b % 2]
                fw.dma("sp", lambda: nc.sync.dma_start(out=xtok[:], in_=Xs[b * BLK:(b + 1) * BLK, :].rearrange("(t p) d -> p t d", p=128)), reads=[Xs], writes=[xtok])
                for st in range(NST):
                    for kc in range(8):
                        fw.op("pe", lambda: nc.tensor.transpose(out=ptr[:, kc, :], in_=xtok[:, st, kc * 128:(kc + 1) * 128], identity=ident_b[:]), reads=[xtok, ident_b], writes=[ptr], chain=True)
                    if st % 2 == 0:
                        fw.op("act", lambda: nc.scalar.copy(out=XT[:, :, st * 128:(st + 1) * 128], in_=ptr[:]), reads=[ptr], writes=[XT])
                    else:
                        fw.op("dve", lambda: nc.vector.tensor_copy(out=XT[:, :, st * 128:(st + 1) * 128], in_=ptr[:]), reads=[ptr], writes=[XT])
                for (c0, cn) in chunks:
                    for j in range(8):
                        k_ = kk % 2; kk += 1
                        g_, u_ = pg[k_], pu[k_]
                        for kc in range(8):
                            fw.op("pe", lambda: nc.tensor.matmul(g_[:, :cn], lhsT=wg_[:, kc, j * 128:(j + 1) * 128], rhs=XT[:, kc, c0:c0 + cn], start=(kc == 0), stop=(kc == 7)),
                                  reads=[wg_, XT], writes=[g_], chain=True)
                        for kc in range(8):
                            fw.op("pe", lambda: nc.tensor.matmul(u_[:, :cn], lhsT=wg_[:, kc, D + j * 128:D + (j + 1) * 128], rhs=XT[:, kc, c0:c0 + cn], start=(kc == 0), stop=(kc == 7)),
                                  reads=[wg_, XT], writes=[u_], chain=True)
                        gs_, sg_, uu_, aa_ = gs[k_], sgm[k_], uu[k_], aa[k_]
                        fw.op("dve", lambda: nc.vector.tensor_scalar(out=gs_[:, :cn], in0=g_[:, :cn], scalar1=bg_[:, j:j + 1], scalar2=7.0, op0=ALU.add, op1=ALU.min), reads=[g_, bg_], writes=[gs_])
                        fw.op("act", lambda: nc.scalar.activation(out=sg_[:, :cn], in_=gs_[:, :cn], func=AF.Sigmoid, scale=1.702), reads=[gs_], writes=[sg_])
                        fw.op("dve", lambda: nc.vector.tensor_scalar(out=uu_[:, :cn], in0=u_[:, :cn], scalar1=bg_[:, 8 + j:9 + j], scalar2=7.0, op0=ALU.add, op1=ALU.min), reads=[u_, bg_], writes=[uu_])
                        fw.op("dve", lambda: nc.vector.tensor_scalar(out=uu_[:, :cn], in0=uu_[:, :cn], scalar1=-7.0, scalar2=1.0, op0=ALU.max, op1=ALU.add), reads=[uu_], writes=[uu_])
                        fw.op("pool", lambda: nc.gpsimd.tensor_tensor(out=aa_[:, :cn], in0=gs_[:, :cn], in1=sg_[:, :cn], op=ALU.mult), reads=[gs_, sg_], writes=[aa_])
                        fw.op("pool", lambda: nc.gpsimd.tensor_tensor(out=actT[:, j, c0:c0 + cn], in0=aa_[:, :cn], in1=uu_[:, :cn], op=ALU.mult), reads=[aa_, uu_], writes=[actT])
                for st in range(NST):
                    yo_ = yo[st % 2]
                    for dh in range(2):
                        p = pd[dh]
                        for fc in range(8):
                            fw.op("pe", lambda: nc.tensor.matmul(p[:, :], lhsT=actT[:, fc, st * 128:(st + 1) * 128], rhs=wd_[:, fc, dh * 512:(dh + 1) * 512], start=(fc == 0), stop=(fc == 7)),
                                  reads=[actT, wd_], writes=[p], chain=True)
                        fw.op("dve", lambda: nc.vector.tensor_tensor(out=yo_[:, dh * 512:(dh + 1) * 512], in0=p[:, :], in1=bd_[:, dh * 512:(dh + 1) * 512], op=ALU.add), reads=[p, bd_], writes=[yo_])
                    r0 = b * BLK + st * 128
                    fw.dma("sp", lambda: nc.sync.dma_start(out=Ys[r0:r0 + 128, :], in_=yo_[:, :]), reads=[yo_], writes=[Ys])

    def phaseG(l, with_ctx, last):
        with ExitStack() as pes:
            def tl(name, shape, dt=F32):
                return fw.sb(name, shape, dt, pes)
            yk = [[tl("yk%d_%d" % (b_, k_), [128, D]) for k_ in range(4)] for b_ in range(2)]
            acc = [tl("acc%d" % i, [128, D]) for i in range(2)]
            xg = [tl("xgG%d" % i, [128, 8, 512]) for i in range(2)]
            pT = [fw.ps("pTG%d" % i, [128, 8, 128], F32, pes) for i in range(2)]
            if last:
                sq = tl("sqG", [128, 8, 512], BF16); rstd = tl("rstdG", [128, 512])
                pss = fw.ps("pssG", [128, 512], F32, pes)
                xo = tl("xoG", [128, 8, 512])
                osb = [tl("osbG%d" % i, [128, D]) for i in range(2)]
                pO = fw.ps("pOG", [128, 8, 128], F32, pes)
            gl = groups() if with_ctx else groups()[:-1]
            for gi, (t0, n, isctx) in enumerate(gl):
                j = 1 if isctx else 0
                x_ = xg[gi % 2]
                fw.dma("sp", lambda: nc.sync.dma_start(out=x_[:, :, :n], in_=xs[:, :, t0:t0 + n]), reads=[xs], writes=[x_])
                for tt in range(n // 128):
                    ti = (t0 // 128) + tt
                    tsl = slice(tt * 128, (tt + 1) * 128)
                    yk_ = yk[ti % 2]; a_ = acc[ti % 2]; p_ = pT[ti % 2]
                    for k_ in range(4):
                        fw.dma("pool", lambda: nc.gpsimd.indirect_dma_start(out=yk_[k_][:, :], out_offset=None, in_=Ys[:, :],
                                                                          in_offset=bass.IndirectOffsetOnAxis(ap=dest_i[:, ti, k_:k_ + 1], axis=0)),
                               reads=[Ys, dest_i], writes=[yk_[k_]])
                    fw.op("dve", lambda: nc.vector.tensor_scalar(out=a_[:], in0=yk_[0][:], scalar1=wk[:, ti, 0:1], scalar2=None, op0=ALU.mult), reads=[yk_[0], wk], writes=[a_])
                    for k_ in range(1, 4):
                        fw.op("dve", lambda: nc.vector.scalar_tensor_tensor(out=a_[:], in0=yk_[k_][:], scalar=wk[:, ti, k_:k_ + 1], in1=a_[:], op0=ALU.mult, op1=ALU.add),
                              reads=[yk_[k_], wk, a_], writes=[a_])
                    for c in range(8):
                        fw.op("pe", lambda: nc.tensor.transpose(out=p_[:, c, :], in_=a_[:, c * 128:(c + 1) * 128], identity=ident_f[:]), reads=[a_, ident_f], writes=[p_], chain=True)
                    for c in range(8):
                        fw.op("dve", lambda: nc.vector.scalar_tensor_tensor(out=x_[:, c, tsl], in0=p_[:, c, :], scalar=modT[:, l, 40 + c, j:j + 1], in1=x_[:, c, tsl],
                                                                          op0=ALU.mult, op1=ALU.add), reads=[p_, modT, x_], writes=[x_])
                if not last:
                    fw.dma("sp", lambda: nc.sync.dma_start(out=xs[:, :, t0:t0 + n], in_=x_[:, :, :n]), reads=[x_], writes=[xs])
                elif not isctx:
                    fw.op("act", lambda: nc.scalar.activation(out=sq[:, :, :n], in_=x_[:, :, :n], func=AF.Square), reads=[x_], writes=[sq])
                    for c in range(8):
                        fw.op("pe", lambda: nc.tensor.matmul(pss[:, :n], lhsT=ones_b[:], rhs=sq[:, c, :n], start=(c == 0), stop=(c == 7)), reads=[sq, ones_b], writes=[pss], chain=True)
                    fw.op("act", lambda: nc.scalar.activation(out=rstd[:, :n], in_=pss[:, :n], func=AF.Sqrt, bias=EPS, scale=1.0 / D), reads=[pss], writes=[rstd])
                    fw.op("dve", lambda: nc.vector.reciprocal(out=rstd[:, :n], in_=rstd[:, :n]), reads=[rstd], writes=[rstd])
                    for c in range(8):
                        fw.op("dve", lambda: nc.vector.scalar_tensor_tensor(out=xo[:, c, :n], in0=x_[:, c, :n], scalar=gfin[:, c:c + 1], in1=rstd[:, :n], op0=ALU.mult, op1=ALU.mult),
                              reads=[x_, gfin, rstd], writes=[xo])
                    for tt in range(n // 128):
                        o_ = osb[tt % 2]
                        for c in range(8):
                            fw.op("pe", lambda: nc.tensor.transpose(out=pO[:, c, :], in_=xo[:, c, tt * 128:(tt + 1) * 128], identity=ident_f[:]), reads=[xo, ident_f], writes=[pO], chain=True)
                        fw.op("act", lambda: nc.scalar.copy(out=o_[:], in_=pO[:].rearrange("p c d -> p (c d)")), reads=[pO], writes=[o_])
                        r0 = t0 + tt * 128
                        fw.dma("sp", lambda: nc.sync.dma_start(out=OUT[r0:r0 + 128, :], in_=o_[:]), reads=[o_], writes=[OUT])

    if "nopre" not in debug:
        prephase()
        fw.barrier()
    phase0()
    fw.barrier()
    for l in range(nlayers):
        with_ctx = l < DEPTH - 1
        phaseA(l)
        fw.barrier()
        if "A" in debug:
            break
        if "noB" not in debug:
            phaseB(l, with_ctx)
            fw.barrier()
        if "noD" not in debug:
            phaseD(l, with_ctx)
            fw.barrier()
        if "noC" not in debug:
            phaseC(l, with_ctx)
            fw.barrier()
        if "BD" in debug:
            break
        phaseE(l, with_ctx)
        fw.barrier()
        if "E" in debug:
            break
        phaseF(l)
        fw.barrier()
        phaseG(l, with_ctx, l == nlayers - 1 and "G" not in debug)
        fw.barrier()

    outs_to_wait = []
    if debug:
        def dump(name, src, shape, dt):
            o = dout("dbg_" + name, shape, dt)
            fw.dma("sp", lambda: nc.sync.dma_start(out=o[:], in_=src[:]), reads=[src], writes=[o])
            outs_to_wait.append(o)
        dump("yT", yT, [8, 128, NTOK], BF16)
        if "moe" in debug:
            dump("Xs", Xs, [NSLOT + 128, D], BF16)
            dump("Ys", Ys, [NSLOT, D], F32)
            od = dout("dbg_dest", [128, NTILE * 4], U32)
            fw.dma("sp", lambda: nc.sync.dma_start(out=od[:], in_=dest_i[:].rearrange("p t k -> p (t k)")), reads=[dest_i], writes=[od])
            outs_to_wait.append(od)
            ow = dout("dbg_wk", [128, NTILE * 4], F32)
            fw.dma("sp", lambda: nc.sync.dma_start(out=ow[:], in_=wk[:].rearrange("p t k -> p (t k)")), reads=[wk], writes=[ow])
            outs_to_wait.append(ow)
        dump("xs", xs, [128, 8, NTOK], F32)
        dump("qa", qa, [3, 128, NTOK], BF16)
        dump("ka", ka, [3, 128, NTOK], BF16)
        dump("va", va, [NTOK, 390], BF16)
        dump("u5", u5, [2, 128, NTOK], F32)
        dump("qs", qs, [3, 128, NTOK], BF16)
        dump("ks", ks, [3, 128, NTOK], BF16)
        dump("vs", vs, [NTOK, 130], BF16)
        om = dout("dbg_modT", [128, DEPTH * 48 * 2], F32)
        fw.dma("sp", lambda: nc.sync.dma_start(out=om[:], in_=modT[:].rearrange("p l c j -> p (l c j)")), reads=[modT], writes=[om])
        outs_to_wait.append(om)
    fw.finish(outs_to_wait + [OUT])
    print("insts", fw.n_inst, "waits", fw.n_wait)
    es.close()
    return nc, hc


def make_inputs(inputs, b, hc):
    m = {}
    m["xin"] = np.ascontiguousarray(np.concatenate([inputs["x"][b], inputs["ctx"][b]], axis=0))
    m["cvec"] = np.ascontiguousarray(np.stack([inputs["c"][b], inputs["c_ctx"]], axis=0))
    for k in ("w_mod", "b_mod", "g_mix", "w_in", "w_out", "na_rpb", "s5_a_re", "s5_a_im", "s5_log_step", "s5_b_re", "s5_b_im",
              "s5_c_re", "s5_c_im", "s5_d", "s5_w_glu", "s5_b_glu", "sw_sinks", "g_ffn", "w_router", "b_router", "w_gate_up",
              "b_gate_up", "w_down", "b_down", "g_final"):
        m[k] = inputs[k]
    for k, v in hc.items():
        m[k] = v
    return m


def kernel(**inputs):
    nc, hc = build()
    in_maps = [make_inputs(inputs, b % 4, hc) for b in range(8)]
    res = run_bass_kernel_spmd(nc, in_maps, core_ids=list(range(8)))
    return np.stack([res.results[b]["out"] for b in range(4)], axis=0)
```

```python
import numpy as np
import ml_dtypes
from contextlib import ExitStack
import concourse.bass as bass
import concourse.mybir as mybir
from concourse.bass_utils import run_bass_kernel_spmd

F32 = mybir.dt.float32
BF16 = mybir.dt.bfloat16
I32 = mybir.dt.int32
U32 = mybir.dt.uint32
AF = mybir.ActivationFunctionType
ALU = mybir.AluOpType
AX = mybir.AxisListType

D = 1024
SEQ = 8192
CTXL = 256
NTOK = SEQ + CTXL
DEPTH = 4
NE = 32
BLK = 896
NB = -(-(NTOK * 4 + NE * (BLK - 1)) // BLK)
NSLOT = NB * BLK
EPS = 1e-6
NEG = -8.0e30


class Buf:
    __slots__ = ("w", "r")

    def __init__(self):
        self.w = {}
        self.r = {}


class T:
    def __init__(self, t, name):
        self.t = t
        self.name = name
        self.b = Buf()

    def __getitem__(self, idx):
        return self.t[idx]


class FW:
    NDMA = 32

    def __init__(self, nc, es):
        self.nc = nc
        self.es = es
        self.engs = {"pe": nc.tensor, "act": nc.scalar, "dve": nc.vector, "pool": nc.gpsimd, "sp": nc.sync}
        self.sem = {}
        self.cnt = {}
        self.sems = {}
        for k in self.engs:
            s = es.enter_context(nc.semaphore("s_" + k))
            self.sem[k] = s
            self.sems["e_" + k] = s
            self.cnt[k] = 0
        self.dma_sems = []
        for i in range(self.NDMA):
            s = es.enter_context(nc.semaphore("d%d" % i))
            self.sems["d%d" % i] = s
            self.dma_sems.append(["d%d" % i, 0])
        self.dma_rr = 0
        self.waited = {k: {} for k in self.engs}
        self.n_inst = 0
        self.n_wait = 0
        self.uid = 0

    def sb(self, name, shape, dt, es=None):
        self.uid += 1
        t = (es or self.es).enter_context(self.nc.sbuf_tensor("%s_%d" % (name, self.uid), list(shape), dt))
        return T(t, name)

    def ps(self, name, shape, dt, es=None):
        self.uid += 1
        t = (es or self.es).enter_context(self.nc.psum_tensor("%s_%d" % (name, self.uid), list(shape), dt))
        return T(t, name)

    def dram(self, name, shape, dt, kind="Internal"):
        t = self.nc.dram_tensor(name, list(shape), dt, kind=kind)
        return T(t.ap(), name)

    def _deps(self, eng, reads, writes, skip_own=False):
        deps = {}

        def add(ev):
            s, v = ev
            if deps.get(s, 0) < v:
                deps[s] = v
        for b in reads:
            for ev in b.b.w.items():
                add(ev)
        for b in writes:
            for ev in b.b.w.items():
                add(ev)
            for ev in b.b.r.items():
                add(ev)
        own = "e_" + eng
        for s, v in deps.items():
            if skip_own and s == own:
                continue
            if self.waited[eng].get(s, 0) >= v:
                continue
            self.engs[eng].wait_ge(self.sems[s], v)
            self.waited[eng][s] = v
            self.n_wait += 1

    def _commit(self, ev, reads, writes):
        s, v = ev
        for b in reads:
            if b.b.r.get(s, 0) < v:
                b.b.r[s] = v
        for b in writes:
            if b.b.w.get(s, 0) < v:
                b.b.w[s] = v
            b.b.r = {}

    def op(self, eng, fn, reads=(), writes=(), chain=False):
        self._deps(eng, reads, writes, skip_own=chain)
        inst = fn()
        self.cnt[eng] += 1
        inst.then_inc(self.sem[eng], 1)
        self._commit(("e_" + eng, self.cnt[eng]), reads, writes)
        self.n_inst += 1
        return inst

    def dma(self, q, fn, reads=(), writes=()):
        slot = self.dma_sems[self.dma_rr]
        self.dma_rr = (self.dma_rr + 1) % self.NDMA
        sid, used = slot
        if used > 0 and self.waited[q].get(sid, 0) < used * 16:
            self.engs[q].wait_ge(self.sems[sid], used * 16)
            self.waited[q][sid] = used * 16
        self._deps(q, reads, writes)
        inst = fn()
        slot[1] = used + 1
        inst.then_inc(self.sems[sid], 16)
        self._commit((sid, (used + 1) * 16), reads, writes)
        self.n_inst += 1
        return inst

    def barrier(self):
        evs = {}
        for k in self.engs:
            if self.cnt[k] > 0:
                evs["e_" + k] = self.cnt[k]
        for sid, used in self.dma_sems:
            if used > 0:
                evs[sid] = used * 16
        for k in self.engs:
            for s, v in evs.items():
                if s == "e_" + k and k in ("sp",):
                    continue
                if self.waited[k].get(s, 0) >= v:
                    continue
                self.engs[k].wait_ge(self.sems[s], v)
                self.waited[k][s] = v
                self.n_wait += 1

    def finish(self, outs, eng="sp"):
        for b in outs:
            for s, v in b.b.w.items():
                self.engs[eng].wait_ge(self.sems[s], v)


def host_consts():
    c = {}
    c["ident_f"] = np.eye(128, dtype=np.float32)
    c["ident_b"] = np.eye(128).astype(ml_dtypes.bfloat16)
    c["ones_b"] = np.ones((128, 128)).astype(ml_dtypes.bfloat16)
    c["ones_f"] = np.ones((128, 128), dtype=np.float32)
    t = np.arange(SEQ)
    row = (t // 64).astype(np.float32)
    col = (t % 64).astype(np.float32)
    inv = (10000.0 ** (-np.arange(16, dtype=np.float32) / 16)).astype(np.float32)
    ar = row[:, None] * inv
    ac = col[:, None] * inv
    ang = np.concatenate([ar, ar, ac, ac], axis=-1)
    cos = np.cos(ang).astype(np.float32).T
    sin = np.sin(ang).astype(np.float32).T
    sign = np.where((np.arange(64) % 32) < 16, -1.0, 1.0).astype(np.float32)[:, None]
    c["rope_c"] = np.ascontiguousarray(np.concatenate([cos, cos], 0))
    c["rope_s"] = np.ascontiguousarray(np.concatenate([sin * sign, sin * sign], 0))
    k = np.arange(128)[:, None]
    q = np.arange(128)[None, :]
    c["mask_l"] = np.where(k >= q, 0.0, NEG).astype(ml_dtypes.bfloat16)
    c["mask_u"] = np.where(k <= q, 0.0, NEG).astype(ml_dtypes.bfloat16)
    qc = np.arange(64)[None, :]
    kc = np.arange(64)[:, None]
    ws = np.clip(qc - 8, 0, 48)
    valid = (kc >= ws) & (kc < ws + 16)
    v2 = np.concatenate([valid, valid], 0)
    c["na_valid8"] = (v2 * 8.0).astype(np.float32)
    c["na_pen"] = np.where(v2, 0.0, NEG).astype(np.float32)
    c["iota32"] = np.broadcast_to(np.arange(32, dtype=np.float32), (128, 32)).copy()
    c["iota512"] = np.broadcast_to(np.arange(512, dtype=np.float32), (128, 512)).copy()
    c["iotablk"] = np.broadcast_to((np.arange(NB) * BLK).astype(np.float32), (128, NB)).copy()
    c["iota_p"] = np.arange(128, dtype=np.float32).reshape(128, 1)
    c["ustrict"] = (np.arange(128)[:, None] < np.arange(128)[None, :]).astype(np.float32)
    return c


CONST_SPECS = None


def groups():
    g = [(i * 512, 512, False) for i in range(SEQ // 512)]
    g.append((SEQ, CTXL, True))
    return g


def build(nlayers=DEPTH, debug=()):
    nc = bass.Bass("TRN2", target_bir_lowering=False)
    es = ExitStack()
    fw = FW(nc, es)

    def din(name, shape, dt=F32):
        return T(nc.dram_tensor(name, list(shape), dt, kind="ExternalInput").ap(), name)

    def dout(name, shape, dt=F32):
        return T(nc.dram_tensor(name, list(shape), dt, kind="ExternalOutput").ap(), name)

    I = {}
    I["xin"] = din("xin", [NTOK, D])
    I["cvec"] = din("cvec", [2, D])
    specs = {"w_mod": [DEPTH, D, 6 * D], "b_mod": [DEPTH, 6 * D], "g_mix": [DEPTH, D], "w_in": [DEPTH, D, 2048],
             "w_out": [DEPTH, D, D], "na_rpb": [DEPTH, 6, 15, 31], "s5_a_re": [DEPTH, 2, 16, 64], "s5_a_im": [DEPTH, 2, 16, 64],
             "s5_log_step": [DEPTH, 2, 16], "s5_b_re": [DEPTH, 2, 16, 64, 16], "s5_b_im": [DEPTH, 2, 16, 64, 16],
             "s5_c_re": [DEPTH, 2, 16, 16, 64], "s5_c_im": [DEPTH, 2, 16, 16, 64], "s5_d": [DEPTH, 256],
             "s5_w_glu": [DEPTH, 256, 256], "s5_b_glu": [DEPTH, 256], "sw_sinks": [DEPTH, 6], "g_ffn": [DEPTH, D],
             "w_router": [DEPTH, D, NE], "b_router": [DEPTH, NE], "w_gate_up": [DEPTH, NE, D, 2 * D],
             "b_gate_up": [DEPTH, NE, 2 * D], "w_down": [DEPTH, NE, D, D], "b_down": [DEPTH, NE, D], "g_final": [D]}
    for k, s in specs.items():
        I[k] = din(k, s)
    hc = host_consts()
    for k, v in hc.items():
        I[k] = din(k, list(v.shape), BF16 if v.dtype == ml_dtypes.bfloat16 else F32)
    OUT = dout("out", [SEQ, D])
    DBG = {}

    xs = fw.dram("xs", [128, 8, NTOK], F32)
    qa = fw.dram("qa", [3, 128, NTOK], BF16)
    ka = fw.dram("ka", [3, 128, NTOK], BF16)
    va = fw.dram("va", [NTOK, 6 * 65], BF16)
    u5 = fw.dram("u5", [2, 128, NTOK], F32)
    qs = fw.dram("qs", [3, 128, NTOK], BF16)
    ks = fw.dram("ks", [3, 128, NTOK], BF16)
    vs = fw.dram("vs", [NTOK, 2 * 65], BF16)
    yT = fw.dram("yT", [8, 128, NTOK], BF16)

    ident_f = fw.sb("ident_f", [128, 128], F32)
    ident_b = fw.sb("ident_b", [128, 128], BF16)
    ones_b = fw.sb("ones_b", [128, 128], BF16)
    ones_f = fw.sb("ones_f", [128, 128], F32)
    for tl, nm in ((ident_f, "ident_f"), (ident_b, "ident_b"), (ones_b, "ones_b"), (ones_f, "ones_f")):
        fw.dma("sp", lambda tl=tl, nm=nm: nc.sync.dma_start(out=tl[:], in_=I[nm][:]), reads=[I[nm]], writes=[tl])
    modT = fw.sb("modT", [128, DEPTH, 48, 2], F32)
    gmix = fw.sb("gmix", [128, DEPTH, 8], F32)
    gffn = fw.sb("gffn", [128, DEPTH, 8], F32)
    gfin = fw.sb("gfin", [128, 8], F32)
    with nc.allow_non_contiguous_dma(reason="tiny per-feature vectors"):
        fw.dma("sp", lambda: nc.sync.dma_start(out=gmix[:], in_=I["g_mix"][:].rearrange("l (c p) -> p l c", p=128)), reads=[I["g_mix"]], writes=[gmix])
        fw.dma("sp", lambda: nc.sync.dma_start(out=gffn[:], in_=I["g_ffn"][:].rearrange("l (c p) -> p l c", p=128)), reads=[I["g_ffn"]], writes=[gffn])
        fw.dma("sp", lambda: nc.sync.dma_start(out=gfin[:], in_=I["g_final"][:].rearrange("(c p) -> p c", p=128)), reads=[I["g_final"]], writes=[gfin])

    def phase0():
        with ExitStack() as pes:
            condT = fw.sb("condT", [128, 2, 8], F32, pes)
            bmT = fw.sb("bmT", [128, DEPTH, 48], F32, pes)
            with nc.allow_non_contiguous_dma(reason="tiny"):
                for jj in range(2):
                    fw.dma("sp", lambda: nc.sync.dma_start(out=condT[:, jj, :], in_=I["cvec"][jj, :].rearrange("(c p) -> p c", p=128)), reads=[I["cvec"]], writes=[condT])
                fw.dma("sp", lambda: nc.sync.dma_start(out=bmT[:], in_=I["b_mod"][:].rearrange("l (c p) -> p l c", p=128)), reads=[I["b_mod"]], writes=[bmT])
            fw.op("act", lambda: nc.scalar.activation(out=condT[:], in_=condT[:], func=AF.Silu), reads=[condT], writes=[condT])
            wbuf = [fw.sb("wmod%d" % i, [128, 8, 768], F32, pes) for i in range(2)]
            pm = [fw.ps("pmod%d" % i, [128, 6, 2], F32, pes) for i in range(2)]
            it = 0
            for l in range(nlayers):
                for piece in range(8):
                    wb = wbuf[it % 2]
                    pp = pm[it % 2]
                    it += 1
                    q = "sp" if piece % 2 == 0 else "act"
                    eng = nc.sync if piece % 2 == 0 else nc.scalar
                    fw.dma(q, lambda: eng.dma_start(out=wb[:], in_=I["w_mod"][l, :, piece * 768:(piece + 1) * 768].rearrange("(c p) n -> p c n", p=128)),
                           reads=[I["w_mod"]], writes=[wb])
                    for cc in range(6):
                        for kc in range(8):
                            fw.op("pe", lambda: nc.tensor.matmul(pp[:, cc, :], lhsT=wb[:, kc, cc * 128:(cc + 1) * 128], rhs=condT[:, :, kc],
                                                               start=(kc == 0), stop=(kc == 7)),
                                  reads=[wb, condT], writes=[pp], chain=True)
                    for j in range(2):
                        fw.op("dve", lambda: nc.vector.tensor_tensor(out=modT[:, l, piece * 6:(piece + 1) * 6, j], in0=pp[:, :, j],
                                                                   in1=bmT[:, l, piece * 6:(piece + 1) * 6], op=ALU.add),
                              reads=[pp, bmT], writes=[modT])
            for l in range(nlayers):
                for i in (1, 4):
                    fw.op("dve", lambda: nc.vector.tensor_scalar_add(out=modT[:, l, i * 8:(i + 1) * 8, :], in0=modT[:, l, i * 8:(i + 1) * 8, :], scalar1=1.0),
                          reads=[modT], writes=[modT])

    def prephase():
        with ExitStack() as pes:
            xt = [fw.sb("xt%d" % i, [128, D], F32, pes) for i in range(2)]
            xo = [fw.sb("xo%d" % i, [128, 8, 128], F32, pes) for i in range(2)]
            pt = [fw.ps("ptr%d" % i, [128, 8, 128], F32, pes) for i in range(2)]
            for ti in range(NTOK // 128):
                a = xt[ti % 2]; o = xo[ti % 2]; p = pt[ti % 2]
                fw.dma("sp", lambda: nc.sync.dma_start(out=a[:], in_=I["xin"][ti * 128:(ti + 1) * 128, :]), reads=[I["xin"]], writes=[a])
                for c in range(8):
                    fw.op("pe", lambda: nc.tensor.transpose(out=p[:, c, :], in_=a[:, c * 128:(c + 1) * 128], identity=ident_f[:]),
                          reads=[a, ident_f], writes=[p], chain=True)
                if ti % 2 == 0:
                    fw.op("act", lambda: nc.scalar.copy(out=o[:], in_=p[:]), reads=[p], writes=[o])
                else:
                    fw.op("dve", lambda: nc.vector.tensor_copy(out=o[:], in_=p[:]), reads=[p], writes=[o])
                fw.dma("act", lambda: nc.scalar.dma_start(out=xs[:, :, ti * 128:(ti + 1) * 128], in_=o[:]), reads=[o], writes=[xs])

    def norm_mod(pes_bufs, xg, n, gvec, l, ishift, iscale, j, out_bf=None, out_f=None):
        sq, pss, rstd, GS = pes_bufs
        fw.op("act", lambda: nc.scalar.activation(out=sq[:, :, :n], in_=xg[:, :, :n], func=AF.Square), reads=[xg], writes=[sq])
        for c in range(8):
            fw.op("pe", lambda: nc.tensor.matmul(pss[:, :n], lhsT=ones_b[:], rhs=sq[:, c, :n], start=(c == 0), stop=(c == 7)),
                  reads=[sq, ones_b], writes=[pss], chain=True)
        fw.op("act", lambda: nc.scalar.activation(out=rstd[:, :n], in_=pss[:, :n], func=AF.Sqrt, bias=EPS, scale=1.0 / D), reads=[pss], writes=[rstd])
        fw.op("dve", lambda: nc.vector.reciprocal(out=rstd[:, :n], in_=rstd[:, :n]), reads=[rstd], writes=[rstd])
        fw.op("dve", lambda: nc.vector.tensor_tensor(out=GS[:, 0, :], in0=gvec, in1=modT[:, l, iscale * 8:(iscale + 1) * 8, j], op=ALU.mult),
              reads=[modT, gmix, gffn], writes=[GS])
        for c in range(8):
            tgt = out_f if out_f is not None else sq
            if out_f is not None:
                fw.op("dve", lambda: nc.vector.scalar_tensor_tensor(out=out_f[:, c, :n], in0=xg[:, c, :n], scalar=GS[:, 0, c:c + 1], in1=rstd[:, :n],
                                                                  op0=ALU.mult, op1=ALU.mult), reads=[xg, GS, rstd], writes=[out_f])
                fw.op("dve", lambda: nc.vector.tensor_scalar_add(out=out_f[:, c, :n], in0=out_f[:, c, :n], scalar1=modT[:, l, ishift * 8 + c, j:j + 1]),
                      reads=[out_f, modT], writes=[out_f])
                if out_bf is not None:
                    fw.op("act", lambda: nc.scalar.copy(out=out_bf[:, c, :n], in_=out_f[:, c, :n]), reads=[out_f], writes=[out_bf])
            else:
                fw.op("dve", lambda: nc.vector.scalar_tensor_tensor(out=xg[:, c, :n], in0=xg[:, c, :n], scalar=GS[:, 0, c:c + 1], in1=rstd[:, :n],
                                                                  op0=ALU.mult, op1=ALU.mult), reads=[xg, GS, rstd], writes=[xg])
                fw.op("act", lambda: nc.scalar.activation(out=out_bf[:, c, :n], in_=xg[:, c, :n], func=AF.Identity,
                                                        bias=modT[:, l, ishift * 8 + c, j:j + 1], scale=1.0), reads=[xg, modT], writes=[out_bf])

    NCOLF = 20 * 128

    def phaseA(l):
        with ExitStack() as pes:
            w2 = fw.sb("w2", [128, 8, NCOLF], BF16, pes)
            wv = fw.sb("wv", [128, 8, 512], BF16, pes)
            win = I["w_in"]

            def wl(dst, dlo, slo, n, q="pool"):
                fw.dma("pool", lambda: nc.gpsimd.dma_start(out=dst[:, :, dlo:dlo + n], in_=win[l, :, slo:slo + n].rearrange("(c p) n -> p c n", p=128)),
                       reads=[win], writes=[dst])
            wl(w2, 0, 0, 384)
            wl(w2, 384, 384, 384)
            wl(w2, 768, 1152, 256)
            wl(w2, 1024, 1408, 384)
            for ti, (h0, h1) in enumerate(((0, 0), (0, 1), (1, 1))):
                wl(w2, 1792 + ti * 128, 1792 + h0 * 64, 64)
                wl(w2, 1792 + ti * 128 + 64, 1792 + h1 * 64, 64)
            wl(wv, 0, 768, 384)
            wl(wv, 384, 1920, 128)
            for (src, dst, nh) in ((1024, 1408, 6), (1792, 2176, 6)):
                sv = w2[:, :, src:src + nh * 64].rearrange("p c (h a j f) -> p c h a j f", h=nh, a=2, j=2, f=16)
                dv = w2[:, :, dst:dst + nh * 64].rearrange("p c (h a j f) -> p c h a j f", h=nh, a=2, j=2, f=16)
                for c in range(8):
                    for jj in range(2):
                        fw.op("pool", lambda: nc.gpsimd.tensor_copy(out=dv[:, c, :, :, jj, :], in_=sv[:, c, :, :, 1 - jj, :]), reads=[w2], writes=[w2])
            xg = [fw.sb("xg%d" % i, [128, 8, 512], F32, pes) for i in range(2)]
            hT = [fw.sb("hT%d" % i, [128, 8, 512], BF16, pes) for i in range(2)]
            sq = fw.sb("sq", [128, 8, 512], BF16, pes)
            rstd = fw.sb("rstd", [128, 512], F32, pes)
            GS = fw.sb("GS", [128, 1, 8], F32, pes)
            pss = fw.ps("pss", [128, 512], F32, pes)
            pp = [fw.ps("ppA%d" % i, [128, 512], F32, pes) for i in range(4)]
            pv = [fw.ps("ppV%d" % i, [128, 512], F32, pes) for i in range(2)]
            ob = [fw.sb("obA%d" % i, [128, 512], BF16, pes) for i in range(4)]
            of = [fw.sb("ofA%d" % i, [128, 512], F32, pes) for i in range(2)]
            t1 = [fw.sb("t1A%d" % i, [128, 512], F32, pes) for i in range(2)]
            t2 = [fw.sb("t2A%d" % i, [128, 512], F32, pes) for i in range(2)]
            rc = [fw.sb("rc%d" % i, [128, 512], F32, pes) for i in range(2)]
            rs_ = [fw.sb("rs%d" % i, [128, 512], F32, pes) for i in range(2)]
            vst = [fw.sb("vst%d" % i, [128, 8, 65], BF16, pes) for i in range(2)]
            for v in vst:
                fw.op("dve", lambda: nc.vector.memset(v[:], 1.0), writes=[v])
            k = 0
            ko = 0
            for gi, (t0, n, isctx) in enumerate(groups()):
                j = 1 if isctx else 0
                x_ = xg[gi % 2]; h_ = hT[gi % 2]
                fw.dma("sp", lambda: nc.sync.dma_start(out=x_[:, :, :n], in_=xs[:, :, t0:t0 + n]), reads=[xs], writes=[x_])
                if not isctx:
                    r_c = rc[gi % 2]; r_s = rs_[gi % 2]
                    fw.dma("act", lambda: nc.scalar.dma_start(out=r_c[:], in_=I["rope_c"][:, t0:t0 + n]), reads=[I["rope_c"]], writes=[r_c])
                    fw.dma("act", lambda: nc.scalar.dma_start(out=r_s[:], in_=I["rope_s"][:, t0:t0 + n]), reads=[I["rope_s"]], writes=[r_s])
                norm_mod((sq, pss, rstd, GS), x_, n, gmix[:, l, :], l, 0, 1, j, out_bf=h_)

                def proj(colblk):
                    nonlocal k
                    p = pp[k % 4]; k += 1
                    for kc in range(8):
                        fw.op("pe", lambda: nc.tensor.matmul(p[:, :n], lhsT=w2[:, kc, colblk * 128:(colblk + 1) * 128], rhs=h_[:, kc, :n],
                                                           start=(kc == 0), stop=(kc == 7)), reads=[w2, h_], writes=[p], chain=True)
                    return p
                for blk in range(8):
                    p = proj(blk)
                    if blk < 6:
                        o = ob[ko % 4]; ko += 1
                        if blk % 2 == 0:
                            fw.op("act", lambda: nc.scalar.copy(out=o[:, :n], in_=p[:, :n]), reads=[p], writes=[o])
                        else:
                            fw.op("dve", lambda: nc.vector.tensor_copy(out=o[:, :n], in_=p[:, :n]), reads=[p], writes=[o])
                        dst = qa if blk < 3 else ka
                        fw.dma("sp", lambda: nc.sync.dma_start(out=dst[blk % 3, :, t0:t0 + n], in_=o[:, :n]), reads=[o], writes=[dst])
                    else:
                        o = of[blk % 2]
                        fw.op("act", lambda: nc.scalar.copy(out=o[:, :n], in_=p[:, :n]), reads=[p], writes=[o])
                        fw.dma("sp", lambda: nc.sync.dma_start(out=u5[blk - 6, :, t0:t0 + n], in_=o[:, :n]), reads=[o], writes=[u5])
                for which, base, dst in ((0, 8, qs), (1, 14, ks)):
                    for ti in range(3):
                        p = proj(base + ti)
                        o = ob[ko % 4]; ko += 1
                        if isctx:
                            fw.op("act", lambda: nc.scalar.copy(out=o[:, :n], in_=p[:, :n]), reads=[p], writes=[o])
                        else:
                            pr = proj(base + 3 + ti)
                            a = t1[ti % 2]; b = t2[ti % 2]
                            fw.op("dve", lambda: nc.vector.tensor_tensor(out=a[:, :n], in0=p[:, :n], in1=r_c[:, :n], op=ALU.mult), reads=[p, r_c], writes=[a])
                            fw.op("dve", lambda: nc.vector.tensor_tensor(out=b[:, :n], in0=pr[:, :n], in1=r_s[:, :n], op=ALU.mult), reads=[pr, r_s], writes=[b])
                            fw.op("pool", lambda: nc.gpsimd.tensor_tensor(out=o[:, :n], in0=a[:, :n], in1=b[:, :n], op=ALU.add), reads=[a, b], writes=[o])
                        fw.dma("sp", lambda: nc.sync.dma_start(out=dst[ti, :, t0:t0 + n], in_=o[:, :n]), reads=[o], writes=[dst])
                for tt in range(n // 128):
                    p = pv[tt % 2]; v = vst[tt % 2]
                    for kc in range(8):
                        fw.op("pe", lambda: nc.tensor.matmul(p[:, :], lhsT=h_[:, kc, tt * 128:(tt + 1) * 128], rhs=wv[:, kc, :],
                                                           start=(kc == 0), stop=(kc == 7)), reads=[wv, h_], writes=[p], chain=True)
                    fw.op("act", lambda: nc.scalar.copy(out=v[:, :, 0:64], in_=p[:, :].rearrange("p (h d) -> p h d", d=64)), reads=[p], writes=[v])
                    r0 = t0 + tt * 128
                    fw.dma("act", lambda: nc.scalar.dma_start(out=va[r0:r0 + 128, :].rearrange("t (h d) -> t h d", d=65), in_=v[:, 0:6, :]), reads=[v], writes=[va])
                    fw.dma("act", lambda: nc.scalar.dma_start(out=vs[r0:r0 + 128, :].rearrange("t (h d) -> t h d", d=65), in_=v[:, 6:8, :]), reads=[v], writes=[vs])


    def attn_norm1(pes_t, po, n, h, sink=None):
        rden, osb, ysb, pbc = pes_t
        if sink is not None:
            fw.op("dve", lambda: nc.vector.tensor_scalar(out=rden[64:65, :n], in0=po[64:65, :n], scalar1=sink[64:65, h:h + 1], scalar2=None, op0=ALU.add),
                  reads=[po, sink], writes=[rden])
            fw.op("dve", lambda: nc.vector.reciprocal(out=rden[64:65, :n], in_=rden[64:65, :n]), reads=[rden], writes=[rden])
        else:
            fw.op("dve", lambda: nc.vector.reciprocal(out=rden[64:65, :n], in_=po[64:65, :n]), reads=[po], writes=[rden])
        fw.op("act", lambda: nc.scalar.copy(out=osb[0:64, :n], in_=po[0:64, :n]), reads=[po], writes=[osb])

    def attn_norm2(pes_t, n, t0, chtile, base):
        rden, osb, ysb, pbc = pes_t
        fw.op("pe", lambda: nc.tensor.matmul(pbc[0:64, :n], lhsT=ones_f[64:65, 0:64], rhs=rden[64:65, :n], start=True, stop=True),
              reads=[rden, ones_f], writes=[pbc])
        fw.op("dve", lambda: nc.vector.tensor_tensor(out=ysb[0:64, :n], in0=osb[0:64, :n], in1=pbc[0:64, :n], op=ALU.mult), reads=[osb, pbc], writes=[ysb])
        fw.dma("sp", lambda: nc.sync.dma_start(out=yT[chtile, base:base + 64, t0:t0 + n], in_=ysb[0:64, :n]), reads=[ysb], writes=[yT])

    class Deferred:
        def __init__(self):
            self.p = None

        def push(self, *a):
            self.flush()
            self.p = a

        def flush(self):
            if self.p is not None:
                attn_norm2(*self.p)
                self.p = None

    rp = fw.dram("rp", [1, 3072], F32)

    def phaseB(l, with_ctx):
        with ExitStack() as pes:
            z = fw.sb("zrp", [1, 3072], F32, pes)
            fw.op("dve", lambda: nc.vector.memset(z[:], 0.0), writes=[z])
            fw.dma("sp", lambda: nc.sync.dma_start(out=z[0:1, 64:64 + 2790], in_=I["na_rpb"][l:l + 1].rearrange("o h a b -> o (h a b)")), reads=[I["na_rpb"]], writes=[z])
            fw.dma("sp", lambda: nc.sync.dma_start(out=rp[:], in_=z[:]), reads=[z], writes=[rp])
            BB = fw.sb("BB", [64, 6, 15, 64], F32, pes)
            for h in range(6):
                src = bass.AP(tensor=rp.t.tensor, offset=64 + h * 465 + 15 - 63, ap=[[1, 64], [31, 15], [1, 64]])
                fw.dma("sp", lambda: nc.sync.dma_start(out=BB[:, h, :, :], in_=src), reads=[rp], writes=[BB])
            pen = fw.sb("napen", [128, 64], F32, pes)
            fw.dma("sp", lambda: nc.sync.dma_start(out=pen[:], in_=I["na_pen"][:]), reads=[I["na_pen"]], writes=[pen])
            biasT = fw.sb("biasT", [128, 6, 14, 64], BF16, pes)
            with ExitStack() as pes2:
                pb = [fw.ps("pbias%d" % i, [128, 64], F32, pes2) for i in range(2)]
                it = 0
                for h in range(6):
                    for a in range(14):
                        p = pb[it % 2]; it += 1
                        fw.op("pe", lambda: nc.tensor.transpose(out=p[:, :], in_=BB[:, h, a:a + 2, :].rearrange("p a k -> p (a k)"), identity=ident_f[0:64, 0:64]),
                              reads=[BB, ident_f], writes=[p])
                        fw.op("dve", lambda: nc.vector.scalar_tensor_tensor(out=biasT[:, h, a, :], in0=p[:, ::-1], scalar=8.0, in1=pen[:], op0=ALU.mult, op1=ALU.add),
                              reads=[p, pen], writes=[biasT])
                fw.barrier()
            qT = fw.sb("qT", [128, NTOK], BF16, pes)
            kT = fw.sb("kT", [128, NTOK], BF16, pes)
            v0 = fw.sb("v0", [128, 66, 2, 65], BF16, pes)
            v1 = fw.sb("v1", [128, 63, 2, 65], BF16, pes)
            pc = fw.ps("pc", [128, 2, 512], F32, pes)
            pn = [fw.ps("pn%d" % i, [128, 4, 4, 64], F32, pes) for i in range(2)]
            po = fw.ps("po", [128, 512], F32, pes)
            pbc = fw.ps("pbc", [128, 512], F32, pes)
            PcT = fw.sb("PcT", [128, 2, 512], BF16, pes)
            PnT = [fw.sb("PnT%d" % i, [128, 4, 4, 64], BF16, pes) for i in range(2)]
            nt = [(fw.sb("rden%d" % i, [128, 512], F32, pes), fw.sb("osb%d" % i, [128, 512], F32, pes), fw.sb("ysb%d" % i, [128, 512], BF16, pes), pbc) for i in range(2)]
            dfr = Deferred(); po_i = 0
            ih = 0
            for jp in range(3):
                fw.dma("sp", lambda: nc.sync.dma_start(out=qT[:], in_=qa[jp]), reads=[qa], writes=[qT])
                fw.dma("act", lambda: nc.scalar.dma_start(out=kT[:], in_=ka[jp]), reads=[ka], writes=[kT])
                fw.dma("sp", lambda: nc.sync.dma_start(out=v0[:].rearrange("p t h d -> p t (h d)"),
                                                      in_=va[:, jp * 130:(jp + 1) * 130].rearrange("(t p) c -> p t c", p=128)), reads=[va], writes=[v0])
                fw.dma("act", lambda: nc.scalar.dma_start(out=v1[:].rearrange("p t h d -> p t (h d)"),
                                                        in_=va[64:64 + 63 * 128, jp * 130:(jp + 1) * 130].rearrange("(t p) c -> p t c", p=128)), reads=[va], writes=[v1])
                for hh in range(2):
                    h = 2 * jp + hh
                    b0 = 64 * hh
                    for G in range(16):
                        t0 = 512 * G
                        for ct in range(2):
                            fw.op("pe", lambda: nc.tensor.matmul(pc[:, ct, :], lhsT=kT[b0:b0 + 64, SEQ + 128 * ct:SEQ + 128 * ct + 128], rhs=qT[b0:b0 + 64, t0:t0 + 512],
                                                               start=True, stop=True), reads=[kT, qT], writes=[pc])
                        fw.op("act", lambda: nc.scalar.activation(out=PcT[:], in_=pc[:], func=AF.Exp, scale=0.125), reads=[pc], writes=[PcT])
                        for half in range(2):
                            p_ = pn[ih % 2]; P_ = PnT[ih % 2]; ih += 1
                            for rr in range(4):
                                r = 8 * G + 4 * half + rr
                                rs = min(max(r - 4, 0), 120)
                                for j in range(4):
                                    k0 = (rs + 2 * j) * 64
                                    a = rs + 2 * j - r + 7
                                    fw.op("pe", lambda: nc.tensor.matmul(p_[:, rr, j, :], lhsT=kT[b0:b0 + 64, k0:k0 + 128], rhs=qT[b0:b0 + 64, r * 64:r * 64 + 64],
                                                                       start=True, stop=False), reads=[kT, qT], writes=[p_], chain=True)
                                    fw.op("pe", lambda: nc.tensor.matmul(p_[:, rr, j, :], lhsT=ident_b[:, :], rhs=biasT[:, h, a, :],
                                                                       start=False, stop=True), reads=[biasT, ident_b], writes=[p_], chain=True)
                            if half == 0:
                                dfr.flush()
                            fw.op("act", lambda: nc.scalar.activation(out=P_[:], in_=p_[:], func=AF.Exp, scale=0.125), reads=[p_], writes=[P_])
                            for rr in range(4):
                                r = 8 * G + 4 * half + rr
                                rs = min(max(r - 4, 0), 120)
                                c0 = (4 * half + rr) * 64
                                for j in range(4):
                                    k0 = (rs + 2 * j) * 64
                                    vt = v0[:, k0 // 128, hh, :] if rs % 2 == 0 else v1[:, (k0 - 64) // 128, hh, :]
                                    fw.op("pe", lambda: nc.tensor.matmul(po[0:65, c0:c0 + 64], lhsT=vt, rhs=P_[:, rr, j, :], start=(j == 0), stop=False),
                                          reads=[v0, v1, P_], writes=[po], chain=True)
                                for ct in range(2):
                                    fw.op("pe", lambda: nc.tensor.matmul(po[0:65, c0:c0 + 64], lhsT=v0[:, 64 + ct, hh, :], rhs=PcT[:, ct, c0:c0 + 64], start=False, stop=(ct == 1)),
                                          reads=[v0, PcT], writes=[po], chain=True)
                        attn_norm1(nt[po_i % 2], po, 512, h)
                        dfr.push(nt[po_i % 2], 512, t0, jp, b0); po_i += 1
                    if with_ctx:
                        for ct in range(2):
                            fw.op("pe", lambda: nc.tensor.matmul(pc[:, ct, :CTXL], lhsT=kT[b0:b0 + 64, SEQ + 128 * ct:SEQ + 128 * ct + 128], rhs=qT[b0:b0 + 64, SEQ:SEQ + CTXL],
                                                               start=True, stop=True), reads=[kT, qT], writes=[pc])
                        fw.op("act", lambda: nc.scalar.activation(out=PcT[:, :, :CTXL], in_=pc[:, :, :CTXL], func=AF.Exp, scale=0.125), reads=[pc], writes=[PcT])
                        for ct in range(2):
                            fw.op("pe", lambda: nc.tensor.matmul(po[0:65, :CTXL], lhsT=v0[:, 64 + ct, hh, :], rhs=PcT[:, ct, :CTXL], start=(ct == 0), stop=(ct == 1)),
                                  reads=[v0, PcT], writes=[po], chain=True)
                        attn_norm1(nt[po_i % 2], po, CTXL, h)
                        dfr.push(nt[po_i % 2], CTXL, SEQ, jp, b0); po_i += 1

            dfr.flush()

    def phaseD(l, with_ctx):
        with ExitStack() as pes:
            sk = fw.sb("sinks", [128, 6], F32, pes)
            fw.dma("sp", lambda: nc.sync.dma_start(out=sk[64:65, :], in_=I["sw_sinks"][l:l + 1, :]), reads=[I["sw_sinks"]], writes=[sk])
            fw.op("act", lambda: nc.scalar.activation(out=sk[64:65, :], in_=sk[64:65, :], func=AF.Exp), reads=[sk], writes=[sk])
            ml = fw.sb("mask_l", [128, 128], BF16, pes)
            mu = fw.sb("mask_u", [128, 128], BF16, pes)
            fw.dma("sp", lambda: nc.sync.dma_start(out=ml[:], in_=I["mask_l"][:]), reads=[I["mask_l"]], writes=[ml])
            fw.dma("sp", lambda: nc.sync.dma_start(out=mu[:], in_=I["mask_u"][:]), reads=[I["mask_u"]], writes=[mu])
            qT = fw.sb("qTs", [128, NTOK], BF16, pes)
            kT = fw.sb("kTs", [128, NTOK], BF16, pes)
            vS = fw.sb("vS", [128, 66, 2, 65], BF16, pes)
            fw.dma("sp", lambda: nc.sync.dma_start(out=vS[:].rearrange("p t h d -> p t (h d)"), in_=vs[:, :].rearrange("(t p) c -> p t c", p=128)), reads=[vs], writes=[vS])
            pc = fw.ps("pcs", [128, 2, 512], F32, pes)
            pn = [fw.ps("pns%d" % i, [128, 3, 128], F32, pes) for i in range(2)]
            po = fw.ps("pos", [128, 512], F32, pes)
            pbc = fw.ps("pbcs", [128, 512], F32, pes)
            PcT = fw.sb("PcTs", [128, 2, 512], BF16, pes)
            PnT = [fw.sb("PnTs%d" % i, [128, 3, 128], BF16, pes) for i in range(2)]
            nt = [(fw.sb("rdens%d" % i, [128, 512], F32, pes), fw.sb("osbs%d" % i, [128, 512], F32, pes), fw.sb("ysbs%d" % i, [128, 512], BF16, pes), pbc) for i in range(2)]
            dfr = Deferred(); po_i = 0
            ih = 0
            NB = SEQ // 128
            for jp in range(3):
                fw.dma("sp", lambda: nc.sync.dma_start(out=qT[:], in_=qs[jp]), reads=[qs], writes=[qT])
                fw.dma("act", lambda: nc.scalar.dma_start(out=kT[:], in_=ks[jp]), reads=[ks], writes=[kT])
                for hh in range(2):
                    h = 2 * jp + hh
                    kvh = h // 3
                    b0 = 64 * hh
                    for G in range(16):
                        t0 = 512 * G
                        for ct in range(2):
                            fw.op("pe", lambda: nc.tensor.matmul(pc[:, ct, :], lhsT=kT[b0:b0 + 64, SEQ + 128 * ct:SEQ + 128 * ct + 128], rhs=qT[b0:b0 + 64, t0:t0 + 512],
                                                               start=True, stop=True), reads=[kT, qT], writes=[pc])
                        fw.op("act", lambda: nc.scalar.activation(out=PcT[:], in_=pc[:], func=AF.Exp, scale=0.125), reads=[pc], writes=[PcT])
                        for nb_ in range(4):
                            n = 4 * G + nb_
                            p_ = pn[ih % 2]; P_ = PnT[ih % 2]; ih += 1
                            kbs = [kb for kb in (n - 1, n, n + 1) if 0 <= kb < NB]
                            lo = kbs[0] - (n - 1)
                            hi = kbs[-1] - (n - 1) + 1
                            for kb in kbs:
                                ki = kb - (n - 1)
                                fw.op("pe", lambda: nc.tensor.matmul(p_[:, ki, :], lhsT=kT[b0:b0 + 64, kb * 128:kb * 128 + 128], rhs=qT[b0:b0 + 64, n * 128:n * 128 + 128],
                                                                   start=True, stop=(kb == n)), reads=[kT, qT], writes=[p_], chain=True)
                                if kb != n:
                                    mk = ml if kb < n else mu
                                    fw.op("pe", lambda: nc.tensor.matmul(p_[:, ki, :], lhsT=ident_b[:, :], rhs=mk[:, :], start=False, stop=True),
                                          reads=[mk, ident_b], writes=[p_], chain=True)
                            if nb_ == 0:
                                dfr.flush()
                            fw.op("act", lambda: nc.scalar.activation(out=P_[:, lo:hi, :], in_=p_[:, lo:hi, :], func=AF.Exp, scale=0.125), reads=[p_], writes=[P_])
                            c0 = nb_ * 128
                            for kb in kbs:
                                ki = kb - (n - 1)
                                fw.op("pe", lambda: nc.tensor.matmul(po[0:65, c0:c0 + 128], lhsT=vS[:, kb, kvh, :], rhs=P_[:, ki, :], start=(kb == kbs[0]), stop=False),
                                      reads=[vS, P_], writes=[po], chain=True)
                            for ct in range(2):
                                fw.op("pe", lambda: nc.tensor.matmul(po[0:65, c0:c0 + 128], lhsT=vS[:, 64 + ct, kvh, :], rhs=PcT[:, ct, c0:c0 + 128], start=False, stop=(ct == 1)),
                                      reads=[vS, PcT], writes=[po], chain=True)
                        attn_norm1(nt[po_i % 2], po, 512, h, sink=sk)
                        dfr.push(nt[po_i % 2], 512, t0, 5 + jp, b0); po_i += 1
                    if with_ctx:
                        for ct in range(2):
                            fw.op("pe", lambda: nc.tensor.matmul(pc[:, ct, :CTXL], lhsT=kT[b0:b0 + 64, SEQ + 128 * ct:SEQ + 128 * ct + 128], rhs=qT[b0:b0 + 64, SEQ:SEQ + CTXL],
                                                               start=True, stop=True), reads=[kT, qT], writes=[pc])
                        fw.op("act", lambda: nc.scalar.activation(out=PcT[:, :, :CTXL], in_=pc[:, :, :CTXL], func=AF.Exp, scale=0.125), reads=[pc], writes=[PcT])
                        for ct in range(2):
                            fw.op("pe", lambda: nc.tensor.matmul(po[0:65, :CTXL], lhsT=vS[:, 64 + ct, kvh, :], rhs=PcT[:, ct, :CTXL], start=(ct == 0), stop=(ct == 1)),
                                  reads=[vS, PcT], writes=[po], chain=True)
                        attn_norm1(nt[po_i % 2], po, CTXL, h, sink=sk)
                        dfr.push(nt[po_i % 2], CTXL, SEQ, 5 + jp, b0); po_i += 1
            dfr.flush()

    yf = fw.dram("yf", [2, 128, NTOK], F32)
    TWO_PI = 6.283185307179586
    C1 = 6.28125
    C2 = TWO_PI - 6.28125
    PI = 3.141592653589793

    def phaseC(l, with_ctx):
        with ExitStack() as pes:
            def tl(name, shape, dt=F32):
                return fw.sb(name, shape, dt, pes)
            scr_i = tl("scr_i", [128, 512], I32)
            scr_k = tl("scr_k", [128, 512])
            scr_t = tl("scr_t", [128, 512])
            scr_a = tl("scr_a", [128, 512])
            scr_b = tl("scr_b", [128, 512])

            def dve(fn, reads, writes):
                fw.op("dve", fn, reads=reads, writes=writes)

            def reduce_angle(ang, n, srcs):
                dve(lambda: nc.vector.tensor_scalar(out=scr_k[:, :n], in0=ang, scalar1=1.0 / TWO_PI, scalar2=None, op0=ALU.mult), srcs, [scr_k])
                dve(lambda: nc.vector.tensor_copy(out=scr_i[:, :n], in_=scr_k[:, :n]), [scr_k], [scr_i])
                dve(lambda: nc.vector.tensor_copy(out=scr_k[:, :n], in_=scr_i[:, :n]), [scr_i], [scr_k])
                dve(lambda: nc.vector.scalar_tensor_tensor(out=scr_a[:, :n], in0=scr_k[:, :n], scalar=-C1, in1=ang, op0=ALU.mult, op1=ALU.add), [scr_k] + srcs, [scr_a])
                dve(lambda: nc.vector.scalar_tensor_tensor(out=scr_a[:, :n], in0=scr_k[:, :n], scalar=-C2, in1=scr_a[:, :n], op0=ALU.mult, op1=ALU.add), [scr_k, scr_a], [scr_a])
                wrap(scr_a, n)

            def wrap(t, n):
                dve(lambda: nc.vector.tensor_scalar(out=scr_t[:, :n], in0=t[:, :n], scalar1=PI, scalar2=None, op0=ALU.is_gt), [t], [scr_t])
                dve(lambda: nc.vector.scalar_tensor_tensor(out=t[:, :n], in0=scr_t[:, :n], scalar=-TWO_PI, in1=t[:, :n], op0=ALU.mult, op1=ALU.add), [scr_t, t], [t])
                dve(lambda: nc.vector.tensor_scalar(out=scr_t[:, :n], in0=t[:, :n], scalar1=-PI, scalar2=None, op0=ALU.is_lt), [t], [scr_t])
                dve(lambda: nc.vector.scalar_tensor_tensor(out=t[:, :n], in0=scr_t[:, :n], scalar=TWO_PI, in1=t[:, :n], op0=ALU.mult, op1=ALU.add), [scr_t, t], [t])

            def sincos(ang, n, srcs, out_s, out_c, outs):
                reduce_angle(ang, n, srcs)
                fw.op("act", lambda: nc.scalar.activation(out=out_s, in_=scr_a[:, :n], func=AF.Sin), reads=[scr_a], writes=outs)
                dve(lambda: nc.vector.tensor_scalar(out=scr_b[:, :n], in0=scr_a[:, :n], scalar1=PI / 2, scalar2=None, op0=ALU.add), [scr_a], [scr_b])
                wrap(scr_b, n)
                fw.op("act", lambda: nc.scalar.activation(out=out_c, in_=scr_b[:, :n], func=AF.Sin), reads=[scr_b], writes=outs)

            are = tl("are", [128, 16]); aim = tl("aim", [128, 16]); stp = tl("stp", [128, 16])
            with nc.allow_non_contiguous_dma(reason="tiny s5 params"):
                for nm, dst in (("s5_a_re", are), ("s5_a_im", aim)):
                    for d_ in range(2):
                        fw.dma("sp", lambda: nc.sync.dma_start(out=dst[:, d_ * 8:(d_ + 1) * 8], in_=I[nm][l, d_].rearrange("g p -> (g p)").rearrange("(i q) -> q i", q=128)),
                               reads=[I[nm]], writes=[dst])
                for d_ in range(2):
                    for g2 in range(2):
                        src = bass.AP(tensor=I["s5_log_step"].t.tensor, offset=l * 32 + d_ * 16 + g2, ap=[[0, 64], [2, 8]])
                        fw.dma("sp", lambda: nc.sync.dma_start(out=stp[64 * g2:64 * g2 + 64, d_ * 8:(d_ + 1) * 8], in_=src), reads=[I["s5_log_step"]], writes=[stp])
            fw.op("act", lambda: nc.scalar.activation(out=stp[:], in_=stp[:], func=AF.Exp), reads=[stp], writes=[stp])
            dve(lambda: nc.vector.tensor_scalar(out=are[:], in0=are[:], scalar1=-1e-4, scalar2=None, op0=ALU.min), [are], [are])
            lr = tl("lr", [128, 16]); li = tl("li", [128, 16]); rr = tl("rr", [128, 16])
            dve(lambda: nc.vector.tensor_tensor(out=lr[:], in0=are[:], in1=stp[:], op=ALU.mult), [are, stp], [lr])
            dve(lambda: nc.vector.tensor_tensor(out=li[:], in0=aim[:], in1=stp[:], op=ALU.mult), [aim, stp], [li])
            fw.op("act", lambda: nc.scalar.activation(out=rr[:], in_=lr[:], func=AF.Exp), reads=[lr], writes=[rr])
            s1 = tl("s1", [128, 16]); c1 = tl("c1", [128, 16])
            sincos(li[:, :], 16, [li], s1[:, :], c1[:, :], [s1, c1])
            sL = {}; cL = {}
            angL = tl("angL", [128, 16])
            for L in (512, 256):
                sL[L] = tl("sL%d" % L, [128, 16]); cL[L] = tl("cL%d" % L, [128, 16])
                dve(lambda: nc.vector.tensor_scalar(out=angL[:], in0=li[:], scalar1=float(L), scalar2=None, op0=ALU.mult), [li], [angL])
                sincos(angL[:, :], 16, [angL], sL[L][:, :], cL[L][:, :], [sL[L], cL[L]])
            lbr = tl("lbr", [128, 16]); lbi = tl("lbi", [128, 16]); den = tl("den", [128, 16]); tq = tl("tq", [128, 16])
            cr = tl("cr", [128, 16]); ci = tl("ci", [128, 16]); nci = tl("nci", [128, 16])
            dve(lambda: nc.vector.tensor_tensor(out=lbr[:], in0=rr[:], in1=c1[:], op=ALU.mult), [rr, c1], [lbr])
            dve(lambda: nc.vector.tensor_scalar(out=lbr[:], in0=lbr[:], scalar1=-1.0, scalar2=None, op0=ALU.add), [lbr], [lbr])
            dve(lambda: nc.vector.tensor_tensor(out=lbi[:], in0=rr[:], in1=s1[:], op=ALU.mult), [rr, s1], [lbi])
            dve(lambda: nc.vector.tensor_tensor(out=den[:], in0=are[:], in1=are[:], op=ALU.mult), [are], [den])
            dve(lambda: nc.vector.tensor_tensor(out=tq[:], in0=aim[:], in1=aim[:], op=ALU.mult), [aim], [tq])
            dve(lambda: nc.vector.tensor_tensor(out=den[:], in0=den[:], in1=tq[:], op=ALU.add), [den, tq], [den])
            dve(lambda: nc.vector.reciprocal(out=den[:], in_=den[:]), [den], [den])
            dve(lambda: nc.vector.tensor_tensor(out=cr[:], in0=lbr[:], in1=are[:], op=ALU.mult), [lbr, are], [cr])
            dve(lambda: nc.vector.tensor_tensor(out=tq[:], in0=lbi[:], in1=aim[:], op=ALU.mult), [lbi, aim], [tq])
            dve(lambda: nc.vector.tensor_tensor(out=cr[:], in0=cr[:], in1=tq[:], op=ALU.add), [cr, tq], [cr])
            dve(lambda: nc.vector.tensor_tensor(out=cr[:], in0=cr[:], in1=den[:], op=ALU.mult), [cr, den], [cr])
            dve(lambda: nc.vector.tensor_tensor(out=ci[:], in0=lbi[:], in1=are[:], op=ALU.mult), [lbi, are], [ci])
            dve(lambda: nc.vector.tensor_tensor(out=tq[:], in0=lbr[:], in1=aim[:], op=ALU.mult), [lbr, aim], [tq])
            dve(lambda: nc.vector.tensor_tensor(out=ci[:], in0=ci[:], in1=tq[:], op=ALU.subtract), [ci, tq], [ci])
            dve(lambda: nc.vector.tensor_tensor(out=ci[:], in0=ci[:], in1=den[:], op=ALU.mult), [ci, den], [ci])
            dve(lambda: nc.vector.tensor_scalar(out=nci[:], in0=ci[:], scalar1=-1.0, scalar2=None, op0=ALU.mult), [ci], [nci])
            bsr = tl("bsr", [128, 2, 2, 128]); bsi = tl("bsi", [128, 2, 2, 128])
            bbr = tl("bbr", [128, 2, 2, 128]); bbi = tl("bbi", [128, 2, 2, 128])
            csr = tl("csr", [128, 2, 2, 128]); csi = tl("csi", [128, 2, 2, 128])
            for t_ in (bsr, bsi, csr, csi):
                dve(lambda: nc.vector.memset(t_[:], 0.0), [], [t_])
            qn = 0
            for d_ in range(2):
                for g in range(16):
                    i = g // 2; g2 = g % 2; i4 = i // 4; im = i % 4
                    for nm, dst in (("s5_b_re", bsr), ("s5_b_im", bsi)):
                        q = ("sp", nc.sync) if qn % 2 == 0 else ("act", nc.scalar); qn += 1
                        fw.dma(q[0], lambda: q[1].dma_start(out=dst[64 * g2:64 * g2 + 64, d_, i4, 32 * im + 16 * g2:32 * im + 16 * g2 + 16], in_=I[nm][l, d_, g]),
                               reads=[I[nm]], writes=[dst])
                    for nm, dst in (("s5_c_re", csr), ("s5_c_im", csi)):
                        q = ("sp", nc.sync) if qn % 2 == 0 else ("act", nc.scalar); qn += 1
                        fw.dma(q[0], lambda: q[1].dma_start(out=dst[32 * im + 16 * g2:32 * im + 16 * g2 + 16, d_, i4, 64 * g2:64 * g2 + 64], in_=I[nm][l, d_, g]),
                               reads=[I[nm]], writes=[dst])
            for d_ in range(2):
                for i in range(8):
                    col = d_ * 8 + i; i4 = i // 4; im = i % 4
                    sl = slice(32 * im, 32 * im + 32)
                    dve(lambda: nc.vector.tensor_scalar(out=bbr[:, d_, i4, sl], in0=bsr[:, d_, i4, sl], scalar1=cr[:, col:col + 1], scalar2=None, op0=ALU.mult), [bsr, cr], [bbr])
                    dve(lambda: nc.vector.scalar_tensor_tensor(out=bbr[:, d_, i4, sl], in0=bsi[:, d_, i4, sl], scalar=nci[:, col:col + 1], in1=bbr[:, d_, i4, sl],
                                                               op0=ALU.mult, op1=ALU.add), [bsi, nci, bbr], [bbr])
                    dve(lambda: nc.vector.tensor_scalar(out=bbi[:, d_, i4, sl], in0=bsi[:, d_, i4, sl], scalar1=cr[:, col:col + 1], scalar2=None, op0=ALU.mult), [bsi, cr], [bbi])
                    dve(lambda: nc.vector.scalar_tensor_tensor(out=bbi[:, d_, i4, sl], in0=bsr[:, d_, i4, sl], scalar=ci[:, col:col + 1], in1=bbi[:, d_, i4, sl],
                                                               op0=ALU.mult, op1=ALU.add), [bsr, ci, bbi], [bbi])
            BbT = tl("BbT", [128, 2, 2, 2, 128], BF16)
            CT = tl("CT", [128, 2, 2, 2, 128], BF16)
            with ExitStack() as pes2:
                ptp = [fw.ps("ptp%d" % k_, [128, 128], F32, pes2) for k_ in range(2)]
                it = 0
                for d_ in range(2):
                    for i4 in range(2):
                        for ri, (bsrc, csrc) in enumerate(((bbr, csr), (bbi, csi))):
                            p = ptp[it % 2]; it += 1
                            fw.op("pe", lambda: nc.tensor.transpose(out=p[:, :], in_=bsrc[:, d_, i4, :], identity=ident_f[:]), reads=[bsrc, ident_f], writes=[p])
                            fw.op("act", lambda: nc.scalar.copy(out=BbT[:, d_, i4, ri, :], in_=p[:, :]), reads=[p], writes=[BbT])
                            p = ptp[it % 2]; it += 1
                            fw.op("pe", lambda: nc.tensor.transpose(out=p[:, :], in_=csrc[:, d_, i4, :], identity=ident_f[:]), reads=[csrc, ident_f], writes=[p])
                            fw.op("act", lambda: nc.scalar.mul(out=CT[:, d_, i4, ri, :], in_=p[:, :], mul=(1.0 if ri == 0 else -1.0)), reads=[p], writes=[CT])
                fw.barrier()
            tcos = tl("tcos", [128, 16, 512]); tsin = tl("tsin", [128, 16, 512])
            rful = tl("rful", [128, 16, 512])
            io = tl("io512", [128, 512])
            fw.dma("sp", lambda: nc.sync.dma_start(out=io[:], in_=I["iota512"][:]), reads=[I["iota512"]], writes=[io])
            angt = tl("angt", [128, 512])
            for col in range(16):
                dve(lambda: nc.vector.tensor_scalar(out=angt[:], in0=io[:], scalar1=li[:, col:col + 1], scalar2=None, op0=ALU.mult), [io, li], [angt])
                sincos(angt[:, :], 512, [angt], tsin[:, col, :], tcos[:, col, :], [tsin, tcos])
                fw.op("pool", lambda: nc.gpsimd.tensor_scalar(out=rful[:, col, :], in0=io[:], scalar1=0.0, scalar2=rr[:, col:col + 1], op0=ALU.mult, op1=ALU.add), reads=[io, rr], writes=[rful])
            dsk = tl("dsk", [128, 2]); bgl = tl("bgl", [128, 2])
            wgl = tl("wgl", [128, 2, 256], BF16)
            with nc.allow_non_contiguous_dma(reason="tiny"):
                fw.dma("sp", lambda: nc.sync.dma_start(out=dsk[:], in_=I["s5_d"][l].rearrange("(c p) -> p c", p=128)), reads=[I["s5_d"]], writes=[dsk])
                fw.dma("sp", lambda: nc.sync.dma_start(out=bgl[:], in_=I["s5_b_glu"][l].rearrange("(c p) -> p c", p=128)), reads=[I["s5_b_glu"]], writes=[bgl])
            fw.dma("pool", lambda: nc.gpsimd.dma_start(out=wgl[:], in_=I["s5_w_glu"][l].rearrange("(c p) n -> p c n", p=128)), reads=[I["s5_w_glu"]], writes=[wgl])
            zst = tl("zst", [128, 16, 2])
            zin = tl("zin", [128, 16, 2])
            dve(lambda: nc.vector.memset(zin[:], 0.0), [], [zin])
            uf = [tl("uf%d" % k_, [128, 2, 512]) for k_ in range(2)]
            ub = [tl("ub%d" % k_, [128, 2, 512], BF16) for k_ in range(2)]
            pA = [fw.ps("pA%d" % k_, [128, 512], F32, pes) for k_ in range(2)]
            pB = [fw.ps("pB%d" % k_, [128, 512], F32, pes) for k_ in range(2)]
            py = [fw.ps("py%d" % k_, [128, 512], F32, pes) for k_ in range(2)]
            pg = [fw.ps("pg%d" % k_, [128, 512], F32, pes) for k_ in range(2)]
            W = {}
            for nm in ("t1", "t2", "t3", "t4", "dr", "di", "zr", "zi"):
                W[nm] = [tl(nm + "_%d" % k_, [128, 512]) for k_ in range(2)]
            xr = [tl("xr%d" % k_, [128, 512], BF16) for k_ in range(2)]
            xi = [tl("xi%d" % k_, [128, 512], BF16) for k_ in range(2)]
            ysb = [tl("ysbC0", [128, 2, 512])] * 2
            yfl = tl("yfl", [128, 2, 512])
            gq = tl("gq", [128, 2, 512]); gp = tl("gp", [128, 2, 512]); gg = gq
            gb = tl("gb", [128, 2, 512], BF16); sg = gp; yo = tl("yo", [128, 2, 512], BF16)
            lat = [(g_ * 512, 512) for g_ in range(16)]
            order = {0: [(SEQ, CTXL)] + lat, 1: [(SEQ, CTXL)] + lat[::-1]}
            it = 0
            ci_ = 0
            for d_ in range(2):
                prevL = None
                for (t0, L) in order[d_]:
                    isctx = t0 >= SEQ
                    u_ = uf[ci_ % 2]; ub_ = ub[ci_ % 2]; ys_ = ysb[ci_ % 2]; ci_ += 1
                    fw.dma("sp", lambda: nc.sync.dma_start(out=u_[:, :, :L], in_=u5[:, :, t0:t0 + L].rearrange("c p t -> p c t")), reads=[u5], writes=[u_])
                    if d_ == 0:
                        fw.op("act", lambda: nc.scalar.copy(out=ub_[:, :, :L], in_=u_[:, :, :L]), reads=[u_], writes=[ub_])
                    else:
                        fw.op("act", lambda: nc.scalar.copy(out=ub_[:, :, :L], in_=u_[:, :, L - 1::-1] if False else u_[:, :, :L][:, :, ::-1]), reads=[u_], writes=[ub_])
                        fw.dma("act", lambda: nc.scalar.dma_start(out=yfl[:, :, :L], in_=yf[:, :, t0:t0 + L].rearrange("c p t -> p c t")), reads=[yf], writes=[yfl])
                    for i in range(8):
                        col = d_ * 8 + i; i4 = i // 4; im = i % 4
                        k_ = it % 2; it += 1
                        A = pA[k_]; B = pB[k_]
                        t1, t2, t3, t4 = W["t1"][k_], W["t2"][k_], W["t3"][k_], W["t4"][k_]
                        dr, di, zr, zi = W["dr"][k_], W["di"][k_], W["zr"][k_], W["zi"][k_]
                        u1, u2, u3, u4 = t1, t2, t3, t4
                        rf = rful
                        xr_, xi_ = xr[k_], xi[k_]
                        ps_ = slice(32 * im, 32 * im + 32)
                        fw.op("pe", lambda: nc.tensor.matmul(A[:, :L], lhsT=BbT[ps_, d_, i4, 0, :], rhs=ub_[ps_, i4, :L], start=True, stop=True, tile_position=(32 * im, 0)),
                              reads=[BbT, ub_], writes=[A])
                        fw.op("pe", lambda: nc.tensor.matmul(B[:, :L], lhsT=BbT[ps_, d_, i4, 1, :], rhs=ub_[ps_, i4, :L], start=True, stop=True, tile_position=(32 * im, 0)),
                              reads=[BbT, ub_], writes=[B])
                        if prevL is not None:
                            dve(lambda: nc.vector.tensor_scalar(out=zin[:, col, 0:1], in0=zst[:, col, 1:2], scalar1=sL[prevL][:, col:col + 1], scalar2=None, op0=ALU.mult), [zst, sL[prevL]], [zin])
                            dve(lambda: nc.vector.scalar_tensor_tensor(out=zin[:, col, 0:1], in0=zst[:, col, 0:1], scalar=cL[prevL][:, col:col + 1], in1=zin[:, col, 0:1],
                                                                       op0=ALU.mult, op1=ALU.subtract), [zst, cL[prevL], zin], [zin])
                            dve(lambda: nc.vector.tensor_scalar(out=zin[:, col, 1:2], in0=zst[:, col, 1:2], scalar1=cL[prevL][:, col:col + 1], scalar2=None, op0=ALU.mult), [zst, cL[prevL]], [zin])
                            dve(lambda: nc.vector.scalar_tensor_tensor(out=zin[:, col, 1:2], in0=zst[:, col, 0:1], scalar=sL[prevL][:, col:col + 1], in1=zin[:, col, 1:2],
                                                                       op0=ALU.mult, op1=ALU.add), [zst, sL[prevL], zin], [zin])
                        cs = tcos[:, col, :L]; sn = tsin[:, col, :L]
                        dve(lambda: nc.vector.tensor_tensor(out=t1[:, :L], in0=A[:, :L], in1=cs, op=ALU.mult), [A, tcos], [t1])
                        dve(lambda: nc.vector.tensor_tensor(out=t2[:, :L], in0=B[:, :L], in1=sn, op=ALU.mult), [B, tsin], [t2])
                        fw.op("pool", lambda: nc.gpsimd.tensor_tensor(out=dr[:, :L], in0=t1[:, :L], in1=t2[:, :L], op=ALU.add), reads=[t1, t2], writes=[dr])
                        dve(lambda: nc.vector.tensor_tensor(out=t3[:, :L], in0=B[:, :L], in1=cs, op=ALU.mult), [B, tcos], [t3])
                        dve(lambda: nc.vector.tensor_tensor(out=t4[:, :L], in0=A[:, :L], in1=sn, op=ALU.mult), [A, tsin], [t4])
                        fw.op("pool", lambda: nc.gpsimd.tensor_tensor(out=di[:, :L], in0=t3[:, :L], in1=t4[:, :L], op=ALU.subtract), reads=[t3, t4], writes=[di])
                        dve(lambda: nc.vector.tensor_tensor_scan(out=zr[:, :L], data0=rful[:, col, :L], data1=dr[:, :L], initial=zin[:, col, 0:1], op0=ALU.mult, op1=ALU.add),
                            [rf, dr, zin], [zr])
                        dve(lambda: nc.vector.tensor_tensor_scan(out=zi[:, :L], data0=rful[:, col, :L], data1=di[:, :L], initial=zin[:, col, 1:2], op0=ALU.mult, op1=ALU.add),
                            [rf, di, zin], [zi])
                        dve(lambda: nc.vector.tensor_copy(out=zst[:, col, 0:1], in_=zr[:, L - 1:L]), [zr], [zst])
                        dve(lambda: nc.vector.tensor_copy(out=zst[:, col, 1:2], in_=zi[:, L - 1:L]), [zi], [zst])
                        dve(lambda: nc.vector.tensor_tensor(out=u1[:, :L], in0=zr[:, :L], in1=cs, op=ALU.mult), [zr, tcos], [u1])
                        fw.op("pool", lambda: nc.gpsimd.tensor_tensor(out=u2[:, :L], in0=zi[:, :L], in1=sn, op=ALU.mult), reads=[zi, tsin], writes=[u2])
                        fw.op("pool", lambda: nc.gpsimd.tensor_tensor(out=xr_[:, :L], in0=u1[:, :L], in1=u2[:, :L], op=ALU.subtract), reads=[u1, u2], writes=[xr_])
                        dve(lambda: nc.vector.tensor_tensor(out=u3[:, :L], in0=zr[:, :L], in1=sn, op=ALU.mult), [zr, tsin], [u3])
                        fw.op("pool", lambda: nc.gpsimd.tensor_tensor(out=u4[:, :L], in0=zi[:, :L], in1=cs, op=ALU.mult), reads=[zi, tcos], writes=[u4])
                        fw.op("pool", lambda: nc.gpsimd.tensor_tensor(out=xi_[:, :L], in0=u3[:, :L], in1=u4[:, :L], op=ALU.add), reads=[u3, u4], writes=[xi_])
                        if isctx and not with_ctx:
                            continue
                        yq = py[i4]
                        fw.op("pe", lambda: nc.tensor.matmul(yq[ps_, :L], lhsT=CT[:, d_, i4, 0, ps_], rhs=xr_[:, :L], start=True, stop=False, tile_position=(0, 32 * im)),
                              reads=[CT, xr_], writes=[yq])
                        fw.op("pe", lambda: nc.tensor.matmul(yq[ps_, :L], lhsT=CT[:, d_, i4, 1, ps_], rhs=xi_[:, :L], start=False, stop=True, tile_position=(0, 32 * im)),
                              reads=[CT, xi_], writes=[yq])
                    prevL = L
                    if isctx and not with_ctx:
                        continue
                    if d_ == 0:
                        for ct in range(2):
                            dve(lambda: nc.vector.scalar_tensor_tensor(out=ys_[:, ct, :L], in0=u_[:, ct, :L], scalar=dsk[:, ct:ct + 1], in1=py[ct][:, :L], op0=ALU.mult, op1=ALU.add),
                                [u_, dsk, py[ct]], [ys_])
                        fw.dma("sp", lambda: nc.sync.dma_start(out=yf[:, :, t0:t0 + L].rearrange("c p t -> p c t"), in_=ys_[:, :, :L]), reads=[ys_], writes=[yf])
                    else:
                        for ct in range(2):
                            dve(lambda: nc.vector.tensor_tensor(out=ys_[:, ct, :L], in0=py[ct][:, :L][:, ::-1], in1=yfl[:, ct, :L], op=ALU.add), [py[ct], yfl], [ys_])
                        fw.op("act", lambda: nc.scalar.activation(out=gq[:, :, :L], in_=ys_[:, :, :L], func=AF.Square), reads=[ys_], writes=[gq])
                        dve(lambda: nc.vector.tensor_scalar(out=gq[:, :, :L], in0=gq[:, :, :L], scalar1=0.044715, scalar2=1.0, op0=ALU.mult, op1=ALU.add), [gq], [gq])
                        fw.op("pool", lambda: nc.gpsimd.tensor_tensor(out=gp[:, :, :L], in0=gq[:, :, :L], in1=ys_[:, :, :L], op=ALU.mult), reads=[gq, ys_], writes=[gp])
                        fw.op("act", lambda: nc.scalar.activation(out=gp[:, :, :L], in_=gp[:, :, :L], func=AF.Sigmoid, scale=1.5957691216057308), reads=[gp], writes=[gp])
                        fw.op("pool", lambda: nc.gpsimd.tensor_tensor(out=gg[:, :, :L], in0=gp[:, :, :L], in1=ys_[:, :, :L], op=ALU.mult), reads=[gp, ys_], writes=[gg])
                        fw.op("act", lambda: nc.scalar.copy(out=gb[:, :, :L], in_=gg[:, :, :L]), reads=[gg], writes=[gb])
                        for co in range(2):
                            for cin in range(2):
                                fw.op("pe", lambda: nc.tensor.matmul(pg[co][:, :L], lhsT=wgl[:, cin, co * 128:(co + 1) * 128], rhs=gb[:, cin, :L], start=(cin == 0), stop=(cin == 1)),
                                      reads=[wgl, gb], writes=[pg[co]], chain=True)
                            fw.op("act", lambda: nc.scalar.activation(out=sg[:, co, :L], in_=pg[co][:, :L], func=AF.Sigmoid, bias=bgl[:, co:co + 1], scale=1.0), reads=[pg[co], bgl], writes=[sg])
                        dve(lambda: nc.vector.tensor_tensor(out=yo[:, :, :L], in0=gg[:, :, :L], in1=sg[:, :, :L], op=ALU.mult), [gg, sg], [yo])
                        fw.dma("sp", lambda: nc.sync.dma_start(out=yT[3:5, :, t0:t0 + L].rearrange("c p t -> p c t"), in_=yo[:, :, :L]), reads=[yo], writes=[yT])


    Xs = fw.dram("Xs", [NSLOT + 128, D], BF16)
    Ys = fw.dram("Ys", [NSLOT, D], F32)
    h2tok = fw.dram("h2tok", [NTOK, D], BF16)
    NTILE = NTOK // 128
    dest_i = fw.sb("dest_i", [128, NTILE, 4], U32)
    wk = fw.sb("wk", [128, NTILE, 4], F32)
    idxw = fw.sb("idxw", [128, NB, 8], U32)
    idxbg = fw.sb("idxbg", [128, NB], U32)
    idxbd = fw.sb("idxbd", [128, NB], U32)
    iop = fw.sb("iop", [128, 1], F32)
    fw.dma("sp", lambda: nc.sync.dma_start(out=iop[:], in_=I["iota_p"][:]), reads=[I["iota_p"]], writes=[iop])

    def phaseE(l, with_ctx):
        with ExitStack() as pes:
            def tl(name, shape, dt=F32):
                return fw.sb(name, shape, dt, pes)
            wout = tl("wout", [128, 8, D], BF16)
            fw.dma("pool", lambda: nc.gpsimd.dma_start(out=wout[:], in_=I["w_out"][l].rearrange("(c p) n -> p c n", p=128)), reads=[I["w_out"]], writes=[wout])
            wr = tl("wr", [128, 8, NE])
            fw.dma("sp", lambda: nc.sync.dma_start(out=wr[:], in_=I["w_router"][l].rearrange("(c p) e -> p c e", p=128)), reads=[I["w_router"]], writes=[wr])
            brow = tl("brow", [128, NE])
            fw.dma("sp", lambda: nc.sync.dma_start(out=brow[:], in_=I["b_router"][l].partition_broadcast(128)), reads=[I["b_router"]], writes=[brow])
            io32 = tl("io32", [128, NE]); ust = tl("ust", [128, 128]); iob = tl("iob", [128, NB])
            fw.dma("sp", lambda: nc.sync.dma_start(out=io32[:], in_=I["iota32"][:]), reads=[I["iota32"]], writes=[io32])
            fw.dma("sp", lambda: nc.sync.dma_start(out=ust[:], in_=I["ustrict"][:]), reads=[I["ustrict"]], writes=[ust])
            fw.dma("sp", lambda: nc.sync.dma_start(out=iob[:], in_=I["iotablk"][:]), reads=[I["iotablk"]], writes=[iob])
            runm = tl("runm", [128, NE])
            fw.op("dve", lambda: nc.vector.memset(runm[:], 0.0), writes=[runm])
            posall = tl("posall", [128, NTILE, NE]); idxall = tl("idxall", [128, NTILE, 4])
            xg = [tl("xgE%d" % i, [128, 8, 512]) for i in range(2)]
            yg = [tl("ygE%d" % i, [128, 8, 512], BF16) for i in range(2)]
            h2f = tl("h2f", [128, 8, 512]); h2b = tl("h2b", [128, 8, 512], BF16)
            sq = tl("sqE", [128, 8, 512], BF16); rstd = tl("rstdE", [128, 512]); GS = tl("GSE", [128, 1, 8])
            pss = fw.ps("pssE", [128, 512], F32, pes)
            pp = [fw.ps("ppE%d" % i, [128, 512], F32, pes) for i in range(2)]
            plg = fw.ps("plg", [128, NE], F32, pes)
            ppos = fw.ps("ppos", [128, NE], F32, pes)
            ptr = [fw.ps("ptrE%d" % i, [128, 8, 128], BF16, pes) for i in range(2)]
            htok = [tl("htok%d" % i, [128, D], BF16) for i in range(2)]
            lg = tl("lg", [128, NE]); m8 = tl("m8", [128, 8]); idx8 = tl("idx8", [128, 8], U32); mask = tl("mask", [128, NE])
            negmx = tl("negmx", [128, 1]); e4 = tl("e4", [128, 4]); ssum = tl("ssum", [128, 1])
            oh = tl("oh", [128, NE]); junk = tl("junk", [128, NE]); posk = tl("posk", [128, 4]); posf = tl("posf", [128, NE])
            gl = groups() if with_ctx else groups()[:-1]
            tiles_done = []
            for gi, (t0, n, isctx) in enumerate(gl):
                j = 1 if isctx else 0
                x_ = xg[gi % 2]; y_ = yg[gi % 2]
                fw.dma("sp", lambda: nc.sync.dma_start(out=x_[:, :, :n], in_=xs[:, :, t0:t0 + n]), reads=[xs], writes=[x_])
                fw.dma("act", lambda: nc.scalar.dma_start(out=y_[:, :, :n], in_=yT[:, :, t0:t0 + n].rearrange("c p t -> p c t")), reads=[yT], writes=[y_])
                for dc in range(8):
                    p = pp[dc % 2]
                    for ct in range(8):
                        fw.op("pe", lambda: nc.tensor.matmul(p[:, :n], lhsT=wout[:, ct, dc * 128:(dc + 1) * 128], rhs=y_[:, ct, :n], start=(ct == 0), stop=(ct == 7)),
                              reads=[wout, y_], writes=[p], chain=True)
                    fw.op("dve", lambda: nc.vector.scalar_tensor_tensor(out=x_[:, dc, :n], in0=p[:, :n], scalar=modT[:, l, 16 + dc, j:j + 1], in1=x_[:, dc, :n],
                                                                      op0=ALU.mult, op1=ALU.add), reads=[p, modT, x_], writes=[x_])
                fw.dma("sp", lambda: nc.sync.dma_start(out=xs[:, :, t0:t0 + n], in_=x_[:, :, :n]), reads=[x_], writes=[xs])
                norm_mod((sq, pss, rstd, GS), x_, n, gffn[:, l, :], l, 3, 4, j, out_bf=h2b, out_f=h2f)
                for tt in range(n // 128):
                    ti = (t0 // 128) + tt
                    tiles_done.append(ti)
                    tsl = slice(tt * 128, (tt + 1) * 128)
                    for kc in range(8):
                        fw.op("pe", lambda: nc.tensor.matmul(plg[:, :], lhsT=h2f[:, kc, tsl], rhs=wr[:, kc, :], start=(kc == 0), stop=(kc == 7)),
                              reads=[h2f, wr], writes=[plg], chain=True)
                    fw.op("dve", lambda: nc.vector.tensor_tensor(out=lg[:], in0=plg[:], in1=brow[:], op=ALU.add), reads=[plg, brow], writes=[lg])
                    fw.op("dve", lambda: nc.vector.max(out=m8[:], in_=lg[:]), reads=[lg], writes=[m8])
                    fw.op("dve", lambda: nc.vector.max_index(out=idx8[:], in_max=m8[:], in_values=lg[:]), reads=[lg, m8], writes=[idx8])
                    fw.op("dve", lambda: nc.vector.tensor_scalar(out=mask[:], in0=lg[:], scalar1=m8[:, 3:4], scalar2=None, op0=ALU.is_ge), reads=[lg, m8], writes=[mask])
                    fw.op("dve", lambda: nc.vector.tensor_scalar(out=negmx[:], in0=m8[:, 0:1], scalar1=-1.0, scalar2=None, op0=ALU.mult), reads=[m8], writes=[negmx])
                    fw.op("act", lambda: nc.scalar.activation(out=e4[:], in_=m8[:, 0:4], func=AF.Exp, bias=negmx[:, 0:1], scale=1.0, accum_out=ssum[:, 0:1]),
                          reads=[m8, negmx], writes=[e4, ssum])
                    fw.op("dve", lambda: nc.vector.reciprocal(out=ssum[:], in_=ssum[:]), reads=[ssum], writes=[ssum])
                    fw.op("dve", lambda: nc.vector.tensor_scalar(out=wk[:, ti, :], in0=e4[:], scalar1=ssum[:, 0:1], scalar2=None, op0=ALU.mult), reads=[e4, ssum], writes=[wk])
                    fw.op("pe", lambda: nc.tensor.matmul(ppos[:, :], lhsT=ust[:, :], rhs=mask[:, :], start=True, stop=False), reads=[ust, mask], writes=[ppos])
                    fw.op("pe", lambda: nc.tensor.matmul(ppos[:, :], lhsT=ones_f[:, :], rhs=runm[:, :], start=False, stop=True), reads=[ones_f, runm], writes=[ppos], chain=True)
                    fw.op("act", lambda: nc.scalar.copy(out=posall[:, ti, :], in_=ppos[:]), reads=[ppos], writes=[posall])
                    fw.op("pool", lambda: nc.gpsimd.tensor_tensor(out=runm[:], in0=runm[:], in1=mask[:], op=ALU.add), reads=[runm, mask], writes=[runm])
                    fw.op("dve", lambda: nc.vector.tensor_copy(out=idxall[:, ti, :], in_=idx8[:, 0:4]), reads=[idx8], writes=[idxall])
                    pt_ = ptr[ti % 2]; ht = htok[ti % 2]
                    for kc in range(8):
                        fw.op("pe", lambda: nc.tensor.transpose(out=pt_[:, kc, :], in_=h2b[:, kc, tsl], identity=ident_b[:]), reads=[h2b, ident_b], writes=[pt_], chain=True)
                    fw.op("act", lambda: nc.scalar.copy(out=ht[:], in_=pt_[:].rearrange("p c d -> p (c d)")), reads=[pt_], writes=[ht])
                    fw.dma("act", lambda: nc.scalar.dma_start(out=h2tok[ti * 128:(ti + 1) * 128, :], in_=ht[:]), reads=[ht], writes=[h2tok])
            cnt = tl("cnt", [128, NE]); pad = tl("pad", [128, NE]); ends = tl("ends", [128, NE]); pst = tl("pst", [128, NE])
            qi = tl("qi", [128, NE], I32); qf = tl("qf", [128, NE]); gt = tl("gt", [128, NE]); onesr = tl("onesr", [128, NE])
            acc = tl("accb", [128, NB])
            fw.op("pe", lambda: nc.tensor.matmul(ppos[:, :], lhsT=ones_f[:, :], rhs=runm[:, :], start=True, stop=True), reads=[ones_f, runm], writes=[ppos])
            fw.op("dve", lambda: nc.vector.tensor_scalar(out=cnt[:], in0=ppos[:], scalar1=float(BLK - 1), scalar2=1.0 / BLK, op0=ALU.add, op1=ALU.mult), reads=[ppos], writes=[cnt])
            fw.op("dve", lambda: nc.vector.tensor_copy(out=qi[:], in_=cnt[:]), reads=[cnt], writes=[qi])
            fw.op("dve", lambda: nc.vector.tensor_copy(out=qf[:], in_=qi[:]), reads=[qi], writes=[qf])
            fw.op("dve", lambda: nc.vector.tensor_tensor(out=gt[:], in0=qf[:], in1=cnt[:], op=ALU.is_gt), reads=[qf, cnt], writes=[gt])
            fw.op("dve", lambda: nc.vector.tensor_tensor(out=qf[:], in0=qf[:], in1=gt[:], op=ALU.subtract), reads=[qf, gt], writes=[qf])
            fw.op("dve", lambda: nc.vector.tensor_scalar(out=pad[:], in0=qf[:], scalar1=float(BLK), scalar2=None, op0=ALU.mult), reads=[qf], writes=[pad])
            fw.op("dve", lambda: nc.vector.memset(onesr[:], 1.0), writes=[onesr])
            fw.op("dve", lambda: nc.vector.tensor_tensor_scan(out=ends[:], data0=onesr[:], data1=pad[:], initial=0.0, op0=ALU.mult, op1=ALU.add), reads=[onesr, pad], writes=[ends])
            fw.op("dve", lambda: nc.vector.tensor_tensor(out=pst[:], in0=ends[:], in1=pad[:], op=ALU.subtract), reads=[ends, pad], writes=[pst])
            fw.op("dve", lambda: nc.vector.memset(acc[:], 0.0), writes=[acc])
            for e in range(NE):
                fw.op("dve", lambda: nc.vector.scalar_tensor_tensor(out=acc[:], in0=iob[:], scalar=ends[:, e:e + 1], in1=acc[:], op0=ALU.is_ge, op1=ALU.add),
                      reads=[iob, ends, acc], writes=[acc])
            fw.op("dve", lambda: nc.vector.tensor_scalar(out=acc[:], in0=acc[:], scalar1=float(NE - 1), scalar2=None, op0=ALU.min), reads=[acc], writes=[acc])
            tix = tl("tix", [128, NB])
            for kc in range(8):
                fw.op("dve", lambda: nc.vector.tensor_scalar(out=tix[:], in0=acc[:], scalar1=1024.0, scalar2=float(l * NE * 1024 + kc * 128), op0=ALU.mult, op1=ALU.add), reads=[acc], writes=[tix])
                fw.op("dve", lambda: nc.vector.tensor_scalar(out=tix[:], in0=tix[:], scalar1=iop[:, 0:1], scalar2=None, op0=ALU.add), reads=[tix, iop], writes=[tix])
                fw.op("dve", lambda: nc.vector.tensor_copy(out=idxw[:, :, kc], in_=tix[:]), reads=[tix], writes=[idxw])
            fw.op("dve", lambda: nc.vector.tensor_scalar(out=tix[:], in0=acc[:], scalar1=16.0, scalar2=float(l * NE * 16), op0=ALU.mult, op1=ALU.add), reads=[acc], writes=[tix])
            fw.op("dve", lambda: nc.vector.tensor_scalar(out=tix[:], in0=tix[:], scalar1=iop[:, 0:1], scalar2=None, op0=ALU.add), reads=[tix, iop], writes=[tix])
            fw.op("dve", lambda: nc.vector.tensor_copy(out=idxbg[:], in_=tix[:]), reads=[tix], writes=[idxbg])
            fw.op("dve", lambda: nc.vector.tensor_scalar(out=tix[:], in0=acc[:], scalar1=float(l * NE), scalar2=None, op0=ALU.add), reads=[acc], writes=[tix])
            fw.op("dve", lambda: nc.vector.tensor_copy(out=idxbd[:], in_=tix[:]), reads=[tix], writes=[idxbd])
            for ti in tiles_done:
                ht = htok[ti % 2]
                fw.dma("sp", lambda: nc.sync.dma_start(out=ht[:], in_=h2tok[ti * 128:(ti + 1) * 128, :]), reads=[h2tok], writes=[ht])
                fw.op("dve", lambda: nc.vector.tensor_tensor(out=posf[:], in0=posall[:, ti, :], in1=pst[:], op=ALU.add), reads=[posall, pst], writes=[posf])
                for k_ in range(4):
                    fw.op("dve", lambda: nc.vector.tensor_scalar(out=oh[:], in0=io32[:], scalar1=idxall[:, ti, k_:k_ + 1], scalar2=None, op0=ALU.is_equal), reads=[io32, idxall], writes=[oh])
                    fw.op("dve", lambda: nc.vector.scalar_tensor_tensor(out=junk[:], in0=oh[:], scalar=1.0, in1=posf[:], op0=ALU.mult, op1=ALU.mult,
                                                                      accum_out=posk[:, k_:k_ + 1]), reads=[oh, posf], writes=[junk, posk])
                fw.op("dve", lambda: nc.vector.tensor_copy(out=dest_i[:, ti, :], in_=posk[:]), reads=[posk], writes=[dest_i])
                for k_ in range(4):
                    fw.dma("pool", lambda: nc.gpsimd.indirect_dma_start(out=Xs[:, :], out_offset=bass.IndirectOffsetOnAxis(ap=dest_i[:, ti, k_:k_ + 1], axis=0),
                                                                      in_=ht[:, :], in_offset=None), reads=[ht, dest_i], writes=[Xs])

    def phaseF(l):
        with ExitStack() as pes:
            def tl(name, shape, dt=F32):
                return fw.sb(name, shape, dt, pes)
            NST = BLK // 128
            chunks = [(c0, min(512, BLK - c0)) for c0 in range(0, BLK, 512)]
            wgu = [tl("wgu%d" % i, [128, 8, 2 * D], BF16) for i in range(2)]
            wdn = [tl("wdn%d" % i, [128, 8, D], BF16) for i in range(2)]
            bdr = [tl("bdr%d" % i, [128, D]) for i in range(2)]
            bgc = [tl("bgc%d" % i, [128, 16]) for i in range(2)]
            xtok = [tl("xtok%d" % i, [128, NST, D], BF16) for i in range(2)]
            XT = tl("XT", [128, 8, BLK], BF16)
            actT = tl("actT", [128, 8, BLK], BF16)
            gs = [tl("gs%d" % i, [128, 512]) for i in range(2)]
            sgm = [tl("sgm%d" % i, [128, 512]) for i in range(2)]
            uu = [tl("uu%d" % i, [128, 512]) for i in range(2)]
            aa = [tl("aa%d" % i, [128, 512]) for i in range(2)]
            yo = [tl("yoF%d" % i, [128, D]) for i in range(2)]
            ptrs = [fw.ps("ptrF%d" % i, [128, 8, 128], BF16, pes) for i in range(2)]
            pg = [fw.ps("pgF%d" % i, [128, 512], F32, pes) for i in range(2)]
            pu = [fw.ps("puF%d" % i, [128, 512], F32, pes) for i in range(2)]
            pd = [fw.ps("pdF%d" % i, [128, 512], F32, pes) for i in range(2)]

            wgu_flat = I["w_gate_up"][:].rearrange("l e k n -> (l e k) n")
            wdn_flat = I["w_down"][:].rearrange("l e k n -> (l e k) n")
            bgu_flat = I["b_gate_up"][:].rearrange("l e (c p) -> (l e c) p", p=128)
            bdn_flat = I["b_down"][:].rearrange("l e d -> (l e) d")
            bgrow = [tl("bgrow%d" % i, [16, 128]) for i in range(2)]

            def load_w(b):
                wg_ = wgu[b % 2]; wd_ = wdn[b % 2]; bd_ = bdr[b % 2]; br_ = bgrow[b % 2]
                fw.dma("pool", lambda: nc.gpsimd.indirect_dma_start(out=br_[:, :], out_offset=None, in_=bgu_flat,
                                                                  in_offset=bass.IndirectOffsetOnAxis(ap=idxbg[0:16, b:b + 1], axis=0)),
                       reads=[I["b_gate_up"], idxbg], writes=[br_])
                fw.dma("pool", lambda: nc.gpsimd.indirect_dma_start(out=bd_[:, :], out_offset=None, in_=bdn_flat,
                                                                  in_offset=bass.IndirectOffsetOnAxis(ap=idxbd[:, b:b + 1], axis=0)),
                       reads=[I["b_down"], idxbd], writes=[bd_])
                for kc in range(8):
                    fw.dma("pool", lambda: nc.gpsimd.indirect_dma_start(out=wg_[:, kc, :], out_offset=None, in_=wgu_flat,
                                                                      in_offset=bass.IndirectOffsetOnAxis(ap=idxw[:, b, kc:kc + 1], axis=0)),
                           reads=[I["w_gate_up"], idxw], writes=[wg_])
                for fc in range(8):
                    fw.dma("pool", lambda: nc.gpsimd.indirect_dma_start(out=wd_[:, fc, :], out_offset=None, in_=wdn_flat,
                                                                      in_offset=bass.IndirectOffsetOnAxis(ap=idxw[:, b, fc:fc + 1], axis=0)),
                           reads=[I["w_down"], idxw], writes=[wd_])
            load_w(0)
            fw.dma("sp", lambda: nc.sync.dma_start(out=xtok[0][:], in_=Xs[0:BLK, :].rearrange("(t p) d -> p t d", p=128)), reads=[Xs], writes=[xtok[0]])
            kk = 0
            for b in range(NB):
                if b + 1 < NB:
                    load_w(b + 1)
                wg_ = wgu[b % 2]; wd_ = wdn[b % 2]; bd_ = bdr[b % 2]; bg_ = bgc[b % 2]; br_ = bgrow[b % 2]
                xt_ = xtok[b % 2]
                if b + 1 < NB:
                    xn_ = xtok[(b + 1) % 2]
                    fw.dma("sp", lambda: nc.sync.dma_start(out=xn_[:], in_=Xs[(b + 1) * BLK:(b + 2) * BLK, :].rearrange("(t p) d -> p t d", p=128)), reads=[Xs], writes=[xn_])
                fw.op("pe", lambda: nc.tensor.transpose(out=pd[1][:, 0:16], in_=br_[:, :], identity=ident_f[0:16, 0:16]), reads=[br_, ident_f], writes=[pd[1]])
                fw.op("act", lambda: nc.scalar.copy(out=bg_[:], in_=pd[1][:, 0:16]), reads=[pd[1]], writes=[bg_])
                for st in range(NST):
                    ptr = ptrs[st % 2]
                    for kc in range(8):
                        fw.op("pe", lambda: nc.tensor.transpose(out=ptr[:, kc, :], in_=xt_[:, st, kc * 128:(kc + 1) * 128], identity=ident_b[:]), reads=[xt_, ident_b], writes=[ptr], chain=True)
                    if st % 2 == 0:
                        fw.op("act", lambda: nc.scalar.copy(out=XT[:, :, st * 128:(st + 1) * 128], in_=ptr[:]), reads=[ptr], writes=[XT])
                    else:
                        fw.op("dve", lambda: nc.vector.tensor_copy(out=XT[:, :, st * 128:(st + 1) * 128], in_=ptr[:]), reads=[ptr], writes=[XT])
                for (c0, cn) in chunks:
                    for j in range(8):
                        k_ = kk % 2; kk += 1
                        g_, u_ = pg[k_], pu[k_]
                        for kc in range(8):
                            fw.op("pe", lambda: nc.tensor.matmul(g_[:, :cn], lhsT=wg_[:, kc, j * 128:(j + 1) * 128], rhs=XT[:, kc, c0:c0 + cn], start=(kc == 0), stop=(kc == 7)),
                                  reads=[wg_, XT], writes=[g_], chain=True)
                        for kc in range(8):
                            fw.op("pe", lambda: nc.tensor.matmul(u_[:, :cn], lhsT=wg_[:, kc, D + j * 128:D + (j + 1) * 128], rhs=XT[:, kc, c0:c0 + cn], start=(kc == 0), stop=(kc == 7)),
                                  reads=[wg_, XT], writes=[u_], chain=True)
                        gs_, sg_, uu_, aa_ = gs[k_], sgm[k_], uu[k_], aa[k_]
                        fw.op("dve", lambda: nc.vector.tensor_scalar(out=gs_[:, :cn], in0=g_[:, :cn], scalar1=bg_[:, j:j + 1], scalar2=7.0, op0=ALU.add, op1=ALU.min), reads=[g_, bg_], writes=[gs_])
                        fw.op("act", lambda: nc.scalar.activation(out=sg_[:, :cn], in_=gs_[:, :cn], func=AF.Sigmoid, scale=1.702), reads=[gs_], writes=[sg_])
                        fw.op("dve", lambda: nc.vector.tensor_scalar(out=uu_[:, :cn], in0=u_[:, :cn], scalar1=bg_[:, 8 + j:9 + j], scalar2=7.0, op0=ALU.add, op1=ALU.min), reads=[u_, bg_], writes=[uu_])
                        fw.op("dve", lambda: nc.vector.tensor_scalar(out=uu_[:, :cn], in0=uu_[:, :cn], scalar1=-7.0, scalar2=1.0, op0=ALU.max, op1=ALU.add), reads=[uu_], writes=[uu_])
                        fw.op("pool", lambda: nc.gpsimd.tensor_tensor(out=aa_[:, :cn], in0=gs_[:, :cn], in1=sg_[:, :cn], op=ALU.mult), reads=[gs_, sg_], writes=[aa_])
                        fw.op("pool", lambda: nc.gpsimd.tensor_tensor(out=actT[:, j, c0:c0 + cn], in0=aa_[:, :cn], in1=uu_[:, :cn], op=ALU.mult), reads=[aa_, uu_], writes=[actT])
                for st in range(NST):
                    yo_ = yo[st % 2]
                    for dh in range(2):
                        p = pd[dh]
                        for fc in range(8):
                            fw.op("pe", lambda: nc.tensor.matmul(p[:, :], lhsT=actT[:, fc, st * 128:(st + 1) * 128], rhs=wd_[:, fc, dh * 512:(dh + 1) * 512], start=(fc == 0), stop=(fc == 7)),
                                  reads=[actT, wd_], writes=[p], chain=True)
                        fw.op("dve", lambda: nc.vector.tensor_tensor(out=yo_[:, dh * 512:(dh + 1) * 512], in0=p[:, :], in1=bd_[:, dh * 512:(dh + 1) * 512], op=ALU.add), reads=[p, bd_], writes=[yo_])
                    r0 = b * BLK + st * 128
                    fw.dma("sp", lambda: nc.sync.dma_start(out=Ys[r0:r0 + 128, :], in_=yo_[:, :]), reads=[yo_], writes=[Ys])

    def phaseG(l, with_ctx, last):
        with ExitStack() as pes:
            def tl(name, shape, dt=F32):
                return fw.sb(name, shape, dt, pes)
            yk = [[tl("yk%d_%d" % (b_, k_), [128, D]) for k_ in range(4)] for b_ in range(2)]
            acc = [tl("acc%d" % i, [128, D]) for i in range(2)]
            xg = [tl("xgG%d" % i, [128, 8, 512]) for i in range(2)]
            pT = [fw.ps("pTG%d" % i, [128, 8, 128], F32, pes) for i in range(2)]
            if last:
                sq = tl("sqG", [128, 8, 512], BF16); rstd = tl("rstdG", [128, 512])
                pss = fw.ps("pssG", [128, 512], F32, pes)
                xo = tl("xoG", [128, 8, 512])
                osb = [tl("osbG%d" % i, [128, D]) for i in range(2)]
                pO = fw.ps("pOG", [128, 8, 128], F32, pes)
            gl = groups() if with_ctx else groups()[:-1]
            for gi, (t0, n, isctx) in enumerate(gl):
                j = 1 if isctx else 0
                x_ = xg[gi % 2]
                fw.dma("sp", lambda: nc.sync.dma_start(out=x_[:, :, :n], in_=xs[:, :, t0:t0 + n]), reads=[xs], writes=[x_])
                for tt in range(n // 128):
                    ti = (t0 // 128) + tt
                    tsl = slice(tt * 128, (tt + 1) * 128)
                    yk_ = yk[ti % 2]; a_ = acc[ti % 2]; p_ = pT[ti % 2]
                    for k_ in range(4):
                        fw.dma("pool", lambda: nc.gpsimd.indirect_dma_start(out=yk_[k_][:, :], out_offset=None, in_=Ys[:, :],
                                                                          in_offset=bass.IndirectOffsetOnAxis(ap=dest_i[:, ti, k_:k_ + 1], axis=0)),
                               reads=[Ys, dest_i], writes=[yk_[k_]])
                    fw.op("dve", lambda: nc.vector.tensor_scalar(out=a_[:], in0=yk_[0][:], scalar1=wk[:, ti, 0:1], scalar2=None, op0=ALU.mult), reads=[yk_[0], wk], writes=[a_])
                    for k_ in range(1, 4):
                        fw.op("dve", lambda: nc.vector.scalar_tensor_tensor(out=a_[:], in0=yk_[k_][:], scalar=wk[:, ti, k_:k_ + 1], in1=a_[:], op0=ALU.mult, op1=ALU.add),
                              reads=[yk_[k_], wk, a_], writes=[a_])
                    for c in range(8):
                        fw.op("pe", lambda: nc.tensor.transpose(out=p_[:, c, :], in_=a_[:, c * 128:(c + 1) * 128], identity=ident_f[:]), reads=[a_, ident_f], writes=[p_], chain=True)
                    for c in range(8):
                        fw.op("dve", lambda: nc.vector.scalar_tensor_tensor(out=x_[:, c, tsl], in0=p_[:, c, :], scalar=modT[:, l, 40 + c, j:j + 1], in1=x_[:, c, tsl],
                                                                          op0=ALU.mult, op1=ALU.add), reads=[p_, modT, x_], writes=[x_])
                if not last:
                    fw.dma("sp", lambda: nc.sync.dma_start(out=xs[:, :, t0:t0 + n], in_=x_[:, :, :n]), reads=[x_], writes=[xs])
                elif not isctx:
                    fw.op("act", lambda: nc.scalar.activation(out=sq[:, :, :n], in_=x_[:, :, :n], func=AF.Square), reads=[x_], writes=[sq])
                    for c in range(8):
                        fw.op("pe", lambda: nc.tensor.matmul(pss[:, :n], lhsT=ones_b[:], rhs=sq[:, c, :n], start=(c == 0), stop=(c == 7)), reads=[sq, ones_b], writes=[pss], chain=True)
                    fw.op("act", lambda: nc.scalar.activation(out=rstd[:, :n], in_=pss[:, :n], func=AF.Sqrt, bias=EPS, scale=1.0 / D), reads=[pss], writes=[rstd])
                    fw.op("dve", lambda: nc.vector.reciprocal(out=rstd[:, :n], in_=rstd[:, :n]), reads=[rstd], writes=[rstd])
                    for c in range(8):
                        fw.op("dve", lambda: nc.vector.scalar_tensor_tensor(out=xo[:, c, :n], in0=x_[:, c, :n], scalar=gfin[:, c:c + 1], in1=rstd[:, :n], op0=ALU.mult, op1=ALU.mult),
                              reads=[x_, gfin, rstd], writes=[xo])
                    for tt in range(n // 128):
                        o_ = osb[tt % 2]
                        for c in range(8):
                            fw.op("pe", lambda: nc.tensor.transpose(out=pO[:, c, :], in_=xo[:, c, tt * 128:(tt + 1) * 128], identity=ident_f[:]), reads=[xo, ident_f], writes=[pO], chain=True)
                        fw.op("act", lambda: nc.scalar.copy(out=o_[:], in_=pO[:].rearrange("p c d -> p (c d)")), reads=[pO], writes=[o_])
                        r0 = t0 + tt * 128
                        fw.dma("sp", lambda: nc.sync.dma_start(out=OUT[r0:r0 + 128, :], in_=o_[:]), reads=[o_], writes=[OUT])

    if "nopre" not in debug:
        prephase()
        fw.barrier()
    phase0()
    fw.barrier()
    for l in range(nlayers):
        with_ctx = l < DEPTH - 1
        phaseA(l)
        fw.barrier()
        if "A" in debug:
            break
        if "noB" not in debug:
            phaseB(l, with_ctx)
            fw.barrier()
        if "noD" not in debug:
            phaseD(l, with_ctx)
            fw.barrier()
        if "noC" not in debug:
            phaseC(l, with_ctx)
            fw.barrier()
        if "BD" in debug:
            break
        phaseE(l, with_ctx)
        fw.barrier()
        if "E" in debug:
            break
        phaseF(l)
        fw.barrier()
        phaseG(l, with_ctx, l == nlayers - 1 and "G" not in debug)
        fw.barrier()

    outs_to_wait = []
    if debug:
        def dump(name, src, shape, dt):
            o = dout("dbg_" + name, shape, dt)
            fw.dma("sp", lambda: nc.sync.dma_start(out=o[:], in_=src[:]), reads=[src], writes=[o])
            outs_to_wait.append(o)
        dump("yT", yT, [8, 128, NTOK], BF16)
        if "moe" in debug:
            dump("Xs", Xs, [NSLOT + 128, D], BF16)
            dump("Ys", Ys, [NSLOT, D], F32)
            od = dout("dbg_dest", [128, NTILE * 4], U32)
            fw.dma("sp", lambda: nc.sync.dma_start(out=od[:], in_=dest_i[:].rearrange("p t k -> p (t k)")), reads=[dest_i], writes=[od])
            outs_to_wait.append(od)
            ow = dout("dbg_wk", [128, NTILE * 4], F32)
            fw.dma("sp", lambda: nc.sync.dma_start(out=ow[:], in_=wk[:].rearrange("p t k -> p (t k)")), reads=[wk], writes=[ow])
            outs_to_wait.append(ow)
        dump("xs", xs, [128, 8, NTOK], F32)
        dump("qa", qa, [3, 128, NTOK], BF16)
        dump("ka", ka, [3, 128, NTOK], BF16)
        dump("va", va, [NTOK, 390], BF16)
        dump("u5", u5, [2, 128, NTOK], F32)
        dump("qs", qs, [3, 128, NTOK], BF16)
        dump("ks", ks, [3, 128, NTOK], BF16)
        dump("vs", vs, [NTOK, 130], BF16)
        om = dout("dbg_modT", [128, DEPTH * 48 * 2], F32)
        fw.dma("sp", lambda: nc.sync.dma_start(out=om[:], in_=modT[:].rearrange("p l c j -> p (l c j)")), reads=[modT], writes=[om])
        outs_to_wait.append(om)
    fw.finish(outs_to_wait + [OUT])
    print("insts", fw.n_inst, "waits", fw.n_wait)
    es.close()
    return nc, hc


def make_inputs(inputs, b, hc):
    m = {}
    m["xin"] = np.ascontiguousarray(np.concatenate([inputs["x"][b], inputs["ctx"][b]], axis=0))
    m["cvec"] = np.ascontiguousarray(np.stack([inputs["c"][b], inputs["c_ctx"]], axis=0))
    for k in ("w_mod", "b_mod", "g_mix", "w_in", "w_out", "na_rpb", "s5_a_re", "s5_a_im", "s5_log_step", "s5_b_re", "s5_b_im",
              "s5_c_re", "s5_c_im", "s5_d", "s5_w_glu", "s5_b_glu", "sw_sinks", "g_ffn", "w_router", "b_router", "w_gate_up",
              "b_gate_up", "w_down", "b_down", "g_final"):
        m[k] = inputs[k]
    for k, v in hc.items():
        m[k] = v
    return m


def kernel(**inputs):
    nc, hc = build()
    in_maps = [make_inputs(inputs, b % 4, hc) for b in range(8)]
    res = run_bass_kernel_spmd(nc, in_maps, core_ids=list(range(8)))
    return np.stack([res.results[b]["out"] for b in range(4)], axis=0)
```

```python
import numpy as np
import ml_dtypes
from contextlib import ExitStack
import concourse.bass as bass
import concourse.mybir as mybir
from concourse.bass_utils import run_bass_kernel_spmd

F32 = mybir.dt.float32
BF16 = mybir.dt.bfloat16
I32 = mybir.dt.int32
U32 = mybir.dt.uint32
AF = mybir.ActivationFunctionType
ALU = mybir.AluOpType
AX = mybir.AxisListType

D = 1024
SEQ = 8192
CTXL = 256
NTOK = SEQ + CTXL
DEPTH = 4
NE = 32
BLK = 896
NB = -(-(NTOK * 4 + NE * (BLK - 1)) // BLK)
NSLOT = NB * BLK
EPS = 1e-6
NEG = -8.0e30


class Buf:
    __slots__ = ("w", "r")

    def __init__(self):
        self.w = {}
        self.r = {}


class T:
    def __init__(self, t, name):
        self.t = t
        self.name = name
        self.b = Buf()

    def __getitem__(self, idx):
        return self.t[idx]


class FW:
    NDMA = 32

    def __init__(self, nc, es):
        self.nc = nc
        self.es = es
        self.engs = {"pe": nc.tensor, "act": nc.scalar, "dve": nc.vector, "pool": nc.gpsimd, "sp": nc.sync}
        self.sem = {}
        self.cnt = {}
        self.sems = {}
        for k in self.engs:
            s = es.enter_context(nc.semaphore("s_" + k))
            self.sem[k] = s
            self.sems["e_" + k] = s
            self.cnt[k] = 0
        self.dma_sems = []
        self.ring = {}
        base = 0
        for q, n in (("sp", 32), ("act", 16), ("pool", 44)):
            self.ring[q] = [base, n, 0]
            base += n
        for i in range(base):
            s = es.enter_context(nc.semaphore("d%d" % i))
            self.sems["d%d" % i] = s
            self.dma_sems.append(["d%d" % i, 0])
        self.waited = {k: {} for k in self.engs}
        self.n_inst = 0
        self.n_wait = 0
        self.uid = 0

    def sb(self, name, shape, dt, es=None):
        self.uid += 1
        t = (es or self.es).enter_context(self.nc.sbuf_tensor("%s_%d" % (name, self.uid), list(shape), dt))
        return T(t, name)

    def ps(self, name, shape, dt, es=None):
        self.uid += 1
        t = (es or self.es).enter_context(self.nc.psum_tensor("%s_%d" % (name, self.uid), list(shape), dt))
        return T(t, name)

    def dram(self, name, shape, dt, kind="Internal"):
        t = self.nc.dram_tensor(name, list(shape), dt, kind=kind)
        return T(t.ap(), name)

    def _deps(self, eng, reads, writes, skip_own=False):
        deps = {}

        def add(ev):
            s, v = ev
            if deps.get(s, 0) < v:
                deps[s] = v
        for b in reads:
            for ev in b.b.w.items():
                add(ev)
        for b in writes:
            for ev in b.b.w.items():
                add(ev)
            for ev in b.b.r.items():
                add(ev)
        own = "e_" + eng
        for s, v in deps.items():
            if skip_own and s == own:
                continue
            if self.waited[eng].get(s, 0) >= v:
                continue
            self.engs[eng].wait_ge(self.sems[s], v)
            self.waited[eng][s] = v
            self.n_wait += 1

    def _commit(self, ev, reads, writes):
        s, v = ev
        for b in reads:
            if b.b.r.get(s, 0) < v:
                b.b.r[s] = v
        for b in writes:
            if b.b.w.get(s, 0) < v:
                b.b.w[s] = v
            b.b.r = {}

    def op(self, eng, fn, reads=(), writes=(), chain=False):
        self._deps(eng, reads, writes, skip_own=chain)
        inst = fn()
        self.cnt[eng] += 1
        inst.then_inc(self.sem[eng], 1)
        self._commit(("e_" + eng, self.cnt[eng]), reads, writes)
        self.n_inst += 1
        return inst

    def dma(self, q, fn, reads=(), writes=()):
        rg = self.ring[q]
        slot = self.dma_sems[rg[0] + rg[2]]
        rg[2] = (rg[2] + 1) % rg[1]
        sid, used = slot
        if used > 0 and self.waited[q].get(sid, 0) < used * 16:
            self.engs[q].wait_ge(self.sems[sid], used * 16)
            self.waited[q][sid] = used * 16
        self._deps(q, reads, writes)
        inst = fn()
        slot[1] = used + 1
        inst.then_inc(self.sems[sid], 16)
        self._commit((sid, (used + 1) * 16), reads, writes)
        self.n_inst += 1
        return inst

    def barrier(self):
        evs = {}
        for k in self.engs:
            if self.cnt[k] > 0:
                evs["e_" + k] = self.cnt[k]
        for sid, used in self.dma_sems:
            if used > 0:
                evs[sid] = used * 16
        for k in self.engs:
            for s, v in evs.items():
                if s == "e_" + k and k in ("sp",):
                    continue
                if self.waited[k].get(s, 0) >= v:
                    continue
                self.engs[k].wait_ge(self.sems[s], v)
                self.waited[k][s] = v
                self.n_wait += 1

    def finish(self, outs, eng="sp"):
        for b in outs:
            for s, v in b.b.w.items():
                self.engs[eng].wait_ge(self.sems[s], v)


def host_consts():
    c = {}
    c["ident_f"] = np.eye(128, dtype=np.float32)
    c["ident_b"] = np.eye(128).astype(ml_dtypes.bfloat16)
    c["ones_b"] = np.ones((128, 128)).astype(ml_dtypes.bfloat16)
    c["ones_f"] = np.ones((128, 128), dtype=np.float32)
    t = np.arange(SEQ)
    row = (t // 64).astype(np.float32)
    col = (t % 64).astype(np.float32)
    inv = (10000.0 ** (-np.arange(16, dtype=np.float32) / 16)).astype(np.float32)
    ar = row[:, None] * inv
    ac = col[:, None] * inv
    ang = np.concatenate([ar, ar, ac, ac], axis=-1)
    cos = np.cos(ang).astype(np.float32).T
    sin = np.sin(ang).astype(np.float32).T
    sign = np.where((np.arange(64) % 32) < 16, -1.0, 1.0).astype(np.float32)[:, None]
    c["rope_c"] = np.ascontiguousarray(np.concatenate([cos, cos], 0))
    c["rope_s"] = np.ascontiguousarray(np.concatenate([sin * sign, sin * sign], 0))
    k = np.arange(128)[:, None]
    q = np.arange(128)[None, :]
    c["mask_l"] = np.where(k >= q, 0.0, NEG).astype(ml_dtypes.bfloat16)
    c["mask_u"] = np.where(k <= q, 0.0, NEG).astype(ml_dtypes.bfloat16)
    qc = np.arange(64)[None, :]
    kc = np.arange(64)[:, None]
    ws = np.clip(qc - 8, 0, 48)
    valid = (kc >= ws) & (kc < ws + 16)
    v2 = np.concatenate([valid, valid], 0)
    c["na_valid8"] = (v2 * 8.0).astype(np.float32)
    c["na_pen"] = np.where(v2, 0.0, NEG).astype(np.float32)
    c["iota32"] = np.broadcast_to(np.arange(32, dtype=np.float32), (128, 32)).copy()
    c["iota512"] = np.broadcast_to(np.arange(512, dtype=np.float32), (128, 512)).copy()
    c["iotablk"] = np.broadcast_to((np.arange(NB) * BLK).astype(np.float32), (128, NB)).copy()
    c["iota_p"] = np.arange(128, dtype=np.float32).reshape(128, 1)
    c["ustrict"] = (np.arange(128)[:, None] < np.arange(128)[None, :]).astype(np.float32)
    return c


CONST_SPECS = None


def groups():
    g = [(i * 512, 512, False) for i in range(SEQ // 512)]
    g.append((SEQ, CTXL, True))
    return g


def build(nlayers=DEPTH, debug=()):
    nc = bass.Bass("TRN2", target_bir_lowering=False)
    es = ExitStack()
    fw = FW(nc, es)

    def din(name, shape, dt=F32):
        return T(nc.dram_tensor(name, list(shape), dt, kind="ExternalInput").ap(), name)

    def dout(name, shape, dt=F32):
        return T(nc.dram_tensor(name, list(shape), dt, kind="ExternalOutput").ap(), name)

    I = {}
    I["xin"] = din("xin", [NTOK, D])
    I["cvec"] = din("cvec", [2, D])
    specs = {"w_mod": [DEPTH, D, 6 * D], "b_mod": [DEPTH, 6 * D], "g_mix": [DEPTH, D], "w_in": [DEPTH, D, 2048],
             "w_out": [DEPTH, D, D], "na_rpb": [DEPTH, 6, 15, 31], "s5_a_re": [DEPTH, 2, 16, 64], "s5_a_im": [DEPTH, 2, 16, 64],
             "s5_log_step": [DEPTH, 2, 16], "s5_b_re": [DEPTH, 2, 16, 64, 16], "s5_b_im": [DEPTH, 2, 16, 64, 16],
             "s5_c_re": [DEPTH, 2, 16, 16, 64], "s5_c_im": [DEPTH, 2, 16, 16, 64], "s5_d": [DEPTH, 256],
             "s5_w_glu": [DEPTH, 256, 256], "s5_b_glu": [DEPTH, 256], "sw_sinks": [DEPTH, 6], "g_ffn": [DEPTH, D],
             "w_router": [DEPTH, D, NE], "b_router": [DEPTH, NE], "w_gate_up": [DEPTH, NE, D, 2 * D],
             "b_gate_up": [DEPTH, NE, 2 * D], "w_down": [DEPTH, NE, D, D], "b_down": [DEPTH, NE, D], "g_final": [D]}
    for k, s in specs.items():
        I[k] = din(k, s)
    hc = host_consts()
    for k, v in hc.items():
        I[k] = din(k, list(v.shape), BF16 if v.dtype == ml_dtypes.bfloat16 else F32)
    OUT = dout("out", [SEQ, D])
    DBG = {}

    xs = fw.dram("xs", [128, 8, NTOK], F32)
    qa = fw.dram("qa", [3, 128, NTOK], BF16)
    ka = fw.dram("ka", [3, 128, NTOK], BF16)
    va = fw.dram("va", [NTOK, 6 * 65], BF16)
    u5 = fw.dram("u5", [2, 128, NTOK], F32)
    qs = fw.dram("qs", [3, 128, NTOK], BF16)
    ks = fw.dram("ks", [3, 128, NTOK], BF16)
    vs = fw.dram("vs", [NTOK, 2 * 65], BF16)
    yT = fw.dram("yT", [8, 128, NTOK], BF16)

    ident_f = fw.sb("ident_f", [128, 128], F32)
    ident_b = fw.sb("ident_b", [128, 128], BF16)
    ones_b = fw.sb("ones_b", [128, 128], BF16)
    ones_f = fw.sb("ones_f", [128, 128], F32)
    for tl, nm in ((ident_f, "ident_f"), (ident_b, "ident_b"), (ones_b, "ones_b"), (ones_f, "ones_f")):
        fw.dma("sp", lambda tl=tl, nm=nm: nc.sync.dma_start(out=tl[:], in_=I[nm][:]), reads=[I[nm]], writes=[tl])
    modT = fw.sb("modT", [128, DEPTH, 48, 2], F32)
    gmix = fw.sb("gmix", [128, DEPTH, 8], F32)
    gffn = fw.sb("gffn", [128, DEPTH, 8], F32)
    gfin = fw.sb("gfin", [128, 8], F32)
    with nc.allow_non_contiguous_dma(reason="tiny per-feature vectors"):
        fw.dma("sp", lambda: nc.sync.dma_start(out=gmix[:], in_=I["g_mix"][:].rearrange("l (c p) -> p l c", p=128)), reads=[I["g_mix"]], writes=[gmix])
        fw.dma("sp", lambda: nc.sync.dma_start(out=gffn[:], in_=I["g_ffn"][:].rearrange("l (c p) -> p l c", p=128)), reads=[I["g_ffn"]], writes=[gffn])
        fw.dma("sp", lambda: nc.sync.dma_start(out=gfin[:], in_=I["g_final"][:].rearrange("(c p) -> p c", p=128)), reads=[I["g_final"]], writes=[gfin])

    def phase0():
        with ExitStack() as pes:
            condT = fw.sb("condT", [128, 2, 8], F32, pes)
            bmT = fw.sb("bmT", [128, DEPTH, 48], F32, pes)
            with nc.allow_non_contiguous_dma(reason="tiny"):
                for jj in range(2):
                    fw.dma("sp", lambda: nc.sync.dma_start(out=condT[:, jj, :], in_=I["cvec"][jj, :].rearrange("(c p) -> p c", p=128)), reads=[I["cvec"]], writes=[condT])
                fw.dma("sp", lambda: nc.sync.dma_start(out=bmT[:], in_=I["b_mod"][:].rearrange("l (c p) -> p l c", p=128)), reads=[I["b_mod"]], writes=[bmT])
            fw.op("act", lambda: nc.scalar.activation(out=condT[:], in_=condT[:], func=AF.Silu), reads=[condT], writes=[condT])
            wbuf = [fw.sb("wmod%d" % i, [128, 8, 768], F32, pes) for i in range(2)]
            pm = [fw.ps("pmod%d" % i, [128, 6, 2], F32, pes) for i in range(2)]
            it = 0
            for l in range(nlayers):
                for piece in range(8):
                    wb = wbuf[it % 2]
                    pp = pm[it % 2]
                    it += 1
                    q = "sp" if piece % 2 == 0 else "act"
                    eng = nc.sync if piece % 2 == 0 else nc.scalar
                    fw.dma(q, lambda: eng.dma_start(out=wb[:], in_=I["w_mod"][l, :, piece * 768:(piece + 1) * 768].rearrange("(c p) n -> p c n", p=128)),
                           reads=[I["w_mod"]], writes=[wb])
                    for cc in range(6):
                        for kc in range(8):
                            fw.op("pe", lambda: nc.tensor.matmul(pp[:, cc, :], lhsT=wb[:, kc, cc * 128:(cc + 1) * 128], rhs=condT[:, :, kc],
                                                               start=(kc == 0), stop=(kc == 7)),
                                  reads=[wb, condT], writes=[pp], chain=True)
                    for j in range(2):
                        fw.op("dve", lambda: nc.vector.tensor_tensor(out=modT[:, l, piece * 6:(piece + 1) * 6, j], in0=pp[:, :, j],
                                                                   in1=bmT[:, l, piece * 6:(piece + 1) * 6], op=ALU.add),
                              reads=[pp, bmT], writes=[modT])
            for l in range(nlayers):
                for i in (1, 4):
                    fw.op("dve", lambda: nc.vector.tensor_scalar_add(out=modT[:, l, i * 8:(i + 1) * 8, :], in0=modT[:, l, i * 8:(i + 1) * 8, :], scalar1=1.0),
                          reads=[modT], writes=[modT])

    def prephase():
        with ExitStack() as pes:
            xt = [fw.sb("xt%d" % i, [128, D], F32, pes) for i in range(2)]
            xo = [fw.sb("xo%d" % i, [128, 8, 128], F32, pes) for i in range(2)]
            pt = [fw.ps("ptr%d" % i, [128, 8, 128], F32, pes) for i in range(2)]
            for ti in range(NTOK // 128):
                a = xt[ti % 2]; o = xo[ti % 2]; p = pt[ti % 2]
                fw.dma("sp", lambda: nc.sync.dma_start(out=a[:], in_=I["xin"][ti * 128:(ti + 1) * 128, :]), reads=[I["xin"]], writes=[a])
                for c in range(8):
                    fw.op("pe", lambda: nc.tensor.transpose(out=p[:, c, :], in_=a[:, c * 128:(c + 1) * 128], identity=ident_f[:]),
                          reads=[a, ident_f], writes=[p], chain=True)
                if ti % 2 == 0:
                    fw.op("act", lambda: nc.scalar.copy(out=o[:], in_=p[:]), reads=[p], writes=[o])
                else:
                    fw.op("dve", lambda: nc.vector.tensor_copy(out=o[:], in_=p[:]), reads=[p], writes=[o])
                fw.dma("act", lambda: nc.scalar.dma_start(out=xs[:, :, ti * 128:(ti + 1) * 128], in_=o[:]), reads=[o], writes=[xs])

    def norm_mod(pes_bufs, xg, n, gvec, l, ishift, iscale, j, out_bf=None, out_f=None):
        sq, pss, rstd, GS = pes_bufs
        fw.op("act", lambda: nc.scalar.activation(out=sq[:, :, :n], in_=xg[:, :, :n], func=AF.Square), reads=[xg], writes=[sq])
        for c in range(8):
            fw.op("pe", lambda: nc.tensor.matmul(pss[:, :n], lhsT=ones_b[:], rhs=sq[:, c, :n], start=(c == 0), stop=(c == 7)),
                  reads=[sq, ones_b], writes=[pss], chain=True)
        fw.op("act", lambda: nc.scalar.activation(out=rstd[:, :n], in_=pss[:, :n], func=AF.Sqrt, bias=EPS, scale=1.0 / D), reads=[pss], writes=[rstd])
        fw.op("dve", lambda: nc.vector.reciprocal(out=rstd[:, :n], in_=rstd[:, :n]), reads=[rstd], writes=[rstd])
        fw.op("dve", lambda: nc.vector.tensor_tensor(out=GS[:, 0, :], in0=gvec, in1=modT[:, l, iscale * 8:(iscale + 1) * 8, j], op=ALU.mult),
              reads=[modT, gmix, gffn], writes=[GS])
        for c in range(8):
            tgt = out_f if out_f is not None else sq
            if out_f is not None:
                fw.op("dve", lambda: nc.vector.scalar_tensor_tensor(out=out_f[:, c, :n], in0=xg[:, c, :n], scalar=GS[:, 0, c:c + 1], in1=rstd[:, :n],
                                                                  op0=ALU.mult, op1=ALU.mult), reads=[xg, GS, rstd], writes=[out_f])
                fw.op("dve", lambda: nc.vector.tensor_scalar_add(out=out_f[:, c, :n], in0=out_f[:, c, :n], scalar1=modT[:, l, ishift * 8 + c, j:j + 1]),
                      reads=[out_f, modT], writes=[out_f])
                if out_bf is not None:
                    fw.op("act", lambda: nc.scalar.copy(out=out_bf[:, c, :n], in_=out_f[:, c, :n]), reads=[out_f], writes=[out_bf])
            else:
                fw.op("dve", lambda: nc.vector.scalar_tensor_tensor(out=xg[:, c, :n], in0=xg[:, c, :n], scalar=GS[:, 0, c:c + 1], in1=rstd[:, :n],
                                                                  op0=ALU.mult, op1=ALU.mult), reads=[xg, GS, rstd], writes=[xg])
                fw.op("act", lambda: nc.scalar.activation(out=out_bf[:, c, :n], in_=xg[:, c, :n], func=AF.Identity,
                                                        bias=modT[:, l, ishift * 8 + c, j:j + 1], scale=1.0), reads=[xg, modT], writes=[out_bf])

    NCOLF = 20 * 128

    def phaseA(l):
        with ExitStack() as pes:
            w2 = fw.sb("w2", [128, 8, NCOLF], BF16, pes)
            wv = fw.sb("wv", [128, 8, 512], BF16, pes)
            win = I["w_in"]

            def wl(dst, dlo, slo, n, q="pool"):
                fw.dma("pool", lambda: nc.gpsimd.dma_start(out=dst[:, :, dlo:dlo + n], in_=win[l, :, slo:slo + n].rearrange("(c p) n -> p c n", p=128)),
                       reads=[win], writes=[dst])
            wl(w2, 0, 0, 384)
            wl(w2, 384, 384, 384)
            wl(w2, 768, 1152, 256)
            wl(w2, 1024, 1408, 384)
            for ti, (h0, h1) in enumerate(((0, 0), (0, 1), (1, 1))):
                wl(w2, 1792 + ti * 128, 1792 + h0 * 64, 64)
                wl(w2, 1792 + ti * 128 + 64, 1792 + h1 * 64, 64)
            wl(wv, 0, 768, 384)
            wl(wv, 384, 1920, 128)
            for (src, dst, nh) in ((1024, 1408, 6), (1792, 2176, 6)):
                sv = w2[:, :, src:src + nh * 64].rearrange("p c (h a j f) -> p c h a j f", h=nh, a=2, j=2, f=16)
                dv = w2[:, :, dst:dst + nh * 64].rearrange("p c (h a j f) -> p c h a j f", h=nh, a=2, j=2, f=16)
                for c in range(8):
                    for jj in range(2):
                        fw.op("pool", lambda: nc.gpsimd.tensor_copy(out=dv[:, c, :, :, jj, :], in_=sv[:, c, :, :, 1 - jj, :]), reads=[w2], writes=[w2])
            xg = [fw.sb("xg%d" % i, [128, 8, 512], F32, pes) for i in range(2)]
            hT = [fw.sb("hT%d" % i, [128, 8, 512], BF16, pes) for i in range(2)]
            sq = fw.sb("sq", [128, 8, 512], BF16, pes)
            rstd = fw.sb("rstd", [128, 512], F32, pes)
            GS = fw.sb("GS", [128, 1, 8], F32, pes)
            pss = fw.ps("pss", [128, 512], F32, pes)
            pp = [fw.ps("ppA%d" % i, [128, 512], F32, pes) for i in range(4)]
            pv = [fw.ps("ppV%d" % i, [128, 512], F32, pes) for i in range(2)]
            ob = [fw.sb("obA%d" % i, [128, 512], BF16, pes) for i in range(4)]
            of = [fw.sb("ofA%d" % i, [128, 512], F32, pes) for i in range(2)]
            t1 = [fw.sb("t1A%d" % i, [128, 512], F32, pes) for i in range(2)]
            t2 = [fw.sb("t2A%d" % i, [128, 512], F32, pes) for i in range(2)]
            rc = [fw.sb("rc%d" % i, [128, 512], F32, pes) for i in range(2)]
            rs_ = [fw.sb("rs%d" % i, [128, 512], F32, pes) for i in range(2)]
            vst = [fw.sb("vst%d" % i, [128, 8, 65], BF16, pes) for i in range(2)]
            for v in vst:
                fw.op("dve", lambda: nc.vector.memset(v[:], 1.0), writes=[v])
            k = 0
            ko = 0
            for gi, (t0, n, isctx) in enumerate(groups()):
                j = 1 if isctx else 0
                x_ = xg[gi % 2]; h_ = hT[gi % 2]
                fw.dma("sp", lambda: nc.sync.dma_start(out=x_[:, :, :n], in_=xs[:, :, t0:t0 + n]), reads=[xs], writes=[x_])
                if not isctx:
                    r_c = rc[gi % 2]; r_s = rs_[gi % 2]
                    fw.dma("act", lambda: nc.scalar.dma_start(out=r_c[:], in_=I["rope_c"][:, t0:t0 + n]), reads=[I["rope_c"]], writes=[r_c])
                    fw.dma("act", lambda: nc.scalar.dma_start(out=r_s[:], in_=I["rope_s"][:, t0:t0 + n]), reads=[I["rope_s"]], writes=[r_s])
                norm_mod((sq, pss, rstd, GS), x_, n, gmix[:, l, :], l, 0, 1, j, out_bf=h_)

                def proj(colblk):
                    nonlocal k
                    p = pp[k % 4]; k += 1
                    for kc in range(8):
                        fw.op("pe", lambda: nc.tensor.matmul(p[:, :n], lhsT=w2[:, kc, colblk * 128:(colblk + 1) * 128], rhs=h_[:, kc, :n],
                                                           start=(kc == 0), stop=(kc == 7)), reads=[w2, h_], writes=[p], chain=True)
                    return p
                for blk in range(8):
                    p = proj(blk)
                    if blk < 6:
                        o = ob[ko % 4]; ko += 1
                        if blk % 2 == 0:
                            fw.op("act", lambda: nc.scalar.copy(out=o[:, :n], in_=p[:, :n]), reads=[p], writes=[o])
                        else:
                            fw.op("dve", lambda: nc.vector.tensor_copy(out=o[:, :n], in_=p[:, :n]), reads=[p], writes=[o])
                        dst = qa if blk < 3 else ka
                        fw.dma("sp", lambda: nc.sync.dma_start(out=dst[blk % 3, :, t0:t0 + n], in_=o[:, :n]), reads=[o], writes=[dst])
                    else:
                        o = of[blk % 2]
                        fw.op("act", lambda: nc.scalar.copy(out=o[:, :n], in_=p[:, :n]), reads=[p], writes=[o])
                        fw.dma("sp", lambda: nc.sync.dma_start(out=u5[blk - 6, :, t0:t0 + n], in_=o[:, :n]), reads=[o], writes=[u5])
                for which, base, dst in ((0, 8, qs), (1, 14, ks)):
                    for ti in range(3):
                        p = proj(base + ti)
                        o = ob[ko % 4]; ko += 1
                        if isctx:
                            fw.op("act", lambda: nc.scalar.copy(out=o[:, :n], in_=p[:, :n]), reads=[p], writes=[o])
                        else:
                            pr = proj(base + 3 + ti)
                            a = t1[ti % 2]; b = t2[ti % 2]
                            fw.op("dve", lambda: nc.vector.tensor_tensor(out=a[:, :n], in0=p[:, :n], in1=r_c[:, :n], op=ALU.mult), reads=[p, r_c], writes=[a])
                            fw.op("dve", lambda: nc.vector.tensor_tensor(out=b[:, :n], in0=pr[:, :n], in1=r_s[:, :n], op=ALU.mult), reads=[pr, r_s], writes=[b])
                            fw.op("pool", lambda: nc.gpsimd.tensor_tensor(out=o[:, :n], in0=a[:, :n], in1=b[:, :n], op=ALU.add), reads=[a, b], writes=[o])
                        fw.dma("sp", lambda: nc.sync.dma_start(out=dst[ti, :, t0:t0 + n], in_=o[:, :n]), reads=[o], writes=[dst])
                for tt in range(n // 128):
                    p = pv[tt % 2]; v = vst[tt % 2]
                    for kc in range(8):
                        fw.op("pe", lambda: nc.tensor.matmul(p[:, :], lhsT=h_[:, kc, tt * 128:(tt + 1) * 128], rhs=wv[:, kc, :],
                                                           start=(kc == 0), stop=(kc == 7)), reads=[wv, h_], writes=[p], chain=True)
                    fw.op("act", lambda: nc.scalar.copy(out=v[:, :, 0:64], in_=p[:, :].rearrange("p (h d) -> p h d", d=64)), reads=[p], writes=[v])
                    r0 = t0 + tt * 128
                    fw.dma("act", lambda: nc.scalar.dma_start(out=va[r0:r0 + 128, :].rearrange("t (h d) -> t h d", d=65), in_=v[:, 0:6, :]), reads=[v], writes=[va])
                    fw.dma("act", lambda: nc.scalar.dma_start(out=vs[r0:r0 + 128, :].rearrange("t (h d) -> t h d", d=65), in_=v[:, 6:8, :]), reads=[v], writes=[vs])


    def attn_norm1(pes_t, po, n, h, sink=None):
        rden, osb, ysb, pbc = pes_t
        if sink is not None:
            fw.op("dve", lambda: nc.vector.tensor_scalar(out=rden[64:65, :n], in0=po[64:65, :n], scalar1=sink[64:65, h:h + 1], scalar2=None, op0=ALU.add),
                  reads=[po, sink], writes=[rden])
            fw.op("dve", lambda: nc.vector.reciprocal(out=rden[64:65, :n], in_=rden[64:65, :n]), reads=[rden], writes=[rden])
        else:
            fw.op("dve", lambda: nc.vector.reciprocal(out=rden[64:65, :n], in_=po[64:65, :n]), reads=[po], writes=[rden])
        fw.op("act", lambda: nc.scalar.copy(out=osb[0:64, :n], in_=po[0:64, :n]), reads=[po], writes=[osb])

    def attn_norm2(pes_t, n, t0, chtile, base):
        rden, osb, ysb, pbc = pes_t
        fw.op("pe", lambda: nc.tensor.matmul(pbc[0:64, :n], lhsT=ones_f[64:65, 0:64], rhs=rden[64:65, :n], start=True, stop=True),
              reads=[rden, ones_f], writes=[pbc])
        fw.op("dve", lambda: nc.vector.tensor_tensor(out=ysb[0:64, :n], in0=osb[0:64, :n], in1=pbc[0:64, :n], op=ALU.mult), reads=[osb, pbc], writes=[ysb])
        fw.dma("sp", lambda: nc.sync.dma_start(out=yT[chtile, base:base + 64, t0:t0 + n], in_=ysb[0:64, :n]), reads=[ysb], writes=[yT])

    class Deferred:
        def __init__(self):
            self.p = None

        def push(self, *a):
            self.flush()
            self.p = a

        def flush(self):
            if self.p is not None:
                attn_norm2(*self.p)
                self.p = None

    rp = fw.dram("rp", [1, 3072], F32)

    def phaseB(l, with_ctx):
        with ExitStack() as pes:
            z = fw.sb("zrp", [1, 3072], F32, pes)
            fw.op("dve", lambda: nc.vector.memset(z[:], 0.0), writes=[z])
            fw.dma("sp", lambda: nc.sync.dma_start(out=z[0:1, 64:64 + 2790], in_=I["na_rpb"][l:l + 1].rearrange("o h a b -> o (h a b)")), reads=[I["na_rpb"]], writes=[z])
            fw.dma("sp", lambda: nc.sync.dma_start(out=rp[:], in_=z[:]), reads=[z], writes=[rp])
            BB = fw.sb("BB", [64, 6, 15, 64], F32, pes)
            for h in range(6):
                src = bass.AP(tensor=rp.t.tensor, offset=64 + h * 465 + 15 - 63, ap=[[1, 64], [31, 15], [1, 64]])
                fw.dma("sp", lambda: nc.sync.dma_start(out=BB[:, h, :, :], in_=src), reads=[rp], writes=[BB])
            pen = fw.sb("napen", [128, 64], F32, pes)
            fw.dma("sp", lambda: nc.sync.dma_start(out=pen[:], in_=I["na_pen"][:]), reads=[I["na_pen"]], writes=[pen])
            biasT = fw.sb("biasT", [128, 6, 14, 64], BF16, pes)
            with ExitStack() as pes2:
                pb = [fw.ps("pbias%d" % i, [128, 64], F32, pes2) for i in range(2)]
                it = 0
                for h in range(6):
                    for a in range(14):
                        p = pb[it % 2]; it += 1
                        fw.op("pe", lambda: nc.tensor.transpose(out=p[:, :], in_=BB[:, h, a:a + 2, :].rearrange("p a k -> p (a k)"), identity=ident_f[0:64, 0:64]),
                              reads=[BB, ident_f], writes=[p])
                        fw.op("dve", lambda: nc.vector.scalar_tensor_tensor(out=biasT[:, h, a, :], in0=p[:, ::-1], scalar=8.0, in1=pen[:], op0=ALU.mult, op1=ALU.add),
                              reads=[p, pen], writes=[biasT])
                fw.barrier()
            qT = fw.sb("qT", [128, NTOK], BF16, pes)
            kT = fw.sb("kT", [128, NTOK], BF16, pes)
            v0 = fw.sb("v0", [128, 66, 2, 65], BF16, pes)
            v1 = fw.sb("v1", [128, 63, 2, 65], BF16, pes)
            pc = fw.ps("pc", [128, 2, 512], F32, pes)
            pn = [fw.ps("pn%d" % i, [128, 4, 4, 64], F32, pes) for i in range(2)]
            po = fw.ps("po", [128, 512], F32, pes)
            pbc = fw.ps("pbc", [128, 512], F32, pes)
            PcT = fw.sb("PcT", [128, 2, 512], BF16, pes)
            PnT = [fw.sb("PnT%d" % i, [128, 4, 4, 64], BF16, pes) for i in range(2)]
            nt = [(fw.sb("rden%d" % i, [128, 512], F32, pes), fw.sb("osb%d" % i, [128, 512], F32, pes), fw.sb("ysb%d" % i, [128, 512], BF16, pes), pbc) for i in range(2)]
            dfr = Deferred(); po_i = 0
            ih = 0
            for jp in range(3):
                fw.dma("sp", lambda: nc.sync.dma_start(out=qT[:], in_=qa[jp]), reads=[qa], writes=[qT])
                fw.dma("act", lambda: nc.scalar.dma_start(out=kT[:], in_=ka[jp]), reads=[ka], writes=[kT])
                fw.dma("sp", lambda: nc.sync.dma_start(out=v0[:].rearrange("p t h d -> p t (h d)"),
                                                      in_=va[:, jp * 130:(jp + 1) * 130].rearrange("(t p) c -> p t c", p=128)), reads=[va], writes=[v0])
                fw.dma("act", lambda: nc.scalar.dma_start(out=v1[:].rearrange("p t h d -> p t (h d)"),
                                                        in_=va[64:64 + 63 * 128, jp * 130:(jp + 1) * 130].rearrange("(t p) c -> p t c", p=128)), reads=[va], writes=[v1])
                for hh in range(2):
                    h = 2 * jp + hh
                    b0 = 64 * hh
                    for G in range(16):
                        t0 = 512 * G
                        for ct in range(2):
                            fw.op("pe", lambda: nc.tensor.matmul(pc[:, ct, :], lhsT=kT[b0:b0 + 64, SEQ + 128 * ct:SEQ + 128 * ct + 128], rhs=qT[b0:b0 + 64, t0:t0 + 512],
                                                               start=True, stop=True), reads=[kT, qT], writes=[pc])
                        fw.op("act", lambda: nc.scalar.activation(out=PcT[:], in_=pc[:], func=AF.Exp, scale=0.125), reads=[pc], writes=[PcT])
                        for half in range(2):
                            p_ = pn[ih % 2]; P_ = PnT[ih % 2]; ih += 1
                            for rr in range(4):
                                r = 8 * G + 4 * half + rr
                                rs = min(max(r - 4, 0), 120)
                                for j in range(4):
                                    k0 = (rs + 2 * j) * 64
                                    a = rs + 2 * j - r + 7
                                    fw.op("pe", lambda: nc.tensor.matmul(p_[:, rr, j, :], lhsT=kT[b0:b0 + 64, k0:k0 + 128], rhs=qT[b0:b0 + 64, r * 64:r * 64 + 64],
                                                                       start=True, stop=False), reads=[kT, qT], writes=[p_], chain=True)
                                    fw.op("pe", lambda: nc.tensor.matmul(p_[:, rr, j, :], lhsT=ident_b[:, :], rhs=biasT[:, h, a, :],
                                                                       start=False, stop=True), reads=[biasT, ident_b], writes=[p_], chain=True)
                            if half == 0:
                                dfr.flush()
                            fw.op("act", lambda: nc.scalar.activation(out=P_[:], in_=p_[:], func=AF.Exp, scale=0.125), reads=[p_], writes=[P_])
                            for rr in range(4):
                                r = 8 * G + 4 * half + rr
                                rs = min(max(r - 4, 0), 120)
                                c0 = (4 * half + rr) * 64
                                for j in range(4):
                                    k0 = (rs + 2 * j) * 64
                                    vt = v0[:, k0 // 128, hh, :] if rs % 2 == 0 else v1[:, (k0 - 64) // 128, hh, :]
                                    fw.op("pe", lambda: nc.tensor.matmul(po[0:65, c0:c0 + 64], lhsT=vt, rhs=P_[:, rr, j, :], start=(j == 0), stop=False),
                                          reads=[v0, v1, P_], writes=[po], chain=True)
                                for ct in range(2):
                                    fw.op("pe", lambda: nc.tensor.matmul(po[0:65, c0:c0 + 64], lhsT=v0[:, 64 + ct, hh, :], rhs=PcT[:, ct, c0:c0 + 64], start=False, stop=(ct == 1)),
                                          reads=[v0, PcT], writes=[po], chain=True)
                        attn_norm1(nt[po_i % 2], po, 512, h)
                        dfr.push(nt[po_i % 2], 512, t0, jp, b0); po_i += 1
                    if with_ctx:
                        for ct in range(2):
                            fw.op("pe", lambda: nc.tensor.matmul(pc[:, ct, :CTXL], lhsT=kT[b0:b0 + 64, SEQ + 128 * ct:SEQ + 128 * ct + 128], rhs=qT[b0:b0 + 64, SEQ:SEQ + CTXL],
                                                               start=True, stop=True), reads=[kT, qT], writes=[pc])
                        fw.op("act", lambda: nc.scalar.activation(out=PcT[:, :, :CTXL], in_=pc[:, :, :CTXL], func=AF.Exp, scale=0.125), reads=[pc], writes=[PcT])
                        for ct in range(2):
                            fw.op("pe", lambda: nc.tensor.matmul(po[0:65, :CTXL], lhsT=v0[:, 64 + ct, hh, :], rhs=PcT[:, ct, :CTXL], start=(ct == 0), stop=(ct == 1)),
                                  reads=[v0, PcT], writes=[po], chain=True)
                        attn_norm1(nt[po_i % 2], po, CTXL, h)
                        dfr.push(nt[po_i % 2], CTXL, SEQ, jp, b0); po_i += 1

            dfr.flush()

    def phaseD(l, with_ctx):
        with ExitStack() as pes:
            sk = fw.sb("sinks", [128, 6], F32, pes)
            fw.dma("sp", lambda: nc.sync.dma_start(out=sk[64:65, :], in_=I["sw_sinks"][l:l + 1, :]), reads=[I["sw_sinks"]], writes=[sk])
            fw.op("act", lambda: nc.scalar.activation(out=sk[64:65, :], in_=sk[64:65, :], func=AF.Exp), reads=[sk], writes=[sk])
            ml = fw.sb("mask_l", [128, 128], BF16, pes)
            mu = fw.sb("mask_u", [128, 128], BF16, pes)
            fw.dma("sp", lambda: nc.sync.dma_start(out=ml[:], in_=I["mask_l"][:]), reads=[I["mask_l"]], writes=[ml])
            fw.dma("sp", lambda: nc.sync.dma_start(out=mu[:], in_=I["mask_u"][:]), reads=[I["mask_u"]], writes=[mu])
            qT = fw.sb("qTs", [128, NTOK], BF16, pes)
            kT = fw.sb("kTs", [128, NTOK], BF16, pes)
            vS = fw.sb("vS", [128, 66, 2, 65], BF16, pes)
            fw.dma("sp", lambda: nc.sync.dma_start(out=vS[:].rearrange("p t h d -> p t (h d)"), in_=vs[:, :].rearrange("(t p) c -> p t c", p=128)), reads=[vs], writes=[vS])
            pc = fw.ps("pcs", [128, 2, 512], F32, pes)
            pn = [fw.ps("pns%d" % i, [128, 3, 128], F32, pes) for i in range(2)]
            po = fw.ps("pos", [128, 512], F32, pes)
            pbc = fw.ps("pbcs", [128, 512], F32, pes)
            PcT = fw.sb("PcTs", [128, 2, 512], BF16, pes)
            PnT = [fw.sb("PnTs%d" % i, [128, 3, 128], BF16, pes) for i in range(2)]
            nt = [(fw.sb("rdens%d" % i, [128, 512], F32, pes), fw.sb("osbs%d" % i, [128, 512], F32, pes), fw.sb("ysbs%d" % i, [128, 512], BF16, pes), pbc) for i in range(2)]
            dfr = Deferred(); po_i = 0
            ih = 0
            NB = SEQ // 128
            for jp in range(3):
                fw.dma("sp", lambda: nc.sync.dma_start(out=qT[:], in_=qs[jp]), reads=[qs], writes=[qT])
                fw.dma("act", lambda: nc.scalar.dma_start(out=kT[:], in_=ks[jp]), reads=[ks], writes=[kT])
                for hh in range(2):
                    h = 2 * jp + hh
                    kvh = h // 3
                    b0 = 64 * hh
                    for G in range(16):
                        t0 = 512 * G
                        for ct in range(2):
                            fw.op("pe", lambda: nc.tensor.matmul(pc[:, ct, :], lhsT=kT[b0:b0 + 64, SEQ + 128 * ct:SEQ + 128 * ct + 128], rhs=qT[b0:b0 + 64, t0:t0 + 512],
                                                               start=True, stop=True), reads=[kT, qT], writes=[pc])
                        fw.op("act", lambda: nc.scalar.activation(out=PcT[:], in_=pc[:], func=AF.Exp, scale=0.125), reads=[pc], writes=[PcT])
                        for nb_ in range(4):
                            n = 4 * G + nb_
                            p_ = pn[ih % 2]; P_ = PnT[ih % 2]; ih += 1
                            kbs = [kb for kb in (n - 1, n, n + 1) if 0 <= kb < NB]
                            lo = kbs[0] - (n - 1)
                            hi = kbs[-1] - (n - 1) + 1
                            for kb in kbs:
                                ki = kb - (n - 1)
                                fw.op("pe", lambda: nc.tensor.matmul(p_[:, ki, :], lhsT=kT[b0:b0 + 64, kb * 128:kb * 128 + 128], rhs=qT[b0:b0 + 64, n * 128:n * 128 + 128],
                                                                   start=True, stop=(kb == n)), reads=[kT, qT], writes=[p_], chain=True)
                                if kb != n:
                                    mk = ml if kb < n else mu
                                    fw.op("pe", lambda: nc.tensor.matmul(p_[:, ki, :], lhsT=ident_b[:, :], rhs=mk[:, :], start=False, stop=True),
                                          reads=[mk, ident_b], writes=[p_], chain=True)
                            if nb_ == 0:
                                dfr.flush()
                            fw.op("act", lambda: nc.scalar.activation(out=P_[:, lo:hi, :], in_=p_[:, lo:hi, :], func=AF.Exp, scale=0.125), reads=[p_], writes=[P_])
                            c0 = nb_ * 128
                            for kb in kbs:
                                ki = kb - (n - 1)
                                fw.op("pe", lambda: nc.tensor.matmul(po[0:65, c0:c0 + 128], lhsT=vS[:, kb, kvh, :], rhs=P_[:, ki, :], start=(kb == kbs[0]), stop=False),
                                      reads=[vS, P_], writes=[po], chain=True)
                            for ct in range(2):
                                fw.op("pe", lambda: nc.tensor.matmul(po[0:65, c0:c0 + 128], lhsT=vS[:, 64 + ct, kvh, :], rhs=PcT[:, ct, c0:c0 + 128], start=False, stop=(ct == 1)),
                                      reads=[vS, PcT], writes=[po], chain=True)
                        attn_norm1(nt[po_i % 2], po, 512, h, sink=sk)
                        dfr.push(nt[po_i % 2], 512, t0, 5 + jp, b0); po_i += 1
                    if with_ctx:
                        for ct in range(2):
                            fw.op("pe", lambda: nc.tensor.matmul(pc[:, ct, :CTXL], lhsT=kT[b0:b0 + 64, SEQ + 128 * ct:SEQ + 128 * ct + 128], rhs=qT[b0:b0 + 64, SEQ:SEQ + CTXL],
                                                               start=True, stop=True), reads=[kT, qT], writes=[pc])
                        fw.op("act", lambda: nc.scalar.activation(out=PcT[:, :, :CTXL], in_=pc[:, :, :CTXL], func=AF.Exp, scale=0.125), reads=[pc], writes=[PcT])
                        for ct in range(2):
                            fw.op("pe", lambda: nc.tensor.matmul(po[0:65, :CTXL], lhsT=vS[:, 64 + ct, kvh, :], rhs=PcT[:, ct, :CTXL], start=(ct == 0), stop=(ct == 1)),
                                  reads=[vS, PcT], writes=[po], chain=True)
                        attn_norm1(nt[po_i % 2], po, CTXL, h, sink=sk)
                        dfr.push(nt[po_i % 2], CTXL, SEQ, 5 + jp, b0); po_i += 1
            dfr.flush()

    yf = fw.dram("yf", [2, 128, NTOK], F32)
    TWO_PI = 6.283185307179586
    C1 = 6.28125
    C2 = TWO_PI - 6.28125
    PI = 3.141592653589793

    def phaseC(l, with_ctx):
        with ExitStack() as pes:
            def tl(name, shape, dt=F32):
                return fw.sb(name, shape, dt, pes)
            scr_i = tl("scr_i", [128, 512], I32)
            scr_k = tl("scr_k", [128, 512])
            scr_t = tl("scr_t", [128, 512])
            scr_a = tl("scr_a", [128, 512])
            scr_b = tl("scr_b", [128, 512])

            def dve(fn, reads, writes):
                fw.op("dve", fn, reads=reads, writes=writes)

            def reduce_angle(ang, n, srcs):
                dve(lambda: nc.vector.tensor_scalar(out=scr_k[:, :n], in0=ang, scalar1=1.0 / TWO_PI, scalar2=None, op0=ALU.mult), srcs, [scr_k])
                dve(lambda: nc.vector.tensor_copy(out=scr_i[:, :n], in_=scr_k[:, :n]), [scr_k], [scr_i])
                dve(lambda: nc.vector.tensor_copy(out=scr_k[:, :n], in_=scr_i[:, :n]), [scr_i], [scr_k])
                dve(lambda: nc.vector.scalar_tensor_tensor(out=scr_a[:, :n], in0=scr_k[:, :n], scalar=-C1, in1=ang, op0=ALU.mult, op1=ALU.add), [scr_k] + srcs, [scr_a])
                dve(lambda: nc.vector.scalar_tensor_tensor(out=scr_a[:, :n], in0=scr_k[:, :n], scalar=-C2, in1=scr_a[:, :n], op0=ALU.mult, op1=ALU.add), [scr_k, scr_a], [scr_a])
                wrap(scr_a, n)

            def wrap(t, n):
                dve(lambda: nc.vector.tensor_scalar(out=scr_t[:, :n], in0=t[:, :n], scalar1=PI, scalar2=None, op0=ALU.is_gt), [t], [scr_t])
                dve(lambda: nc.vector.scalar_tensor_tensor(out=t[:, :n], in0=scr_t[:, :n], scalar=-TWO_PI, in1=t[:, :n], op0=ALU.mult, op1=ALU.add), [scr_t, t], [t])
                dve(lambda: nc.vector.tensor_scalar(out=scr_t[:, :n], in0=t[:, :n], scalar1=-PI, scalar2=None, op0=ALU.is_lt), [t], [scr_t])
                dve(lambda: nc.vector.scalar_tensor_tensor(out=t[:, :n], in0=scr_t[:, :n], scalar=TWO_PI, in1=t[:, :n], op0=ALU.mult, op1=ALU.add), [scr_t, t], [t])

            def sincos(ang, n, srcs, out_s, out_c, outs):
                reduce_angle(ang, n, srcs)
                fw.op("act", lambda: nc.scalar.activation(out=out_s, in_=scr_a[:, :n], func=AF.Sin), reads=[scr_a], writes=outs)
                dve(lambda: nc.vector.tensor_scalar(out=scr_b[:, :n], in0=scr_a[:, :n], scalar1=PI / 2, scalar2=None, op0=ALU.add), [scr_a], [scr_b])
                wrap(scr_b, n)
                fw.op("act", lambda: nc.scalar.activation(out=out_c, in_=scr_b[:, :n], func=AF.Sin), reads=[scr_b], writes=outs)

            are = tl("are", [128, 16]); aim = tl("aim", [128, 16]); stp = tl("stp", [128, 16])
            with nc.allow_non_contiguous_dma(reason="tiny s5 params"):
                for nm, dst in (("s5_a_re", are), ("s5_a_im", aim)):
                    for d_ in range(2):
                        fw.dma("sp", lambda: nc.sync.dma_start(out=dst[:, d_ * 8:(d_ + 1) * 8], in_=I[nm][l, d_].rearrange("g p -> (g p)").rearrange("(i q) -> q i", q=128)),
                               reads=[I[nm]], writes=[dst])
                for d_ in range(2):
                    for g2 in range(2):
                        src = bass.AP(tensor=I["s5_log_step"].t.tensor, offset=l * 32 + d_ * 16 + g2, ap=[[0, 64], [2, 8]])
                        fw.dma("sp", lambda: nc.sync.dma_start(out=stp[64 * g2:64 * g2 + 64, d_ * 8:(d_ + 1) * 8], in_=src), reads=[I["s5_log_step"]], writes=[stp])
            fw.op("act", lambda: nc.scalar.activation(out=stp[:], in_=stp[:], func=AF.Exp), reads=[stp], writes=[stp])
            dve(lambda: nc.vector.tensor_scalar(out=are[:], in0=are[:], scalar1=-1e-4, scalar2=None, op0=ALU.min), [are], [are])
            lr = tl("lr", [128, 16]); li = tl("li", [128, 16]); rr = tl("rr", [128, 16])
            dve(lambda: nc.vector.tensor_tensor(out=lr[:], in0=are[:], in1=stp[:], op=ALU.mult), [are, stp], [lr])
            dve(lambda: nc.vector.tensor_tensor(out=li[:], in0=aim[:], in1=stp[:], op=ALU.mult), [aim, stp], [li])
            fw.op("act", lambda: nc.scalar.activation(out=rr[:], in_=lr[:], func=AF.Exp), reads=[lr], writes=[rr])
            s1 = tl("s1", [128, 16]); c1 = tl("c1", [128, 16])
            sincos(li[:, :], 16, [li], s1[:, :], c1[:, :], [s1, c1])
            sL = {}; cL = {}
            angL = tl("angL", [128, 16])
            for L in (512, 256):
                sL[L] = tl("sL%d" % L, [128, 16]); cL[L] = tl("cL%d" % L, [128, 16])
                dve(lambda: nc.vector.tensor_scalar(out=angL[:], in0=li[:], scalar1=float(L), scalar2=None, op0=ALU.mult), [li], [angL])
                sincos(angL[:, :], 16, [angL], sL[L][:, :], cL[L][:, :], [sL[L], cL[L]])
            lbr = tl("lbr", [128, 16]); lbi = tl("lbi", [128, 16]); den = tl("den", [128, 16]); tq = tl("tq", [128, 16])
            cr = tl("cr", [128, 16]); ci = tl("ci", [128, 16]); nci = tl("nci", [128, 16])
            dve(lambda: nc.vector.tensor_tensor(out=lbr[:], in0=rr[:], in1=c1[:], op=ALU.mult), [rr, c1], [lbr])
            dve(lambda: nc.vector.tensor_scalar(out=lbr[:], in0=lbr[:], scalar1=-1.0, scalar2=None, op0=ALU.add), [lbr], [lbr])
            dve(lambda: nc.vector.tensor_tensor(out=lbi[:], in0=rr[:], in1=s1[:], op=ALU.mult), [rr, s1], [lbi])
            dve(lambda: nc.vector.tensor_tensor(out=den[:], in0=are[:], in1=are[:], op=ALU.mult), [are], [den])
            dve(lambda: nc.vector.tensor_tensor(out=tq[:], in0=aim[:], in1=aim[:], op=ALU.mult), [aim], [tq])
            dve(lambda: nc.vector.tensor_tensor(out=den[:], in0=den[:], in1=tq[:], op=ALU.add), [den, tq], [den])
            dve(lambda: nc.vector.reciprocal(out=den[:], in_=den[:]), [den], [den])
            dve(lambda: nc.vector.tensor_tensor(out=cr[:], in0=lbr[:], in1=are[:], op=ALU.mult), [lbr, are], [cr])
            dve(lambda: nc.vector.tensor_tensor(out=tq[:], in0=lbi[:], in1=aim[:], op=ALU.mult), [lbi, aim], [tq])
            dve(lambda: nc.vector.tensor_tensor(out=cr[:], in0=cr[:], in1=tq[:], op=ALU.add), [cr, tq], [cr])
            dve(lambda: nc.vector.tensor_tensor(out=cr[:], in0=cr[:], in1=den[:], op=ALU.mult), [cr, den], [cr])
            dve(lambda: nc.vector.tensor_tensor(out=ci[:], in0=lbi[:], in1=are[:], op=ALU.mult), [lbi, are], [ci])
            dve(lambda: nc.vector.tensor_tensor(out=tq[:], in0=lbr[:], in1=aim[:], op=ALU.mult), [lbr, aim], [tq])
            dve(lambda: nc.vector.tensor_tensor(out=ci[:], in0=ci[:], in1=tq[:], op=ALU.subtract), [ci, tq], [ci])
            dve(lambda: nc.vector.tensor_tensor(out=ci[:], in0=ci[:], in1=den[:], op=ALU.mult), [ci, den], [ci])
            dve(lambda: nc.vector.tensor_scalar(out=nci[:], in0=ci[:], scalar1=-1.0, scalar2=None, op0=ALU.mult), [ci], [nci])
            bsr = tl("bsr", [128, 2, 2, 128]); bsi = tl("bsi", [128, 2, 2, 128])
            bbr = tl("bbr", [128, 2, 2, 128]); bbi = tl("bbi", [128, 2, 2, 128])
            csr = tl("csr", [128, 2, 2, 128]); csi = tl("csi", [128, 2, 2, 128])
            for t_ in (bsr, bsi, csr, csi):
                dve(lambda: nc.vector.memset(t_[:], 0.0), [], [t_])
            qn = 0
            for d_ in range(2):
                for g in range(16):
                    i = g // 2; g2 = g % 2; i4 = i // 4; im = i % 4
                    for nm, dst in (("s5_b_re", bsr), ("s5_b_im", bsi)):
                        q = ("sp", nc.sync) if qn % 2 == 0 else ("act", nc.scalar); qn += 1
                        fw.dma(q[0], lambda: q[1].dma_start(out=dst[64 * g2:64 * g2 + 64, d_, i4, 32 * im + 16 * g2:32 * im + 16 * g2 + 16], in_=I[nm][l, d_, g]),
                               reads=[I[nm]], writes=[dst])
                    for nm, dst in (("s5_c_re", csr), ("s5_c_im", csi)):
                        q = ("sp", nc.sync) if qn % 2 == 0 else ("act", nc.scalar); qn += 1
                        fw.dma(q[0], lambda: q[1].dma_start(out=dst[32 * im + 16 * g2:32 * im + 16 * g2 + 16, d_, i4, 64 * g2:64 * g2 + 64], in_=I[nm][l, d_, g]),
                               reads=[I[nm]], writes=[dst])
            for d_ in range(2):
                for i in range(8):
                    col = d_ * 8 + i; i4 = i // 4; im = i % 4
                    sl = slice(32 * im, 32 * im + 32)
                    dve(lambda: nc.vector.tensor_scalar(out=bbr[:, d_, i4, sl], in0=bsr[:, d_, i4, sl], scalar1=cr[:, col:col + 1], scalar2=None, op0=ALU.mult), [bsr, cr], [bbr])
                    dve(lambda: nc.vector.scalar_tensor_tensor(out=bbr[:, d_, i4, sl], in0=bsi[:, d_, i4, sl], scalar=nci[:, col:col + 1], in1=bbr[:, d_, i4, sl],
                                                               op0=ALU.mult, op1=ALU.add), [bsi, nci, bbr], [bbr])
                    dve(lambda: nc.vector.tensor_scalar(out=bbi[:, d_, i4, sl], in0=bsi[:, d_, i4, sl], scalar1=cr[:, col:col + 1], scalar2=None, op0=ALU.mult), [bsi, cr], [bbi])
                    dve(lambda: nc.vector.scalar_tensor_tensor(out=bbi[:, d_, i4, sl], in0=bsr[:, d_, i4, sl], scalar=ci[:, col:col + 1], in1=bbi[:, d_, i4, sl],
                                                               op0=ALU.mult, op1=ALU.add), [bsr, ci, bbi], [bbi])
            BbT = tl("BbT", [128, 2, 2, 2, 128], BF16)
            CT = tl("CT", [128, 2, 2, 2, 128], BF16)
            with ExitStack() as pes2:
                ptp = [fw.ps("ptp%d" % k_, [128, 128], F32, pes2) for k_ in range(2)]
                it = 0
                for d_ in range(2):
                    for i4 in range(2):
                        for ri, (bsrc, csrc) in enumerate(((bbr, csr), (bbi, csi))):
                            p = ptp[it % 2]; it += 1
                            fw.op("pe", lambda: nc.tensor.transpose(out=p[:, :], in_=bsrc[:, d_, i4, :], identity=ident_f[:]), reads=[bsrc, ident_f], writes=[p])
                            fw.op("act", lambda: nc.scalar.copy(out=BbT[:, d_, i4, ri, :], in_=p[:, :]), reads=[p], writes=[BbT])
                            p = ptp[it % 2]; it += 1
                            fw.op("pe", lambda: nc.tensor.transpose(out=p[:, :], in_=csrc[:, d_, i4, :], identity=ident_f[:]), reads=[csrc, ident_f], writes=[p])
                            fw.op("act", lambda: nc.scalar.mul(out=CT[:, d_, i4, ri, :], in_=p[:, :], mul=(1.0 if ri == 0 else -1.0)), reads=[p], writes=[CT])
                fw.barrier()
            tcos = tl("tcos", [128, 16, 512]); tsin = tl("tsin", [128, 16, 512])
            rful = tl("rful", [128, 16, 512])
            io = tl("io512", [128, 512])
            fw.dma("sp", lambda: nc.sync.dma_start(out=io[:], in_=I["iota512"][:]), reads=[I["iota512"]], writes=[io])
            angt = tl("angt", [128, 512])
            for col in range(16):
                dve(lambda: nc.vector.tensor_scalar(out=angt[:], in0=io[:], scalar1=li[:, col:col + 1], scalar2=None, op0=ALU.mult), [io, li], [angt])
                sincos(angt[:, :], 512, [angt], tsin[:, col, :], tcos[:, col, :], [tsin, tcos])
                fw.op("pool", lambda: nc.gpsimd.tensor_scalar(out=rful[:, col, :], in0=io[:], scalar1=0.0, scalar2=rr[:, col:col + 1], op0=ALU.mult, op1=ALU.add), reads=[io, rr], writes=[rful])
            dsk = tl("dsk", [128, 2]); bgl = tl("bgl", [128, 2])
            wgl = tl("wgl", [128, 2, 256], BF16)
            with nc.allow_non_contiguous_dma(reason="tiny"):
                fw.dma("sp", lambda: nc.sync.dma_start(out=dsk[:], in_=I["s5_d"][l].rearrange("(c p) -> p c", p=128)), reads=[I["s5_d"]], writes=[dsk])
                fw.dma("sp", lambda: nc.sync.dma_start(out=bgl[:], in_=I["s5_b_glu"][l].rearrange("(c p) -> p c", p=128)), reads=[I["s5_b_glu"]], writes=[bgl])
            fw.dma("pool", lambda: nc.gpsimd.dma_start(out=wgl[:], in_=I["s5_w_glu"][l].rearrange("(c p) n -> p c n", p=128)), reads=[I["s5_w_glu"]], writes=[wgl])
            zst = tl("zst", [128, 16, 2])
            zin = tl("zin", [128, 16, 2])
            dve(lambda: nc.vector.memset(zin[:], 0.0), [], [zin])
            uf = [tl("uf%d" % k_, [128, 2, 512]) for k_ in range(2)]
            ub = [tl("ub%d" % k_, [128, 2, 512], BF16) for k_ in range(2)]
            pA = [fw.ps("pA%d" % k_, [128, 512], F32, pes) for k_ in range(2)]
            pB = [fw.ps("pB%d" % k_, [128, 512], F32, pes) for k_ in range(2)]
            py = [fw.ps("py%d" % k_, [128, 512], F32, pes) for k_ in range(2)]
            pg = [fw.ps("pg%d" % k_, [128, 512], F32, pes) for k_ in range(2)]
            W = {}
            for nm in ("t1", "t2", "t3", "t4", "dr", "di", "zr", "zi"):
                W[nm] = [tl(nm + "_%d" % k_, [128, 512]) for k_ in range(2)]
            xr = [tl("xr%d" % k_, [128, 512], BF16) for k_ in range(2)]
            xi = [tl("xi%d" % k_, [128, 512], BF16) for k_ in range(2)]
            ysb = [tl("ysbC0", [128, 2, 512])] * 2
            yfl = tl("yfl", [128, 2, 512])
            gq = tl("gq", [128, 2, 512]); gp = tl("gp", [128, 2, 512]); gg = gq
            gb = tl("gb", [128, 2, 512], BF16); sg = gp; yo = tl("yo", [128, 2, 512], BF16)
            lat = [(g_ * 512, 512) for g_ in range(16)]
            order = {0: [(SEQ, CTXL)] + lat, 1: [(SEQ, CTXL)] + lat[::-1]}
            it = 0
            ci_ = 0
            for d_ in range(2):
                prevL = None
                for (t0, L) in order[d_]:
                    isctx = t0 >= SEQ
                    u_ = uf[ci_ % 2]; ub_ = ub[ci_ % 2]; ys_ = ysb[ci_ % 2]; ci_ += 1
                    fw.dma("sp", lambda: nc.sync.dma_start(out=u_[:, :, :L], in_=u5[:, :, t0:t0 + L].rearrange("c p t -> p c t")), reads=[u5], writes=[u_])
                    if d_ == 0:
                        fw.op("act", lambda: nc.scalar.copy(out=ub_[:, :, :L], in_=u_[:, :, :L]), reads=[u_], writes=[ub_])
                    else:
                        fw.op("act", lambda: nc.scalar.copy(out=ub_[:, :, :L], in_=u_[:, :, L - 1::-1] if False else u_[:, :, :L][:, :, ::-1]), reads=[u_], writes=[ub_])
                        fw.dma("act", lambda: nc.scalar.dma_start(out=yfl[:, :, :L], in_=yf[:, :, t0:t0 + L].rearrange("c p t -> p c t")), reads=[yf], writes=[yfl])
                    for i in range(8):
                        col = d_ * 8 + i; i4 = i // 4; im = i % 4
                        k_ = it % 2; it += 1
                        A = pA[k_]; B = pB[k_]
                        t1, t2, t3, t4 = W["t1"][k_], W["t2"][k_], W["t3"][k_], W["t4"][k_]
                        dr, di, zr, zi = W["dr"][k_], W["di"][k_], W["zr"][k_], W["zi"][k_]
                        u1, u2, u3, u4 = t1, t2, t3, t4
                        rf = rful
                        xr_, xi_ = xr[k_], xi[k_]
                        ps_ = slice(32 * im, 32 * im + 32)
                        fw.op("pe", lambda: nc.tensor.matmul(A[:, :L], lhsT=BbT[ps_, d_, i4, 0, :], rhs=ub_[ps_, i4, :L], start=True, stop=True, tile_position=(32 * im, 0)),
                              reads=[BbT, ub_], writes=[A])
                        fw.op("pe", lambda: nc.tensor.matmul(B[:, :L], lhsT=BbT[ps_, d_, i4, 1, :], rhs=ub_[ps_, i4, :L], start=True, stop=True, tile_position=(32 * im, 0)),
                              reads=[BbT, ub_], writes=[B])
                        if prevL is not None:
                            dve(lambda: nc.vector.tensor_scalar(out=zin[:, col, 0:1], in0=zst[:, col, 1:2], scalar1=sL[prevL][:, col:col + 1], scalar2=None, op0=ALU.mult), [zst, sL[prevL]], [zin])
                            dve(lambda: nc.vector.scalar_tensor_tensor(out=zin[:, col, 0:1], in0=zst[:, col, 0:1], scalar=cL[prevL][:, col:col + 1], in1=zin[:, col, 0:1],
                                                                       op0=ALU.mult, op1=ALU.subtract), [zst, cL[prevL], zin], [zin])
                            dve(lambda: nc.vector.tensor_scalar(out=zin[:, col, 1:2], in0=zst[:, col, 1:2], scalar1=cL[prevL][:, col:col + 1], scalar2=None, op0=ALU.mult), [zst, cL[prevL]], [zin])
                            dve(lambda: nc.vector.scalar_tensor_tensor(out=zin[:, col, 1:2], in0=zst[:, col, 0:1], scalar=sL[prevL][:, col:col + 1], in1=zin[:, col, 1:2],
                                                                       op0=ALU.mult, op1=ALU.add), [zst, sL[prevL], zin], [zin])
                        cs = tcos[:, col, :L]; sn = tsin[:, col, :L]
                        dve(lambda: nc.vector.tensor_tensor(out=t1[:, :L], in0=A[:, :L], in1=cs, op=ALU.mult), [A, tcos], [t1])
                        dve(lambda: nc.vector.tensor_tensor(out=t2[:, :L], in0=B[:, :L], in1=sn, op=ALU.mult), [B, tsin], [t2])
                        fw.op("pool", lambda: nc.gpsimd.tensor_tensor(out=dr[:, :L], in0=t1[:, :L], in1=t2[:, :L], op=ALU.add), reads=[t1, t2], writes=[dr])
                        dve(lambda: nc.vector.tensor_tensor(out=t3[:, :L], in0=B[:, :L], in1=cs, op=ALU.mult), [B, tcos], [t3])
                        dve(lambda: nc.vector.tensor_tensor(out=t4[:, :L], in0=A[:, :L], in1=sn, op=ALU.mult), [A, tsin], [t4])
                        fw.op("pool", lambda: nc.gpsimd.tensor_tensor(out=di[:, :L], in0=t3[:, :L], in1=t4[:, :L], op=ALU.subtract), reads=[t3, t4], writes=[di])
                        dve(lambda: nc.vector.tensor_tensor_scan(out=zr[:, :L], data0=rful[:, col, :L], data1=dr[:, :L], initial=zin[:, col, 0:1], op0=ALU.mult, op1=ALU.add),
                            [rf, dr, zin], [zr])
                        dve(lambda: nc.vector.tensor_tensor_scan(out=zi[:, :L], data0=rful[:, col, :L], data1=di[:, :L], initial=zin[:, col, 1:2], op0=ALU.mult, op1=ALU.add),
                            [rf, di, zin], [zi])
                        dve(lambda: nc.vector.tensor_copy(out=zst[:, col, 0:1], in_=zr[:, L - 1:L]), [zr], [zst])
                        dve(lambda: nc.vector.tensor_copy(out=zst[:, col, 1:2], in_=zi[:, L - 1:L]), [zi], [zst])
                        dve(lambda: nc.vector.tensor_tensor(out=u1[:, :L], in0=zr[:, :L], in1=cs, op=ALU.mult), [zr, tcos], [u1])
                        fw.op("pool", lambda: nc.gpsimd.tensor_tensor(out=u2[:, :L], in0=zi[:, :L], in1=sn, op=ALU.mult), reads=[zi, tsin], writes=[u2])
                        fw.op("pool", lambda: nc.gpsimd.tensor_tensor(out=xr_[:, :L], in0=u1[:, :L], in1=u2[:, :L], op=ALU.subtract), reads=[u1, u2], writes=[xr_])
                        dve(lambda: nc.vector.tensor_tensor(out=u3[:, :L], in0=zr[:, :L], in1=sn, op=ALU.mult), [zr, tsin], [u3])
                        fw.op("pool", lambda: nc.gpsimd.tensor_tensor(out=u4[:, :L], in0=zi[:, :L], in1=cs, op=ALU.mult), reads=[zi, tcos], writes=[u4])
                        fw.op("pool", lambda: nc.gpsimd.tensor_tensor(out=xi_[:, :L], in0=u3[:, :L], in1=u4[:, :L], op=ALU.add), reads=[u3, u4], writes=[xi_])
                        if isctx and not with_ctx:
                            continue
                        yq = py[i4]
                        fw.op("pe", lambda: nc.tensor.matmul(yq[ps_, :L], lhsT=CT[:, d_, i4, 0, ps_], rhs=xr_[:, :L], start=True, stop=False, tile_position=(0, 32 * im)),
                              reads=[CT, xr_], writes=[yq])
                        fw.op("pe", lambda: nc.tensor.matmul(yq[ps_, :L], lhsT=CT[:, d_, i4, 1, ps_], rhs=xi_[:, :L], start=False, stop=True, tile_position=(0, 32 * im)),
                              reads=[CT, xi_], writes=[yq])
                    prevL = L
                    if isctx and not with_ctx:
                        continue
                    if d_ == 0:
                        for ct in range(2):
                            dve(lambda: nc.vector.scalar_tensor_tensor(out=ys_[:, ct, :L], in0=u_[:, ct, :L], scalar=dsk[:, ct:ct + 1], in1=py[ct][:, :L], op0=ALU.mult, op1=ALU.add),
                                [u_, dsk, py[ct]], [ys_])
                        fw.dma("sp", lambda: nc.sync.dma_start(out=yf[:, :, t0:t0 + L].rearrange("c p t -> p c t"), in_=ys_[:, :, :L]), reads=[ys_], writes=[yf])
                    else:
                        for ct in range(2):
                            dve(lambda: nc.vector.tensor_tensor(out=ys_[:, ct, :L], in0=py[ct][:, :L][:, ::-1], in1=yfl[:, ct, :L], op=ALU.add), [py[ct], yfl], [ys_])
                        fw.op("act", lambda: nc.scalar.activation(out=gq[:, :, :L], in_=ys_[:, :, :L], func=AF.Square), reads=[ys_], writes=[gq])
                        dve(lambda: nc.vector.tensor_scalar(out=gq[:, :, :L], in0=gq[:, :, :L], scalar1=0.044715, scalar2=1.0, op0=ALU.mult, op1=ALU.add), [gq], [gq])
                        fw.op("pool", lambda: nc.gpsimd.tensor_tensor(out=gp[:, :, :L], in0=gq[:, :, :L], in1=ys_[:, :, :L], op=ALU.mult), reads=[gq, ys_], writes=[gp])
                        fw.op("act", lambda: nc.scalar.activation(out=gp[:, :, :L], in_=gp[:, :, :L], func=AF.Sigmoid, scale=1.5957691216057308), reads=[gp], writes=[gp])
                        fw.op("pool", lambda: nc.gpsimd.tensor_tensor(out=gg[:, :, :L], in0=gp[:, :, :L], in1=ys_[:, :, :L], op=ALU.mult), reads=[gp, ys_], writes=[gg])
                        fw.op("act", lambda: nc.scalar.copy(out=gb[:, :, :L], in_=gg[:, :, :L]), reads=[gg], writes=[gb])
                        for co in range(2):
                            for cin in range(2):
                                fw.op("pe", lambda: nc.tensor.matmul(pg[co][:, :L], lhsT=wgl[:, cin, co * 128:(co + 1) * 128], rhs=gb[:, cin, :L], start=(cin == 0), stop=(cin == 1)),
                                      reads=[wgl, gb], writes=[pg[co]], chain=True)
                            fw.op("act", lambda: nc.scalar.activation(out=sg[:, co, :L], in_=pg[co][:, :L], func=AF.Sigmoid, bias=bgl[:, co:co + 1], scale=1.0), reads=[pg[co], bgl], writes=[sg])
                        dve(lambda: nc.vector.tensor_tensor(out=yo[:, :, :L], in0=gg[:, :, :L], in1=sg[:, :, :L], op=ALU.mult), [gg, sg], [yo])
                        fw.dma("sp", lambda: nc.sync.dma_start(out=yT[3:5, :, t0:t0 + L].rearrange("c p t -> p c t"), in_=yo[:, :, :L]), reads=[yo], writes=[yT])


    Xs = fw.dram("Xs", [NSLOT + 128, D], BF16)
    Ys = fw.dram("Ys", [NSLOT, D], F32)
    h2tok = fw.dram("h2tok", [NTOK, D], BF16)
    NTILE = NTOK // 128
    dest_i = fw.sb("dest_i", [128, NTILE, 4], U32)
    wk = fw.sb("wk", [128, NTILE, 4], F32)
    idxw = fw.sb("idxw", [128, NB, 8], U32)
    idxbg = fw.sb("idxbg", [128, NB], U32)
    idxbd = fw.sb("idxbd", [128, NB], U32)
    iop = fw.sb("iop", [128, 1], F32)
    fw.dma("sp", lambda: nc.sync.dma_start(out=iop[:], in_=I["iota_p"][:]), reads=[I["iota_p"]], writes=[iop])

    def phaseE(l, with_ctx):
        with ExitStack() as pes:
            def tl(name, shape, dt=F32):
                return fw.sb(name, shape, dt, pes)
            wout = tl("wout", [128, 8, D], BF16)
            fw.dma("pool", lambda: nc.gpsimd.dma_start(out=wout[:], in_=I["w_out"][l].rearrange("(c p) n -> p c n", p=128)), reads=[I["w_out"]], writes=[wout])
            wr = tl("wr", [128, 8, NE])
            fw.dma("sp", lambda: nc.sync.dma_start(out=wr[:], in_=I["w_router"][l].rearrange("(c p) e -> p c e", p=128)), reads=[I["w_router"]], writes=[wr])
            brow = tl("brow", [128, NE])
            fw.dma("sp", lambda: nc.sync.dma_start(out=brow[:], in_=I["b_router"][l].partition_broadcast(128)), reads=[I["b_router"]], writes=[brow])
            io32 = tl("io32", [128, NE]); ust = tl("ust", [128, 128]); iob = tl("iob", [128, NB])
            fw.dma("sp", lambda: nc.sync.dma_start(out=io32[:], in_=I["iota32"][:]), reads=[I["iota32"]], writes=[io32])
            fw.dma("sp", lambda: nc.sync.dma_start(out=ust[:], in_=I["ustrict"][:]), reads=[I["ustrict"]], writes=[ust])
            fw.dma("sp", lambda: nc.sync.dma_start(out=iob[:], in_=I["iotablk"][:]), reads=[I["iotablk"]], writes=[iob])
            runm = tl("runm", [128, NE])
            fw.op("dve", lambda: nc.vector.memset(runm[:], 0.0), writes=[runm])
            posall = tl("posall", [128, NTILE, NE]); idxall = tl("idxall", [128, NTILE, 4])
            xg = [tl("xgE%d" % i, [128, 8, 512]) for i in range(2)]
            yg = [tl("ygE%d" % i, [128, 8, 512], BF16) for i in range(2)]
            h2f = tl("h2f", [128, 8, 512]); h2b = tl("h2b", [128, 8, 512], BF16)
            sq = tl("sqE", [128, 8, 512], BF16); rstd = tl("rstdE", [128, 512]); GS = tl("GSE", [128, 1, 8])
            pss = fw.ps("pssE", [128, 512], F32, pes)
            pp = [fw.ps("ppE%d" % i, [128, 512], F32, pes) for i in range(2)]
            plg = fw.ps("plg", [128, NE], F32, pes)
            ppos = fw.ps("ppos", [128, NE], F32, pes)
            ptr = [fw.ps("ptrE%d" % i, [128, 8, 128], BF16, pes) for i in range(2)]
            htok = [tl("htok%d" % i, [128, D], BF16) for i in range(2)]
            lg = tl("lg", [128, NE]); m8 = tl("m8", [128, 8]); idx8 = tl("idx8", [128, 8], U32); mask = tl("mask", [128, NE])
            negmx = tl("negmx", [128, 1]); e4 = tl("e4", [128, 4]); ssum = tl("ssum", [128, 1])
            oh = tl("oh", [128, NE]); junk = tl("junk", [128, NE]); posk = tl("posk", [128, 4]); posf = tl("posf", [128, NE])
            gl = groups() if with_ctx else groups()[:-1]
            tiles_done = []
            for gi, (t0, n, isctx) in enumerate(gl):
                j = 1 if isctx else 0
                x_ = xg[gi % 2]; y_ = yg[gi % 2]
                fw.dma("sp", lambda: nc.sync.dma_start(out=x_[:, :, :n], in_=xs[:, :, t0:t0 + n]), reads=[xs], writes=[x_])
                fw.dma("act", lambda: nc.scalar.dma_start(out=y_[:, :, :n], in_=yT[:, :, t0:t0 + n].rearrange("c p t -> p c t")), reads=[yT], writes=[y_])
                for dc in range(8):
                    p = pp[dc % 2]
                    for ct in range(8):
                        fw.op("pe", lambda: nc.tensor.matmul(p[:, :n], lhsT=wout[:, ct, dc * 128:(dc + 1) * 128], rhs=y_[:, ct, :n], start=(ct == 0), stop=(ct == 7)),
                              reads=[wout, y_], writes=[p], chain=True)
                    fw.op("dve", lambda: nc.vector.scalar_tensor_tensor(out=x_[:, dc, :n], in0=p[:, :n], scalar=modT[:, l, 16 + dc, j:j + 1], in1=x_[:, dc, :n],
                                                                      op0=ALU.mult, op1=ALU.add), reads=[p, modT, x_], writes=[x_])
                fw.dma("sp", lambda: nc.sync.dma_start(out=xs[:, :, t0:t0 + n], in_=x_[:, :, :n]), reads=[x_], writes=[xs])
                norm_mod((sq, pss, rstd, GS), x_, n, gffn[:, l, :], l, 3, 4, j, out_bf=h2b, out_f=h2f)
                for tt in range(n // 128):
                    ti = (t0 // 128) + tt
                    tiles_done.append(ti)
                    tsl = slice(tt * 128, (tt + 1) * 128)
                    for kc in range(8):
                        fw.op("pe", lambda: nc.tensor.matmul(plg[:, :], lhsT=h2f[:, kc, tsl], rhs=wr[:, kc, :], start=(kc == 0), stop=(kc == 7)),
                              reads=[h2f, wr], writes=[plg], chain=True)
                    fw.op("dve", lambda: nc.vector.tensor_tensor(out=lg[:], in0=plg[:], in1=brow[:], op=ALU.add), reads=[plg, brow], writes=[lg])
                    fw.op("dve", lambda: nc.vector.max(out=m8[:], in_=lg[:]), reads=[lg], writes=[m8])
                    fw.op("dve", lambda: nc.vector.max_index(out=idx8[:], in_max=m8[:], in_values=lg[:]), reads=[lg, m8], writes=[idx8])
                    fw.op("dve", lambda: nc.vector.tensor_scalar(out=mask[:], in0=lg[:], scalar1=m8[:, 3:4], scalar2=None, op0=ALU.is_ge), reads=[lg, m8], writes=[mask])
                    fw.op("dve", lambda: nc.vector.tensor_scalar(out=negmx[:], in0=m8[:, 0:1], scalar1=-1.0, scalar2=None, op0=ALU.mult), reads=[m8], writes=[negmx])
                    fw.op("act", lambda: nc.scalar.activation(out=e4[:], in_=m8[:, 0:4], func=AF.Exp, bias=negmx[:, 0:1], scale=1.0, accum_out=ssum[:, 0:1]),
                          reads=[m8, negmx], writes=[e4, ssum])
                    fw.op("dve", lambda: nc.vector.reciprocal(out=ssum[:], in_=ssum[:]), reads=[ssum], writes=[ssum])
                    fw.op("dve", lambda: nc.vector.tensor_scalar(out=wk[:, ti, :], in0=e4[:], scalar1=ssum[:, 0:1], scalar2=None, op0=ALU.mult), reads=[e4, ssum], writes=[wk])
                    fw.op("pe", lambda: nc.tensor.matmul(ppos[:, :], lhsT=ust[:, :], rhs=mask[:, :], start=True, stop=False), reads=[ust, mask], writes=[ppos])
                    fw.op("pe", lambda: nc.tensor.matmul(ppos[:, :], lhsT=ones_f[:, :], rhs=runm[:, :], start=False, stop=True), reads=[ones_f, runm], writes=[ppos], chain=True)
                    fw.op("act", lambda: nc.scalar.copy(out=posall[:, ti, :], in_=ppos[:]), reads=[ppos], writes=[posall])
                    fw.op("pool", lambda: nc.gpsimd.tensor_tensor(out=runm[:], in0=runm[:], in1=mask[:], op=ALU.add), reads=[runm, mask], writes=[runm])
                    fw.op("dve", lambda: nc.vector.tensor_copy(out=idxall[:, ti, :], in_=idx8[:, 0:4]), reads=[idx8], writes=[idxall])
                    pt_ = ptr[ti % 2]; ht = htok[ti % 2]
                    for kc in range(8):
                        fw.op("pe", lambda: nc.tensor.transpose(out=pt_[:, kc, :], in_=h2b[:, kc, tsl], identity=ident_b[:]), reads=[h2b, ident_b], writes=[pt_], chain=True)
                    fw.op("act", lambda: nc.scalar.copy(out=ht[:], in_=pt_[:].rearrange("p c d -> p (c d)")), reads=[pt_], writes=[ht])
                    fw.dma("act", lambda: nc.scalar.dma_start(out=h2tok[ti * 128:(ti + 1) * 128, :], in_=ht[:]), reads=[ht], writes=[h2tok])
            cnt = tl("cnt", [128, NE]); pad = tl("pad", [128, NE]); ends = tl("ends", [128, NE]); pst = tl("pst", [128, NE])
            qi = tl("qi", [128, NE], I32); qf = tl("qf", [128, NE]); gt = tl("gt", [128, NE]); onesr = tl("onesr", [128, NE])
            acc = tl("accb", [128, NB])
            fw.op("pe", lambda: nc.tensor.matmul(ppos[:, :], lhsT=ones_f[:, :], rhs=runm[:, :], start=True, stop=True), reads=[ones_f, runm], writes=[ppos])
            fw.op("dve", lambda: nc.vector.tensor_scalar(out=cnt[:], in0=ppos[:], scalar1=float(BLK - 1), scalar2=1.0 / BLK, op0=ALU.add, op1=ALU.mult), reads=[ppos], writes=[cnt])
            fw.op("dve", lambda: nc.vector.tensor_copy(out=qi[:], in_=cnt[:]), reads=[cnt], writes=[qi])
            fw.op("dve", lambda: nc.vector.tensor_copy(out=qf[:], in_=qi[:]), reads=[qi], writes=[qf])
            fw.op("dve", lambda: nc.vector.tensor_tensor(out=gt[:], in0=qf[:], in1=cnt[:], op=ALU.is_gt), reads=[qf, cnt], writes=[gt])
            fw.op("dve", lambda: nc.vector.tensor_tensor(out=qf[:], in0=qf[:], in1=gt[:], op=ALU.subtract), reads=[qf, gt], writes=[qf])
            fw.op("dve", lambda: nc.vector.tensor_scalar(out=pad[:], in0=qf[:], scalar1=float(BLK), scalar2=None, op0=ALU.mult), reads=[qf], writes=[pad])
            fw.op("dve", lambda: nc.vector.memset(onesr[:], 1.0), writes=[onesr])
            fw.op("dve", lambda: nc.vector.tensor_tensor_scan(out=ends[:], data0=onesr[:], data1=pad[:], initial=0.0, op0=ALU.mult, op1=ALU.add), reads=[onesr, pad], writes=[ends])
            fw.op("dve", lambda: nc.vector.tensor_tensor(out=pst[:], in0=ends[:], in1=pad[:], op=ALU.subtract), reads=[ends, pad], writes=[pst])
            fw.op("dve", lambda: nc.vector.memset(acc[:], 0.0), writes=[acc])
            for e in range(NE):
                fw.op("dve", lambda: nc.vector.scalar_tensor_tensor(out=acc[:], in0=iob[:], scalar=ends[:, e:e + 1], in1=acc[:], op0=ALU.is_ge, op1=ALU.add),
                      reads=[iob, ends, acc], writes=[acc])
            fw.op("dve", lambda: nc.vector.tensor_scalar(out=acc[:], in0=acc[:], scalar1=float(NE - 1), scalar2=None, op0=ALU.min), reads=[acc], writes=[acc])
            tix = tl("tix", [128, NB])
            for kc in range(8):
                fw.op("dve", lambda: nc.vector.tensor_scalar(out=tix[:], in0=acc[:], scalar1=1024.0, scalar2=float(l * NE * 1024 + kc * 128), op0=ALU.mult, op1=ALU.add), reads=[acc], writes=[tix])
                fw.op("dve", lambda: nc.vector.tensor_scalar(out=tix[:], in0=tix[:], scalar1=iop[:, 0:1], scalar2=None, op0=ALU.add), reads=[tix, iop], writes=[tix])
                fw.op("dve", lambda: nc.vector.tensor_copy(out=idxw[:, :, kc], in_=tix[:]), reads=[tix], writes=[idxw])
            fw.op("dve", lambda: nc.vector.tensor_scalar(out=tix[:], in0=acc[:], scalar1=16.0, scalar2=float(l * NE * 16), op0=ALU.mult, op1=ALU.add), reads=[acc], writes=[tix])
            fw.op("dve", lambda: nc.vector.tensor_scalar(out=tix[:], in0=tix[:], scalar1=iop[:, 0:1], scalar2=None, op0=ALU.add), reads=[tix, iop], writes=[tix])
            fw.op("dve", lambda: nc.vector.tensor_copy(out=idxbg[:], in_=tix[:]), reads=[tix], writes=[idxbg])
            fw.op("dve", lambda: nc.vector.tensor_scalar(out=tix[:], in0=acc[:], scalar1=float(l * NE), scalar2=None, op0=ALU.add), reads=[acc], writes=[tix])
            fw.op("dve", lambda: nc.vector.tensor_copy(out=idxbd[:], in_=tix[:]), reads=[tix], writes=[idxbd])
            for ti in tiles_done:
                ht = htok[ti % 2]
                fw.dma("sp", lambda: nc.sync.dma_start(out=ht[:], in_=h2tok[ti * 128:(ti + 1) * 128, :]), reads=[h2tok], writes=[ht])
                fw.op("dve", lambda: nc.vector.tensor_tensor(out=posf[:], in0=posall[:, ti, :], in1=pst[:], op=ALU.add), reads=[posall, pst], writes=[posf])
                for k_ in range(4):
                    fw.op("dve", lambda: nc.vector.tensor_scalar(out=oh[:], in0=io32[:], scalar1=idxall[:, ti, k_:k_ + 1], scalar2=None, op0=ALU.is_equal), reads=[io32, idxall], writes=[oh])
                    fw.op("dve", lambda: nc.vector.scalar_tensor_tensor(out=junk[:], in0=oh[:], scalar=1.0, in1=posf[:], op0=ALU.mult, op1=ALU.mult,
                                                                      accum_out=posk[:, k_:k_ + 1]), reads=[oh, posf], writes=[junk, posk])
                fw.op("dve", lambda: nc.vector.tensor_copy(out=dest_i[:, ti, :], in_=posk[:]), reads=[posk], writes=[dest_i])
                for k_ in range(4):
                    fw.dma("pool", lambda: nc.gpsimd.indirect_dma_start(out=Xs[:, :], out_offset=bass.IndirectOffsetOnAxis(ap=dest_i[:, ti, k_:k_ + 1], axis=0),
                                                                      in_=ht[:, :], in_offset=None), reads=[ht, dest_i], writes=[Xs])

    def phaseF(l):
        with ExitStack() as pes:
            def tl(name, shape, dt=F32):
                return fw.sb(name, shape, dt, pes)
            NST = BLK // 128
            chunks = [(c0, min(512, BLK - c0)) for c0 in range(0, BLK, 512)]
            wgu = [tl("wgu%d" % i, [128, 8, 2 * D], BF16) for i in range(2)]
            wdn = [tl("wdn%d" % i, [128, 8, D], BF16) for i in range(2)]
            bdr = [tl("bdr%d" % i, [128, D]) for i in range(2)]
            bgc = [tl("bgc%d" % i, [128, 16]) for i in range(2)]
            xtok = [tl("xtok%d" % i, [128, NST, D], BF16) for i in range(2)]
            XT = tl("XT", [128, 8, BLK], BF16)
            actT = tl("actT", [128, 8, BLK], BF16)
            gs = [tl("gs%d" % i, [128, 512]) for i in range(2)]
            sgm = [tl("sgm%d" % i, [128, 512]) for i in range(2)]
            uu = [tl("uu%d" % i, [128, 512]) for i in range(2)]
            aa = [tl("aa%d" % i, [128, 512]) for i in range(2)]
            yo = [tl("yoF%d" % i, [128, D]) for i in range(2)]
            ptrs = [fw.ps("ptrF%d" % i, [128, 8, 128], BF16, pes) for i in range(2)]
            pg = [fw.ps("pgF%d" % i, [128, 512], F32, pes) for i in range(2)]
            pu = [fw.ps("puF%d" % i, [128, 512], F32, pes) for i in range(2)]
            pd = [fw.ps("pdF%d" % i, [128, 512], F32, pes) for i in range(2)]

            wgu_flat = I["w_gate_up"][:].rearrange("l e k n -> (l e k) n")
            wdn_flat = I["w_down"][:].rearrange("l e k n -> (l e k) n")
            bgu_flat = I["b_gate_up"][:].rearrange("l e (c p) -> (l e c) p", p=128)
            bdn_flat = I["b_down"][:].rearrange("l e d -> (l e) d")
            bgrow = [tl("bgrow%d" % i, [16, 128]) for i in range(2)]

            def load_w(b):
                wg_ = wgu[b % 2]; wd_ = wdn[b % 2]; bd_ = bdr[b % 2]; br_ = bgrow[b % 2]
                fw.dma("pool", lambda: nc.gpsimd.indirect_dma_start(out=br_[:, :], out_offset=None, in_=bgu_flat,
                                                                  in_offset=bass.IndirectOffsetOnAxis(ap=idxbg[0:16, b:b + 1], axis=0)),
                       reads=[I["b_gate_up"], idxbg], writes=[br_])
                fw.dma("pool", lambda: nc.gpsimd.indirect_dma_start(out=bd_[:, :], out_offset=None, in_=bdn_flat,
                                                                  in_offset=bass.IndirectOffsetOnAxis(ap=idxbd[:, b:b + 1], axis=0)),
                       reads=[I["b_down"], idxbd], writes=[bd_])
                for kc in range(8):
                    fw.dma("pool", lambda: nc.gpsimd.indirect_dma_start(out=wg_[:, kc, :], out_offset=None, in_=wgu_flat,
                                                                      in_offset=bass.IndirectOffsetOnAxis(ap=idxw[:, b, kc:kc + 1], axis=0)),
                           reads=[I["w_gate_up"], idxw], writes=[wg_])
                for fc in range(8):
                    fw.dma("pool", lambda: nc.gpsimd.indirect_dma_start(out=wd_[:, fc, :], out_offset=None, in_=wdn_flat,
                                                                      in_offset=bass.IndirectOffsetOnAxis(ap=idxw[:, b, fc:fc + 1], axis=0)),
                           reads=[I["w_down"], idxw], writes=[wd_])
            load_w(0)
            fw.dma("sp", lambda: nc.sync.dma_start(out=xtok[0][:], in_=Xs[0:BLK, :].rearrange("(t p) d -> p t d", p=128)), reads=[Xs], writes=[xtok[0]])
            kk = 0
            for b in range(NB):
                if b + 1 < NB:
                    load_w(b + 1)
                wg_ = wgu[b % 2]; wd_ = wdn[b % 2]; bd_ = bdr[b % 2]; bg_ = bgc[b % 2]; br_ = bgrow[b % 2]
                xt_ = xtok[b % 2]
                if b + 1 < NB:
                    xn_ = xtok[(b + 1) % 2]
                    fw.dma("sp", lambda: nc.sync.dma_start(out=xn_[:], in_=Xs[(b + 1) * BLK:(b + 2) * BLK, :].rearrange("(t p) d -> p t d", p=128)), reads=[Xs], writes=[xn_])
                fw.op("pe", lambda: nc.tensor.transpose(out=pd[1][:, 0:16], in_=br_[:, :], identity=ident_f[0:16, 0:16]), reads=[br_, ident_f], writes=[pd[1]])
                fw.op("act", lambda: nc.scalar.copy(out=bg_[:], in_=pd[1][:, 0:16]), reads=[pd[1]], writes=[bg_])
                for st in range(NST):
                    ptr = ptrs[st % 2]
                    for kc in range(8):
                        fw.op("pe", lambda: nc.tensor.transpose(out=ptr[:, kc, :], in_=xt_[:, st, kc * 128:(kc + 1) * 128], identity=ident_b[:]), reads=[xt_, ident_b], writes=[ptr], chain=True)
                    if st % 2 == 0:
                        fw.op("act", lambda: nc.scalar.copy(out=XT[:, :, st * 128:(st + 1) * 128], in_=ptr[:]), reads=[ptr], writes=[XT])
                    else:
                        fw.op("dve", lambda: nc.vector.tensor_copy(out=XT[:, :, st * 128:(st + 1) * 128], in_=ptr[:]), reads=[ptr], writes=[XT])
                for (c0, cn) in chunks:
                    for j in range(8):
                        k_ = kk % 2; kk += 1
                        g_, u_ = pg[k_], pu[k_]
                        for kc in range(8):
                            fw.op("pe", lambda: nc.tensor.matmul(g_[:, :cn], lhsT=wg_[:, kc, j * 128:(j + 1) * 128], rhs=XT[:, kc, c0:c0 + cn], start=(kc == 0), stop=(kc == 7)),
                                  reads=[wg_, XT], writes=[g_], chain=True)
                        for kc in range(8):
                            fw.op("pe", lambda: nc.tensor.matmul(u_[:, :cn], lhsT=wg_[:, kc, D + j * 128:D + (j + 1) * 128], rhs=XT[:, kc, c0:c0 + cn], start=(kc == 0), stop=(kc == 7)),
                                  reads=[wg_, XT], writes=[u_], chain=True)
                        gs_, sg_, uu_, aa_ = gs[k_], sgm[k_], uu[k_], aa[k_]
                        fw.op("dve", lambda: nc.vector.tensor_scalar(out=gs_[:, :cn], in0=g_[:, :cn], scalar1=bg_[:, j:j + 1], scalar2=7.0, op0=ALU.add, op1=ALU.min), reads=[g_, bg_], writes=[gs_])
                        fw.op("act", lambda: nc.scalar.activation(out=sg_[:, :cn], in_=gs_[:, :cn], func=AF.Sigmoid, scale=1.702), reads=[gs_], writes=[sg_])
                        fw.op("dve", lambda: nc.vector.tensor_scalar(out=uu_[:, :cn], in0=u_[:, :cn], scalar1=bg_[:, 8 + j:9 + j], scalar2=7.0, op0=ALU.add, op1=ALU.min), reads=[u_, bg_], writes=[uu_])
                        fw.op("dve", lambda: nc.vector.tensor_scalar(out=uu_[:, :cn], in0=uu_[:, :cn], scalar1=-7.0, scalar2=1.0, op0=ALU.max, op1=ALU.add), reads=[uu_], writes=[uu_])
                        fw.op("pool", lambda: nc.gpsimd.tensor_tensor(out=aa_[:, :cn], in0=gs_[:, :cn], in1=sg_[:, :cn], op=ALU.mult), reads=[gs_, sg_], writes=[aa_])
                        fw.op("pool", lambda: nc.gpsimd.tensor_tensor(out=actT[:, j, c0:c0 + cn], in0=aa_[:, :cn], in1=uu_[:, :cn], op=ALU.mult), reads=[aa_, uu_], writes=[actT])
                for st in range(NST):
                    yo_ = yo[st % 2]
                    for dh in range(2):
                        p = pd[dh]
                        for fc in range(8):
                            fw.op("pe", lambda: nc.tensor.matmul(p[:, :], lhsT=actT[:, fc, st * 128:(st + 1) * 128], rhs=wd_[:, fc, dh * 512:(dh + 1) * 512], start=(fc == 0), stop=(fc == 7)),
                                  reads=[actT, wd_], writes=[p], chain=True)
                        fw.op("dve", lambda: nc.vector.tensor_tensor(out=yo_[:, dh * 512:(dh + 1) * 512], in0=p[:, :], in1=bd_[:, dh * 512:(dh + 1) * 512], op=ALU.add), reads=[p, bd_], writes=[yo_])
                    r0 = b * BLK + st * 128
                    fw.dma("sp", lambda: nc.sync.dma_start(out=Ys[r0:r0 + 128, :], in_=yo_[:, :]), reads=[yo_], writes=[Ys])

    def phaseG(l, with_ctx, last):
        with ExitStack() as pes:
            def tl(name, shape, dt=F32):
                return fw.sb(name, shape, dt, pes)
            yk = [[tl("yk%d_%d" % (b_, k_), [128, D]) for k_ in range(4)] for b_ in range(2)]
            acc = [tl("acc%d" % i, [128, D]) for i in range(2)]
            xg = [tl("xgG%d" % i, [128, 8, 512]) for i in range(2)]
            pT = [fw.ps("pTG%d" % i, [128, 8, 128], F32, pes) for i in range(2)]
            if last:
                sq = tl("sqG", [128, 8, 512], BF16); rstd = tl("rstdG", [128, 512])
                pss = fw.ps("pssG", [128, 512], F32, pes)
                xo = tl("xoG", [128, 8, 512])
                osb = [tl("osbG%d" % i, [128, D]) for i in range(2)]
                pO = fw.ps("pOG", [128, 8, 128], F32, pes)
            gl = groups() if with_ctx else groups()[:-1]
            for gi, (t0, n, isctx) in enumerate(gl):
                j = 1 if isctx else 0
                x_ = xg[gi % 2]
                fw.dma("sp", lambda: nc.sync.dma_start(out=x_[:, :, :n], in_=xs[:, :, t0:t0 + n]), reads=[xs], writes=[x_])
                for tt in range(n // 128):
                    ti = (t0 // 128) + tt
                    tsl = slice(tt * 128, (tt + 1) * 128)
                    yk_ = yk[ti % 2]; a_ = acc[ti % 2]; p_ = pT[ti % 2]
                    for k_ in range(4):
                        fw.dma("pool", lambda: nc.gpsimd.indirect_dma_start(out=yk_[k_][:, :], out_offset=None, in_=Ys[:, :],
                                                                          in_offset=bass.IndirectOffsetOnAxis(ap=dest_i[:, ti, k_:k_ + 1], axis=0)),
                               reads=[Ys, dest_i], writes=[yk_[k_]])
                    fw.op("dve", lambda: nc.vector.tensor_scalar(out=a_[:], in0=yk_[0][:], scalar1=wk[:, ti, 0:1], scalar2=None, op0=ALU.mult), reads=[yk_[0], wk], writes=[a_])
                    for k_ in range(1, 4):
                        fw.op("dve", lambda: nc.vector.scalar_tensor_tensor(out=a_[:], in0=yk_[k_][:], scalar=wk[:, ti, k_:k_ + 1], in1=a_[:], op0=ALU.mult, op1=ALU.add),
                              reads=[yk_[k_], wk, a_], writes=[a_])
                    for c in range(8):
                        fw.op("pe", lambda: nc.tensor.transpose(out=p_[:, c, :], in_=a_[:, c * 128:(c + 1) * 128], identity=ident_f[:]), reads=[a_, ident_f], writes=[p_], chain=True)
                    for c in range(8):
                        fw.op("dve", lambda: nc.vector.scalar_tensor_tensor(out=x_[:, c, tsl], in0=p_[:, c, :], scalar=modT[:, l, 40 + c, j:j + 1], in1=x_[:, c, tsl],
                                                                          op0=ALU.mult, op1=ALU.add), reads=[p_, modT, x_], writes=[x_])
                if not last:
                    fw.dma("sp", lambda: nc.sync.dma_start(out=xs[:, :, t0:t0 + n], in_=x_[:, :, :n]), reads=[x_], writes=[xs])
                elif not isctx:
                    fw.op("act", lambda: nc.scalar.activation(out=sq[:, :, :n], in_=x_[:, :, :n], func=AF.Square), reads=[x_], writes=[sq])
                    for c in range(8):
                        fw.op("pe", lambda: nc.tensor.matmul(pss[:, :n], lhsT=ones_b[:], rhs=sq[:, c, :n], start=(c == 0), stop=(c == 7)), reads=[sq, ones_b], writes=[pss], chain=True)
                    fw.op("act", lambda: nc.scalar.activation(out=rstd[:, :n], in_=pss[:, :n], func=AF.Sqrt, bias=EPS, scale=1.0 / D), reads=[pss], writes=[rstd])
                    fw.op("dve", lambda: nc.vector.reciprocal(out=rstd[:, :n], in_=rstd[:, :n]), reads=[rstd], writes=[rstd])
                    for c in range(8):
                        fw.op("dve", lambda: nc.vector.scalar_tensor_tensor(out=xo[:, c, :n], in0=x_[:, c, :n], scalar=gfin[:, c:c + 1], in1=rstd[:, :n], op0=ALU.mult, op1=ALU.mult),
                              reads=[x_, gfin, rstd], writes=[xo])
                    for tt in range(n // 128):
                        o_ = osb[tt % 2]
                        for c in range(8):
                            fw.op("pe", lambda: nc.tensor.transpose(out=pO[:, c, :], in_=xo[:, c, tt * 128:(tt + 1) * 128], identity=ident_f[:]), reads=[xo, ident_f], writes=[pO], chain=True)
                        fw.op("act", lambda: nc.scalar.copy(out=o_[:], in_=pO[:].rearrange("p c d -> p (c d)")), reads=[pO], writes=[o_])
                        r0 = t0 + tt * 128
                        fw.dma("sp", lambda: nc.sync.dma_start(out=OUT[r0:r0 + 128, :], in_=o_[:]), reads=[o_], writes=[OUT])

    if "nopre" not in debug:
        prephase()
        fw.barrier()
    phase0()
    fw.barrier()
    for l in range(nlayers):
        with_ctx = l < DEPTH - 1
        phaseA(l)
        fw.barrier()
        if "A" in debug:
            break
        if "noB" not in debug:
            phaseB(l, with_ctx)
            fw.barrier()
        if "noD" not in debug:
            phaseD(l, with_ctx)
            fw.barrier()
        if "noC" not in debug:
            phaseC(l, with_ctx)
            fw.barrier()
        if "BD" in debug:
            break
        phaseE(l, with_ctx)
        fw.barrier()
        if "E" in debug:
            break
        phaseF(l)
        fw.barrier()
        phaseG(l, with_ctx, l == nlayers - 1 and "G" not in debug)
        fw.barrier()

    outs_to_wait = []
    if debug:
        def dump(name, src, shape, dt):
            o = dout("dbg_" + name, shape, dt)
            fw.dma("sp", lambda: nc.sync.dma_start(out=o[:], in_=src[:]), reads=[src], writes=[o])
            outs_to_wait.append(o)
        dump("yT", yT, [8, 128, NTOK], BF16)
        if "moe" in debug:
            dump("Xs", Xs, [NSLOT + 128, D], BF16)
            dump("Ys", Ys, [NSLOT, D], F32)
            od = dout("dbg_dest", [128, NTILE * 4], U32)
            fw.dma("sp", lambda: nc.sync.dma_start(out=od[:], in_=dest_i[:].rearrange("p t k -> p (t k)")), reads=[dest_i], writes=[od])
            outs_to_wait.append(od)
            ow = dout("dbg_wk", [128, NTILE * 4], F32)
            fw.dma("sp", lambda: nc.sync.dma_start(out=ow[:], in_=wk[:].rearrange("p t k -> p (t k)")), reads=[wk], writes=[ow])
            outs_to_wait.append(ow)
        dump("xs", xs, [128, 8, NTOK], F32)
        dump("qa", qa, [3, 128, NTOK], BF16)
        dump("ka", ka, [3, 128, NTOK], BF16)
        dump("va", va, [NTOK, 390], BF16)
        dump("u5", u5, [2, 128, NTOK], F32)
        dump("qs", qs, [3, 128, NTOK], BF16)
        dump("ks", ks, [3, 128, NTOK], BF16)
        dump("vs", vs, [NTOK, 130], BF16)
        om = dout("dbg_modT", [128, DEPTH * 48 * 2], F32)
        fw.dma("sp", lambda: nc.sync.dma_start(out=om[:], in_=modT[:].rearrange("p l c j -> p (l c j)")), reads=[modT], writes=[om])
        outs_to_wait.append(om)
    fw.finish(outs_to_wait + [OUT])
    print("insts", fw.n_inst, "waits", fw.n_wait)
    es.close()
    return nc, hc


def make_inputs(inputs, b, hc):
    m = {}
    m["xin"] = np.ascontiguousarray(np.concatenate([inputs["x"][b], inputs["ctx"][b]], axis=0))
    m["cvec"] = np.ascontiguousarray(np.stack([inputs["c"][b], inputs["c_ctx"]], axis=0))
    for k in ("w_mod", "b_mod", "g_mix", "w_in", "w_out", "na_rpb", "s5_a_re", "s5_a_im", "s5_log_step", "s5_b_re", "s5_b_im",
              "s5_c_re", "s5_c_im", "s5_d", "s5_w_glu", "s5_b_glu", "sw_sinks", "g_ffn", "w_router", "b_router", "w_gate_up",
              "b_gate_up", "w_down", "b_down", "g_final"):
        m[k] = inputs[k]
    for k, v in hc.items():
        m[k] = v
    return m


def kernel(**inputs):
    nc, hc = build()
    in_maps = [make_inputs(inputs, b % 4, hc) for b in range(8)]
    res = run_bass_kernel_spmd(nc, in_maps, core_ids=list(range(8)))
    return np.stack([res.results[b]["out"] for b in range(4)], axis=0)
```

```python
import numpy as np
import ml_dtypes
from contextlib import ExitStack
import concourse.bass as bass
import concourse.mybir as mybir
from concourse.bass_utils import run_bass_kernel_spmd

F32 = mybir.dt.float32
BF16 = mybir.dt.bfloat16
I32 = mybir.dt.int32
U32 = mybir.dt.uint32
AF = mybir.ActivationFunctionType
ALU = mybir.AluOpType
AX = mybir.AxisListType

D = 1024
SEQ = 8192
CTXL = 256
NTOK = SEQ + CTXL
DEPTH = 4
NE = 32
BLK = 896
NB = -(-(NTOK * 4 + NE * (BLK - 1)) // BLK)
NSLOT = NB * BLK
EPS = 1e-6
NEG = -8.0e30


class Buf:
    __slots__ = ("w", "r")

    def __init__(self):
        self.w = {}
        self.r = {}


class T:
    def __init__(self, t, name):
        self.t = t
        self.name = name
        self.b = Buf()

    def __getitem__(self, idx):
        return self.t[idx]


class FW:
    NDMA = 32

    def __init__(self, nc, es):
        self.nc = nc
        self.es = es
        self.engs = {"pe": nc.tensor, "act": nc.scalar, "dve": nc.vector, "pool": nc.gpsimd, "sp": nc.sync}
        self.sem = {}
        self.cnt = {}
        self.sems = {}
        for k in self.engs:
            s = es.enter_context(nc.semaphore("s_" + k))
            self.sem[k] = s
            self.sems["e_" + k] = s
            self.cnt[k] = 0
        self.dma_sems = []
        self.ring = {}
        base = 0
        for q, n in (("sp", 32), ("act", 16), ("pool", 44)):
            self.ring[q] = [base, n, 0]
            base += n
        for i in range(base):
            s = es.enter_context(nc.semaphore("d%d" % i))
            self.sems["d%d" % i] = s
            self.dma_sems.append(["d%d" % i, 0])
        self.waited = {k: {} for k in self.engs}
        self.n_inst = 0
        self.n_wait = 0
        self.uid = 0

    def sb(self, name, shape, dt, es=None):
        self.uid += 1
        t = (es or self.es).enter_context(self.nc.sbuf_tensor("%s_%d" % (name, self.uid), list(shape), dt))
        return T(t, name)

    def ps(self, name, shape, dt, es=None):
        self.uid += 1
        t = (es or self.es).enter_context(self.nc.psum_tensor("%s_%d" % (name, self.uid), list(shape), dt))
        return T(t, name)

    def dram(self, name, shape, dt, kind="Internal"):
        t = self.nc.dram_tensor(name, list(shape), dt, kind=kind)
        return T(t.ap(), name)

    def _deps(self, eng, reads, writes, skip_own=False):
        deps = {}

        def add(ev):
            s, v = ev
            if deps.get(s, 0) < v:
                deps[s] = v
        for b in reads:
            for ev in b.b.w.items():
                add(ev)
        for b in writes:
            for ev in b.b.w.items():
                add(ev)
            for ev in b.b.r.items():
                add(ev)
        own = "e_" + eng
        for s, v in deps.items():
            if skip_own and s == own:
                continue
            if self.waited[eng].get(s, 0) >= v:
                continue
            self.engs[eng].wait_ge(self.sems[s], v)
            self.waited[eng][s] = v
            self.n_wait += 1

    def _commit(self, ev, reads, writes):
        s, v = ev
        for b in reads:
            if b.b.r.get(s, 0) < v:
                b.b.r[s] = v
        for b in writes:
            if b.b.w.get(s, 0) < v:
                b.b.w[s] = v
            b.b.r = {}

    def op(self, eng, fn, reads=(), writes=(), chain=False):
        self._deps(eng, reads, writes, skip_own=chain)
        inst = fn()
        self.cnt[eng] += 1
        inst.then_inc(self.sem[eng], 1)
        self._commit(("e_" + eng, self.cnt[eng]), reads, writes)
        self.n_inst += 1
        return inst

    def dma(self, q, fn, reads=(), writes=()):
        rg = self.ring[q]
        slot = self.dma_sems[rg[0] + rg[2]]
        rg[2] = (rg[2] + 1) % rg[1]
        sid, used = slot
        if used > 0 and self.waited[q].get(sid, 0) < used * 16:
            self.engs[q].wait_ge(self.sems[sid], used * 16)
            self.waited[q][sid] = used * 16
        self._deps(q, reads, writes)
        inst = fn()
        slot[1] = used + 1
        inst.then_inc(self.sems[sid], 16)
        self._commit((sid, (used + 1) * 16), reads, writes)
        self.n_inst += 1
        return inst

    def barrier(self):
        evs = {}
        for k in self.engs:
            if self.cnt[k] > 0:
                evs["e_" + k] = self.cnt[k]
        for sid, used in self.dma_sems:
            if used > 0:
                evs[sid] = used * 16
        for k in self.engs:
            for s, v in evs.items():
                if s == "e_" + k and k in ("sp",):
                    continue
                if self.waited[k].get(s, 0) >= v:
                    continue
                self.engs[k].wait_ge(self.sems[s], v)
                self.waited[k][s] = v
                self.n_wait += 1

    def finish(self, outs, eng="sp"):
        for b in outs:
            for s, v in b.b.w.items():
                self.engs[eng].wait_ge(self.sems[s], v)


def host_consts():
    c = {}
    c["ident_f"] = np.eye(128, dtype=np.float32)
    c["ident_b"] = np.eye(128).astype(ml_dtypes.bfloat16)
    c["ones_b"] = np.ones((128, 128)).astype(ml_dtypes.bfloat16)
    c["ones_f"] = np.ones((128, 128), dtype=np.float32)
    t = np.arange(SEQ)
    row = (t // 64).astype(np.float32)
    col = (t % 64).astype(np.float32)
    inv = (10000.0 ** (-np.arange(16, dtype=np.float32) / 16)).astype(np.float32)
    ar = row[:, None] * inv
    ac = col[:, None] * inv
    ang = np.concatenate([ar, ar, ac, ac], axis=-1)
    cos = np.cos(ang).astype(np.float32).T
    sin = np.sin(ang).astype(np.float32).T
    sign = np.where((np.arange(64) % 32) < 16, -1.0, 1.0).astype(np.float32)[:, None]
    c["rope_c"] = np.ascontiguousarray(np.concatenate([cos, cos], 0))
    c["rope_s"] = np.ascontiguousarray(np.concatenate([sin * sign, sin * sign], 0))
    k = np.arange(128)[:, None]
    q = np.arange(128)[None, :]
    c["mask_l"] = np.where(k >= q, 0.0, NEG).astype(ml_dtypes.bfloat16)
    c["mask_u"] = np.where(k <= q, 0.0, NEG).astype(ml_dtypes.bfloat16)
    qc = np.arange(64)[None, :]
    kc = np.arange(64)[:, None]
    ws = np.clip(qc - 8, 0, 48)
    valid = (kc >= ws) & (kc < ws + 16)
    v2 = np.concatenate([valid, valid], 0)
    c["na_valid8"] = (v2 * 8.0).astype(np.float32)
    c["na_pen"] = np.where(v2, 0.0, NEG).astype(np.float32)
    c["iota32"] = np.broadcast_to(np.arange(32, dtype=np.float32), (128, 32)).copy()
    c["iota512"] = np.broadcast_to(np.arange(512, dtype=np.float32), (128, 512)).copy()
    c["iotablk"] = np.broadcast_to((np.arange(NB) * BLK).astype(np.float32), (128, NB)).copy()
    c["iota_p"] = np.arange(128, dtype=np.float32).reshape(128, 1)
    c["ustrict"] = (np.arange(128)[:, None] < np.arange(128)[None, :]).astype(np.float32)
    return c


CONST_SPECS = None


def groups():
    g = [(i * 512, 512, False) for i in range(SEQ // 512)]
    g.append((SEQ, CTXL, True))
    return g


def build(nlayers=DEPTH, debug=()):
    nc = bass.Bass("TRN2", target_bir_lowering=False)
    es = ExitStack()
    fw = FW(nc, es)

    def din(name, shape, dt=F32):
        return T(nc.dram_tensor(name, list(shape), dt, kind="ExternalInput").ap(), name)

    def dout(name, shape, dt=F32):
        return T(nc.dram_tensor(name, list(shape), dt, kind="ExternalOutput").ap(), name)

    I = {}
    I["xin"] = din("xin", [NTOK, D])
    I["cvec"] = din("cvec", [2, D])
    specs = {"w_mod": [DEPTH, D, 6 * D], "b_mod": [DEPTH, 6 * D], "g_mix": [DEPTH, D], "w_in": [DEPTH, D, 2048],
             "w_out": [DEPTH, D, D], "na_rpb": [DEPTH, 6, 15, 31], "s5_a_re": [DEPTH, 2, 16, 64], "s5_a_im": [DEPTH, 2, 16, 64],
             "s5_log_step": [DEPTH, 2, 16], "s5_b_re": [DEPTH, 2, 16, 64, 16], "s5_b_im": [DEPTH, 2, 16, 64, 16],
             "s5_c_re": [DEPTH, 2, 16, 16, 64], "s5_c_im": [DEPTH, 2, 16, 16, 64], "s5_d": [DEPTH, 256],
             "s5_w_glu": [DEPTH, 256, 256], "s5_b_glu": [DEPTH, 256], "sw_sinks": [DEPTH, 6], "g_ffn": [DEPTH, D],
             "w_router": [DEPTH, D, NE], "b_router": [DEPTH, NE], "w_gate_up": [DEPTH, NE, D, 2 * D],
             "b_gate_up": [DEPTH, NE, 2 * D], "w_down": [DEPTH, NE, D, D], "b_down": [DEPTH, NE, D], "g_final": [D]}
    for k, s in specs.items():
        I[k] = din(k, s)
    hc = host_consts()
    for k, v in hc.items():
        I[k] = din(k, list(v.shape), BF16 if v.dtype == ml_dtypes.bfloat16 else F32)
    OUT = dout("out", [SEQ, D])
    DBG = {}

    xs = fw.dram("xs", [128, 8, NTOK], F32)
    qa = fw.dram("qa", [3, 128, NTOK], BF16)
    ka = fw.dram("ka", [3, 128, NTOK], BF16)
    va = fw.dram("va", [NTOK, 6 * 65], BF16)
    u5 = fw.dram("u5", [2, 128, NTOK], F32)
    qs = fw.dram("qs", [3, 128, NTOK], BF16)
    ks = fw.dram("ks", [3, 128, NTOK], BF16)
    vs = fw.dram("vs", [NTOK, 2 * 65], BF16)
    yT = fw.dram("yT", [8, 128, NTOK], BF16)

    ident_f = fw.sb("ident_f", [128, 128], F32)
    ident_b = fw.sb("ident_b", [128, 128], BF16)
    ones_b = fw.sb("ones_b", [128, 128], BF16)
    ones_f = fw.sb("ones_f", [128, 128], F32)
    for tl, nm in ((ident_f, "ident_f"), (ident_b, "ident_b"), (ones_b, "ones_b"), (ones_f, "ones_f")):
        fw.dma("sp", lambda tl=tl, nm=nm: nc.sync.dma_start(out=tl[:], in_=I[nm][:]), reads=[I[nm]], writes=[tl])
    modT = fw.sb("modT", [128, DEPTH, 48, 2], F32)
    gmix = fw.sb("gmix", [128, DEPTH, 8], F32)
    gffn = fw.sb("gffn", [128, DEPTH, 8], F32)
    gfin = fw.sb("gfin", [128, 8], F32)
    with nc.allow_non_contiguous_dma(reason="tiny per-feature vectors"):
        fw.dma("sp", lambda: nc.sync.dma_start(out=gmix[:], in_=I["g_mix"][:].rearrange("l (c p) -> p l c", p=128)), reads=[I["g_mix"]], writes=[gmix])
        fw.dma("sp", lambda: nc.sync.dma_start(out=gffn[:], in_=I["g_ffn"][:].rearrange("l (c p) -> p l c", p=128)), reads=[I["g_ffn"]], writes=[gffn])
        fw.dma("sp", lambda: nc.sync.dma_start(out=gfin[:], in_=I["g_final"][:].rearrange("(c p) -> p c", p=128)), reads=[I["g_final"]], writes=[gfin])

    def phase0():
        with ExitStack() as pes:
            condT = fw.sb("condT", [128, 2, 8], F32, pes)
            bmT = fw.sb("bmT", [128, DEPTH, 48], F32, pes)
            with nc.allow_non_contiguous_dma(reason="tiny"):
                for jj in range(2):
                    fw.dma("sp", lambda: nc.sync.dma_start(out=condT[:, jj, :], in_=I["cvec"][jj, :].rearrange("(c p) -> p c", p=128)), reads=[I["cvec"]], writes=[condT])
                fw.dma("sp", lambda: nc.sync.dma_start(out=bmT[:], in_=I["b_mod"][:].rearrange("l (c p) -> p l c", p=128)), reads=[I["b_mod"]], writes=[bmT])
            fw.op("act", lambda: nc.scalar.activation(out=condT[:], in_=condT[:], func=AF.Silu), reads=[condT], writes=[condT])
            wbuf = [fw.sb("wmod%d" % i, [128, 8, 768], F32, pes) for i in range(2)]
            pm = [fw.ps("pmod%d" % i, [128, 6, 2], F32, pes) for i in range(2)]
            it = 0
            for l in range(nlayers):
                for piece in range(8):
                    wb = wbuf[it % 2]
                    pp = pm[it % 2]
                    it += 1
                    q = "sp" if piece % 2 == 0 else "act"
                    eng = nc.sync if piece % 2 == 0 else nc.scalar
                    fw.dma(q, lambda: eng.dma_start(out=wb[:], in_=I["w_mod"][l, :, piece * 768:(piece + 1) * 768].rearrange("(c p) n -> p c n", p=128)),
                           reads=[I["w_mod"]], writes=[wb])
                    for cc in range(6):
                        for kc in range(8):
                            fw.op("pe", lambda: nc.tensor.matmul(pp[:, cc, :], lhsT=wb[:, kc, cc * 128:(cc + 1) * 128], rhs=condT[:, :, kc],
                                                               start=(kc == 0), stop=(kc == 7)),
                                  reads=[wb, condT], writes=[pp], chain=True)
                    for j in range(2):
                        fw.op("dve", lambda: nc.vector.tensor_tensor(out=modT[:, l, piece * 6:(piece + 1) * 6, j], in0=pp[:, :, j],
                                                                   in1=bmT[:, l, piece * 6:(piece + 1) * 6], op=ALU.add),
                              reads=[pp, bmT], writes=[modT])
            for l in range(nlayers):
                for i in (1, 4):
                    fw.op("dve", lambda: nc.vector.tensor_scalar_add(out=modT[:, l, i * 8:(i + 1) * 8, :], in0=modT[:, l, i * 8:(i + 1) * 8, :], scalar1=1.0),
                          reads=[modT], writes=[modT])

    def prephase():
        with ExitStack() as pes:
            xt = [fw.sb("xt%d" % i, [128, D], F32, pes) for i in range(2)]
            xo = [fw.sb("xo%d" % i, [128, 8, 128], F32, pes) for i in range(2)]
            pt = [fw.ps("ptr%d" % i, [128, 8, 128], F32, pes) for i in range(2)]
            for ti in range(NTOK // 128):
                a = xt[ti % 2]; o = xo[ti % 2]; p = pt[ti % 2]
                fw.dma("sp", lambda: nc.sync.dma_start(out=a[:], in_=I["xin"][ti * 128:(ti + 1) * 128, :]), reads=[I["xin"]], writes=[a])
                for c in range(8):
                    fw.op("pe", lambda: nc.tensor.transpose(out=p[:, c, :], in_=a[:, c * 128:(c + 1) * 128], identity=ident_f[:]),
                          reads=[a, ident_f], writes=[p], chain=True)
                if ti % 2 == 0:
                    fw.op("act", lambda: nc.scalar.copy(out=o[:], in_=p[:]), reads=[p], writes=[o])
                else:
                    fw.op("dve", lambda: nc.vector.tensor_copy(out=o[:], in_=p[:]), reads=[p], writes=[o])
                fw.dma("act", lambda: nc.scalar.dma_start(out=xs[:, :, ti * 128:(ti + 1) * 128], in_=o[:]), reads=[o], writes=[xs])

    def norm_mod(pes_bufs, xg, n, gvec, l, ishift, iscale, j, out_bf=None, out_f=None):
        sq, pss, rstd, GS = pes_bufs
        fw.op("act", lambda: nc.scalar.activation(out=sq[:, :, :n], in_=xg[:, :, :n], func=AF.Square), reads=[xg], writes=[sq])
        for c in range(8):
            fw.op("pe", lambda: nc.tensor.matmul(pss[:, :n], lhsT=ones_b[:], rhs=sq[:, c, :n], start=(c == 0), stop=(c == 7)),
                  reads=[sq, ones_b], writes=[pss], chain=True)
        fw.op("act", lambda: nc.scalar.activation(out=rstd[:, :n], in_=pss[:, :n], func=AF.Sqrt, bias=EPS, scale=1.0 / D), reads=[pss], writes=[rstd])
        fw.op("dve", lambda: nc.vector.reciprocal(out=rstd[:, :n], in_=rstd[:, :n]), reads=[rstd], writes=[rstd])
        fw.op("dve", lambda: nc.vector.tensor_tensor(out=GS[:, 0, :], in0=gvec, in1=modT[:, l, iscale * 8:(iscale + 1) * 8, j], op=ALU.mult),
              reads=[modT, gmix, gffn], writes=[GS])
        for c in range(8):
            tgt = out_f if out_f is not None else sq
            if out_f is not None:
                fw.op("dve", lambda: nc.vector.scalar_tensor_tensor(out=out_f[:, c, :n], in0=xg[:, c, :n], scalar=GS[:, 0, c:c + 1], in1=rstd[:, :n],
                                                                  op0=ALU.mult, op1=ALU.mult), reads=[xg, GS, rstd], writes=[out_f])
                fw.op("dve", lambda: nc.vector.tensor_scalar_add(out=out_f[:, c, :n], in0=out_f[:, c, :n], scalar1=modT[:, l, ishift * 8 + c, j:j + 1]),
                      reads=[out_f, modT], writes=[out_f])
                if out_bf is not None:
                    fw.op("act", lambda: nc.scalar.copy(out=out_bf[:, c, :n], in_=out_f[:, c, :n]), reads=[out_f], writes=[out_bf])
            else:
                fw.op("dve", lambda: nc.vector.scalar_tensor_tensor(out=xg[:, c, :n], in0=xg[:, c, :n], scalar=GS[:, 0, c:c + 1], in1=rstd[:, :n],
                                                                  op0=ALU.mult, op1=ALU.mult), reads=[xg, GS, rstd], writes=[xg])
                fw.op("act", lambda: nc.scalar.activation(out=out_bf[:, c, :n], in_=xg[:, c, :n], func=AF.Identity,
                                                        bias=modT[:, l, ishift * 8 + c, j:j + 1], scale=1.0), reads=[xg, modT], writes=[out_bf])

    NCOLF = 20 * 128

    def phaseA(l):
        with ExitStack() as pes:
            w2 = fw.sb("w2", [128, 8, NCOLF], BF16, pes)
            wv = fw.sb("wv", [128, 8, 512], BF16, pes)
            win = I["w_in"]

            def wl(dst, dlo, slo, n, q="pool"):
                fw.dma("pool", lambda: nc.gpsimd.dma_start(out=dst[:, :, dlo:dlo + n], in_=win[l, :, slo:slo + n].rearrange("(c p) n -> p c n", p=128)),
                       reads=[win], writes=[dst])
            wl(w2, 0, 0, 384)
            wl(w2, 384, 384, 384)
            wl(w2, 768, 1152, 256)
            wl(w2, 1024, 1408, 384)
            for ti, (h0, h1) in enumerate(((0, 0), (0, 1), (1, 1))):
                wl(w2, 1792 + ti * 128, 1792 + h0 * 64, 64)
                wl(w2, 1792 + ti * 128 + 64, 1792 + h1 * 64, 64)
            wl(wv, 0, 768, 384)
            wl(wv, 384, 1920, 128)
            for (src, dst, nh) in ((1024, 1408, 6), (1792, 2176, 6)):
                sv = w2[:, :, src:src + nh * 64].rearrange("p c (h a j f) -> p c h a j f", h=nh, a=2, j=2, f=16)
                dv = w2[:, :, dst:dst + nh * 64].rearrange("p c (h a j f) -> p c h a j f", h=nh, a=2, j=2, f=16)
                for c in range(8):
                    for jj in range(2):
                        fw.op("pool", lambda: nc.gpsimd.tensor_copy(out=dv[:, c, :, :, jj, :], in_=sv[:, c, :, :, 1 - jj, :]), reads=[w2], writes=[w2])
            xg = [fw.sb("xg%d" % i, [128, 8, 512], F32, pes) for i in range(2)]
            hT = [fw.sb("hT%d" % i, [128, 8, 512], BF16, pes) for i in range(2)]
            sq = fw.sb("sq", [128, 8, 512], BF16, pes)
            rstd = fw.sb("rstd", [128, 512], F32, pes)
            GS = fw.sb("GS", [128, 1, 8], F32, pes)
            pss = fw.ps("pss", [128, 512], F32, pes)
            pp = [fw.ps("ppA%d" % i, [128, 512], F32, pes) for i in range(4)]
            pv = [fw.ps("ppV%d" % i, [128, 512], F32, pes) for i in range(2)]
            ob = [fw.sb("obA%d" % i, [128, 512], BF16, pes) for i in range(4)]
            of = [fw.sb("ofA%d" % i, [128, 512], F32, pes) for i in range(2)]
            t1 = [fw.sb("t1A%d" % i, [128, 512], F32, pes) for i in range(2)]
            t2 = [fw.sb("t2A%d" % i, [128, 512], F32, pes) for i in range(2)]
            rc = [fw.sb("rc%d" % i, [128, 512], F32, pes) for i in range(2)]
            rs_ = [fw.sb("rs%d" % i, [128, 512], F32, pes) for i in range(2)]
            vst = [fw.sb("vst%d" % i, [128, 8, 65], BF16, pes) for i in range(2)]
            for v in vst:
                fw.op("dve", lambda: nc.vector.memset(v[:], 1.0), writes=[v])
            k = 0
            ko = 0
            for gi, (t0, n, isctx) in enumerate(groups()):
                j = 1 if isctx else 0
                x_ = xg[gi % 2]; h_ = hT[gi % 2]
                fw.dma("sp", lambda: nc.sync.dma_start(out=x_[:, :, :n], in_=xs[:, :, t0:t0 + n]), reads=[xs], writes=[x_])
                if not isctx:
                    r_c = rc[gi % 2]; r_s = rs_[gi % 2]
                    fw.dma("act", lambda: nc.scalar.dma_start(out=r_c[:], in_=I["rope_c"][:, t0:t0 + n]), reads=[I["rope_c"]], writes=[r_c])
                    fw.dma("act", lambda: nc.scalar.dma_start(out=r_s[:], in_=I["rope_s"][:, t0:t0 + n]), reads=[I["rope_s"]], writes=[r_s])
                norm_mod((sq, pss, rstd, GS), x_, n, gmix[:, l, :], l, 0, 1, j, out_bf=h_)

                def proj(colblk):
                    nonlocal k
                    p = pp[k % 4]; k += 1
                    for kc in range(8):
                        fw.op("pe", lambda: nc.tensor.matmul(p[:, :n], lhsT=w2[:, kc, colblk * 128:(colblk + 1) * 128], rhs=h_[:, kc, :n],
                                                           start=(kc == 0), stop=(kc == 7)), reads=[w2, h_], writes=[p], chain=True)
                    return p
                for blk in range(8):
                    p = proj(blk)
                    if blk < 6:
                        o = ob[ko % 4]; ko += 1
                        if blk % 2 == 0:
                            fw.op("act", lambda: nc.scalar.copy(out=o[:, :n], in_=p[:, :n]), reads=[p], writes=[o])
                        else:
                            fw.op("dve", lambda: nc.vector.tensor_copy(out=o[:, :n], in_=p[:, :n]), reads=[p], writes=[o])
                        dst = qa if blk < 3 else ka
                        fw.dma("sp", lambda: nc.sync.dma_start(out=dst[blk % 3, :, t0:t0 + n], in_=o[:, :n]), reads=[o], writes=[dst])
                    else:
                        o = of[blk % 2]
                        fw.op("act", lambda: nc.scalar.copy(out=o[:, :n], in_=p[:, :n]), reads=[p], writes=[o])
                        fw.dma("sp", lambda: nc.sync.dma_start(out=u5[blk - 6, :, t0:t0 + n], in_=o[:, :n]), reads=[o], writes=[u5])
                for which, base, dst in ((0, 8, qs), (1, 14, ks)):
                    for ti in range(3):
                        p = proj(base + ti)
                        o = ob[ko % 4]; ko += 1
                        if isctx:
                            fw.op("act", lambda: nc.scalar.copy(out=o[:, :n], in_=p[:, :n]), reads=[p], writes=[o])
                        else:
                            pr = proj(base + 3 + ti)
                            a = t1[ti % 2]; b = t2[ti % 2]
                            fw.op("dve", lambda: nc.vector.tensor_tensor(out=a[:, :n], in0=p[:, :n], in1=r_c[:, :n], op=ALU.mult), reads=[p, r_c], writes=[a])
                            fw.op("dve", lambda: nc.vector.tensor_tensor(out=b[:, :n], in0=pr[:, :n], in1=r_s[:, :n], op=ALU.mult), reads=[pr, r_s], writes=[b])
                            fw.op("pool", lambda: nc.gpsimd.tensor_tensor(out=o[:, :n], in0=a[:, :n], in1=b[:, :n], op=ALU.add), reads=[a, b], writes=[o])
                        fw.dma("sp", lambda: nc.sync.dma_start(out=dst[ti, :, t0:t0 + n], in_=o[:, :n]), reads=[o], writes=[dst])
                for tt in range(n // 128):
                    p = pv[tt % 2]; v = vst[tt % 2]
                    for kc in range(8):
                        fw.op("pe", lambda: nc.tensor.matmul(p[:, :], lhsT=h_[:, kc, tt * 128:(tt + 1) * 128], rhs=wv[:, kc, :],
                                                           start=(kc == 0), stop=(kc == 7)), reads=[wv, h_], writes=[p], chain=True)
                    fw.op("act", lambda: nc.scalar.copy(out=v[:, :, 0:64], in_=p[:, :].rearrange("p (h d) -> p h d", d=64)), reads=[p], writes=[v])
                    r0 = t0 + tt * 128
                    fw.dma("act", lambda: nc.scalar.dma_start(out=va[r0:r0 + 128, :].rearrange("t (h d) -> t h d", d=65), in_=v[:, 0:6, :]), reads=[v], writes=[va])
                    fw.dma("act", lambda: nc.scalar.dma_start(out=vs[r0:r0 + 128, :].rearrange("t (h d) -> t h d", d=65), in_=v[:, 6:8, :]), reads=[v], writes=[vs])


    def attn_norm1(pes_t, po, n, h, sink=None):
        rden, osb, ysb, pbc = pes_t
        if sink is not None:
            fw.op("dve", lambda: nc.vector.tensor_scalar(out=rden[64:65, :n], in0=po[64:65, :n], scalar1=sink[64:65, h:h + 1], scalar2=None, op0=ALU.add),
                  reads=[po, sink], writes=[rden])
            fw.op("dve", lambda: nc.vector.reciprocal(out=rden[64:65, :n], in_=rden[64:65, :n]), reads=[rden], writes=[rden])
        else:
            fw.op("dve", lambda: nc.vector.reciprocal(out=rden[64:65, :n], in_=po[64:65, :n]), reads=[po], writes=[rden])
        fw.op("act", lambda: nc.scalar.copy(out=osb[0:64, :n], in_=po[0:64, :n]), reads=[po], writes=[osb])

    def attn_norm2(pes_t, n, t0, chtile, base):
        rden, osb, ysb, pbc = pes_t
        fw.op("pe", lambda: nc.tensor.matmul(pbc[0:64, :n], lhsT=ones_f[64:65, 0:64], rhs=rden[64:65, :n], start=True, stop=True),
              reads=[rden, ones_f], writes=[pbc])
        fw.op("dve", lambda: nc.vector.tensor_tensor(out=ysb[0:64, :n], in0=osb[0:64, :n], in1=pbc[0:64, :n], op=ALU.mult), reads=[osb, pbc], writes=[ysb])
        fw.dma("sp", lambda: nc.sync.dma_start(out=yT[chtile, base:base + 64, t0:t0 + n], in_=ysb[0:64, :n]), reads=[ysb], writes=[yT])

    class Deferred:
        def __init__(self):
            self.p = None

        def push(self, *a):
            self.flush()
            self.p = a

        def flush(self):
            if self.p is not None:
                attn_norm2(*self.p)
                self.p = None

    rp = fw.dram("rp", [1, 3072], F32)

    def phaseB(l, with_ctx):
        with ExitStack() as pes:
            z = fw.sb("zrp", [1, 3072], F32, pes)
            fw.op("dve", lambda: nc.vector.memset(z[:], 0.0), writes=[z])
            fw.dma("sp", lambda: nc.sync.dma_start(out=z[0:1, 64:64 + 2790], in_=I["na_rpb"][l:l + 1].rearrange("o h a b -> o (h a b)")), reads=[I["na_rpb"]], writes=[z])
            fw.dma("sp", lambda: nc.sync.dma_start(out=rp[:], in_=z[:]), reads=[z], writes=[rp])
            BB = fw.sb("BB", [64, 6, 15, 64], F32, pes)
            for h in range(6):
                src = bass.AP(tensor=rp.t.tensor, offset=64 + h * 465 + 15 - 63, ap=[[1, 64], [31, 15], [1, 64]])
                fw.dma("sp", lambda: nc.sync.dma_start(out=BB[:, h, :, :], in_=src), reads=[rp], writes=[BB])
            pen = fw.sb("napen", [128, 64], F32, pes)
            fw.dma("sp", lambda: nc.sync.dma_start(out=pen[:], in_=I["na_pen"][:]), reads=[I["na_pen"]], writes=[pen])
            biasT = fw.sb("biasT", [128, 6, 14, 64], BF16, pes)
            with ExitStack() as pes2:
                pb = [fw.ps("pbias%d" % i, [128, 64], F32, pes2) for i in range(2)]
                it = 0
                for h in range(6):
                    for a in range(14):
                        p = pb[it % 2]; it += 1
                        fw.op("pe", lambda: nc.tensor.transpose(out=p[:, :], in_=BB[:, h, a:a + 2, :].rearrange("p a k -> p (a k)"), identity=ident_f[0:64, 0:64]),
                              reads=[BB, ident_f], writes=[p])
                        fw.op("dve", lambda: nc.vector.scalar_tensor_tensor(out=biasT[:, h, a, :], in0=p[:, ::-1], scalar=8.0, in1=pen[:], op0=ALU.mult, op1=ALU.add),
                              reads=[p, pen], writes=[biasT])
                fw.barrier()
            qT = fw.sb("qT", [128, NTOK], BF16, pes)
            kT = fw.sb("kT", [128, NTOK], BF16, pes)
            v0 = fw.sb("v0", [128, 66, 2, 65], BF16, pes)
            v1 = fw.sb("v1", [128, 63, 2, 65], BF16, pes)
            pc = fw.ps("pc", [128, 2, 512], F32, pes)
            pn = [fw.ps("pn%d" % i, [128, 4, 4, 64], F32, pes) for i in range(2)]
            po = fw.ps("po", [128, 512], F32, pes)
            pbc = fw.ps("pbc", [128, 512], F32, pes)
            PcT = fw.sb("PcT", [128, 2, 512], BF16, pes)
            PnT = [fw.sb("PnT%d" % i, [128, 4, 4, 64], BF16, pes) for i in range(2)]
            nt = [(fw.sb("rden%d" % i, [128, 512], F32, pes), fw.sb("osb%d" % i, [128, 512], F32, pes), fw.sb("ysb%d" % i, [128, 512], BF16, pes), pbc) for i in range(2)]
            dfr = Deferred(); po_i = 0
            ih = 0
            for jp in range(3):
                fw.dma("sp", lambda: nc.sync.dma_start(out=qT[:], in_=qa[jp]), reads=[qa], writes=[qT])
                fw.dma("act", lambda: nc.scalar.dma_start(out=kT[:], in_=ka[jp]), reads=[ka], writes=[kT])
                fw.dma("sp", lambda: nc.sync.dma_start(out=v0[:].rearrange("p t h d -> p t (h d)"),
                                                      in_=va[:, jp * 130:(jp + 1) * 130].rearrange("(t p) c -> p t c", p=128)), reads=[va], writes=[v0])
                fw.dma("act", lambda: nc.scalar.dma_start(out=v1[:].rearrange("p t h d -> p t (h d)"),
                                                        in_=va[64:64 + 63 * 128, jp * 130:(jp + 1) * 130].rearrange("(t p) c -> p t c", p=128)), reads=[va], writes=[v1])
                for hh in range(2):
                    h = 2 * jp + hh
                    b0 = 64 * hh
                    for G in range(16):
                        t0 = 512 * G
                        for ct in range(2):
                            fw.op("pe", lambda: nc.tensor.matmul(pc[:, ct, :], lhsT=kT[b0:b0 + 64, SEQ + 128 * ct:SEQ + 128 * ct + 128], rhs=qT[b0:b0 + 64, t0:t0 + 512],
                                                               start=True, stop=True), reads=[kT, qT], writes=[pc])
                        fw.op("act", lambda: nc.scalar.activation(out=PcT[:], in_=pc[:], func=AF.Exp, scale=0.125), reads=[pc], writes=[PcT])
                        for half in range(2):
                            p_ = pn[ih % 2]; P_ = PnT[ih % 2]; ih += 1
                            for rr in range(4):
                                r = 8 * G + 4 * half + rr
                                rs = min(max(r - 4, 0), 120)
                                for j in range(4):
                                    k0 = (rs + 2 * j) * 64
                                    a = rs + 2 * j - r + 7
                                    fw.op("pe", lambda: nc.tensor.matmul(p_[:, rr, j, :], lhsT=kT[b0:b0 + 64, k0:k0 + 128], rhs=qT[b0:b0 + 64, r * 64:r * 64 + 64],
                                                                       start=True, stop=False), reads=[kT, qT], writes=[p_], chain=True)
                                    fw.op("pe", lambda: nc.tensor.matmul(p_[:, rr, j, :], lhsT=ident_b[:, :], rhs=biasT[:, h, a, :],
                                                                       start=False, stop=True), reads=[biasT, ident_b], writes=[p_], chain=True)
                            if half == 0:
                                dfr.flush()
                            fw.op("act", lambda: nc.scalar.activation(out=P_[:], in_=p_[:], func=AF.Exp, scale=0.125), reads=[p_], writes=[P_])
                            for rr in range(4):
                                r = 8 * G + 4 * half + rr
                                rs = min(max(r - 4, 0), 120)
                                c0 = (4 * half + rr) * 64
                                for j in range(4):
                                    k0 = (rs + 2 * j) * 64
                                    vt = v0[:, k0 // 128, hh, :] if rs % 2 == 0 else v1[:, (k0 - 64) // 128, hh, :]
                                    fw.op("pe", lambda: nc.tensor.matmul(po[0:65, c0:c0 + 64], lhsT=vt, rhs=P_[:, rr, j, :], start=(j == 0), stop=False),
                                          reads=[v0, v1, P_], writes=[po], chain=True)
                                for ct in range(2):
                                    fw.op("pe", lambda: nc.tensor.matmul(po[0:65, c0:c0 + 64], lhsT=v0[:, 64 + ct, hh, :], rhs=PcT[:, ct, c0:c0 + 64], start=False, stop=(ct == 1)),
                                          reads=[v0, PcT], writes=[po], chain=True)
                        attn_norm1(nt[po_i % 2], po, 512, h)
                        dfr.push(nt[po_i % 2], 512, t0, jp, b0); po_i += 1
                    if with_ctx:
                        for ct in range(2):
                            fw.op("pe", lambda: nc.tensor.matmul(pc[:, ct, :CTXL], lhsT=kT[b0:b0 + 64, SEQ + 128 * ct:SEQ + 128 * ct + 128], rhs=qT[b0:b0 + 64, SEQ:SEQ + CTXL],
                                                               start=True, stop=True), reads=[kT, qT], writes=[pc])
                        fw.op("act", lambda: nc.scalar.activation(out=PcT[:, :, :CTXL], in_=pc[:, :, :CTXL], func=AF.Exp, scale=0.125), reads=[pc], writes=[PcT])
                        for ct in range(2):
                            fw.op("pe", lambda: nc.tensor.matmul(po[0:65, :CTXL], lhsT=v0[:, 64 + ct, hh, :], rhs=PcT[:, ct, :CTXL], start=(ct == 0), stop=(ct == 1)),
                                  reads=[v0, PcT], writes=[po], chain=True)
                        attn_norm1(nt[po_i % 2], po, CTXL, h)
                        dfr.push(nt[po_i % 2], CTXL, SEQ, jp, b0); po_i += 1

            dfr.flush()

    def phaseD(l, with_ctx):
        with ExitStack() as pes:
            sk = fw.sb("sinks", [128, 6], F32, pes)
            fw.dma("sp", lambda: nc.sync.dma_start(out=sk[64:65, :], in_=I["sw_sinks"][l:l + 1, :]), reads=[I["sw_sinks"]], writes=[sk])
            fw.op("act", lambda: nc.scalar.activation(out=sk[64:65, :], in_=sk[64:65, :], func=AF.Exp), reads=[sk], writes=[sk])
            ml = fw.sb("mask_l", [128, 128], BF16, pes)
            mu = fw.sb("mask_u", [128, 128], BF16, pes)
            fw.dma("sp", lambda: nc.sync.dma_start(out=ml[:], in_=I["mask_l"][:]), reads=[I["mask_l"]], writes=[ml])
            fw.dma("sp", lambda: nc.sync.dma_start(out=mu[:], in_=I["mask_u"][:]), reads=[I["mask_u"]], writes=[mu])
            qT = fw.sb("qTs", [128, NTOK], BF16, pes)
            kT = fw.sb("kTs", [128, NTOK], BF16, pes)
            vS = fw.sb("vS", [128, 66, 2, 65], BF16, pes)
            fw.dma("sp", lambda: nc.sync.dma_start(out=vS[:].rearrange("p t h d -> p t (h d)"), in_=vs[:, :].rearrange("(t p) c -> p t c", p=128)), reads=[vs], writes=[vS])
            pc = fw.ps("pcs", [128, 2, 512], F32, pes)
            pn = [fw.ps("pns%d" % i, [128, 3, 128], F32, pes) for i in range(2)]
            po = fw.ps("pos", [128, 512], F32, pes)
            pbc = fw.ps("pbcs", [128, 512], F32, pes)
            PcT = fw.sb("PcTs", [128, 2, 512], BF16, pes)
            PnT = [fw.sb("PnTs%d" % i, [128, 3, 128], BF16, pes) for i in range(2)]
            nt = [(fw.sb("rdens%d" % i, [128, 512], F32, pes), fw.sb("osbs%d" % i, [128, 512], F32, pes), fw.sb("ysbs%d" % i, [128, 512], BF16, pes), pbc) for i in range(2)]
            dfr = Deferred(); po_i = 0
            ih = 0
            NB = SEQ // 128
            for jp in range(3):
                fw.dma("sp", lambda: nc.sync.dma_start(out=qT[:], in_=qs[jp]), reads=[qs], writes=[qT])
                fw.dma("act", lambda: nc.scalar.dma_start(out=kT[:], in_=ks[jp]), reads=[ks], writes=[kT])
                for hh in range(2):
                    h = 2 * jp + hh
                    kvh = h // 3
                    b0 = 64 * hh
                    for G in range(16):
                        t0 = 512 * G
                        for ct in range(2):
                            fw.op("pe", lambda: nc.tensor.matmul(pc[:, ct, :], lhsT=kT[b0:b0 + 64, SEQ + 128 * ct:SEQ + 128 * ct + 128], rhs=qT[b0:b0 + 64, t0:t0 + 512],
                                                               start=True, stop=True), reads=[kT, qT], writes=[pc])
                        fw.op("act", lambda: nc.scalar.activation(out=PcT[:], in_=pc[:], func=AF.Exp, scale=0.125), reads=[pc], writes=[PcT])
                        for nb_ in range(4):
                            n = 4 * G + nb_
                            p_ = pn[ih % 2]; P_ = PnT[ih % 2]; ih += 1
                            kbs = [kb for kb in (n - 1, n, n + 1) if 0 <= kb < NB]
                            lo = kbs[0] - (n - 1)
                            hi = kbs[-1] - (n - 1) + 1
                            for kb in kbs:
                                ki = kb - (n - 1)
                                fw.op("pe", lambda: nc.tensor.matmul(p_[:, ki, :], lhsT=kT[b0:b0 + 64, kb * 128:kb * 128 + 128], rhs=qT[b0:b0 + 64, n * 128:n * 128 + 128],
                                                                   start=True, stop=(kb == n)), reads=[kT, qT], writes=[p_], chain=True)
                                if kb != n:
                                    mk = ml if kb < n else mu
                                    fw.op("pe", lambda: nc.tensor.matmul(p_[:, ki, :], lhsT=ident_b[:, :], rhs=mk[:, :], start=False, stop=True),
                                          reads=[mk, ident_b], writes=[p_], chain=True)
                            if nb_ == 0:
                                dfr.flush()
                            fw.op("act", lambda: nc.scalar.activation(out=P_[:, lo:hi, :], in_=p_[:, lo:hi, :], func=AF.Exp, scale=0.125), reads=[p_], writes=[P_])
                            c0 = nb_ * 128
                            for kb in kbs:
                                ki = kb - (n - 1)
                                fw.op("pe", lambda: nc.tensor.matmul(po[0:65, c0:c0 + 128], lhsT=vS[:, kb, kvh, :], rhs=P_[:, ki, :], start=(kb == kbs[0]), stop=False),
                                      reads=[vS, P_], writes=[po], chain=True)
                            for ct in range(2):
                                fw.op("pe", lambda: nc.tensor.matmul(po[0:65, c0:c0 + 128], lhsT=vS[:, 64 + ct, kvh, :], rhs=PcT[:, ct, c0:c0 + 128], start=False, stop=(ct == 1)),
                                      reads=[vS, PcT], writes=[po], chain=True)
                        attn_norm1(nt[po_i % 2], po, 512, h, sink=sk)
                        dfr.push(nt[po_i % 2], 512, t0, 5 + jp, b0); po_i += 1
                    if with_ctx:
                        for ct in range(2):
                            fw.op("pe", lambda: nc.tensor.matmul(pc[:, ct, :CTXL], lhsT=kT[b0:b0 + 64, SEQ + 128 * ct:SEQ + 128 * ct + 128], rhs=qT[b0:b0 + 64, SEQ:SEQ + CTXL],
                                                               start=True, stop=True), reads=[kT, qT], writes=[pc])
                        fw.op("act", lambda: nc.scalar.activation(out=PcT[:, :, :CTXL], in_=pc[:, :, :CTXL], func=AF.Exp, scale=0.125), reads=[pc], writes=[PcT])
                        for ct in range(2):
                            fw.op("pe", lambda: nc.tensor.matmul(po[0:65, :CTXL], lhsT=vS[:, 64 + ct, kvh, :], rhs=PcT[:, ct, :CTXL], start=(ct == 0), stop=(ct == 1)),
                                  reads=[vS, PcT], writes=[po], chain=True)
                        attn_norm1(nt[po_i % 2], po, CTXL, h, sink=sk)
                        dfr.push(nt[po_i % 2], CTXL, SEQ, 5 + jp, b0); po_i += 1
            dfr.flush()

    yf = fw.dram("yf", [2, 128, NTOK], F32)
    TWO_PI = 6.283185307179586
    C1 = 6.28125
    C2 = TWO_PI - 6.28125
    PI = 3.141592653589793

    def phaseC(l, with_ctx):
        with ExitStack() as pes:
            def tl(name, shape, dt=F32):
                return fw.sb(name, shape, dt, pes)
            scr_i = tl("scr_i", [128, 512], I32)
            scr_k = tl("scr_k", [128, 512])
            scr_t = tl("scr_t", [128, 512])
            scr_a = tl("scr_a", [128, 512])
            scr_b = tl("scr_b", [128, 512])

            def dve(fn, reads, writes):
                fw.op("dve", fn, reads=reads, writes=writes)

            def reduce_angle(ang, n, srcs):
                dve(lambda: nc.vector.tensor_scalar(out=scr_k[:, :n], in0=ang, scalar1=1.0 / TWO_PI, scalar2=None, op0=ALU.mult), srcs, [scr_k])
                dve(lambda: nc.vector.tensor_copy(out=scr_i[:, :n], in_=scr_k[:, :n]), [scr_k], [scr_i])
                dve(lambda: nc.vector.tensor_copy(out=scr_k[:, :n], in_=scr_i[:, :n]), [scr_i], [scr_k])
                dve(lambda: nc.vector.scalar_tensor_tensor(out=scr_a[:, :n], in0=scr_k[:, :n], scalar=-C1, in1=ang, op0=ALU.mult, op1=ALU.add), [scr_k] + srcs, [scr_a])
                dve(lambda: nc.vector.scalar_tensor_tensor(out=scr_a[:, :n], in0=scr_k[:, :n], scalar=-C2, in1=scr_a[:, :n], op0=ALU.mult, op1=ALU.add), [scr_k, scr_a], [scr_a])
                wrap(scr_a, n)

            def wrap(t, n):
                dve(lambda: nc.vector.tensor_scalar(out=scr_t[:, :n], in0=t[:, :n], scalar1=PI, scalar2=None, op0=ALU.is_gt), [t], [scr_t])
                dve(lambda: nc.vector.scalar_tensor_tensor(out=t[:, :n], in0=scr_t[:, :n], scalar=-TWO_PI, in1=t[:, :n], op0=ALU.mult, op1=ALU.add), [scr_t, t], [t])
                dve(lambda: nc.vector.tensor_scalar(out=scr_t[:, :n], in0=t[:, :n], scalar1=-PI, scalar2=None, op0=ALU.is_lt), [t], [scr_t])
                dve(lambda: nc.vector.scalar_tensor_tensor(out=t[:, :n], in0=scr_t[:, :n], scalar=TWO_PI, in1=t[:, :n], op0=ALU.mult, op1=ALU.add), [scr_t, t], [t])

            def sincos(ang, n, srcs, out_s, out_c, outs):
                reduce_angle(ang, n, srcs)
                fw.op("act", lambda: nc.scalar.activation(out=out_s, in_=scr_a[:, :n], func=AF.Sin), reads=[scr_a], writes=outs)
                dve(lambda: nc.vector.tensor_scalar(out=scr_b[:, :n], in0=scr_a[:, :n], scalar1=PI / 2, scalar2=None, op0=ALU.add), [scr_a], [scr_b])
                wrap(scr_b, n)
                fw.op("act", lambda: nc.scalar.activation(out=out_c, in_=scr_b[:, :n], func=AF.Sin), reads=[scr_b], writes=outs)

            are = tl("are", [128, 16]); aim = tl("aim", [128, 16]); stp = tl("stp", [128, 16])
            with nc.allow_non_contiguous_dma(reason="tiny s5 params"):
                for nm, dst in (("s5_a_re", are), ("s5_a_im", aim)):
                    for d_ in range(2):
                        fw.dma("sp", lambda: nc.sync.dma_start(out=dst[:, d_ * 8:(d_ + 1) * 8], in_=I[nm][l, d_].rearrange("g p -> (g p)").rearrange("(i q) -> q i", q=128)),
                               reads=[I[nm]], writes=[dst])
                for d_ in range(2):
                    for g2 in range(2):
                        src = bass.AP(tensor=I["s5_log_step"].t.tensor, offset=l * 32 + d_ * 16 + g2, ap=[[0, 64], [2, 8]])
                        fw.dma("sp", lambda: nc.sync.dma_start(out=stp[64 * g2:64 * g2 + 64, d_ * 8:(d_ + 1) * 8], in_=src), reads=[I["s5_log_step"]], writes=[stp])
            fw.op("act", lambda: nc.scalar.activation(out=stp[:], in_=stp[:], func=AF.Exp), reads=[stp], writes=[stp])
            dve(lambda: nc.vector.tensor_scalar(out=are[:], in0=are[:], scalar1=-1e-4, scalar2=None, op0=ALU.min), [are], [are])
            lr = tl("lr", [128, 16]); li = tl("li", [128, 16]); rr = tl("rr", [128, 16])
            dve(lambda: nc.vector.tensor_tensor(out=lr[:], in0=are[:], in1=stp[:], op=ALU.mult), [are, stp], [lr])
            dve(lambda: nc.vector.tensor_tensor(out=li[:], in0=aim[:], in1=stp[:], op=ALU.mult), [aim, stp], [li])
            fw.op("act", lambda: nc.scalar.activation(out=rr[:], in_=lr[:], func=AF.Exp), reads=[lr], writes=[rr])
            s1 = tl("s1", [128, 16]); c1 = tl("c1", [128, 16])
            sincos(li[:, :], 16, [li], s1[:, :], c1[:, :], [s1, c1])
            sL = {}; cL = {}
            angL = tl("angL", [128, 16])
            for L in (512, 256):
                sL[L] = tl("sL%d" % L, [128, 16]); cL[L] = tl("cL%d" % L, [128, 16])
                dve(lambda: nc.vector.tensor_scalar(out=angL[:], in0=li[:], scalar1=float(L), scalar2=None, op0=ALU.mult), [li], [angL])
                sincos(angL[:, :], 16, [angL], sL[L][:, :], cL[L][:, :], [sL[L], cL[L]])
            lbr = tl("lbr", [128, 16]); lbi = tl("lbi", [128, 16]); den = tl("den", [128, 16]); tq = tl("tq", [128, 16])
            cr = tl("cr", [128, 16]); ci = tl("ci", [128, 16]); nci = tl("nci", [128, 16])
            dve(lambda: nc.vector.tensor_tensor(out=lbr[:], in0=rr[:], in1=c1[:], op=ALU.mult), [rr, c1], [lbr])
            dve(lambda: nc.vector.tensor_scalar(out=lbr[:], in0=lbr[:], scalar1=-1.0, scalar2=None, op0=ALU.add), [lbr], [lbr])
            dve(lambda: nc.vector.tensor_tensor(out=lbi[:], in0=rr[:], in1=s1[:], op=ALU.mult), [rr, s1], [lbi])
            dve(lambda: nc.vector.tensor_tensor(out=den[:], in0=are[:], in1=are[:], op=ALU.mult), [are], [den])
            dve(lambda: nc.vector.tensor_tensor(out=tq[:], in0=aim[:], in1=aim[:], op=ALU.mult), [aim], [tq])
            dve(lambda: nc.vector.tensor_tensor(out=den[:], in0=den[:], in1=tq[:], op=ALU.add), [den, tq], [den])
            dve(lambda: nc.vector.reciprocal(out=den[:], in_=den[:]), [den], [den])
            dve(lambda: nc.vector.tensor_tensor(out=cr[:], in0=lbr[:], in1=are[:], op=ALU.mult), [lbr, are], [cr])
            dve(lambda: nc.vector.tensor_tensor(out=tq[:], in0=lbi[:], in1=aim[:], op=ALU.mult), [lbi, aim], [tq])
            dve(lambda: nc.vector.tensor_tensor(out=cr[:], in0=cr[:], in1=tq[:], op=ALU.add), [cr, tq], [cr])
            dve(lambda: nc.vector.tensor_tensor(out=cr[:], in0=cr[:], in1=den[:], op=ALU.mult), [cr, den], [cr])
            dve(lambda: nc.vector.tensor_tensor(out=ci[:], in0=lbi[:], in1=are[:], op=ALU.mult), [lbi, are], [ci])
            dve(lambda: nc.vector.tensor_tensor(out=tq[:], in0=lbr[:], in1=aim[:], op=ALU.mult), [lbr, aim], [tq])
            dve(lambda: nc.vector.tensor_tensor(out=ci[:], in0=ci[:], in1=tq[:], op=ALU.subtract), [ci, tq], [ci])
            dve(lambda: nc.vector.tensor_tensor(out=ci[:], in0=ci[:], in1=den[:], op=ALU.mult), [ci, den], [ci])
            dve(lambda: nc.vector.tensor_scalar(out=nci[:], in0=ci[:], scalar1=-1.0, scalar2=None, op0=ALU.mult), [ci], [nci])
            bsr = tl("bsr", [128, 2, 2, 128]); bsi = tl("bsi", [128, 2, 2, 128])
            bbr = tl("bbr", [128, 2, 2, 128]); bbi = tl("bbi", [128, 2, 2, 128])
            csr = tl("csr", [128, 2, 2, 128]); csi = tl("csi", [128, 2, 2, 128])
            for t_ in (bsr, bsi, csr, csi):
                dve(lambda: nc.vector.memset(t_[:], 0.0), [], [t_])
            qn = 0
            for d_ in range(2):
                for g in range(16):
                    i = g // 2; g2 = g % 2; i4 = i // 4; im = i % 4
                    for nm, dst in (("s5_b_re", bsr), ("s5_b_im", bsi)):
                        q = ("sp", nc.sync) if qn % 2 == 0 else ("act", nc.scalar); qn += 1
                        fw.dma(q[0], lambda: q[1].dma_start(out=dst[64 * g2:64 * g2 + 64, d_, i4, 32 * im + 16 * g2:32 * im + 16 * g2 + 16], in_=I[nm][l, d_, g]),
                               reads=[I[nm]], writes=[dst])
                    for nm, dst in (("s5_c_re", csr), ("s5_c_im", csi)):
                        q = ("sp", nc.sync) if qn % 2 == 0 else ("act", nc.scalar); qn += 1
                        fw.dma(q[0], lambda: q[1].dma_start(out=dst[32 * im + 16 * g2:32 * im + 16 * g2 + 16, d_, i4, 64 * g2:64 * g2 + 64], in_=I[nm][l, d_, g]),
                               reads=[I[nm]], writes=[dst])
            for d_ in range(2):
                for i in range(8):
                    col = d_ * 8 + i; i4 = i // 4; im = i % 4
                    sl = slice(32 * im, 32 * im + 32)
                    dve(lambda: nc.vector.tensor_scalar(out=bbr[:, d_, i4, sl], in0=bsr[:, d_, i4, sl], scalar1=cr[:, col:col + 1], scalar2=None, op0=ALU.mult), [bsr, cr], [bbr])
                    dve(lambda: nc.vector.scalar_tensor_tensor(out=bbr[:, d_, i4, sl], in0=bsi[:, d_, i4, sl], scalar=nci[:, col:col + 1], in1=bbr[:, d_, i4, sl],
                                                               op0=ALU.mult, op1=ALU.add), [bsi, nci, bbr], [bbr])
                    dve(lambda: nc.vector.tensor_scalar(out=bbi[:, d_, i4, sl], in0=bsi[:, d_, i4, sl], scalar1=cr[:, col:col + 1], scalar2=None, op0=ALU.mult), [bsi, cr], [bbi])
                    dve(lambda: nc.vector.scalar_tensor_tensor(out=bbi[:, d_, i4, sl], in0=bsr[:, d_, i4, sl], scalar=ci[:, col:col + 1], in1=bbi[:, d_, i4, sl],
                                                               op0=ALU.mult, op1=ALU.add), [bsr, ci, bbi], [bbi])
            BbT = tl("BbT", [128, 2, 2, 2, 128], BF16)
            CT = tl("CT", [128, 2, 2, 2, 128], BF16)
            with ExitStack() as pes2:
                ptp = [fw.ps("ptp%d" % k_, [128, 128], F32, pes2) for k_ in range(2)]
                it = 0
                for d_ in range(2):
                    for i4 in range(2):
                        for ri, (bsrc, csrc) in enumerate(((bbr, csr), (bbi, csi))):
                            p = ptp[it % 2]; it += 1
                            fw.op("pe", lambda: nc.tensor.transpose(out=p[:, :], in_=bsrc[:, d_, i4, :], identity=ident_f[:]), reads=[bsrc, ident_f], writes=[p])
                            fw.op("act", lambda: nc.scalar.copy(out=BbT[:, d_, i4, ri, :], in_=p[:, :]), reads=[p], writes=[BbT])
                            p = ptp[it % 2]; it += 1
                            fw.op("pe", lambda: nc.tensor.transpose(out=p[:, :], in_=csrc[:, d_, i4, :], identity=ident_f[:]), reads=[csrc, ident_f], writes=[p])
                            fw.op("act", lambda: nc.scalar.mul(out=CT[:, d_, i4, ri, :], in_=p[:, :], mul=(1.0 if ri == 0 else -1.0)), reads=[p], writes=[CT])
                fw.barrier()
            tcos = tl("tcos", [128, 16, 512]); tsin = tl("tsin", [128, 16, 512])
            rful = tl("rful", [128, 16, 512])
            io = tl("io512", [128, 512])
            fw.dma("sp", lambda: nc.sync.dma_start(out=io[:], in_=I["iota512"][:]), reads=[I["iota512"]], writes=[io])
            angt = tl("angt", [128, 512])
            for col in range(16):
                dve(lambda: nc.vector.tensor_scalar(out=angt[:], in0=io[:], scalar1=li[:, col:col + 1], scalar2=None, op0=ALU.mult), [io, li], [angt])
                sincos(angt[:, :], 512, [angt], tsin[:, col, :], tcos[:, col, :], [tsin, tcos])
                fw.op("pool", lambda: nc.gpsimd.tensor_scalar(out=rful[:, col, :], in0=io[:], scalar1=0.0, scalar2=rr[:, col:col + 1], op0=ALU.mult, op1=ALU.add), reads=[io, rr], writes=[rful])
            dsk = tl("dsk", [128, 2]); bgl = tl("bgl", [128, 2])
            wgl = tl("wgl", [128, 2, 256], BF16)
            with nc.allow_non_contiguous_dma(reason="tiny"):
                fw.dma("sp", lambda: nc.sync.dma_start(out=dsk[:], in_=I["s5_d"][l].rearrange("(c p) -> p c", p=128)), reads=[I["s5_d"]], writes=[dsk])
                fw.dma("sp", lambda: nc.sync.dma_start(out=bgl[:], in_=I["s5_b_glu"][l].rearrange("(c p) -> p c", p=128)), reads=[I["s5_b_glu"]], writes=[bgl])
            fw.dma("pool", lambda: nc.gpsimd.dma_start(out=wgl[:], in_=I["s5_w_glu"][l].rearrange("(c p) n -> p c n", p=128)), reads=[I["s5_w_glu"]], writes=[wgl])
            zst = tl("zst", [128, 16, 2])
            zin = tl("zin", [128, 16, 2])
            dve(lambda: nc.vector.memset(zin[:], 0.0), [], [zin])
            uf = [tl("uf%d" % k_, [128, 2, 512]) for k_ in range(2)]
            ub = [tl("ub%d" % k_, [128, 2, 512], BF16) for k_ in range(2)]
            pA = [fw.ps("pA%d" % k_, [128, 512], F32, pes) for k_ in range(2)]
            pB = [fw.ps("pB%d" % k_, [128, 512], F32, pes) for k_ in range(2)]
            py = [fw.ps("py%d" % k_, [128, 512], F32, pes) for k_ in range(2)]
            pg = [fw.ps("pg%d" % k_, [128, 512], F32, pes) for k_ in range(2)]
            W = {}
            for nm in ("t1", "t2", "t3", "t4", "dr", "di", "zr", "zi"):
                W[nm] = [tl(nm + "_%d" % k_, [128, 512]) for k_ in range(2)]
            xr = [tl("xr%d" % k_, [128, 512], BF16) for k_ in range(2)]
            xi = [tl("xi%d" % k_, [128, 512], BF16) for k_ in range(2)]
            ysb = [tl("ysbC0", [128, 2, 512])] * 2
            yfl = tl("yfl", [128, 2, 512])
            gq = tl("gq", [128, 2, 512]); gp = tl("gp", [128, 2, 512]); gg = gq
            gb = tl("gb", [128, 2, 512], BF16); sg = gp; yo = tl("yo", [128, 2, 512], BF16)
            lat = [(g_ * 512, 512) for g_ in range(16)]
            order = {0: [(SEQ, CTXL)] + lat, 1: [(SEQ, CTXL)] + lat[::-1]}
            it = 0
            ci_ = 0
            for d_ in range(2):
                prevL = None
                for (t0, L) in order[d_]:
                    isctx = t0 >= SEQ
                    u_ = uf[ci_ % 2]; ub_ = ub[ci_ % 2]; ys_ = ysb[ci_ % 2]; ci_ += 1
                    fw.dma("sp", lambda: nc.sync.dma_start(out=u_[:, :, :L], in_=u5[:, :, t0:t0 + L].rearrange("c p t -> p c t")), reads=[u5], writes=[u_])
                    if d_ == 0:
                        fw.op("act", lambda: nc.scalar.copy(out=ub_[:, :, :L], in_=u_[:, :, :L]), reads=[u_], writes=[ub_])
                    else:
                        fw.op("act", lambda: nc.scalar.copy(out=ub_[:, :, :L], in_=u_[:, :, L - 1::-1] if False else u_[:, :, :L][:, :, ::-1]), reads=[u_], writes=[ub_])
                        fw.dma("act", lambda: nc.scalar.dma_start(out=yfl[:, :, :L], in_=yf[:, :, t0:t0 + L].rearrange("c p t -> p c t")), reads=[yf], writes=[yfl])
                    for i in range(8):
                        col = d_ * 8 + i; i4 = i // 4; im = i % 4
                        k_ = it % 2; it += 1
                        A = pA[k_]; B = pB[k_]
                        t1, t2, t3, t4 = W["t1"][k_], W["t2"][k_], W["t3"][k_], W["t4"][k_]
                        dr, di, zr, zi = W["dr"][k_], W["di"][k_], W["zr"][k_], W["zi"][k_]
                        u1, u2, u3, u4 = t1, t2, t3, t4
                        rf = rful
                        xr_, xi_ = xr[k_], xi[k_]
                        ps_ = slice(32 * im, 32 * im + 32)
                        fw.op("pe", lambda: nc.tensor.matmul(A[:, :L], lhsT=BbT[ps_, d_, i4, 0, :], rhs=ub_[ps_, i4, :L], start=True, stop=True, tile_position=(32 * im, 0)),
                              reads=[BbT, ub_], writes=[A])
                        fw.op("pe", lambda: nc.tensor.matmul(B[:, :L], lhsT=BbT[ps_, d_, i4, 1, :], rhs=ub_[ps_, i4, :L], start=True, stop=True, tile_position=(32 * im, 0)),
                              reads=[BbT, ub_], writes=[B])
                        if prevL is not None:
                            dve(lambda: nc.vector.tensor_scalar(out=zin[:, col, 0:1], in0=zst[:, col, 1:2], scalar1=sL[prevL][:, col:col + 1], scalar2=None, op0=ALU.mult), [zst, sL[prevL]], [zin])
                            dve(lambda: nc.vector.scalar_tensor_tensor(out=zin[:, col, 0:1], in0=zst[:, col, 0:1], scalar=cL[prevL][:, col:col + 1], in1=zin[:, col, 0:1],
                                                                       op0=ALU.mult, op1=ALU.subtract), [zst, cL[prevL], zin], [zin])
                            dve(lambda: nc.vector.tensor_scalar(out=zin[:, col, 1:2], in0=zst[:, col, 1:2], scalar1=cL[prevL][:, col:col + 1], scalar2=None, op0=ALU.mult), [zst, cL[prevL]], [zin])
                            dve(lambda: nc.vector.scalar_tensor_tensor(out=zin[:, col, 1:2], in0=zst[:, col, 0:1], scalar=sL[prevL][:, col:col + 1], in1=zin[:, col, 1:2],
                                                                       op0=ALU.mult, op1=ALU.add), [zst, sL[prevL], zin], [zin])
                        cs = tcos[:, col, :L]; sn = tsin[:, col, :L]
                        dve(lambda: nc.vector.tensor_tensor(out=t1[:, :L], in0=A[:, :L], in1=cs, op=ALU.mult), [A, tcos], [t1])
                        dve(lambda: nc.vector.tensor_tensor(out=t2[:, :L], in0=B[:, :L], in1=sn, op=ALU.mult), [B, tsin], [t2])
                        fw.op("pool", lambda: nc.gpsimd.tensor_tensor(out=dr[:, :L], in0=t1[:, :L], in1=t2[:, :L], op=ALU.add), reads=[t1, t2], writes=[dr])
                        dve(lambda: nc.vector.tensor_tensor(out=t3[:, :L], in0=B[:, :L], in1=cs, op=ALU.mult), [B, tcos], [t3])
                        dve(lambda: nc.vector.tensor_tensor(out=t4[:, :L], in0=A[:, :L], in1=sn, op=ALU.mult), [A, tsin], [t4])
                        fw.op("pool", lambda: nc.gpsimd.tensor_tensor(out=di[:, :L], in0=t3[:, :L], in1=t4[:, :L], op=ALU.subtract), reads=[t3, t4], writes=[di])
                        dve(lambda: nc.vector.tensor_tensor_scan(out=zr[:, :L], data0=rful[:, col, :L], data1=dr[:, :L], initial=zin[:, col, 0:1], op0=ALU.mult, op1=ALU.add),
                            [rf, dr, zin], [zr])
                        dve(lambda: nc.vector.tensor_tensor_scan(out=zi[:, :L], data0=rful[:, col, :L], data1=di[:, :L], initial=zin[:, col, 1:2], op0=ALU.mult, op1=ALU.add),
                            [rf, di, zin], [zi])
                        dve(lambda: nc.vector.tensor_copy(out=zst[:, col, 0:1], in_=zr[:, L - 1:L]), [zr], [zst])
                        dve(lambda: nc.vector.tensor_copy(out=zst[:, col, 1:2], in_=zi[:, L - 1:L]), [zi], [zst])
                        dve(lambda: nc.vector.tensor_tensor(out=u1[:, :L], in0=zr[:, :L], in1=cs, op=ALU.mult), [zr, tcos], [u1])
                        fw.op("pool", lambda: nc.gpsimd.tensor_tensor(out=u2[:, :L], in0=zi[:, :L], in1=sn, op=ALU.mult), reads=[zi, tsin], writes=[u2])
                        fw.op("pool", lambda: nc.gpsimd.tensor_tensor(out=xr_[:, :L], in0=u1[:, :L], in1=u2[:, :L], op=ALU.subtract), reads=[u1, u2], writes=[xr_])
                        dve(lambda: nc.vector.tensor_tensor(out=u3[:, :L], in0=zr[:, :L], in1=sn, op=ALU.mult), [zr, tsin], [u3])
                        fw.op("pool", lambda: nc.gpsimd.tensor_tensor(out=u4[:, :L], in0=zi[:, :L], in1=cs, op=ALU.mult), reads=[zi, tcos], writes=[u4])
                        fw.op("pool", lambda: nc.gpsimd.tensor_tensor(out=xi_[:, :L], in0=u3[:, :L], in1=u4[:, :L], op=ALU.add), reads=[u3, u4], writes=[xi_])
                        if isctx and not with_ctx:
                            continue
                        yq = py[i4]
                        fw.op("pe", lambda: nc.tensor.matmul(yq[ps_, :L], lhsT=CT[:, d_, i4, 0, ps_], rhs=xr_[:, :L], start=True, stop=False, tile_position=(0, 32 * im)),
                              reads=[CT, xr_], writes=[yq])
                        fw.op("pe", lambda: nc.tensor.matmul(yq[ps_, :L], lhsT=CT[:, d_, i4, 1, ps_], rhs=xi_[:, :L], start=False, stop=True, tile_position=(0, 32 * im)),
                              reads=[CT, xi_], writes=[yq])
                    prevL = L
                    if isctx and not with_ctx:
                        continue
                    if d_ == 0:
                        for ct in range(2):
                            dve(lambda: nc.vector.scalar_tensor_tensor(out=ys_[:, ct, :L], in0=u_[:, ct, :L], scalar=dsk[:, ct:ct + 1], in1=py[ct][:, :L], op0=ALU.mult, op1=ALU.add),
                                [u_, dsk, py[ct]], [ys_])
                        fw.dma("sp", lambda: nc.sync.dma_start(out=yf[:, :, t0:t0 + L].rearrange("c p t -> p c t"), in_=ys_[:, :, :L]), reads=[ys_], writes=[yf])
                    else:
                        for ct in range(2):
                            dve(lambda: nc.vector.tensor_tensor(out=ys_[:, ct, :L], in0=py[ct][:, :L][:, ::-1], in1=yfl[:, ct, :L], op=ALU.add), [py[ct], yfl], [ys_])
                        fw.op("act", lambda: nc.scalar.activation(out=gq[:, :, :L], in_=ys_[:, :, :L], func=AF.Square), reads=[ys_], writes=[gq])
                        dve(lambda: nc.vector.tensor_scalar(out=gq[:, :, :L], in0=gq[:, :, :L], scalar1=0.044715, scalar2=1.0, op0=ALU.mult, op1=ALU.add), [gq], [gq])
                        fw.op("pool", lambda: nc.gpsimd.tensor_tensor(out=gp[:, :, :L], in0=gq[:, :, :L], in1=ys_[:, :, :L], op=ALU.mult), reads=[gq, ys_], writes=[gp])
                        fw.op("act", lambda: nc.scalar.activation(out=gp[:, :, :L], in_=gp[:, :, :L], func=AF.Sigmoid, scale=1.5957691216057308), reads=[gp], writes=[gp])
                        fw.op("pool", lambda: nc.gpsimd.tensor_tensor(out=gg[:, :, :L], in0=gp[:, :, :L], in1=ys_[:, :, :L], op=ALU.mult), reads=[gp, ys_], writes=[gg])
                        fw.op("act", lambda: nc.scalar.copy(out=gb[:, :, :L], in_=gg[:, :, :L]), reads=[gg], writes=[gb])
                        for co in range(2):
                            for cin in range(2):
                                fw.op("pe", lambda: nc.tensor.matmul(pg[co][:, :L], lhsT=wgl[:, cin, co * 128:(co + 1) * 128], rhs=gb[:, cin, :L], start=(cin == 0), stop=(cin == 1)),
                                      reads=[wgl, gb], writes=[pg[co]], chain=True)
                            fw.op("act", lambda: nc.scalar.activation(out=sg[:, co, :L], in_=pg[co][:, :L], func=AF.Sigmoid, bias=bgl[:, co:co + 1], scale=1.0), reads=[pg[co], bgl], writes=[sg])
                        dve(lambda: nc.vector.tensor_tensor(out=yo[:, :, :L], in0=gg[:, :, :L], in1=sg[:, :, :L], op=ALU.mult), [gg, sg], [yo])
                        fw.dma("sp", lambda: nc.sync.dma_start(out=yT[3:5, :, t0:t0 + L].rearrange("c p t -> p c t"), in_=yo[:, :, :L]), reads=[yo], writes=[yT])


    Xs = fw.dram("Xs", [NSLOT + 128, D], BF16)
    Ys = fw.dram("Ys", [NSLOT, D], F32)
    h2tok = fw.dram("h2tok", [NTOK, D], BF16)
    NTILE = NTOK // 128
    dest_i = fw.sb("dest_i", [128, NTILE, 4], U32)
    wk = fw.sb("wk", [128, NTILE, 4], F32)
    idxw = fw.sb("idxw", [128, NB, 8], U32)
    idxbg = fw.sb("idxbg", [128, NB], U32)
    idxbd = fw.sb("idxbd", [128, NB], U32)
    iop = fw.sb("iop", [128, 1], F32)
    fw.dma("sp", lambda: nc.sync.dma_start(out=iop[:], in_=I["iota_p"][:]), reads=[I["iota_p"]], writes=[iop])

    def phaseE(l, with_ctx):
        with ExitStack() as pes:
            def tl(name, shape, dt=F32):
                return fw.sb(name, shape, dt, pes)
            wout = tl("wout", [128, 8, D], BF16)
            fw.dma("pool", lambda: nc.gpsimd.dma_start(out=wout[:], in_=I["w_out"][l].rearrange("(c p) n -> p c n", p=128)), reads=[I["w_out"]], writes=[wout])
            wr = tl("wr", [128, 8, NE])
            fw.dma("sp", lambda: nc.sync.dma_start(out=wr[:], in_=I["w_router"][l].rearrange("(c p) e -> p c e", p=128)), reads=[I["w_router"]], writes=[wr])
            brow = tl("brow", [128, NE])
            fw.dma("sp", lambda: nc.sync.dma_start(out=brow[:], in_=I["b_router"][l].partition_broadcast(128)), reads=[I["b_router"]], writes=[brow])
            io32 = tl("io32", [128, NE]); ust = tl("ust", [128, 128]); iob = tl("iob", [128, NB])
            fw.dma("sp", lambda: nc.sync.dma_start(out=io32[:], in_=I["iota32"][:]), reads=[I["iota32"]], writes=[io32])
            fw.dma("sp", lambda: nc.sync.dma_start(out=ust[:], in_=I["ustrict"][:]), reads=[I["ustrict"]], writes=[ust])
            fw.dma("sp", lambda: nc.sync.dma_start(out=iob[:], in_=I["iotablk"][:]), reads=[I["iotablk"]], writes=[iob])
            runm = tl("runm", [128, NE])
            fw.op("dve", lambda: nc.vector.memset(runm[:], 0.0), writes=[runm])
            posall = tl("posall", [128, NTILE, NE]); idxall = tl("idxall", [128, NTILE, 4])
            xg = [tl("xgE%d" % i, [128, 8, 512]) for i in range(2)]
            yg = [tl("ygE%d" % i, [128, 8, 512], BF16) for i in range(2)]
            h2f = tl("h2f", [128, 8, 512]); h2b = tl("h2b", [128, 8, 512], BF16)
            sq = tl("sqE", [128, 8, 512], BF16); rstd = tl("rstdE", [128, 512]); GS = tl("GSE", [128, 1, 8])
            pss = fw.ps("pssE", [128, 512], F32, pes)
            pp = [fw.ps("ppE%d" % i, [128, 512], F32, pes) for i in range(2)]
            plg = fw.ps("plg", [128, NE], F32, pes)
            ppos = fw.ps("ppos", [128, NE], F32, pes)
            ptr = [fw.ps("ptrE%d" % i, [128, 8, 128], BF16, pes) for i in range(2)]
            htok = [tl("htok%d" % i, [128, D], BF16) for i in range(2)]
            lg = tl("lg", [128, NE]); m8 = tl("m8", [128, 8]); idx8 = tl("idx8", [128, 8], U32); mask = tl("mask", [128, NE])
            negmx = tl("negmx", [128, 1]); e4 = tl("e4", [128, 4]); ssum = tl("ssum", [128, 1])
            oh = tl("oh", [128, NE]); junk = tl("junk", [128, NE]); posk = tl("posk", [128, 4]); posf = tl("posf", [128, NE])
            gl = groups() if with_ctx else groups()[:-1]
            tiles_done = []
            for gi, (t0, n, isctx) in enumerate(gl):
                j = 1 if isctx else 0
                x_ = xg[gi % 2]; y_ = yg[gi % 2]
                fw.dma("sp", lambda: nc.sync.dma_start(out=x_[:, :, :n], in_=xs[:, :, t0:t0 + n]), reads=[xs], writes=[x_])
                fw.dma("act", lambda: nc.scalar.dma_start(out=y_[:, :, :n], in_=yT[:, :, t0:t0 + n].rearrange("c p t -> p c t")), reads=[yT], writes=[y_])
                for dc in range(8):
                    p = pp[dc % 2]
                    for ct in range(8):
                        fw.op("pe", lambda: nc.tensor.matmul(p[:, :n], lhsT=wout[:, ct, dc * 128:(dc + 1) * 128], rhs=y_[:, ct, :n], start=(ct == 0), stop=(ct == 7)),
                              reads=[wout, y_], writes=[p], chain=True)
                    fw.op("dve", lambda: nc.vector.scalar_tensor_tensor(out=x_[:, dc, :n], in0=p[:, :n], scalar=modT[:, l, 16 + dc, j:j + 1], in1=x_[:, dc, :n],
                                                                      op0=ALU.mult, op1=ALU.add), reads=[p, modT, x_], writes=[x_])
                fw.dma("sp", lambda: nc.sync.dma_start(out=xs[:, :, t0:t0 + n], in_=x_[:, :, :n]), reads=[x_], writes=[xs])
                norm_mod((sq, pss, rstd, GS), x_, n, gffn[:, l, :], l, 3, 4, j, out_bf=h2b, out_f=h2f)
                for tt in range(n // 128):
                    ti = (t0 // 128) + tt
                    tiles_done.append(ti)
                    tsl = slice(tt * 128, (tt + 1) * 128)
                    for kc in range(8):
                        fw.op("pe", lambda: nc.tensor.matmul(plg[:, :], lhsT=h2f[:, kc, tsl], rhs=wr[:, kc, :], start=(kc == 0), stop=(kc == 7)),
                              reads=[h2f, wr], writes=[plg], chain=True)
                    fw.op("dve", lambda: nc.vector.tensor_tensor(out=lg[:], in0=plg[:], in1=brow[:], op=ALU.add), reads=[plg, brow], writes=[lg])
                    fw.op("dve", lambda: nc.vector.max(out=m8[:], in_=lg[:]), reads=[lg], writes=[m8])
                    fw.op("dve", lambda: nc.vector.max_index(out=idx8[:], in_max=m8[:], in_values=lg[:]), reads=[lg, m8], writes=[idx8])
                    fw.op("dve", lambda: nc.vector.tensor_scalar(out=mask[:], in0=lg[:], scalar1=m8[:, 3:4], scalar2=None, op0=ALU.is_ge), reads=[lg, m8], writes=[mask])
                    fw.op("dve", lambda: nc.vector.tensor_scalar(out=negmx[:], in0=m8[:, 0:1], scalar1=-1.0, scalar2=None, op0=ALU.mult), reads=[m8], writes=[negmx])
                    fw.op("act", lambda: nc.scalar.activation(out=e4[:], in_=m8[:, 0:4], func=AF.Exp, bias=negmx[:, 0:1], scale=1.0, accum_out=ssum[:, 0:1]),
                          reads=[m8, negmx], writes=[e4, ssum])
                    fw.op("dve", lambda: nc.vector.reciprocal(out=ssum[:], in_=ssum[:]), reads=[ssum], writes=[ssum])
                    fw.op("dve", lambda: nc.vector.tensor_scalar(out=wk[:, ti, :], in0=e4[:], scalar1=ssum[:, 0:1], scalar2=None, op0=ALU.mult), reads=[e4, ssum], writes=[wk])
                    fw.op("pe", lambda: nc.tensor.matmul(ppos[:, :], lhsT=ust[:, :], rhs=mask[:, :], start=True, stop=False), reads=[ust, mask], writes=[ppos])
                    fw.op("pe", lambda: nc.tensor.matmul(ppos[:, :], lhsT=ones_f[:, :], rhs=runm[:, :], start=False, stop=True), reads=[ones_f, runm], writes=[ppos], chain=True)
                    fw.op("act", lambda: nc.scalar.copy(out=posall[:, ti, :], in_=ppos[:]), reads=[ppos], writes=[posall])
                    fw.op("pool", lambda: nc.gpsimd.tensor_tensor(out=runm[:], in0=runm[:], in1=mask[:], op=ALU.add), reads=[runm, mask], writes=[runm])
                    fw.op("dve", lambda: nc.vector.tensor_copy(out=idxall[:, ti, :], in_=idx8[:, 0:4]), reads=[idx8], writes=[idxall])
                    pt_ = ptr[ti % 2]; ht = htok[ti % 2]
                    for kc in range(8):
                        fw.op("pe", lambda: nc.tensor.transpose(out=pt_[:, kc, :], in_=h2b[:, kc, tsl], identity=ident_b[:]), reads=[h2b, ident_b], writes=[pt_], chain=True)
                    fw.op("act", lambda: nc.scalar.copy(out=ht[:], in_=pt_[:].rearrange("p c d -> p (c d)")), reads=[pt_], writes=[ht])
                    fw.dma("act", lambda: nc.scalar.dma_start(out=h2tok[ti * 128:(ti + 1) * 128, :], in_=ht[:]), reads=[ht], writes=[h2tok])
            cnt = tl("cnt", [128, NE]); pad = tl("pad", [128, NE]); ends = tl("ends", [128, NE]); pst = tl("pst", [128, NE])
            qi = tl("qi", [128, NE], I32); qf = tl("qf", [128, NE]); gt = tl("gt", [128, NE]); onesr = tl("onesr", [128, NE])
            acc = tl("accb", [128, NB])
            fw.op("pe", lambda: nc.tensor.matmul(ppos[:, :], lhsT=ones_f[:, :], rhs=runm[:, :], start=True, stop=True), reads=[ones_f, runm], writes=[ppos])
            fw.op("dve", lambda: nc.vector.tensor_scalar(out=cnt[:], in0=ppos[:], scalar1=float(BLK - 1), scalar2=1.0 / BLK, op0=ALU.add, op1=ALU.mult), reads=[ppos], writes=[cnt])
            fw.op("dve", lambda: nc.vector.tensor_copy(out=qi[:], in_=cnt[:]), reads=[cnt], writes=[qi])
            fw.op("dve", lambda: nc.vector.tensor_copy(out=qf[:], in_=qi[:]), reads=[qi], writes=[qf])
            fw.op("dve", lambda: nc.vector.tensor_tensor(out=gt[:], in0=qf[:], in1=cnt[:], op=ALU.is_gt), reads=[qf, cnt], writes=[gt])
            fw.op("dve", lambda: nc.vector.tensor_tensor(out=qf[:], in0=qf[:], in1=gt[:], op=ALU.subtract), reads=[qf, gt], writes=[qf])
            fw.op("dve", lambda: nc.vector.tensor_scalar(out=pad[:], in0=qf[:], scalar1=float(BLK), scalar2=None, op0=ALU.mult), reads=[qf], writes=[pad])
            fw.op("dve", lambda: nc.vector.memset(onesr[:], 1.0), writes=[onesr])
            fw.op("dve", lambda: nc.vector.tensor_tensor_scan(out=ends[:], data0=onesr[:], data1=pad[:], initial=0.0, op0=ALU.mult, op1=ALU.add), reads=[onesr, pad], writes=[ends])
            fw.op("dve", lambda: nc.vector.tensor_tensor(out=pst[:], in0=ends[:], in1=pad[:], op=ALU.subtract), reads=[ends, pad], writes=[pst])
            fw.op("dve", lambda: nc.vector.memset(acc[:], 0.0), writes=[acc])
            for e in range(NE):
                fw.op("dve", lambda: nc.vector.scalar_tensor_tensor(out=acc[:], in0=iob[:], scalar=ends[:, e:e + 1], in1=acc[:], op0=ALU.is_ge, op1=ALU.add),
                      reads=[iob, ends, acc], writes=[acc])
            fw.op("dve", lambda: nc.vector.tensor_scalar(out=acc[:], in0=acc[:], scalar1=float(NE - 1), scalar2=None, op0=ALU.min), reads=[acc], writes=[acc])
            tix = tl("tix", [128, NB])
            for kc in range(8):
                fw.op("dve", lambda: nc.vector.tensor_scalar(out=tix[:], in0=acc[:], scalar1=1024.0, scalar2=float(l * NE * 1024 + kc * 128), op0=ALU.mult, op1=ALU.add), reads=[acc], writes=[tix])
                fw.op("dve", lambda: nc.vector.tensor_scalar(out=tix[:], in0=tix[:], scalar1=iop[:, 0:1], scalar2=None, op0=ALU.add), reads=[tix, iop], writes=[tix])
                fw.op("dve", lambda: nc.vector.tensor_copy(out=idxw[:, :, kc], in_=tix[:]), reads=[tix], writes=[idxw])
            fw.op("dve", lambda: nc.vector.tensor_scalar(out=tix[:], in0=acc[:], scalar1=16.0, scalar2=float(l * NE * 16), op0=ALU.mult, op1=ALU.add), reads=[acc], writes=[tix])
            fw.op("dve", lambda: nc.vector.tensor_scalar(out=tix[:], in0=tix[:], scalar1=iop[:, 0:1], scalar2=None, op0=ALU.add), reads=[tix, iop], writes=[tix])
            fw.op("dve", lambda: nc.vector.tensor_copy(out=idxbg[:], in_=tix[:]), reads=[tix], writes=[idxbg])
            fw.op("dve", lambda: nc.vector.tensor_scalar(out=tix[:], in0=acc[:], scalar1=float(l * NE), scalar2=None, op0=ALU.add), reads=[acc], writes=[tix])
            fw.op("dve", lambda: nc.vector.tensor_copy(out=idxbd[:], in_=tix[:]), reads=[tix], writes=[idxbd])
            for ti in tiles_done:
                ht = htok[ti % 2]
                fw.dma("sp", lambda: nc.sync.dma_start(out=ht[:], in_=h2tok[ti * 128:(ti + 1) * 128, :]), reads=[h2tok], writes=[ht])
                fw.op("dve", lambda: nc.vector.tensor_tensor(out=posf[:], in0=posall[:, ti, :], in1=pst[:], op=ALU.add), reads=[posall, pst], writes=[posf])
                for k_ in range(4):
                    fw.op("dve", lambda: nc.vector.tensor_scalar(out=oh[:], in0=io32[:], scalar1=idxall[:, ti, k_:k_ + 1], scalar2=None, op0=ALU.is_equal), reads=[io32, idxall], writes=[oh])
                    fw.op("dve", lambda: nc.vector.scalar_tensor_tensor(out=junk[:], in0=oh[:], scalar=1.0, in1=posf[:], op0=ALU.mult, op1=ALU.mult,
                                                                      accum_out=posk[:, k_:k_ + 1]), reads=[oh, posf], writes=[junk, posk])
                fw.op("dve", lambda: nc.vector.tensor_copy(out=dest_i[:, ti, :], in_=posk[:]), reads=[posk], writes=[dest_i])
                for k_ in range(4):
                    fw.dma("pool", lambda: nc.gpsimd.indirect_dma_start(out=Xs[:, :], out_offset=bass.IndirectOffsetOnAxis(ap=dest_i[:, ti, k_:k_ + 1], axis=0),
                                                                      in_=ht[:, :], in_offset=None), reads=[ht, dest_i], writes=[Xs])

    def phaseF(l):
        with ExitStack() as pes:
            def tl(name, shape, dt=F32):
                return fw.sb(name, shape, dt, pes)
            NST = BLK // 128
            chunks = [(c0, min(512, BLK - c0)) for c0 in range(0, BLK, 512)]
            wgu = [tl("wgu%d" % i, [128, 8, 2 * D], BF16) for i in range(2)]
            wdn = [tl("wdn%d" % i, [128, 8, D], BF16) for i in range(2)]
            bdr = [tl("bdr%d" % i, [128, D]) for i in range(2)]
            bgc = [tl("bgc%d" % i, [128, 16]) for i in range(2)]
            xtok = [tl("xtok%d" % i, [128, NST, D], BF16) for i in range(2)]
            XT = tl("XT", [128, 8, BLK], BF16)
            actT = tl("actT", [128, 8, BLK], BF16)
            gs = [tl("gs%d" % i, [128, 512]) for i in range(2)]
            sgm = [tl("sgm%d" % i, [128, 512]) for i in range(2)]
            uu = [tl("uu%d" % i, [128, 512]) for i in range(2)]
            aa = [tl("aa%d" % i, [128, 512]) for i in range(2)]
            yo = [tl("yoF%d" % i, [128, D]) for i in range(2)]
            ptrs = [fw.ps("ptrF%d" % i, [128, 8, 128], BF16, pes) for i in range(2)]
            pg = [fw.ps("pgF%d" % i, [128, 512], F32, pes) for i in range(2)]
            pu = [fw.ps("puF%d" % i, [128, 512], F32, pes) for i in range(2)]
            pd = [fw.ps("pdF%d" % i, [128, 512], F32, pes) for i in range(2)]

            wgu_flat = I["w_gate_up"][:].rearrange("l e k n -> (l e k) n")
            wdn_flat = I["w_down"][:].rearrange("l e k n -> (l e k) n")
            bgu_flat = I["b_gate_up"][:].rearrange("l e (c p) -> (l e c) p", p=128)
            bdn_flat = I["b_down"][:].rearrange("l e d -> (l e) d")
            bgrow = [tl("bgrow%d" % i, [16, 128]) for i in range(2)]

            def load_w(b):
                wg_ = wgu[b % 2]; wd_ = wdn[b % 2]; bd_ = bdr[b % 2]; br_ = bgrow[b % 2]
                fw.dma("pool", lambda: nc.gpsimd.indirect_dma_start(out=br_[:, :], out_offset=None, in_=bgu_flat,
                                                                  in_offset=bass.IndirectOffsetOnAxis(ap=idxbg[0:16, b:b + 1], axis=0)),
                       reads=[I["b_gate_up"], idxbg], writes=[br_])
                fw.dma("pool", lambda: nc.gpsimd.indirect_dma_start(out=bd_[:, :], out_offset=None, in_=bdn_flat,
                                                                  in_offset=bass.IndirectOffsetOnAxis(ap=idxbd[:, b:b + 1], axis=0)),
                       reads=[I["b_down"], idxbd], writes=[bd_])
                for kc in range(8):
                    fw.dma("pool", lambda: nc.gpsimd.indirect_dma_start(out=wg_[:, kc, :], out_offset=None, in_=wgu_flat,
                                                                      in_offset=bass.IndirectOffsetOnAxis(ap=idxw[:, b, kc:kc + 1], axis=0)),
                           reads=[I["w_gate_up"], idxw], writes=[wg_])
                for fc in range(8):
                    fw.dma("pool", lambda: nc.gpsimd.indirect_dma_start(out=wd_[:, fc, :], out_offset=None, in_=wdn_flat,
                                                                      in_offset=bass.IndirectOffsetOnAxis(ap=idxw[:, b, fc:fc + 1], axis=0)),
                           reads=[I["w_down"], idxw], writes=[wd_])
            load_w(0)
            fw.dma("sp", lambda: nc.sync.dma_start(out=xtok[0][:], in_=Xs[0:BLK, :].rearrange("(t p) d -> p t d", p=128)), reads=[Xs], writes=[xtok[0]])
            kk = 0
            for b in range(NB):
                if b + 1 < NB:
                    load_w(b + 1)
                wg_ = wgu[b % 2]; wd_ = wdn[b % 2]; bd_ = bdr[b % 2]; bg_ = bgc[b % 2]; br_ = bgrow[b % 2]
                xt_ = xtok[b % 2]
                if b + 1 < NB:
                    xn_ = xtok[(b + 1) % 2]
                    fw.dma("sp", lambda: nc.sync.dma_start(out=xn_[:], in_=Xs[(b + 1) * BLK:(b + 2) * BLK, :].rearrange("(t p) d -> p t d", p=128)), reads=[Xs], writes=[xn_])
                fw.op("pe", lambda: nc.tensor.transpose(out=pd[1][:, 0:16], in_=br_[:, :], identity=ident_f[0:16, 0:16]), reads=[br_, ident_f], writes=[pd[1]])
                fw.op("act", lambda: nc.scalar.copy(out=bg_[:], in_=pd[1][:, 0:16]), reads=[pd[1]], writes=[bg_])
                for st in range(NST):
                    ptr = ptrs[st % 2]
                    for kc in range(8):
                        fw.op("pe", lambda: nc.tensor.transpose(out=ptr[:, kc, :], in_=xt_[:, st, kc * 128:(kc + 1) * 128], identity=ident_b[:]), reads=[xt_, ident_b], writes=[ptr], chain=True)
                    if st % 2 == 0:
                        fw.op("act", lambda: nc.scalar.copy(out=XT[:, :, st * 128:(st + 1) * 128], in_=ptr[:]), reads=[ptr], writes=[XT])
                    else:
                        fw.op("dve", lambda: nc.vector.tensor_copy(out=XT[:, :, st * 128:(st + 1) * 128], in_=ptr[:]), reads=[ptr], writes=[XT])
                for (c0, cn) in chunks:
                    for j in range(8):
                        k_ = kk % 2; kk += 1
                        g_, u_ = pg[k_], pu[k_]
                        for kc in range(8):
                            fw.op("pe", lambda: nc.tensor.matmul(g_[:, :cn], lhsT=wg_[:, kc, j * 128:(j + 1) * 128], rhs=XT[:, kc, c0:c0 + cn], start=(kc == 0), stop=(kc == 7)),
                                  reads=[wg_, XT], writes=[g_], chain=True)
                        for kc in range(8):
                            fw.op("pe", lambda: nc.tensor.matmul(u_[:, :cn], lhsT=wg_[:, kc, D + j * 128:D + (j + 1) * 128], rhs=XT[:, kc, c0:c0 + cn], start=(kc == 0), stop=(kc == 7)),
                                  reads=[wg_, XT], writes=[u_], chain=True)
                        gs_, sg_, uu_, aa_ = gs[k_], sgm[k_], uu[k_], aa[k_]
                        fw.op("dve", lambda: nc.vector.tensor_scalar(out=gs_[:, :cn], in0=g_[:, :cn], scalar1=bg_[:, j:j + 1], scalar2=7.0, op0=ALU.add, op1=ALU.min), reads=[g_, bg_], writes=[gs_])
                        fw.op("act", lambda: nc.scalar.activation(out=aa_[:, :cn], in_=gs_[:, :cn], func=AF.Gelu_apprx_sigmoid), reads=[gs_], writes=[aa_])
                        fw.op("dve", lambda: nc.vector.tensor_scalar(out=uu_[:, :cn], in0=u_[:, :cn], scalar1=bg_[:, 8 + j:9 + j], scalar2=7.0, op0=ALU.add, op1=ALU.min), reads=[u_, bg_], writes=[uu_])
                        fw.op("dve", lambda: nc.vector.tensor_scalar(out=uu_[:, :cn], in0=uu_[:, :cn], scalar1=-7.0, scalar2=1.0, op0=ALU.max, op1=ALU.add), reads=[uu_], writes=[uu_])
                        fw.op("dve", lambda: nc.vector.tensor_tensor(out=actT[:, j, c0:c0 + cn], in0=aa_[:, :cn], in1=uu_[:, :cn], op=ALU.mult), reads=[aa_, uu_], writes=[actT])
                for st in range(NST):
                    yo_ = yo[st % 2]
                    for dh in range(2):
                        p = pd[dh]
                        for fc in range(8):
                            fw.op("pe", lambda: nc.tensor.matmul(p[:, :], lhsT=actT[:, fc, st * 128:(st + 1) * 128], rhs=wd_[:, fc, dh * 512:(dh + 1) * 512], start=(fc == 0), stop=(fc == 7)),
                                  reads=[actT, wd_], writes=[p], chain=True)
                        fw.op("dve", lambda: nc.vector.tensor_tensor(out=yo_[:, dh * 512:(dh + 1) * 512], in0=p[:, :], in1=bd_[:, dh * 512:(dh + 1) * 512], op=ALU.add), reads=[p, bd_], writes=[yo_])
                    r0 = b * BLK + st * 128
                    fw.dma("sp", lambda: nc.sync.dma_start(out=Ys[r0:r0 + 128, :], in_=yo_[:, :]), reads=[yo_], writes=[Ys])

    def phaseG(l, with_ctx, last):
        with ExitStack() as pes:
            def tl(name, shape, dt=F32):
                return fw.sb(name, shape, dt, pes)
            yk = [[tl("yk%d_%d" % (b_, k_), [128, D]) for k_ in range(4)] for b_ in range(2)]
            acc = [tl("acc%d" % i, [128, D]) for i in range(2)]
            xg = [tl("xgG%d" % i, [128, 8, 512]) for i in range(2)]
            pT = [fw.ps("pTG%d" % i, [128, 8, 128], F32, pes) for i in range(2)]
            if last:
                sq = tl("sqG", [128, 8, 512], BF16); rstd = tl("rstdG", [128, 512])
                pss = fw.ps("pssG", [128, 512], F32, pes)
                xo = tl("xoG", [128, 8, 512])
                osb = [tl("osbG%d" % i, [128, D]) for i in range(2)]
                pO = fw.ps("pOG", [128, 8, 128], F32, pes)
            gl = groups() if with_ctx else groups()[:-1]
            for gi, (t0, n, isctx) in enumerate(gl):
                j = 1 if isctx else 0
                x_ = xg[gi % 2]
                fw.dma("sp", lambda: nc.sync.dma_start(out=x_[:, :, :n], in_=xs[:, :, t0:t0 + n]), reads=[xs], writes=[x_])
                for tt in range(n // 128):
                    ti = (t0 // 128) + tt
                    tsl = slice(tt * 128, (tt + 1) * 128)
                    yk_ = yk[ti % 2]; a_ = acc[ti % 2]; p_ = pT[ti % 2]
                    for k_ in range(4):
                        fw.dma("pool", lambda: nc.gpsimd.indirect_dma_start(out=yk_[k_][:, :], out_offset=None, in_=Ys[:, :],
                                                                          in_offset=bass.IndirectOffsetOnAxis(ap=dest_i[:, ti, k_:k_ + 1], axis=0)),
                               reads=[Ys, dest_i], writes=[yk_[k_]])
                    fw.op("dve", lambda: nc.vector.tensor_scalar(out=a_[:], in0=yk_[0][:], scalar1=wk[:, ti, 0:1], scalar2=None, op0=ALU.mult), reads=[yk_[0], wk], writes=[a_])
                    for k_ in range(1, 4):
                        fw.op("dve", lambda: nc.vector.scalar_tensor_tensor(out=a_[:], in0=yk_[k_][:], scalar=wk[:, ti, k_:k_ + 1], in1=a_[:], op0=ALU.mult, op1=ALU.add),
                              reads=[yk_[k_], wk, a_], writes=[a_])
                    for c in range(8):
                        fw.op("pe", lambda: nc.tensor.transpose(out=p_[:, c, :], in_=a_[:, c * 128:(c + 1) * 128], identity=ident_f[:]), reads=[a_, ident_f], writes=[p_], chain=True)
                    for c in range(8):
                        fw.op("dve", lambda: nc.vector.scalar_tensor_tensor(out=x_[:, c, tsl], in0=p_[:, c, :], scalar=modT[:, l, 40 + c, j:j + 1], in1=x_[:, c, tsl],
                                                                          op0=ALU.mult, op1=ALU.add), reads=[p_, modT, x_], writes=[x_])
                if not last:
                    fw.dma("sp", lambda: nc.sync.dma_start(out=xs[:, :, t0:t0 + n], in_=x_[:, :, :n]), reads=[x_], writes=[xs])
                elif not isctx:
                    fw.op("act", lambda: nc.scalar.activation(out=sq[:, :, :n], in_=x_[:, :, :n], func=AF.Square), reads=[x_], writes=[sq])
                    for c in range(8):
                        fw.op("pe", lambda: nc.tensor.matmul(pss[:, :n], lhsT=ones_b[:], rhs=sq[:, c, :n], start=(c == 0), stop=(c == 7)), reads=[sq, ones_b], writes=[pss], chain=True)
                    fw.op("act", lambda: nc.scalar.activation(out=rstd[:, :n], in_=pss[:, :n], func=AF.Sqrt, bias=EPS, scale=1.0 / D), reads=[pss], writes=[rstd])
                    fw.op("dve", lambda: nc.vector.reciprocal(out=rstd[:, :n], in_=rstd[:, :n]), reads=[rstd], writes=[rstd])
                    for c in range(8):
                        fw.op("dve", lambda: nc.vector.scalar_tensor_tensor(out=xo[:, c, :n], in0=x_[:, c, :n], scalar=gfin[:, c:c + 1], in1=rstd[:, :n], op0=ALU.mult, op1=ALU.mult),
                              reads=[x_, gfin, rstd], writes=[xo])
                    for tt in range(n // 128):
                        o_ = osb[tt % 2]
                        for c in range(8):
                            fw.op("pe", lambda: nc.tensor.transpose(out=pO[:, c, :], in_=xo[:, c, tt * 128:(tt + 1) * 128], identity=ident_f[:]), reads=[xo, ident_f], writes=[pO], chain=True)
                        fw.op("act", lambda: nc.scalar.copy(out=o_[:], in_=pO[:].rearrange("p c d -> p (c d)")), reads=[pO], writes=[o_])
                        r0 = t0 + tt * 128
                        fw.dma("sp", lambda: nc.sync.dma_start(out=OUT[r0:r0 + 128, :], in_=o_[:]), reads=[o_], writes=[OUT])

    if "nopre" not in debug:
        prephase()
        fw.barrier()
    phase0()
    fw.barrier()
    for l in range(nlayers):
        with_ctx = l < DEPTH - 1
        phaseA(l)
        fw.barrier()
        if "A" in debug:
            break
        if "noB" not in debug:
            phaseB(l, with_ctx)
            fw.barrier()
        if "noD" not in debug:
            phaseD(l, with_ctx)
            fw.barrier()
        if "noC" not in debug:
            phaseC(l, with_ctx)
            fw.barrier()
        if "BD" in debug:
            break
        phaseE(l, with_ctx)
        fw.barrier()
        if "E" in debug:
            break
        phaseF(l)
        fw.barrier()
        phaseG(l, with_ctx, l == nlayers - 1 and "G" not in debug)
        fw.barrier()

    outs_to_wait = []
    if debug:
        def dump(name, src, shape, dt):
            o = dout("dbg_" + name, shape, dt)
            fw.dma("sp", lambda: nc.sync.dma_start(out=o[:], in_=src[:]), reads=[src], writes=[o])
            outs_to_wait.append(o)
        dump("yT", yT, [8, 128, NTOK], BF16)
        if "moe" in debug:
            dump("Xs", Xs, [NSLOT + 128, D], BF16)
            dump("Ys", Ys, [NSLOT, D], F32)
            od = dout("dbg_dest", [128, NTILE * 4], U32)
            fw.dma("sp", lambda: nc.sync.dma_start(out=od[:], in_=dest_i[:].rearrange("p t k -> p (t k)")), reads=[dest_i], writes=[od])
            outs_to_wait.append(od)
            ow = dout("dbg_wk", [128, NTILE * 4], F32)
            fw.dma("sp", lambda: nc.sync.dma_start(out=ow[:], in_=wk[:].rearrange("p t k -> p (t k)")), reads=[wk], writes=[ow])
            outs_to_wait.append(ow)
        dump("xs", xs, [128, 8, NTOK], F32)
        dump("qa", qa, [3, 128, NTOK], BF16)
        dump("ka", ka, [3, 128, NTOK], BF16)
        dump("va", va, [NTOK, 390], BF16)
        dump("u5", u5, [2, 128, NTOK], F32)
        dump("qs", qs, [3, 128, NTOK], BF16)
        dump("ks", ks, [3, 128, NTOK], BF16)
        dump("vs", vs, [NTOK, 130], BF16)
        om = dout("dbg_modT", [128, DEPTH * 48 * 2], F32)
        fw.dma("sp", lambda: nc.sync.dma_start(out=om[:], in_=modT[:].rearrange("p l c j -> p (l c j)")), reads=[modT], writes=[om])
        outs_to_wait.append(om)
    fw.finish(outs_to_wait + [OUT])
    print("insts", fw.n_inst, "waits", fw.n_wait)
    es.close()
    return nc, hc


def make_inputs(inputs, b, hc):
    m = {}
    m["xin"] = np.ascontiguousarray(np.concatenate([inputs["x"][b], inputs["ctx"][b]], axis=0))
    m["cvec"] = np.ascontiguousarray(np.stack([inputs["c"][b], inputs["c_ctx"]], axis=0))
    for k in ("w_mod", "b_mod", "g_mix", "w_in", "w_out", "na_rpb", "s5_a_re", "s5_a_im", "s5_log_step", "s5_b_re", "s5_b_im",
              "s5_c_re", "s5_c_im", "s5_d", "s5_w_glu", "s5_b_glu", "sw_sinks", "g_ffn", "w_router", "b_router", "w_gate_up",
              "b_gate_up", "w_down", "b_down", "g_final"):
        m[k] = inputs[k]
    for k, v in hc.items():
        m[k] = v
    return m


def kernel(**inputs):
    nc, hc = build()
    in_maps = [make_inputs(inputs, b % 4, hc) for b in range(8)]
    res = run_bass_kernel_spmd(nc, in_maps, core_ids=list(range(8)))
    return np.stack([res.results[b]["out"] for b in range(4)], axis=0)
```
